# Optimizing a Trainium2 kernel written in Bass

```python
import math
import jax, jax.numpy as jnp
from jax import lax
import numpy as np


D_MODEL = 1024
BATCH = 4
SEQ = 4096
DEPTH = 2
DEC_BATCH = 128
DEC_SEQ = 1
PAST_LEN = 2048
PAGE_SIZE = 128

F32 = jnp.float32

GROUP_CH = 16
N_GROUPS = D_MODEL // GROUP_CH
STATE = 64
DT_MIN = 1e-3
DT_MAX = 1e-1

N_HEADS = 16
HEAD_DIM = D_MODEL // N_HEADS
N_KV = 4
GQ = N_HEADS // N_KV
CMP_BLOCK = 32
CMP_STRIDE = 16
CMP_HIDDEN = 2 * HEAD_DIM
SEL_BLOCK = 64
TOP_N = 16
WINDOW = 512
QBLOCK = 128
Q_COLS = N_HEADS * HEAD_DIM
KV_COLS = 6 * N_KV * HEAD_DIM
GATE_COLS = 3 * N_HEADS
NSA_COLS = Q_COLS + KV_COLS + GATE_COLS

N_BUCKETS = 32
MAX_DISTANCE = 128

D_FF = 2816
N_EXPERTS = 8
TOP_K = 2
EXPERT_FF = 1408

EPS = 1e-6
NEG = -1e30
FORCE = 1e4

kernel_name = 's5_nsa_hybrid_decode_step'


def rmsnorm(x, g):
    x32 = x.astype(F32)
    y = x32 * lax.rsqrt(jnp.mean(x32 * x32, axis=-1, keepdims=True) + EPS) * g.astype(F32)
    return y.astype(x.dtype)


def swiglu(h, w_in, w_out):
    a, b = jnp.split(h @ w_in, 2, axis=-1)
    return (jax.nn.silu(a) * b) @ w_out


def moe_swiglu(h, router, w_in, w_out):
    logits = jnp.einsum('bld,de->ble', h.astype(F32), router.astype(F32))
    top_v, top_i = lax.top_k(logits, TOP_K)
    w = jax.nn.softmax(top_v, axis=-1)
    gate = jnp.sum(jax.nn.one_hot(top_i, N_EXPERTS, dtype=F32) * w[..., None], axis=-2)
    out = jnp.zeros(h.shape, F32)
    for e in range(N_EXPERTS):
        out = out + gate[..., e:e + 1] * swiglu(h, w_in[e], w_out[e])
    return out.astype(h.dtype)


def complex_affine_combine(e1, e2):
    a1r, a1i, b1r, b1i = e1
    a2r, a2i, b2r, b2i = e2
    return (a2r * a1r - a2i * a1i,
            a2r * a1i + a2i * a1r,
            a2r * b1r - a2i * b1i + b2r,
            a2r * b1i + a2i * b1r + b2i)


def s5_ssm(u, h0_re, h0_im, lam_re, lam_im, log_dt, b_re, b_im, c_re, c_im, d_skip):
    bsz, length, _ = u.shape
    u32 = u.astype(F32)
    ug = u32.reshape(bsz, length, N_GROUPS, GROUP_CH)
    dt = jnp.exp(log_dt.astype(F32))[:, None]
    lr = lam_re.astype(F32)
    li = lam_im.astype(F32)
    mag = jnp.exp(lr * dt)
    ab_re = mag * jnp.cos(li * dt)
    ab_im = mag * jnp.sin(li * dt)
    den = lr * lr + li * li
    nr = ab_re - 1.0
    f_re = (nr * lr + ab_im * li) / den
    f_im = (ab_im * lr - nr * li) / den
    br = b_re.astype(F32)
    bim = b_im.astype(F32)
    bb_re = f_re[..., None] * br - f_im[..., None] * bim
    bb_im = f_re[..., None] * bim + f_im[..., None] * br
    x_re = jnp.einsum('blgc,gpc->blgp', ug, bb_re)
    x_im = jnp.einsum('blgc,gpc->blgp', ug, bb_im)
    if h0_re is not None:
        hr = h0_re.astype(F32)
        hi = h0_im.astype(F32)
        x_re = x_re.at[:, 0].add(ab_re * hr - ab_im * hi)
        x_im = x_im.at[:, 0].add(ab_re * hi + ab_im * hr)
    a_re = jnp.broadcast_to(ab_re, (1, length) + ab_re.shape)
    a_im = jnp.broadcast_to(ab_im, (1, length) + ab_im.shape)
    _, _, h_re, h_im = lax.associative_scan(complex_affine_combine, (a_re, a_im, x_re, x_im), axis=1)
    y = (jnp.einsum('blgp,gcp->blgc', h_re, c_re.astype(F32))
         - jnp.einsum('blgp,gcp->blgc', h_im, c_im.astype(F32)))
    y = y.reshape(bsz, length, D_MODEL) + d_skip.astype(F32) * u32
    return y, h_re[:, -1], h_im[:, -1]


def s5_mixer(h, h0_re, h0_im, lam_re, lam_im, log_dt, b_re, b_im, c_re, c_im, d_skip, w_glu):
    y, hr, hi = s5_ssm(h, h0_re, h0_im, lam_re, lam_im, log_dt, b_re, b_im, c_re, c_im, d_skip)
    g = jax.nn.gelu(y)
    a, b = jnp.split(g @ w_glu, 2, axis=-1)
    return (a * jax.nn.sigmoid(b)).astype(h.dtype), hr, hi


def t5_bucket(dist):
    n = jnp.maximum(dist, 0)
    exact = N_BUCKETS // 2
    logpart = exact + (jnp.log(jnp.maximum(n, 1).astype(F32) / exact)
                       / math.log(MAX_DISTANCE / exact) * (N_BUCKETS - exact)).astype(jnp.int32)
    return jnp.where(n < exact, n, jnp.minimum(logpart, N_BUCKETS - 1))


def sel_cover_weights(nc, nb):
    ratio = SEL_BLOCK // CMP_STRIDE
    span = CMP_BLOCK // CMP_STRIDE
    off = (jnp.arange(ratio)[:, None] - jnp.arange(span)[None, :]).reshape(-1)
    target = jnp.arange(nb)[:, None] * ratio + off[None, :]
    return jnp.sum(jnp.arange(nc)[:, None, None] == target[None], axis=-1).astype(F32)


def nsa_project(h, w_in):
    bsz, length, _ = h.shape
    p = h @ w_in
    q = p[..., :Q_COLS].reshape(bsz, length, N_HEADS, HEAD_DIM)
    kv = p[..., Q_COLS:Q_COLS + KV_COLS].reshape(bsz, length, 6, N_KV, HEAD_DIM)
    gate = jax.nn.sigmoid(p[..., Q_COLS + KV_COLS:].astype(F32)).reshape(bsz, length, 3, N_HEADS)
    return q, kv, gate


def nsa_compress(rows, pe, w1, w2):
    length = rows.shape[1]
    nc = (length - CMP_BLOCK) // CMP_STRIDE + 1
    idx = jnp.arange(nc)[:, None] * CMP_STRIDE + jnp.arange(CMP_BLOCK)[None, :]
    win = rows[:, idx] + pe[None, None, :, None, :]
    hid = jax.nn.gelu(jnp.einsum('bnlgd,lde->bnge', win, w1))
    return jnp.einsum('bnge,ef->bngf', hid, w2)


def sel_blocks(rows):
    bsz, length = rows.shape[:2]
    nb = -(-length // SEL_BLOCK)
    rows = jnp.pad(rows, ((0, 0), (0, nb * SEL_BLOCK - length), (0, 0), (0, 0)))
    return rows.reshape(bsz, nb, SEL_BLOCK, N_KV, HEAD_DIM).transpose(0, 3, 1, 2, 4)


def nsa_attend(q, qpos, kc, vc, kb, vb, kw, vw, kwpos, rel_bias):
    bsz, nq = q.shape[:2]
    nc, nb = kc.shape[1], kb.shape[2]
    qg = q.astype(F32).reshape(bsz, nq, N_KV, GQ, HEAD_DIM) * (HEAD_DIM ** -0.5)
    tab = rel_bias.astype(F32).reshape(N_BUCKETS, N_KV, GQ)

    cend = jnp.arange(nc) * CMP_STRIDE + (CMP_BLOCK - 1)
    dist_c = qpos[:, None] - cend[None, :]
    mask_c = (dist_c >= 0)[None, :, None, None, :]
    s_c = (jnp.einsum('bqgrd,bngd->bqgrn', qg, kc)
           + jnp.transpose(tab[t5_bucket(dist_c)], (0, 2, 3, 1))[None])
    p_c = jax.nn.softmax(jnp.where(mask_c, s_c, NEG), axis=-1) * mask_c
    o_c = jnp.einsum('bqgrn,bngd->bqgrd', p_c, vc)

    imp = jnp.einsum('bqgrn,nj->bqgj', p_c, sel_cover_weights(nc, nb))
    j = jnp.arange(nb)
    jt = (qpos // SEL_BLOCK)[:, None]
    forced = ((j == 0) | (j == jt) | (j == jt - 1))[None, :, None, :]
    vis = (j * SEL_BLOCK <= qpos[:, None])[None, :, None, :]
    score = jnp.where(vis, jnp.where(forced, FORCE, imp), NEG)
    _, idx = lax.top_k(score, min(TOP_N, nb))
    bi = jnp.arange(bsz)[:, None, None, None]
    gi = jnp.arange(N_KV)[None, None, :, None]
    ks = kb[bi, gi, idx]
    vs = vb[bi, gi, idx]
    kpos = idx[..., None] * SEL_BLOCK + jnp.arange(SEL_BLOCK)
    dist_s = qpos[None, :, None, None, None] - kpos
    mask_s = (dist_s >= 0)[:, :, :, None]
    bias_s = jnp.moveaxis(tab[t5_bucket(dist_s), gi[..., None]], -1, 3)
    s_s = jnp.where(mask_s, jnp.einsum('bqgrd,bqgkld->bqgrkl', qg, ks) + bias_s, NEG)
    shp = s_s.shape
    p_s = jax.nn.softmax(s_s.reshape(shp[:4] + (-1,)), axis=-1).reshape(shp) * mask_s
    o_s = jnp.einsum('bqgrkl,bqgkld->bqgrd', p_s, vs)

    dist_w = qpos[:, None] - kwpos[None, :]
    mask_w = ((dist_w >= 0) & (dist_w <= WINDOW) & (kwpos[None, :] >= 0))[None, :, None, None, :]
    s_w = (jnp.einsum('bqgrd,bkgd->bqgrk', qg, kw)
           + jnp.transpose(tab[t5_bucket(dist_w)], (0, 2, 3, 1))[None])
    p_w = jax.nn.softmax(jnp.where(mask_w, s_w, NEG), axis=-1) * mask_w
    o_w = jnp.einsum('bqgrk,bkgd->bqgrd', p_w, vw)

    def heads(o):
        return o.reshape(bsz, nq, N_HEADS, HEAD_DIM)
    return heads(o_c), heads(o_s), heads(o_w)


def nsa_merge(o_c, o_s, o_w, gate, w_o, dtype):
    o = gate[:, :, 0, :, None] * o_c + gate[:, :, 1, :, None] * o_s + gate[:, :, 2, :, None] * o_w
    bsz, length = o.shape[:2]
    return (o.reshape(bsz, length, D_MODEL) @ w_o).astype(dtype)


def nsa_prompt(h, rel_bias, w_in, phi_pe, phi_w1, phi_w2, w_o):
    bsz, length, _ = h.shape
    q, kv, gate = nsa_project(h, w_in)
    kc = nsa_compress(kv[:, :, 0], phi_pe[0], phi_w1[0], phi_w2[0])
    vc = nsa_compress(kv[:, :, 1], phi_pe[1], phi_w1[1], phi_w2[1])
    kb = sel_blocks(kv[:, :, 2])
    vb = sel_blocks(kv[:, :, 3])
    pad = ((0, 0), (WINDOW, 0), (0, 0), (0, 0))
    kwp = jnp.pad(kv[:, :, 4], pad)
    vwp = jnp.pad(kv[:, :, 5], pad)
    nqb = length // QBLOCK

    def block(args):
        qb, s = args
        kw = lax.dynamic_slice_in_dim(kwp, s, WINDOW + QBLOCK, axis=1)
        vw = lax.dynamic_slice_in_dim(vwp, s, WINDOW + QBLOCK, axis=1)
        kwpos = s - WINDOW + jnp.arange(WINDOW + QBLOCK)
        qpos = s + jnp.arange(QBLOCK)
        return nsa_attend(qb, qpos, kc, vc, kb, vb, kw, vw, kwpos, rel_bias)

    qbs = q.reshape(bsz, nqb, QBLOCK, N_HEADS, HEAD_DIM).swapaxes(0, 1)
    starts = jnp.arange(nqb, dtype=jnp.int32) * QBLOCK
    o_c, o_s, o_w = lax.map(block, (qbs, starts))

    def unblock(o):
        return o.swapaxes(0, 1).reshape(bsz, length, N_HEADS, HEAD_DIM)
    y = nsa_merge(unblock(o_c), unblock(o_s), unblock(o_w), gate, w_o, h.dtype)
    wp = min(WINDOW, length)
    return y, kv[:, :, :4], kv[:, length - wp:, 4:]


def nsa_sample(h, cache_kv, cache_win, page_table, rel_bias, w_in, phi_pe, phi_w1, phi_w2, w_o):
    bsz, length, _ = h.shape
    q, kv, gate = nsa_project(h, w_in)
    past = cache_kv[page_table]
    past_len = past.shape[1] * past.shape[2]
    full = jnp.concatenate([past.reshape(bsz, past_len, 4, N_KV, HEAD_DIM),
                            kv[:, :, :4].astype(past.dtype)], axis=1)
    kc = nsa_compress(full[:, :, 0], phi_pe[0], phi_w1[0], phi_w2[0])
    vc = nsa_compress(full[:, :, 1], phi_pe[1], phi_w1[1], phi_w2[1])
    kb = sel_blocks(full[:, :, 2])
    vb = sel_blocks(full[:, :, 3])
    wb = cache_win.shape[1]
    win = jnp.concatenate([cache_win, kv[:, :, 4:].astype(cache_win.dtype)], axis=1)
    kwpos = jnp.concatenate([past_len - wb + jnp.arange(wb), past_len + jnp.arange(length)])
    qpos = past_len + jnp.arange(length)
    o_c, o_s, o_w = nsa_attend(q, qpos, kc, vc, kb, vb, win[:, :, 0], win[:, :, 1], kwpos, rel_bias)
    y = nsa_merge(o_c, o_s, o_w, gate, w_o, h.dtype)
    return y, kv[:, :, :4], kv[:, :, 4:]


def setup_inputs(seed: int = 0) -> dict:
    key = jax.random.key(seed)
    k = jax.random.split(key, 30)

    def nrm(kk, shape, scale=1.0):
        return jax.random.normal(kk, shape, F32) * scale

    n_pages = PAST_LEN // PAGE_SIZE
    used = DEC_BATCH * n_pages
    n_pool = used + max(1, used // 4)
    wb = min(WINDOW, PAST_LEN)
    page_table = jax.random.permutation(k[6], n_pool)[:used].reshape(DEC_BATCH, n_pages).astype(jnp.int32)
    n_idx = jnp.arange(STATE, dtype=F32)
    return {
        'x_prompt': nrm(k[0], (BATCH, SEQ, D_MODEL)),
        'x_sample': nrm(k[1], (DEC_BATCH, DEC_SEQ, D_MODEL)),
        'state_s5_re': nrm(k[2], (DEC_BATCH, N_GROUPS, STATE), 0.1),
        'state_s5_im': nrm(k[3], (DEC_BATCH, N_GROUPS, STATE), 0.1),
        'cache_kv': nrm(k[4], (n_pool, PAGE_SIZE, 4, N_KV, HEAD_DIM)),
        'cache_win': nrm(k[5], (DEC_BATCH, wb, 2, N_KV, HEAD_DIM)),
        'page_table': page_table,
        'rel_bias': nrm(k[7], (N_BUCKETS, N_HEADS), 0.1),
        'norm_mix': 1.0 + nrm(k[8], (DEPTH, D_MODEL), 0.01),
        'norm_ffn': 1.0 + nrm(k[9], (DEPTH, D_MODEL), 0.01),
        'norm_final': 1.0 + nrm(k[10], (D_MODEL,), 0.01),
        's5_lam_re': -0.5 + nrm(k[11], (N_GROUPS, STATE), 0.01),
        's5_lam_im': math.pi * n_idx[None, :] + nrm(k[12], (N_GROUPS, STATE), 0.01),
        's5_log_dt': jax.random.uniform(k[13], (N_GROUPS,), F32, math.log(DT_MIN), math.log(DT_MAX)),
        's5_b_re': nrm(k[14], (N_GROUPS, STATE, GROUP_CH), (2 * GROUP_CH) ** -0.5),
        's5_b_im': nrm(k[15], (N_GROUPS, STATE, GROUP_CH), (2 * GROUP_CH) ** -0.5),
        's5_c_re': nrm(k[16], (N_GROUPS, GROUP_CH, STATE), STATE ** -0.5),
        's5_c_im': nrm(k[17], (N_GROUPS, GROUP_CH, STATE), STATE ** -0.5),
        's5_d': nrm(k[18], (D_MODEL,)),
        's5_w_glu': nrm(k[19], (D_MODEL, 2 * D_MODEL), D_MODEL ** -0.5),
        'ffn_w_in': nrm(k[20], (D_MODEL, 2 * D_FF), D_MODEL ** -0.5),
        'ffn_w_out': nrm(k[21], (D_FF, D_MODEL), D_FF ** -0.5),
        'nsa_w_in': nrm(k[22], (D_MODEL, NSA_COLS), D_MODEL ** -0.5),
        'nsa_phi_pe': nrm(k[23], (2, CMP_BLOCK, HEAD_DIM), 0.1),
        'nsa_phi_w1': nrm(k[24], (2, CMP_BLOCK, HEAD_DIM, CMP_HIDDEN), (CMP_BLOCK * HEAD_DIM) ** -0.5),
        'nsa_phi_w2': nrm(k[25], (2, CMP_HIDDEN, HEAD_DIM), CMP_HIDDEN ** -0.5),
        'nsa_w_o': nrm(k[26], (D_MODEL, D_MODEL), D_MODEL ** -0.5),
        'moe_router': nrm(k[27], (D_MODEL, N_EXPERTS), D_MODEL ** -0.5),
        'moe_w_in': nrm(k[28], (N_EXPERTS, D_MODEL, 2 * EXPERT_FF), D_MODEL ** -0.5),
        'moe_w_out': nrm(k[29], (N_EXPERTS, EXPERT_FF, D_MODEL), EXPERT_FF ** -0.5),
    }


def reference(x_prompt, x_sample, state_s5_re, state_s5_im, cache_kv, cache_win, page_table,
              rel_bias, norm_mix, norm_ffn, norm_final, s5_lam_re, s5_lam_im, s5_log_dt,
              s5_b_re, s5_b_im, s5_c_re, s5_c_im, s5_d, s5_w_glu, ffn_w_in, ffn_w_out,
              nsa_w_in, nsa_phi_pe, nsa_phi_w1, nsa_phi_w2, nsa_w_o,
              moe_router, moe_w_in, moe_w_out):
    s5p = (s5_lam_re, s5_lam_im, s5_log_dt, s5_b_re, s5_b_im, s5_c_re, s5_c_im, s5_d, s5_w_glu)
    nsap = (nsa_w_in, nsa_phi_pe, nsa_phi_w1, nsa_phi_w2, nsa_w_o)
    yp, ys = x_prompt, x_sample
    for i in range(DEPTH):
        hp = rmsnorm(yp, norm_mix[i])
        hs = rmsnorm(ys, norm_mix[i])
        if i % 2 == 0:
            op, s5_re_p, s5_im_p = s5_mixer(hp, None, None, *s5p)
            os_, s5_re_s, s5_im_s = s5_mixer(hs, state_s5_re, state_s5_im, *s5p)
        else:
            op, kv_p, win_p = nsa_prompt(hp, rel_bias, *nsap)
            os_, kv_s, win_s = nsa_sample(hs, cache_kv, cache_win, page_table, rel_bias, *nsap)
        yp = yp + op
        ys = ys + os_
        hp = rmsnorm(yp, norm_ffn[i])
        hs = rmsnorm(ys, norm_ffn[i])
        if i % 2 == 0:
            yp = yp + swiglu(hp, ffn_w_in, ffn_w_out).astype(yp.dtype)
            ys = ys + swiglu(hs, ffn_w_in, ffn_w_out).astype(ys.dtype)
        else:
            yp = yp + moe_swiglu(hp, moe_router, moe_w_in, moe_w_out)
            ys = ys + moe_swiglu(hs, moe_router, moe_w_in, moe_w_out)
    y_prompt = rmsnorm(yp, norm_final)
    y_sample = rmsnorm(ys, norm_final)
    return (y_prompt, y_sample, s5_re_p, s5_im_p, kv_p, win_p, s5_re_s, s5_im_s, kv_s, win_s)
```

```python
import math
import os
import numpy as np
STOP = float(os.environ.get('S5STOP', '99'))
NOSELF = os.environ.get('NOSELF', '0') == '1'
from contextlib import ExitStack
import concourse.bass as bass
import concourse.mybir as mybir
from concourse.bass_types import AP
from concourse.bass_utils import run_bass_kernel_spmd

F32 = mybir.dt.float32
BF16 = mybir.dt.bfloat16
I32 = mybir.dt.int32
AF = mybir.ActivationFunctionType
ALU = mybir.AluOpType
AX = mybir.AxisListType

D = 1024
T = 4096
NS = 16
EPS = 1e-6
TWO_PI = 2.0 * math.pi


class Sched:
    def __init__(self, nc, es, ndma=10):
        self.nc = nc
        self.eng = {"pe": nc.tensor, "dve": nc.vector, "act": nc.scalar, "pool": nc.gpsimd, "sp": nc.sync}
        self.sem = {k: es.enter_context(nc.semaphore("s_" + k)) for k in self.eng}
        self.cnt = {k: 0 for k in self.eng}
        self.dsem = {k: [es.enter_context(nc.semaphore("d_%s%d" % (k, i))) for i in range(ndma)] for k in ("sp", "act", "pool")}
        self.dcnt = {k: [0] * ndma for k in self.dsem}
        self.drr = {k: 0 for k in self.dsem}
        self.waited = {k: {} for k in self.eng}
        self.semobj = {}
        self.wr = {}
        self.rd = {}

    def _sid(self, s):
        i = id(s)
        self.semobj[i] = s
        return i

    def _wait(self, e, deps):
        w = self.waited[e]
        for sid, val in deps.items():
            if w.get(sid, 0) >= val:
                continue
            self.eng[e].wait_ge(self.semobj[sid], val)
            w[sid] = val

    def _deps(self, reads, writes, own_sid=None, skip_own=False):
        deps = {}

        def add(d):
            for sid, val in d.items():
                if skip_own and sid == own_sid:
                    continue
                if deps.get(sid, 0) < val:
                    deps[sid] = val
        for k in reads:
            add(self.wr.get(k, {}))
        for k in writes:
            add(self.wr.get(k, {}))
            add(self.rd.get(k, {}))
        return deps

    def op(self, e, fn, reads=(), writes=()):
        s = self.sem[e]
        sid = self._sid(s)
        deps = self._deps(reads, writes, own_sid=sid, skip_own=(e == "pe" or NOSELF))
        self._wait(e, deps)
        inst = fn(self.eng[e])
        self.cnt[e] += 1
        inst.then_inc(s, 1)
        val = self.cnt[e]
        for k in reads:
            self.rd.setdefault(k, {})[sid] = val
        for k in writes:
            self.wr[k] = {sid: val}
            self.rd[k] = {}
        return inst

    def dma(self, q, out, in_, reads=(), writes=(), add_writer=False, **kw):
        pool = self.dsem[q]
        i = self.drr[q]
        self.drr[q] = (i + 1) % len(pool)
        s = pool[i]
        sid = self._sid(s)
        deps = self._deps(reads, writes)
        if self.dcnt[q][i] > 0:
            deps[sid] = max(deps.get(sid, 0), self.dcnt[q][i])
        self._wait(q, deps)
        inst = self.eng[q].dma_start(out=out, in_=in_, **kw)
        self.dcnt[q][i] += 16
        inst.then_inc(s, 16)
        val = self.dcnt[q][i]
        for k in reads:
            self.rd.setdefault(k, {})[sid] = val
        for k in writes:
            if add_writer and k in self.wr:
                self.wr[k][sid] = val
            else:
                self.wr[k] = {sid: val}
                self.rd[k] = {}
        return inst

    def idma(self, out, in_, idx_ap, reads=(), writes=()):
        q = "pool"
        pool = self.dsem[q]
        i = self.drr[q]
        self.drr[q] = (i + 1) % len(pool)
        sm = pool[i]
        sid = self._sid(sm)
        deps = self._deps(reads, writes)
        if self.dcnt[q][i] > 0:
            deps[sid] = max(deps.get(sid, 0), self.dcnt[q][i])
        self._wait(q, deps)
        inst = self.eng[q].indirect_dma_start(out=out, out_offset=None, in_=in_, in_offset=bass.IndirectOffsetOnAxis(ap=idx_ap, axis=0))
        self.dcnt[q][i] += 16
        inst.then_inc(sm, 16)
        val = self.dcnt[q][i]
        for k in reads:
            self.rd.setdefault(k, {})[sid] = val
        for k in writes:
            self.wr[k] = {sid: val}
            self.rd[k] = {}
        return inst

    def barrier(self):
        deps = {}
        for e in self.eng:
            if self.cnt[e] > 0:
                deps[self._sid(self.sem[e])] = self.cnt[e]
        for q in self.dsem:
            for i, sm in enumerate(self.dsem[q]):
                if self.dcnt[q][i] > 0:
                    deps[self._sid(sm)] = self.dcnt[q][i]
        for e in self.eng:
            self._wait(e, dict(deps))

    def finish(self):
        deps = {}
        for k in list(self.wr.keys()):
            for sid, val in self.wr[k].items():
                deps[sid] = max(deps.get(sid, 0), val)
        for q in self.dsem:
            for i, s in enumerate(self.dsem[q]):
                if self.dcnt[q][i] > 0:
                    sid = self._sid(s)
                    deps[sid] = max(deps.get(sid, 0), self.dcnt[q][i])
        self._wait("sp", deps)


class KB:
    def __init__(self, nc, es):
        self.nc = nc
        self.es = es
        self.S = Sched(nc, es)
        self.dr = {}
        self._q = 0

    def sb(self, es, name, shape, dt):
        return es.enter_context(self.nc.sbuf_tensor(name, shape, dt))

    def ps(self, es, name, shape, dt):
        return es.enter_context(self.nc.psum_tensor(name, shape, dt))

    def dram(self, name, shape, dt, kind):
        t = self.nc.dram_tensor(name, shape, dt, kind=kind).ap()
        self.dr[name] = t
        return t

    def V(self, fn, r=(), w=()):
        return self.S.op("dve", fn, r, w)

    def A(self, fn, r=(), w=()):
        return self.S.op("act", fn, r, w)

    def G(self, fn, r=(), w=()):
        return self.S.op("pool", fn, r, w)

    def P(self, fn, r=(), w=()):
        return self.S.op("pe", fn, r, w)

    def dma(self, out, in_, r=(), w=(), q=None, **kw):
        if q is None:
            q = ("sp", "act")[self._q % 2]
            self._q += 1
        return self.S.dma(q, out, in_, r, w, **kw)


def bc(ap, shape):
    return ap.broadcast_to(shape)


def build_consts(K, es):
    c = {}
    idf = K.sb(es, "ident_f", [128, 128], F32)
    idb = K.sb(es, "ident_b", [128, 128], BF16)
    K.G(lambda e: e.memset(idf[:], 1.0), w=["idf"])
    K.G(lambda e: e.affine_select(out=idf[:], in_=idf[:], pattern=[[-1, 128]], compare_op=ALU.is_equal,
                                  fill=0.0, base=0, channel_multiplier=1), r=["idf"], w=["idf"])
    K.V(lambda e: e.tensor_copy(out=idb[:], in_=idf[:]), r=["idf"], w=["idb"])
    c["idf"], c["idb"] = idf, idb
    return c


def s5_stage(K, es_outer, C, I, O):
    nc = K.nc
    idf, idb = C["idf"], C["idb"]
    V, A, G, P, dma = K.V, K.A, K.G, K.P, K.dma
    with ExitStack() as es:
        _s5_body(K, es, C, I, O)
    K.S.barrier()


def _s5_body(K, es, C, I, O):
    idf, idb = C["idf"], C["idb"]
    V, A, G, P, dma = K.V, K.A, K.G, K.P, K.dma

    Win = K.sb(es, "s5Win", [128, 32, 2, 2, 128], BF16)
    Wout = K.sb(es, "s5Wout", [128, 32, 2, 128], BF16)
    Mint = K.sb(es, "s5Mint", [128, 64, 128], BF16)
    CA = K.sb(es, "s5CA", [128, 2, 32], F32)
    CBn = K.sb(es, "s5CBn", [128, 32], F32)
    CBp = K.sb(es, "s5CBp", [128, 32], F32)
    A1 = K.sb(es, "s5A1", [128, 2, 32], F32)
    gain0 = K.sb(es, "gain0", [128, D], F32)
    dskip = K.sb(es, "dskip", [128, D], F32)
    dma(gain0[:], AP(tensor=I["norm_mix"].tensor, offset=0, ap=[[0, 128], [1, D]]), w=["gain0"])
    dma(dskip[:], AP(tensor=I["s5_d"].tensor, offset=0, ap=[[0, 128], [1, D]]), w=["dskip"])

    with ExitStack() as pes:
        raw = K.sb(pes, "s5raw", [64, 3, 2, 64], F32)
        PRM = K.sb(pes, "s5prm", [128, 3, 32], F32)
        Wk = K.sb(pes, "s5wk", [128, 24, 32], F32)
        Wi = K.sb(pes, "s5wi", [128, 32], I32)
        PW = K.sb(pes, "s5pw", [128, 16, 2, 32], F32)
        Bt = K.sb(pes, "s5bt", [128, 2, 32, 16], F32)
        Ct = K.sb(pes, "s5ct", [128, 2, 32, 16], F32)
        BB = K.sb(pes, "s5bb", [128, 2, 32, 16], F32)
        craw = K.sb(pes, "s5craw", [128, 2, 2, 64], F32)
        T3 = K.sb(pes, "s5t3", [128, 4, 32, 16], F32)
        Ubf = K.sb(pes, "s5U", [128, 32, 2, 128], BF16)
        Vpad = K.sb(pes, "s5V", [128, 32, 2, 2, 128], BF16)
        maskLT = K.sb(pes, "s5mask", [128, 128], F32)
        pp = K.ps(pes, "s5pp", [128, 8, 64], F32)
        pc = [K.ps(pes, "s5pc%d" % i, [128, 512], F32) for i in range(2)]
        pm = [K.ps(pes, "s5pm%d" % i, [128, 512], F32) for i in range(2)]
        pw = [K.ps(pes, "s5pw%d" % i, [128, 1024], BF16) for i in range(2)]

        for i, nm in enumerate(["s5_lam_re", "s5_lam_im"]):
            dma(raw[:, i, :, :], AP(tensor=I[nm].tensor, offset=0, ap=[[64, 64], [0, 2], [1, 64]]), w=["raw"], add_writer=True)
        ldt = K.sb(pes, "s5ldt", [64, 1], F32)
        dma(ldt[:], AP(tensor=I["s5_log_dt"].tensor, offset=0, ap=[[1, 64], [1, 1]]), w=["ldt"])
        V(lambda e: e.tensor_copy(out=raw[:, 2, :, :].rearrange("p a b -> p (a b)"), in_=bc(ldt[:, 0:1], [64, 128])), r=["ldt"], w=["raw2"])
        for i in range(3):
            P(lambda e: e.transpose(out=pp[:, i, :], in_=raw[:, i, :, :], identity=idf[0:64, 0:64]), r=["raw", "raw2", "idf"], w=["pp"])
        for gl in range(2):
            sl = slice(64 * gl, 64 * gl + 64)
            V(lambda e: e.tensor_copy(out=PRM[sl, :, :], in_=pp[sl, 0:3, :].rearrange("p i (pr gl) -> p i pr gl", gl=2)[:, :, :, gl]),
              r=["pp"], w=["PRM"])
        if STOP <= 1:
            return
        LR, LI, LD = PRM[:, 0, :], PRM[:, 1, :], PRM[:, 2, :]
        _n = [0]

        def wk():
            _n[0] += 1
            return Wk[:, _n[0] - 1, :]
        rk, wkk = ["PRM", "Wk"], ["Wk"]

        def vtt(out, a, b, op):
            V(lambda e: e.tensor_tensor(out=out, in0=a, in1=b, op=op), r=rk + ["PW", "Bt", "Ct", "BB"], w=wkk)

        def vts(out, a, s1, s2, op0, op1=None):
            if op1 is None:
                V(lambda e: e.tensor_scalar(out=out, in0=a, scalar1=s1, scalar2=None, op0=op0), r=rk, w=wkk)
            else:
                V(lambda e: e.tensor_scalar(out=out, in0=a, scalar1=s1, scalar2=s2, op0=op0, op1=op1), r=rk, w=wkk)

        def sin_turns(out, turns):
            tf, fr, m = wk(), wk(), wk()
            V(lambda e: e.tensor_copy(out=Wi[:], in_=turns), r=rk, w=["Wi"])
            V(lambda e: e.tensor_copy(out=tf, in_=Wi[:]), r=["Wi"], w=wkk)
            vtt(fr, turns, tf, ALU.subtract)
            vts(m, fr, 0.5, None, ALU.is_gt)
            vtt(fr, fr, m, ALU.subtract)
            vts(m, fr, -0.5, None, ALU.is_lt)
            vtt(fr, fr, m, ALU.add)
            A(lambda e: e.activation(out=out, in_=fr, func=AF.Sin, scale=TWO_PI), r=rk, w=wkk)

        dt_, th, turns, turnc, sn, cs, ld, mag = [wk() for _ in range(8)]
        A(lambda e: e.activation(out=dt_, in_=LD, func=AF.Exp), r=rk, w=wkk)
        vtt(th, LI, dt_, ALU.mult)
        vts(turns, th, 1.0 / TWO_PI, None, ALU.mult)
        vts(turnc, turns, 0.25, None, ALU.add)
        sin_turns(sn, turns)
        sin_turns(cs, turnc)
        vtt(ld, LR, dt_, ALU.mult)
        A(lambda e: e.activation(out=mag, in_=ld, func=AF.Exp), r=rk, w=wkk)
        ar, ai = PW[:, 8, 0, :], PW[:, 8, 1, :]
        V(lambda e: e.tensor_tensor(out=ar, in0=mag, in1=cs, op=ALU.mult), r=rk, w=["PW"])
        V(lambda e: e.tensor_tensor(out=ai, in0=mag, in1=sn, op=ALU.mult), r=rk, w=["PW"])
        _n[0] = 8
        den, t1, t2, nr, fre, fim, q = [wk() for _ in range(7)]
        vtt(den, LR, LR, ALU.mult)
        vtt(t1, LI, LI, ALU.mult)
        vtt(den, den, t1, ALU.add)
        V(lambda e: e.reciprocal(out=den, in_=den), r=rk, w=wkk)
        V(lambda e: e.tensor_scalar(out=nr, in0=ar, scalar1=-1.0, scalar2=None, op0=ALU.add), r=rk + ["PW"], w=wkk)
        vtt(t1, nr, LR, ALU.mult)
        vtt(t2, ai, LI, ALU.mult)
        vtt(t1, t1, t2, ALU.add)
        vtt(fre, t1, den, ALU.mult)
        vtt(t1, ai, LR, ALU.mult)
        vtt(t2, nr, LI, ALU.mult)
        vtt(t1, t1, t2, ALU.subtract)
        vtt(fim, t1, den, ALU.mult)
        A(lambda e: e.activation(out=q, in_=ld, func=AF.Exp, scale=-2.0), r=rk, w=wkk)
        V(lambda e: e.memset(PW[:, 7, 0, :], 1.0), w=["PW"])
        V(lambda e: e.memset(PW[:, 7, 1, :], 0.0), w=["PW"])
        V(lambda e: e.tensor_tensor(out=PW[:, 6, 0, :], in0=ar, in1=q, op=ALU.mult), r=rk + ["PW"], w=["PW"])
        V(lambda e: e.scalar_tensor_tensor(out=PW[:, 6, 1, :], in0=ai, scalar=-1.0, in1=q, op0=ALU.mult, op1=ALU.mult),
          r=rk + ["PW"], w=["PW"])

        def cmul(o, x, y):
            a_, b_ = wk(), wk()
            _n[0] -= 2
            V(lambda e: e.tensor_tensor(out=a_, in0=PW[:, x, 0, :], in1=PW[:, y, 0, :], op=ALU.mult), r=["PW", "Wk"], w=wkk)
            V(lambda e: e.tensor_tensor(out=b_, in0=PW[:, x, 1, :], in1=PW[:, y, 1, :], op=ALU.mult), r=["PW", "Wk"], w=wkk)
            V(lambda e: e.tensor_tensor(out=PW[:, o, 0, :], in0=a_, in1=b_, op=ALU.subtract), r=["PW", "Wk"], w=["PW"])
            V(lambda e: e.tensor_tensor(out=a_, in0=PW[:, x, 0, :], in1=PW[:, y, 1, :], op=ALU.mult), r=["PW", "Wk"], w=wkk)
            V(lambda e: e.tensor_tensor(out=b_, in0=PW[:, x, 1, :], in1=PW[:, y, 0, :], op=ALU.mult), r=["PW", "Wk"], w=wkk)
            V(lambda e: e.tensor_tensor(out=PW[:, o, 1, :], in0=a_, in1=b_, op=ALU.add), r=["PW", "Wk"], w=["PW"])
        for k in range(2, 9):
            cmul(7 + k, 7 + k - 1, 8)
        for k in range(2, 8):
            cmul(7 - k, 7 - k + 1, 6)
        V(lambda e: e.tensor_copy(out=CA[:, 0, :], in_=PW[:, 15, 0, :]), r=["PW"], w=["CA"])
        V(lambda e: e.tensor_copy(out=CA[:, 1, :], in_=PW[:, 15, 0, :]), r=["PW"], w=["CA"])
        V(lambda e: e.tensor_copy(out=CBp[:], in_=PW[:, 15, 1, :]), r=["PW"], w=["CBp"])
        V(lambda e: e.tensor_scalar(out=CBn[:], in0=PW[:, 15, 1, :], scalar1=-1.0, scalar2=None, op0=ALU.mult), r=["PW"], w=["CBn"])
        V(lambda e: e.tensor_copy(out=A1[:], in_=PW[:, 8, :, :]), r=["PW"], w=["A1"])

        if STOP <= 2:
            return
        for i, nm in enumerate(["s5_b_re", "s5_b_im"]):
            for q4 in range(8):
                dma(Bt[:, i, 4 * q4:4 * q4 + 4, :], I[nm].rearrange("(pr gl) p c -> (gl p) pr c", gl=2)[:, 4 * q4:4 * q4 + 4, :],
                    w=["Bt"], add_writer=True)
        fre_b = bc(fre.unsqueeze(2), [128, 32, 16])
        fim_b = bc(fim.unsqueeze(2), [128, 32, 16])
        vtt(T3[:, 0], Bt[:, 0], fre_b, ALU.mult)
        vtt(T3[:, 1], Bt[:, 1], fim_b, ALU.mult)
        V(lambda e: e.tensor_tensor(out=BB[:, 0], in0=T3[:, 0], in1=T3[:, 1], op=ALU.subtract), r=["Wk"], w=["BB"])
        vtt(T3[:, 0], Bt[:, 1], fre_b, ALU.mult)
        vtt(T3[:, 1], Bt[:, 0], fim_b, ALU.mult)
        V(lambda e: e.tensor_tensor(out=BB[:, 1], in0=T3[:, 0], in1=T3[:, 1], op=ALU.add), r=["Wk"], w=["BB"])

        if STOP <= 2.3:
            return
        for i, nm in enumerate(["s5_c_re", "s5_c_im"]):
            cflat = I[nm]
            for k in range(8):
                for hh in range(2):
                    dma(craw[:, k % 2, hh, :], AP(tensor=cflat.tensor, offset=k * 128 * 64, ap=[[64, 128], [1, 64]]),
                        w=["craw%d" % (k % 2)], add_writer=(hh == 1))
                P(lambda e: e.transpose(out=pc[k % 2][:, 0:128], in_=craw[:, k % 2, :, :], identity=idf[:]),
                  r=["craw%d" % (k % 2), "idf"], w=["pc%d" % (k % 2)])
                for gl in range(2):
                    sl = slice(64 * gl, 64 * gl + 64)
                    V(lambda e: e.tensor_copy(
                        out=Ct[sl, i, 4 * k:4 * k + 4, :],
                        in_=pc[k % 2][sl, 0:128].rearrange("p (pr gl c) -> p pr gl c", gl=2, c=16)[:, :, gl, :]),
                      r=["pc%d" % (k % 2)], w=["Ct"])

        if STOP <= 2.5:
            return
        Uv = Ubf[:].rearrange("p pr r (s c) -> p pr r s c", c=16)
        for s in range(8):
            pi = 14 - s
            Pr = bc(PW[:, pi, 0, :].unsqueeze(2), [128, 32, 16])
            Pi = bc(PW[:, pi, 1, :].unsqueeze(2), [128, 32, 16])
            vtt(T3[:, 0], BB[:, 0], Pr, ALU.mult)
            vtt(T3[:, 1], BB[:, 1], Pi, ALU.mult)
            V(lambda e: e.tensor_tensor(out=Uv[:, :, 0, s, :], in0=T3[:, 0], in1=T3[:, 1], op=ALU.subtract), r=["Wk"], w=["Ubf"])
            vtt(T3[:, 2], BB[:, 1], Pr, ALU.mult)
            vtt(T3[:, 3], BB[:, 0], Pi, ALU.mult)
            V(lambda e: e.tensor_tensor(out=Uv[:, :, 1, s, :], in0=T3[:, 2], in1=T3[:, 3], op=ALU.add), r=["Wk"], w=["Ubf"])

        if STOP <= 2.7:
            return
        G(lambda e: e.memset(Vpad[:], 0.0), w=["Vpad"])
        G(lambda e: e.memset(Win[:], 0.0), w=["Win"])
        Vv = Vpad[:].rearrange("p pr g r (s c) -> p pr g r s c", c=16)
        Wv = Wout[:].rearrange("p pr r (s c) -> p pr r s c", c=16)
        for s in range(8):
            for which in range(2):
                pi = s if which == 0 else s + 8
                Pr = bc(PW[:, pi, 0, :].unsqueeze(2), [128, 32, 16])
                Pi = bc(PW[:, pi, 1, :].unsqueeze(2), [128, 32, 16])
                vtt(T3[:, 0], Ct[:, 0], Pr, ALU.mult)
                vtt(T3[:, 1], Ct[:, 1], Pi, ALU.mult)
                vtt(T3[:, 0], T3[:, 0], T3[:, 1], ALU.subtract)
                vtt(T3[:, 2], Ct[:, 0], Pi, ALU.mult)
                vtt(T3[:, 3], Ct[:, 1], Pr, ALU.mult)
                vtt(T3[:, 2], T3[:, 2], T3[:, 3], ALU.add)
                if which == 0:
                    for gl in range(2):
                        sl = slice(64 * gl, 64 * gl + 64)
                        V(lambda e: e.tensor_copy(out=Vv[sl, :, gl, 0, s, :], in_=T3[sl, 0]), r=["Wk"], w=["Vpad"])
                        V(lambda e: e.tensor_scalar(out=Vv[sl, :, gl, 1, s, :], in0=T3[sl, 2], scalar1=-1.0, scalar2=None,
                                                    op0=ALU.mult), r=["Wk"], w=["Vpad"])
                else:
                    V(lambda e: e.tensor_copy(out=Wv[:, :, 0, s, :], in_=T3[:, 0]), r=["Wk"], w=["Wout"])
                    V(lambda e: e.tensor_scalar(out=Wv[:, :, 1, s, :], in0=T3[:, 2], scalar1=-1.0, scalar2=None, op0=ALU.mult),
                      r=["Wk"], w=["Wout"])

        if STOP <= 2.9:
            return
        G(lambda e: e.memset(maskLT[:], 1.0), w=["maskLT"])
        G(lambda e: e.affine_select(out=maskLT[:].rearrange("p (s c) -> p s c", c=16), in_=maskLT[:].rearrange("p (s c) -> p s c", c=16),
                                    pattern=[[16, 8], [0, 16]], compare_op=ALU.is_ge, fill=0.0, base=15, channel_multiplier=-1),
          r=["maskLT"], w=["maskLT"])

        if STOP <= 3:
            return
        for pr in range(32):
            for gl in range(2):
                g = 2 * pr + gl
                pmx = pm[g % 2]
                key = "pm%d" % (g % 2)
                P(lambda e: e.matmul(pmx[:, 0:128], lhsT=Ubf[:, pr, 0, :], rhs=Vpad[:, pr, gl, 0, :], start=True, stop=False),
                  r=["Ubf", "Vpad"], w=[key])
                P(lambda e: e.matmul(pmx[:, 0:128], lhsT=Ubf[:, pr, 1, :], rhs=Vpad[:, pr, gl, 1, :], start=False, stop=True),
                  r=["Ubf", "Vpad"], w=[key])
                V(lambda e: e.tensor_tensor(out=Mint[:, g, :], in0=pmx[:, 0:128], in1=maskLT[:], op=ALU.mult), r=[key, "maskLT"], w=["Mint"])
            for ri in range(2):
                pwx = pw[ri]
                key = "pw%d" % ri
                P(lambda e: e.transpose(out=pwx[:, 0:128], in_=Ubf[:, pr, ri, :], identity=idb[:]), r=["Ubf", "idb"], w=[key])
                A(lambda e: e.copy(out=Win[:, pr, 0, ri, 0:64], in_=pwx[:, 0:64]), r=[key], w=["Win"])
                A(lambda e: e.copy(out=Win[:, pr, 1, ri, 64:128], in_=pwx[:, 64:128]), r=[key], w=["Win"])

    if STOP <= 4:
        return
    with ExitStack() as ses:
        NCH = 64
        xh = K.sb(ses, "s5xh", [NCH, 4, D], F32)
        junk = K.sb(ses, "s5junk", [NCH, D], F32)
        useg = K.sb(ses, "s5useg", [NCH, 64, 8, 16], BF16)
        uT = K.sb(ses, "s5uT", [128, 64, NCH], BF16)
        X = K.sb(ses, "s5X", [128, 2, 32, NCH], F32)
        Hbf = K.sb(ses, "s5H", [128, 2, 32, NCH + 1], BF16)
        carry = K.sb(ses, "s5carry", [128, 2, 32], F32)
        t1 = K.sb(ses, "s5t1", [128, 2, 32], F32)
        t2 = K.sb(ses, "s5t2", [128, 2, 32], F32)
        gtok = K.sb(ses, "s5gtok", [NCH, 8, D], BF16)
        ysb = K.sb(ses, "s5ysb", [128, 8, NCH], F32)
        yc = K.sb(ses, "s5yc", [NCH, 8, 128], F32)
        yc2 = K.sb(ses, "s5yc2", [NCH, 8, 128], F32)
        ss = K.sb(ses, "s5ss", [NCH, 8], F32)
        ptr = [K.ps(ses, "s5ptr%d" % i, [128, 16, NCH], BF16) for i in range(2)]
        px = [K.ps(ses, "s5px%d" % i, [128, 4, 2, NCH], F32) for i in range(2)]
        py = K.ps(ses, "s5py", [128, 8, NCH], F32)
        pyt = [K.ps(ses, "s5pyt%d" % i, [NCH, 4, 128], F32) for i in range(2)]

        V(lambda e: e.memset(carry[:], 0.0), w=["carry"])
        xp = I["xp"]
        for seg in range(T // (8 * NCH)):
            rows = xp[seg * 8 * NCH:(seg + 1) * 8 * NCH, :].rearrange("(n s) d -> n s d", s=8)
            for h in range(2):
                dma(xh[:], rows[:, 4 * h:4 * h + 4, :], w=["xh"])
                for s in range(4):
                    A(lambda e: e.activation(out=junk[:], in_=xh[:, s, :], func=AF.Square, accum_out=ss[:, 4 * h + s:4 * h + s + 1]),
                      r=["xh"], w=["junk", "ss"])
                V(lambda e: e.tensor_scalar(out=ss[:, 4 * h:4 * h + 4], in0=ss[:, 4 * h:4 * h + 4], scalar1=1.0 / D, scalar2=EPS,
                                            op0=ALU.mult, op1=ALU.add), r=["ss"], w=["ss"])
                A(lambda e: e.activation(out=ss[:, 4 * h:4 * h + 4], in_=ss[:, 4 * h:4 * h + 4], func=AF.Sqrt), r=["ss"], w=["ss"])
                V(lambda e: e.reciprocal(out=ss[:, 4 * h:4 * h + 4], in_=ss[:, 4 * h:4 * h + 4]), r=["ss"], w=["ss"])
                for s in range(4):
                    V(lambda e: e.scalar_tensor_tensor(out=useg[:, :, 4 * h + s, :], in0=xh[:, s, :].rearrange("n (g c) -> n g c", c=16),
                                                       scalar=ss[:, 4 * h + s:4 * h + s + 1],
                                                       in1=gain0[0:NCH, :].rearrange("n (g c) -> n g c", c=16), op0=ALU.mult, op1=ALU.mult),
                      r=["xh", "ss", "gain0"], w=["useg"])
            if STOP <= 5:
                return
            for g8 in range(8):
                pt_ = ptr[g8 % 2]
                key = "ptr%d" % (g8 % 2)
                for j in range(8):
                    g = 8 * g8 + j
                    P(lambda e: e.transpose(out=pt_[:, j, :], in_=useg[:, g, :, :], identity=idb[0:NCH, 0:NCH]),
                      r=["useg", "idb"], w=[key])
                A(lambda e: e.copy(out=uT[:, 8 * g8:8 * g8 + 8, :], in_=pt_[:, 0:8, :]), r=[key], w=["uT"])
            if STOP <= 6:
                return
            for q in range(8):
                px_ = px[q % 2]
                key = "px%d" % (q % 2)
                for a in range(4):
                    pr = 4 * q + a
                    for ri in range(2):
                        P(lambda e: e.matmul(px_[:, a, ri, :], lhsT=Win[:, pr, 0, ri, :], rhs=uT[:, 2 * pr, :], start=True, stop=False),
                          r=["Win", "uT"], w=[key])
                        P(lambda e: e.matmul(px_[:, a, ri, :], lhsT=Win[:, pr, 1, ri, :], rhs=uT[:, 2 * pr + 1, :], start=False, stop=True),
                          r=["Win", "uT"], w=[key])
                A(lambda e: e.copy(out=X[:, :, 4 * q:4 * q + 4, :], in_=px_[:].rearrange("p a r n -> p r a n")), r=[key], w=["X%d" % (q // 2)])
            if STOP <= 7:
                return
            V(lambda e: e.tensor_copy(out=Hbf[:, :, :, 0], in_=carry[:]), r=["carry"], w=["Hbf"])
            NCHN = 4
            PWD = 32 // NCHN
            for n in range(NCH):
                for c in range(NCHN):
                    cs_ = slice(c * PWD, (c + 1) * PWD)
                    xk, t1k, t2k = "X%d" % c, "t1_%d" % c, "t2_%d" % c
                    Sp = carry[:, :, cs_] if n == 0 else X[:, :, cs_, n - 1]
                    V(lambda e: e.tensor_tensor(out=t1[:, :, cs_], in0=Sp, in1=CA[:, :, cs_], op=ALU.mult), r=[xk, "carry", "CA"], w=[t1k])
                    V(lambda e: e.tensor_tensor(out=t2[:, 0, cs_], in0=Sp[:, 1, :], in1=CBn[:, cs_], op=ALU.mult), r=[xk, "carry", "CBn"], w=[t2k])
                    V(lambda e: e.tensor_tensor(out=t2[:, 1, cs_], in0=Sp[:, 0, :], in1=CBp[:, cs_], op=ALU.mult), r=[xk, "carry", "CBp"], w=[t2k])
                    V(lambda e: e.tensor_tensor(out=t1[:, :, cs_], in0=t1[:, :, cs_], in1=t2[:, :, cs_], op=ALU.add), r=[t1k, t2k], w=[t1k])
                    V(lambda e: e.tensor_tensor(out=X[:, :, cs_, n], in0=X[:, :, cs_, n], in1=t1[:, :, cs_], op=ALU.add), r=[t1k, xk], w=[xk])
            XK = ["X%d" % c for c in range(NCHN)]
            V(lambda e: e.tensor_copy(out=carry[:], in_=X[:, :, :, NCH - 1]), r=XK, w=["carry"])
            A(lambda e: e.copy(out=Hbf[:, :, :, 1:NCH + 1], in_=X[:]), r=XK, w=["Hbf"])
            if STOP <= 8:
                return
            for g8 in range(8):
                for j in range(8):
                    g = 8 * g8 + j
                    pr, gl = g // 2, g % 2
                    sl = slice(64 * gl, 64 * gl + 64)
                    P(lambda e: e.matmul(py[:, j, :], lhsT=Mint[:, g, :], rhs=uT[:, g, :], start=True, stop=False),
                      r=["Mint", "uT"], w=["py"])
                    P(lambda e: e.matmul(py[:, j, :], lhsT=Wout[sl, pr, 0, :], rhs=Hbf[sl, 0, pr, 0:NCH], start=False, stop=False),
                      r=["Wout", "Hbf"], w=["py"])
                    P(lambda e: e.matmul(py[:, j, :], lhsT=Wout[sl, pr, 1, :], rhs=Hbf[sl, 1, pr, 0:NCH], start=False, stop=True),
                      r=["Wout", "Hbf"], w=["py"])
                A(lambda e: e.copy(out=ysb[:], in_=py[:]), r=["py"], w=["ysb"])
                for j in range(8):
                    pyt_ = pyt[j // 4]
                    P(lambda e: e.transpose(out=pyt_[:, j % 4, :], in_=ysb[:, j, :], identity=idf[:]), r=["ysb", "idf"], w=["pyt%d" % (j // 4)])
                cs_ = slice(128 * g8, 128 * g8 + 128)
                for hh in range(2):
                    V(lambda e: e.tensor_copy(
                        out=yc[:, :, 64 * hh:64 * hh + 64].rearrange("n s (j c) -> n s j c", c=16),
                        in_=pyt[hh][:].rearrange("n j (s c) -> n s j c", c=16)), r=["pyt%d" % hh], w=["yc"])
                V(lambda e: e.tensor_tensor(out=yc2[:].rearrange("n s (j c) -> n s j c", c=16),
                                            in0=useg[:, 8 * g8:8 * g8 + 8, :, :].rearrange("n j s c -> n s j c"),
                                            in1=bc(dskip[0:NCH, cs_].unsqueeze(1), [NCH, 8, 128]).rearrange("n s (j c) -> n s j c", c=16), op=ALU.mult),
                  r=["useg", "dskip"], w=["yc2"])
                V(lambda e: e.tensor_tensor(out=yc[:], in0=yc[:], in1=yc2[:], op=ALU.add), r=["yc", "yc2"], w=["yc"])
                V(lambda e: e.tensor_tensor(out=yc2[:], in0=yc[:], in1=yc[:], op=ALU.mult), r=["yc"], w=["yc2"])
                V(lambda e: e.tensor_scalar(out=yc2[:], in0=yc2[:], scalar1=0.044715, scalar2=1.0, op0=ALU.mult, op1=ALU.add), r=["yc2"], w=["yc2"])
                V(lambda e: e.tensor_tensor(out=yc2[:], in0=yc2[:], in1=yc[:], op=ALU.mult), r=["yc", "yc2"], w=["yc2"])
                A(lambda e: e.activation(out=yc2[:], in_=yc2[:], func=AF.Sigmoid, scale=1.5957691216057308), r=["yc2"], w=["yc2"])
                V(lambda e: e.tensor_tensor(out=gtok[:, :, cs_], in0=yc[:], in1=yc2[:], op=ALU.mult), r=["yc", "yc2"], w=["gtok"])
            dma(O["g_scr"][seg * 8 * NCH:(seg + 1) * 8 * NCH, :].rearrange("(n s) d -> n s d", s=8), gtok[:], r=["gtok"], w=["g_scr"],
                add_writer=True)
        stT = K.sb(ses, "s5stT", [32, 2, 128], F32)
        for ri, nm in enumerate(["s5_re_p", "s5_im_p"]):
            P(lambda e: e.transpose(out=py[0:32, ri, :].rearrange("p n -> p n") if False else pyt[0][0:32, ri, :], in_=carry[:, ri, :], identity=idf[:]),
              r=["carry", "idf"], w=["pyt0"])
            V(lambda e: e.tensor_copy(out=stT[:, ri, :], in_=pyt[0][0:32, ri, :]), r=["pyt0"], w=["stT"])
            dma(O[nm].rearrange("(pr gl) p -> pr (gl p)", gl=2), stT[:, ri, :], r=["stT"], w=[nm])


    K.S.barrier()
    _s5_sample(K, C, I, O, Win, Wout, Mint, A1, gain0, dskip)


def gelu_tanh(K, y, ykey, tmp, tkey, out, okey):
    V, A = K.V, K.A
    V(lambda e: e.tensor_tensor(out=tmp, in0=y, in1=y, op=ALU.mult), r=[ykey], w=[tkey])
    V(lambda e: e.tensor_scalar(out=tmp, in0=tmp, scalar1=0.044715, scalar2=1.0, op0=ALU.mult, op1=ALU.add), r=[tkey], w=[tkey])
    V(lambda e: e.tensor_tensor(out=tmp, in0=tmp, in1=y, op=ALU.mult), r=[ykey, tkey], w=[tkey])
    A(lambda e: e.activation(out=tmp, in_=tmp, func=AF.Sigmoid, scale=1.5957691216057308), r=[tkey], w=[tkey])
    V(lambda e: e.tensor_tensor(out=out, in0=y, in1=tmp, op=ALU.mult), r=[ykey, tkey], w=[okey])


def _s5_sample(K, C, I, O, Win, Wout, Mint, A1, gain0, dskip):
    idf, idb = C["idf"], C["idb"]
    V, A, G, P, dma = K.V, K.A, K.G, K.P, K.dma
    N = NS
    with ExitStack() as es:
        xs = K.sb(es, "ss_x", [N, D], F32)
        ss = K.sb(es, "ss_ss", [N, 1], F32)
        us = K.sb(es, "ss_us", [N, D], F32)
        u0 = K.sb(es, "ss_u0", [N, 64, 8, 16], BF16)
        uT0 = K.sb(es, "ss_uT0", [128, 64, N], BF16)
        uT7 = K.sb(es, "ss_uT7", [128, 64, N], BF16)
        st = K.sb(es, "ss_st", [N, 2, 4096], F32)
        stO = st
        H0 = K.sb(es, "ss_H0", [128, 2, 32, N], F32)
        H0b = K.sb(es, "ss_H0b", [128, 2, 32, N], BF16)
        X7 = K.sb(es, "ss_X7", [128, 2, 32, N], F32)
        H1 = K.sb(es, "ss_H1", [128, 2, 32, N], F32)
        t1 = K.sb(es, "ss_t1", [128, 2, 32, N], F32)
        t2 = K.sb(es, "ss_t2", [128, 2, 32, N], F32)
        ys = K.sb(es, "ss_ys", [N, D], F32)
        y2 = K.sb(es, "ss_y2", [N, D], F32)
        junk = y2
        gsb = K.sb(es, "ss_g", [N, D], BF16)
        ptrA = K.ps(es, "ss_ptrA", [128, 64, N], BF16)
        ptrB = K.ps(es, "ss_ptrB", [128, 64, N], BF16)
        pxs = [K.ps(es, "ss_px%d" % i, [128, 16, 2, N], F32) for i in range(2)]
        ph = [K.ps(es, "ss_ph%d" % i, [128, 32, N], F32) for i in range(2)]
        pys = [K.ps(es, "ss_py%d" % i, [N, 32, 16], F32) for i in range(2)]

        dma(xs[:], I["xs"], w=["sxs"])
        dma(st[:, 0, :], I["st_re"], w=["sst"])
        dma(st[:, 1, :], I["st_im"], w=["sst"], add_writer=True)
        A(lambda e: e.activation(out=junk[:], in_=xs[:], func=AF.Square, accum_out=ss[:, 0:1]), r=["sxs"], w=["sy2", "sss"])
        V(lambda e: e.tensor_scalar(out=ss[:], in0=ss[:], scalar1=1.0 / D, scalar2=EPS, op0=ALU.mult, op1=ALU.add), r=["sss"], w=["sss"])
        A(lambda e: e.activation(out=ss[:], in_=ss[:], func=AF.Sqrt), r=["sss"], w=["sss"])
        V(lambda e: e.reciprocal(out=ss[:], in_=ss[:]), r=["sss"], w=["sss"])
        V(lambda e: e.scalar_tensor_tensor(out=us[:], in0=xs[:], scalar=ss[:, 0:1], in1=gain0[0:N, :], op0=ALU.mult, op1=ALU.mult),
          r=["sxs", "sss", "gain0"], w=["sus"])
        usv = us[:].rearrange("n (g c) -> n g c", c=16)
        G(lambda e: e.memset(u0[:], 0.0), w=["su0"])
        V(lambda e: e.tensor_copy(out=u0[:, :, 0, :], in_=usv), r=["sus", "su0"], w=["su0"])
        for g in range(64):
            P(lambda e: e.transpose(out=ptrA[:, g, :], in_=u0[:, g, :, :], identity=idb[0:N, 0:N]), r=["su0", "idb"], w=["sptrA"])
        G(lambda e: e.memset(u0[:], 0.0), w=["su0"])
        V(lambda e: e.tensor_copy(out=u0[:, :, 7, :], in_=usv), r=["sus", "su0"], w=["su0"])
        for g in range(64):
            P(lambda e: e.transpose(out=ptrB[:, g, :], in_=u0[:, g, :, :], identity=idb[0:N, 0:N]), r=["su0", "idb"], w=["sptrB"])
        A(lambda e: e.copy(out=uT0[:], in_=ptrA[:]), r=["sptrA"], w=["suT0"])
        A(lambda e: e.copy(out=uT7[:], in_=ptrB[:]), r=["sptrB"], w=["suT7"])
        for pr in range(32):
            h_, a_ = pr // 16, pr % 16
            for ri in range(2):
                P(lambda e: e.matmul(pxs[h_][:, a_, ri, :], lhsT=Win[:, pr, 0, ri, :], rhs=uT7[:, 2 * pr, :], start=True, stop=False),
                  r=["Win", "suT7"], w=["spx%d" % h_])
                P(lambda e: e.matmul(pxs[h_][:, a_, ri, :], lhsT=Win[:, pr, 1, ri, :], rhs=uT7[:, 2 * pr + 1, :], start=False, stop=True),
                  r=["Win", "suT7"], w=["spx%d" % h_])
        for h_ in range(2):
            A(lambda e: e.copy(out=X7[:, :, 16 * h_:16 * h_ + 16, :], in_=pxs[h_][:].rearrange("p a r n -> p r a n")), r=["spx%d" % h_], w=["sX7"])
        for ri in range(2):
            for pr in range(32):
                P(lambda e: e.transpose(out=ph[ri][:, pr, :], in_=st[:, ri, pr * 128:(pr + 1) * 128], identity=idf[0:N, 0:N]),
                  r=["sst", "idf"], w=["sph%d" % ri])
            V(lambda e: e.tensor_copy(out=H0[:, ri, :, :], in_=ph[ri][:]), r=["sph%d" % ri], w=["sH0"])
        A(lambda e: e.copy(out=H0b[:], in_=H0[:]), r=["sH0"], w=["sH0b"])
        a1r = bc(A1[:, 0, :].unsqueeze(2), [128, 32, N])
        a1i = bc(A1[:, 1, :].unsqueeze(2), [128, 32, N])
        for ri in range(2):
            V(lambda e: e.tensor_tensor(out=t1[:, ri], in0=H0[:, ri], in1=a1r, op=ALU.mult), r=["sH0", "A1"], w=["st1"])
            V(lambda e: e.tensor_tensor(out=t2[:, ri], in0=H0[:, 1 - ri], in1=a1i, op=ALU.mult), r=["sH0", "A1"], w=["st2"])
        V(lambda e: e.tensor_tensor(out=H1[:, 0], in0=t1[:, 0], in1=t2[:, 0], op=ALU.subtract), r=["st1", "st2"], w=["sH1"])
        V(lambda e: e.tensor_tensor(out=H1[:, 1], in0=t1[:, 1], in1=t2[:, 1], op=ALU.add), r=["st1", "st2"], w=["sH1"])
        V(lambda e: e.tensor_tensor(out=H1[:], in0=H1[:], in1=X7[:], op=ALU.add), r=["sH1", "sX7"], w=["sH1"])
        idx = 0
        for ri in range(2):
            for q in range(8):
                b_ = idx % 2
                idx += 1
                pv = pxs[b_][0:N].rearrange("p a r n -> p (a r n)")
                for j in range(4):
                    pr = 4 * q + j
                    P(lambda e: e.transpose(out=pv[:, j * 128:(j + 1) * 128], in_=H1[:, ri, pr, :], identity=idf[:]),
                      r=["sH1", "idf"], w=["spx%d" % b_])
                V(lambda e: e.tensor_copy(out=stO[:, ri, 512 * q:512 * q + 512], in_=pv[:, 0:512]), r=["spx%d" % b_], w=["sst"])
        dma(O["s5_re_s"], stO[:, 0, :], r=["sst"], w=["s5_re_s"])
        dma(O["s5_im_s"], stO[:, 1, :], r=["sst"], w=["s5_im_s"])
        for g in range(64):
            pr, gl = g // 2, g % 2
            sl = slice(64 * gl, 64 * gl + 64)
            h_ = g // 32
            P(lambda e: e.matmul(pys[h_][:, g % 32, :], lhsT=uT0[:, g, :], rhs=Mint[:, g, 0:16], start=True, stop=False),
              r=["suT0", "Mint"], w=["spy%d" % h_])
            P(lambda e: e.matmul(pys[h_][:, g % 32, :], lhsT=H0b[sl, 0, pr, :], rhs=Wout[sl, pr, 0, 0:16], start=False, stop=False),
              r=["sH0b", "Wout"], w=["spy%d" % h_])
            P(lambda e: e.matmul(pys[h_][:, g % 32, :], lhsT=H0b[sl, 1, pr, :], rhs=Wout[sl, pr, 1, 0:16], start=False, stop=True),
              r=["sH0b", "Wout"], w=["spy%d" % h_])
        V(lambda e: e.tensor_tensor(out=y2[:], in0=us[:], in1=dskip[0:N, :], op=ALU.mult), r=["sus", "dskip"], w=["sy2"])
        for h_ in range(2):
            V(lambda e: e.tensor_tensor(out=ys[:, 512 * h_:512 * h_ + 512], in0=pys[h_][:].rearrange("n g c -> n (g c)"),
                                        in1=y2[:, 512 * h_:512 * h_ + 512], op=ALU.add), r=["spy%d" % h_, "sy2"], w=["sys"])
        gelu_tanh(K, ys[:], "sys", y2[:], "sy2", gsb[:], "sgsb")
        dma(O["gs_scr"], gsb[:], r=["sgsb"], w=["gs_scr"])
        V(lambda e: e.tensor_tensor(out=ys[:], in0=ys[:], in1=y2[:], op=ALU.mult), r=["sys", "sy2", "sgsb"], w=["sys"])
        dma(O["gs32_scr"], ys[:], r=["sys"], w=["gs32_scr"])

def load_w_bf16(K, wt, src, ncols, step=1024):
    v = src.rearrange("(k p) n -> p k n", p=128)
    for c0 in range(0, ncols, step):
        c1 = min(ncols, c0 + step)
        K.S.dma("pool", wt[:, :, c0:c1], v[:, :, c0:c1], (), ["w_" + wt.name], add_writer=(c0 > 0))


def rms_to_bf16(K, x_t, xkey, gain, h_t, hkey, junk, ss, sfx, Pn=128):
    V, A = K.V, K.A
    A(lambda e: e.activation(out=junk[0:Pn, :], in_=x_t[0:Pn, :], func=AF.Square, accum_out=ss[0:Pn, 0:1]), r=[xkey], w=["junk" + sfx, "ss" + sfx])
    V(lambda e: e.tensor_scalar(out=ss[0:Pn, 0:1], in0=ss[0:Pn, 0:1], scalar1=1.0 / D, scalar2=EPS, op0=ALU.mult, op1=ALU.add), r=["ss" + sfx], w=["ss" + sfx])
    A(lambda e: e.activation(out=ss[0:Pn, 0:1], in_=ss[0:Pn, 0:1], func=AF.Sqrt), r=["ss" + sfx], w=["ss" + sfx])
    V(lambda e: e.reciprocal(out=ss[0:Pn, 0:1], in_=ss[0:Pn, 0:1]), r=["ss" + sfx], w=["ss" + sfx])
    V(lambda e: e.scalar_tensor_tensor(out=h_t[0:Pn, :], in0=x_t[0:Pn, :], scalar=ss[0:Pn, 0:1], in1=gain[0:Pn, :], op0=ALU.mult, op1=ALU.mult),
      r=[xkey, "ss" + sfx, "gain"], w=[hkey])


def transpose_tiles(K, src, skey, nk, dstT, dkey, ptb, idb, Pn=128):
    for k0 in range(0, nk, 8):
        k1 = min(nk, k0 + 8)
        for k in range(k0, k1):
            K.P(lambda e: e.transpose(out=ptb[:, k - k0, 0:Pn], in_=src[0:Pn, k * 128:(k + 1) * 128], identity=idb[0:Pn, 0:Pn]),
                r=[skey, "idb"], w=["ptb"])
        K.A(lambda e: e.copy(out=dstT[:, k0:k1, 0:Pn], in_=ptb[:, 0:k1 - k0, 0:Pn]), r=["ptb"], w=[dkey])


NTILE = T // 128


def tile_rows(t):
    return (128, slice(t * 128, (t + 1) * 128)) if t < NTILE else (NS, None)


def pick(big, small, t):
    Pn, rs = tile_rows(t)
    return big[rs, :] if rs is not None else small


def glu_stage(K, C, I, O):
    idb = C["idb"]
    V, A, G, P, dma = K.V, K.A, K.G, K.P, K.dma
    with ExitStack() as es:
        wg = K.sb(es, "wglu", [128, 8, 2048], BF16)
        load_w_bf16(K, wg, I["s5_w_glu"], 2048)
        gt = [K.sb(es, "glu_g%d" % i, [128, D], BF16) for i in range(2)]
        xt = [K.sb(es, "glu_x%d" % i, [128, D], F32) for i in range(2)]
        gT = [K.sb(es, "glu_gT%d" % i, [128, 8, 128], BF16) for i in range(2)]
        sg = [K.sb(es, "glu_sg%d" % i, [128, D], F32) for i in range(2)]
        ptb = K.ps(es, "glu_ptb", [128, 8, 128], BF16)
        pa = [K.ps(es, "glu_pa%d" % i, [128, 512], F32) for i in range(4)]
        for t in range(NTILE):
            b = t % 2
            sx = str(b)
            Pn, _ = tile_rows(t)
            dma(gt[b][0:Pn, :], pick(O["g_scr"], O["gs_scr"], t), r=["g_scr", "gs_scr"], w=["gt" + sx])
            dma(xt[b][0:Pn, :], pick(I["xp"], I["xs"], t), w=["xt" + sx])
            transpose_tiles(K, gt[b], "gt" + sx, 8, gT[b], "gT" + sx, ptb, idb, Pn)
            for c in range(4):
                for k in range(8):
                    P(lambda e: e.matmul(pa[c][0:Pn, :], lhsT=gT[b][:, k, 0:Pn], rhs=wg[:, k, c * 512:(c + 1) * 512], start=(k == 0), stop=(k == 7)),
                      r=["gT" + sx, "w_wglu"], w=["pa%d" % c])
            for hh in range(2):
                cs = slice(512 * hh, 512 * hh + 512)
                A(lambda e: e.activation(out=sg[b][0:Pn, cs], in_=pa[2 + hh][0:Pn, :], func=AF.Sigmoid), r=["pa%d" % (2 + hh)], w=["sg" + sx])
                V(lambda e: e.tensor_tensor(out=sg[b][0:Pn, cs], in0=pa[hh][0:Pn, :], in1=sg[b][0:Pn, cs], op=ALU.mult), r=["pa%d" % hh, "sg" + sx], w=["sg" + sx])
            V(lambda e: e.tensor_tensor(out=xt[b][0:Pn, :], in0=xt[b][0:Pn, :], in1=sg[b][0:Pn, :], op=ALU.add), r=["xt" + sx, "sg" + sx], w=["xt" + sx])
            dma(pick(O["y1_scr"], O["y1s_scr"], t), xt[b][0:Pn, :], r=["xt" + sx], w=["y1_scr"], add_writer=True)
    K.S.barrier()


def ffn_stage(K, C, I, O):
    idb = C["idb"]
    V, A, G, P, dma = K.V, K.A, K.G, K.P, K.dma
    FF = 2816
    CW = 352
    with ExitStack() as es:
        w1 = K.sb(es, "ffw1", [128, 8, 2 * FF], BF16)
        w2 = K.sb(es, "ffw2", [128, 22, D], BF16)
        load_w_bf16(K, w1, I["ffn_w_in"], 2 * FF)
        load_w_bf16(K, w2, I["ffn_w_out"], D)
        gain = K.sb(es, "ff_gain", [128, D], F32)
        dma(gain[:], AP(tensor=I["norm_ffn"].tensor, offset=0, ap=[[0, 128], [1, D]]), w=["gain"])
        yt = [K.sb(es, "ff_y%d" % i, [128, D], F32) for i in range(2)]
        hb = [K.sb(es, "ff_h%d" % i, [128, D], BF16) for i in range(2)]
        hT = [K.sb(es, "ff_hT%d" % i, [128, 8, 128], BF16) for i in range(2)]
        act = [K.sb(es, "ff_act%d" % i, [128, FF], BF16) for i in range(2)]
        actT = [K.sb(es, "ff_actT%d" % i, [128, 22, 128], BF16) for i in range(2)]
        sl_ = [K.sb(es, "ff_s%d" % i, [128, CW], F32) for i in range(2)]
        junk = K.sb(es, "ff_junk", [128, D], F32)
        ss = [K.sb(es, "ff_ss%d" % i, [128, 1], F32) for i in range(2)]
        ptb = K.ps(es, "ff_ptb", [128, 8, 128], BF16)
        pa = [K.ps(es, "ff_pa%d" % i, [128, 512], F32) for i in range(2)]
        pb = [K.ps(es, "ff_pb%d" % i, [128, 512], F32) for i in range(2)]
        po = [K.ps(es, "ff_po%d" % i, [128, 512], F32) for i in range(2)]
        for t in range(NTILE):
            b = t % 2
            sx = str(b)
            Pn, _ = tile_rows(t)
            dma(yt[b][0:Pn, :], pick(O["y1_scr"], O["y1s_scr"], t), r=["y1_scr"], w=["yt" + sx])
            rms_to_bf16(K, yt[b], "yt" + sx, gain, hb[b], "hb" + sx, junk, ss[b], sx, Pn)
            transpose_tiles(K, hb[b], "hb" + sx, 8, hT[b], "hT" + sx, ptb, idb, Pn)
            for j in range(8):
                jb = j % 2
                for k in range(8):
                    P(lambda e: e.matmul(pa[jb][0:Pn, 0:CW], lhsT=hT[b][:, k, 0:Pn], rhs=w1[:, k, CW * j:CW * j + CW], start=(k == 0), stop=(k == 7)),
                      r=["hT" + sx, "w_ffw1"], w=["fpa%d" % jb])
                for k in range(8):
                    P(lambda e: e.matmul(pb[jb][0:Pn, 0:CW], lhsT=hT[b][:, k, 0:Pn], rhs=w1[:, k, FF + CW * j:FF + CW * j + CW], start=(k == 0), stop=(k == 7)),
                      r=["hT" + sx, "w_ffw1"], w=["fpb%d" % jb])
                A(lambda e: e.activation(out=sl_[jb][0:Pn, :], in_=pa[jb][0:Pn, 0:CW], func=AF.Silu), r=["fpa%d" % jb], w=["fsl%d" % jb])
                V(lambda e: e.tensor_tensor(out=act[b][0:Pn, CW * j:CW * j + CW], in0=pb[jb][0:Pn, 0:CW], in1=sl_[jb][0:Pn, :], op=ALU.mult),
                  r=["fpb%d" % jb, "fsl%d" % jb], w=["act" + sx])
            transpose_tiles(K, act[b], "act" + sx, 22, actT[b], "actT" + sx, ptb, idb, Pn)
            for c in range(2):
                for k in range(22):
                    P(lambda e: e.matmul(po[c][0:Pn, :], lhsT=actT[b][:, k, 0:Pn], rhs=w2[:, k, c * 512:(c + 1) * 512], start=(k == 0), stop=(k == 21)),
                      r=["actT" + sx, "w_ffw2"], w=["fpo%d" % c])
                V(lambda e: e.tensor_tensor(out=yt[b][0:Pn, c * 512:(c + 1) * 512], in0=po[c][0:Pn, :], in1=yt[b][0:Pn, c * 512:(c + 1) * 512], op=ALU.add),
                  r=["fpo%d" % c, "yt" + sx], w=["yt" + sx])
            dma(pick(O["y2_scr"], O["y2s_scr"], t), yt[b][0:Pn, :], r=["yt" + sx], w=["y2_scr"], add_writer=True)
    K.S.barrier()


def nsa_proj_stage(K, C, I, O):
    idb = C["idb"]
    V, A, G, P, dma = K.V, K.A, K.G, K.P, K.dma
    NCOL = 2608
    with ExitStack() as es:
        wn = K.sb(es, "nsw", [128, 8, NCOL], BF16)
        load_w_bf16(K, wn, I["nsa_w_in"], NCOL)
        gain = K.sb(es, "ns_gain", [128, D], F32)
        dma(gain[:], AP(tensor=I["norm_mix"].tensor, offset=D, ap=[[0, 128], [1, D]]), w=["gain"])
        yt = [K.sb(es, "ns_y%d" % i, [128, D], F32) for i in range(2)]
        hb = [K.sb(es, "ns_h%d" % i, [128, D], BF16) for i in range(2)]
        hT = [K.sb(es, "ns_hT%d" % i, [128, 8, 128], BF16) for i in range(2)]
        kv = [K.sb(es, "ns_kv%d" % i, [128, 1536], F32) for i in range(2)]
        kvb = [K.sb(es, "ns_kvb%d" % i, [128, 1536], BF16) for i in range(2)]
        qb = [K.sb(es, "ns_qb%d" % i, [128, 1024], BF16) for i in range(2)]
        gt_ = [K.sb(es, "ns_gt%d" % i, [128, 48], F32) for i in range(2)]
        junk = K.sb(es, "ns_junk", [128, D], F32)
        ss = [K.sb(es, "ns_ss%d" % i, [128, 1], F32) for i in range(2)]
        ptb = K.ps(es, "ns_ptb", [128, 8, 128], BF16)
        pk = [K.ps(es, "ns_pk%d" % i, [128, 512], F32) for i in range(6)]
        for t in range(NTILE):
            b = t % 2
            sx = str(b)
            Pn, rs = tile_rows(t)
            dma(yt[b][0:Pn, :], pick(O["y2_scr"], O["y2s_scr"], t), r=["y2_scr"], w=["yt" + sx])
            rms_to_bf16(K, yt[b], "yt" + sx, gain, hb[b], "hb" + sx, junk, ss[b], sx, Pn)
            transpose_tiles(K, hb[b], "hb" + sx, 8, hT[b], "hT" + sx, ptb, idb, Pn)
            for c in range(6):
                c0 = 512 * c
                cw = min(512, NCOL - c0)
                for k in range(8):
                    P(lambda e: e.matmul(pk[c][0:Pn, 0:cw], lhsT=hT[b][:, k, 0:Pn], rhs=wn[:, k, c0:c0 + cw], start=(k == 0), stop=(k == 7)),
                      r=["hT" + sx, "w_nsw"], w=["npk%d" % c])
            for c in range(2):
                A(lambda e: e.activation(out=qb[b][0:Pn, 512 * c:512 * c + 512], in_=pk[c][0:Pn, :], func=AF.Identity, scale=0.125),
                  r=["npk%d" % c], w=["qb" + sx])
            for c in range(3):
                A(lambda e: e.copy(out=kv[b][0:Pn, 512 * c:512 * c + 512], in_=pk[2 + c][0:Pn, :]), r=["npk%d" % (2 + c)], w=["kv" + sx])
                V(lambda e: e.tensor_copy(out=kvb[b][0:Pn, 512 * c:512 * c + 512], in_=kv[b][0:Pn, 512 * c:512 * c + 512]), r=["kv" + sx], w=["kvb" + sx])
            A(lambda e: e.activation(out=gt_[b][0:Pn, :], in_=pk[5][0:Pn, 0:48], func=AF.Sigmoid), r=["npk5"], w=["gt_" + sx])
            dma(pick(O["kv_p"], O["kv_s"], t), kv[b][0:Pn, 0:1024], r=["kv" + sx], w=["kv_p"], add_writer=True)
            NSDBG = int(os.environ.get("NSDBG", "0"))
            if not NSDBG & 1:
                dma(pick(O["q_scr"], O["qs_scr"], t), qb[b][0:Pn, :], r=["qb" + sx], w=["q_scr"], add_writer=True)
            if not NSDBG & 2:
                dma(pick(O["kvb_scr"], O["kvbs_scr"], t), kvb[b][0:Pn, :], r=["kvb" + sx], w=["kvb_scr"], add_writer=True)
            if not NSDBG & 4:
                dma(pick(O["gate_scr"], O["gates_scr"], t), gt_[b][0:Pn, :], r=["gt_" + sx], w=["gate_scr"], add_writer=True)
            if rs is None:
                dma(O["win_s"], kv[b][0:Pn, 1024:1536], r=["kv" + sx], w=["win_p"], add_writer=True)
            elif t >= NTILE - 4:
                tt = t - (NTILE - 4)
                dma(O["win_p"][tt * 128:(tt + 1) * 128, :], kv[b][0:Pn, 1024:1536], r=["kv" + sx], w=["win_p"], add_writer=True)
    K.S.barrier()


class Lin32:
    def __init__(self, K, es, C, kcmax, tag):
        self.K, self.C, self.tag = K, C, tag
        self.wch = [K.sb(es, "l32w%s%d" % (tag, i), [128, kcmax, 512], F32) for i in range(2)]
        self.ps = [K.ps(es, "l32p%s%d" % (tag, i), [128, 512], F32) for i in range(2)]
        self.pt = K.ps(es, "l32t%s" % tag, [128, 32, NS], F32)
        self.i = 0

    def transpose(self, src, skey, kc, dstT, dkey):
        K, idf = self.K, self.C["idf"]
        for k in range(kc):
            K.P(lambda e: e.transpose(out=self.pt[:, k, :], in_=src[:, k * 128:(k + 1) * 128], identity=idf[0:NS, 0:NS]),
                r=[skey, "idf"], w=["l32t" + self.tag])
        K.V(lambda e: e.tensor_copy(out=dstT[:, 0:kc, :], in_=self.pt[:, 0:kc, :]), r=["l32t" + self.tag], w=[dkey])

    def run(self, xT, xkey, kc, Wd, ncols, evac):
        K = self.K
        Wv = Wd.rearrange("(k p) n -> p k n", p=128)
        for c0 in range(0, ncols, 512):
            cw = min(512, ncols - c0)
            b = self.i % 2
            self.i += 1
            wk = "l32w%s%d" % (self.tag, b)
            pk = "l32p%s%d" % (self.tag, b)
            K.dma(self.wch[b][:, 0:kc, 0:cw], Wv[:, :, c0:c0 + cw], w=[wk])
            for k in range(kc):
                K.P(lambda e: e.matmul(self.ps[b][0:NS, 0:cw], lhsT=xT[:, k, :], rhs=self.wch[b][:, k, 0:cw], start=(k == 0), stop=(k == kc - 1)),
                    r=[xkey, wk], w=[pk])
            evac(c0, cw, self.ps[b][0:NS, 0:cw], pk)


def rms32(K, x, xkey, gain, out, okey, junk, ss, tag):
    V, A = K.V, K.A
    N = NS
    A(lambda e: e.activation(out=junk[0:N, :], in_=x, func=AF.Square, accum_out=ss[0:N, 0:1]), r=[xkey], w=["junk" + tag, "ss" + tag])
    V(lambda e: e.tensor_scalar(out=ss[0:N, :], in0=ss[0:N, :], scalar1=1.0 / D, scalar2=EPS, op0=ALU.mult, op1=ALU.add), r=["ss" + tag], w=["ss" + tag])
    A(lambda e: e.activation(out=ss[0:N, :], in_=ss[0:N, :], func=AF.Sqrt), r=["ss" + tag], w=["ss" + tag])
    V(lambda e: e.reciprocal(out=ss[0:N, :], in_=ss[0:N, :]), r=["ss" + tag], w=["ss" + tag])
    V(lambda e: e.scalar_tensor_tensor(out=out, in0=x, scalar=ss[0:N, 0:1], in1=gain[0:N, :], op0=ALU.mult, op1=ALU.mult),
      r=[xkey, "ss" + tag, "gain" + tag], w=[okey])


def sample_l0_f32(K, C, I, O):
    V, A, G, P, dma = K.V, K.A, K.G, K.P, K.dma
    N = NS
    FF = 2816
    with ExitStack() as es:
        L = Lin32(K, es, C, 22, "a")
        x = K.sb(es, "f_x", [N, D], F32)
        g32 = K.sb(es, "f_g", [N, D], F32)
        xT = K.sb(es, "f_xT", [128, 22, N], F32)
        big = K.sb(es, "f_big", [N, 2 * FF], F32)
        act = K.sb(es, "f_act", [N, FF], F32)
        h = K.sb(es, "f_h", [N, D], F32)
        junk = K.sb(es, "f_junk", [N, D], F32)
        ss = K.sb(es, "f_ss", [N, 1], F32)
        gain_f = K.sb(es, "f_gainf", [N, D], F32)
        gain_m = K.sb(es, "f_gainm", [N, D], F32)
        qb = K.sb(es, "f_qb", [N, D], BF16)
        kvb = K.sb(es, "f_kvb", [N, 1536], BF16)
        dma(x[:], I["xs"], w=["f_x"])
        dma(g32[:], O["gs32_scr"], r=["gs32_scr"], w=["f_g"])
        dma(gain_f[:], AP(tensor=I["norm_ffn"].tensor, offset=0, ap=[[0, N], [1, D]]), w=["gainf"])
        dma(gain_m[:], AP(tensor=I["norm_mix"].tensor, offset=D, ap=[[0, N], [1, D]]), w=["gainm"])

        def to_big(c0, cw, ps, pk):
            A(lambda e: e.copy(out=big[:, c0:c0 + cw], in_=ps), r=[pk], w=["f_big"])
        L.transpose(g32, "f_g", 8, xT, "f_xT")
        L.run(xT, "f_xT", 8, I["s5_w_glu"], 2048, to_big)
        A(lambda e: e.activation(out=big[:, 1024:2048], in_=big[:, 1024:2048], func=AF.Sigmoid), r=["f_big"], w=["f_big"])
        V(lambda e: e.tensor_tensor(out=big[:, 0:1024], in0=big[:, 0:1024], in1=big[:, 1024:2048], op=ALU.mult), r=["f_big"], w=["f_big"])
        V(lambda e: e.tensor_tensor(out=x[:], in0=x[:], in1=big[:, 0:1024], op=ALU.add), r=["f_big", "f_x"], w=["f_x"])
        rms32(K, x[:], "f_x", gain_f, h[:], "f_h", junk, ss, "f")
        L.transpose(h, "f_h", 8, xT, "f_xT")
        L.run(xT, "f_xT", 8, I["ffn_w_in"], 2 * FF, to_big)
        A(lambda e: e.activation(out=act[:], in_=big[:, 0:FF], func=AF.Silu), r=["f_big"], w=["f_act"])
        V(lambda e: e.tensor_tensor(out=act[:], in0=act[:], in1=big[:, FF:2 * FF], op=ALU.mult), r=["f_big", "f_act"], w=["f_act"])
        L.transpose(act, "f_act", 22, xT, "f_xT")

        def add_x(c0, cw, ps, pk):
            V(lambda e: e.tensor_tensor(out=x[:, c0:c0 + cw], in0=ps, in1=x[:, c0:c0 + cw], op=ALU.add), r=[pk, "f_x"], w=["f_x"])
        L.run(xT, "f_xT", 22, I["ffn_w_out"], D, add_x)
        dma(O["y2s_scr"], x[:], r=["f_x"], w=["y2_scr"], add_writer=True)
        rms32(K, x[:], "f_x", gain_m, h[:], "f_h", junk, ss, "m")
        L.transpose(h, "f_h", 8, xT, "f_xT")
        L.run(xT, "f_xT", 8, I["nsa_w_in"], 2608, to_big)
        A(lambda e: e.activation(out=qb[:], in_=big[:, 0:1024], func=AF.Identity, scale=0.125), r=["f_big"], w=["f_qb"])
        V(lambda e: e.tensor_copy(out=kvb[:], in_=big[:, 1024:2560]), r=["f_big"], w=["f_kvb"])
        A(lambda e: e.activation(out=act[:, 0:48], in_=big[:, 2560:2608], func=AF.Sigmoid), r=["f_big", "f_act"], w=["f_act"])
        dma(O["kv_s"], big[:, 1024:2048], r=["f_big"], w=["kv_p"], add_writer=True)
        dma(O["win_s"], big[:, 2048:2560], r=["f_big"], w=["win_p"], add_writer=True)
        dma(O["qs_scr"], qb[:], r=["f_qb"], w=["q_scr"], add_writer=True)
        dma(O["kvbs_scr"], kvb[:], r=["f_kvb"], w=["kvb_scr"], add_writer=True)
        dma(O["gates_scr"], act[:, 0:48], r=["f_act"], w=["gate_scr"], add_writer=True)
    K.S.barrier()


NEG = -30000.0
NCB = 255


def nsa_attn_prompt(K, C, I, O):
    idf, idb = C["idf"], C["idb"]
    V, A, G, P, dma = K.V, K.A, K.G, K.P, K.dma
    nc = K.nc
    with ExitStack() as es:
        KselE = K.sb(es, "na_KselE", [128, 4, T], BF16)
        KwinE = K.sb(es, "na_KwinE", [128, 4, T], BF16)
        Vsel = K.sb(es, "na_Vsel", [128, NTILE, 4, 65], BF16)
        Vwin = K.sb(es, "na_Vwin", [128, NTILE, 4, 65], BF16)
        kcT = K.sb(es, "na_kcT", [64, 4, 256], BF16)
        vca = K.sb(es, "na_vca", [128, 2, 4, 129], BF16)
        chb = K.sb(es, "na_chb", [128, 16], F32)
        W4 = K.sb(es, "na_W4", [128, 128], F32)
        for g in range(4):
            K.S.dma("pool", KselE[64:128, g, :], I["c_E"], (), ["KselE"], add_writer=True)
            K.S.dma("pool", KwinE[64:128, g, :], I["c_E"], (), ["KwinE"], add_writer=True)
        G(lambda e: e.memset(Vsel[:, :, :, 64:65], 1.0), w=["Vsel"])
        G(lambda e: e.memset(Vwin[:, :, :, 64:65], 1.0), w=["Vwin"])
        G(lambda e: e.memset(vca[:, :, :, 64:65], 1.0), w=["vca"])
        for nt in range(2):
            for g in range(4):
                K.S.dma("pool", vca[:, nt, g, 65:129], I["c_cover"][nt * 128:(nt + 1) * 128, :], (), ["vca"], add_writer=True)
        dma(chb[:], AP(tensor=I["rel_bias"].tensor, offset=31 * 16, ap=[[0, 128], [1, 16]]), w=["chb"])
        G(lambda e: e.memset(W4[:], 0.0), w=["W4"])
        G(lambda e: e.affine_select(out=W4[:], in_=W4[:], pattern=[[-1, 128]], compare_op=ALU.is_ge, fill=NEG, base=0, channel_multiplier=1),
          r=["W4"], w=["W4"])

        with ExitStack() as bes:
            tabs = K.sb(bes, "na_tab", [32, 16], F32)
            ohb = K.sb(bes, "na_ohb", [32, 256], F32)
            Fraw = K.sb(bes, "na_Fraw", [16, 256], F32)
            Fpad = K.sb(bes, "na_Fpad", [16, 384], F32)
            FC = K.sb(bes, "na_FC", [16, 8192], F32)
            pF = K.ps(bes, "na_pF", [128, 512], F32)
            dma(tabs[:], I["rel_bias"], w=["tabs"])
            dma(ohb[:], I["c_ohb"], w=["ohb"])
            P(lambda e: e.matmul(pF[0:16, 0:256], lhsT=tabs[:], rhs=ohb[:], start=True, stop=True), r=["tabs", "ohb"], w=["pF"])
            V(lambda e: e.tensor_copy(out=Fraw[:], in_=pF[0:16, 0:256]), r=["pF"], w=["Fraw"])
            G(lambda e: e.memset(Fpad[:], NEG), w=["Fpad"])
            V(lambda e: e.tensor_scalar(out=Fpad[:, 127:383], in0=Fraw[:], scalar1=Fraw[:, 255:256], scalar2=None, op0=ALU.subtract),
              r=["Fraw", "Fpad"], w=["Fpad"])
            dma(O["fpad_scr"], Fpad[:], r=["Fpad"], w=["fpad_scr"])
            G(lambda e: e.memset(FC[:, 0:4111], NEG), w=["FC"])
            V(lambda e: e.tensor_copy(out=FC[:, 4111:4367], in_=Fraw[:]), r=["Fraw", "FC"], w=["FC"])
            V(lambda e: e.tensor_copy(out=FC[:, 4367:8192], in_=bc(Fraw[:, 255:256], [16, 8192 - 4367])), r=["Fraw", "FC"], w=["FC"])
            dma(O["fc_scr"], FC[:], r=["FC"], w=["fc_scr"])

        K.S.barrier()
        with ExitStack() as ces:
            KcT = K.sb(ces, "na_KcT", [64, 2, 4, T], BF16)
            kvt = [K.sb(ces, "na_kvt%d" % i, [128, 1536], BF16) for i in range(2)]
            W1 = K.sb(ces, "na_W1", [64, 2, 32, 128], BF16)
            W2 = K.sb(ces, "na_W2", [128, 2, 64], BF16)
            pe_f = K.sb(ces, "na_pef", [32, 2, 64], F32)
            pe_b = K.sb(ces, "na_peb", [32, 2, 64], BF16)
            peT = K.sb(ces, "na_peT", [64, 2, 32], BF16)
            cv = K.sb(ces, "na_cv", [128, 2], F32)
            pre = K.sb(ces, "na_pre", [128, 256], F32)
            tmpg = K.sb(ces, "na_tmpg", [128, 256], F32)
            hidT = K.sb(ces, "na_hidT", [128, 256], BF16)
            ptk = [K.ps(ces, "na_ptk%d" % i, [64, 8, 128], BF16) for i in range(2)]
            ph = K.ps(ces, "na_ph", [128, 512], F32)
            pk2 = K.ps(ces, "na_pk2", [128, 512], F32)
            for kvi in range(2):
                K.S.dma("pool", W1[:, kvi, :, :], I["nsa_phi_w1"][kvi].rearrange("l d e -> d l e"), (), ["naW1"], add_writer=True)
                K.S.dma("pool", W2[:, kvi, :], I["nsa_phi_w2"][kvi], (), ["naW2"], add_writer=True)
            dma(pe_f[:], I["nsa_phi_pe"].rearrange("k l d -> l k d"), w=["pe_f"])
            V(lambda e: e.tensor_copy(out=pe_b[:], in_=pe_f[:]), r=["pe_f"], w=["pe_b"])
            for kvi in range(2):
                P(lambda e: e.transpose(out=ptk[0][:, kvi, 0:32], in_=pe_b[:, kvi, :], identity=idb[0:32, 0:32]), r=["pe_b", "idb"], w=["ptk0"])
            V(lambda e: e.tensor_copy(out=peT[:], in_=ptk[0][:, 0:2, 0:32]), r=["ptk0"], w=["peT"])
            for kvi in range(2):
                for l in range(32):
                    P(lambda e: e.matmul(ph[:, kvi:kvi + 1], lhsT=W1[:, kvi, l, :], rhs=peT[:, kvi, l:l + 1], start=(l == 0), stop=(l == 31)),
                      r=["naW1", "peT"], w=["ph"])
            V(lambda e: e.tensor_copy(out=cv[:], in_=ph[:, 0:2]), r=["ph"], w=["cv"])
            G(lambda e: e.memset(hidT[:], 0.0), w=["hidT"])
            for t in range(NTILE):
                b = t % 2
                cs = slice(t * 128, (t + 1) * 128)
                dma(kvt[b][:], O["kvb_scr"][cs, :], r=["kvb_scr"], w=["kvt%d" % b])
                for j in range(8):
                    P(lambda e: e.transpose(out=ptk[0][:, j, :], in_=kvt[b][:, j * 64:(j + 1) * 64], identity=idb[:]), r=["kvt%d" % b, "idb"], w=["ptk0"])
                A(lambda e: e.copy(out=KcT[:, :, :, cs], in_=ptk[0][:].rearrange("p (k g) n -> p k g n", g=4)), r=["ptk0"], w=["KcT"])
                for j in range(4):
                    P(lambda e: e.transpose(out=ptk[1][:, j, :], in_=kvt[b][:, 512 + j * 64:512 + (j + 1) * 64], identity=idb[:]), r=["kvt%d" % b, "idb"], w=["ptk1"])
                    P(lambda e: e.transpose(out=ptk[1][:, 4 + j, :], in_=kvt[b][:, 1024 + j * 64:1024 + (j + 1) * 64], identity=idb[:]), r=["kvt%d" % b, "idb"], w=["ptk1"])
                V(lambda e: e.tensor_copy(out=KselE[0:64, :, cs], in_=ptk[1][:, 0:4, :]), r=["ptk1"], w=["KselE"])
                V(lambda e: e.tensor_copy(out=KwinE[0:64, :, cs], in_=ptk[1][:, 4:8, :]), r=["ptk1", "KselE"], w=["KwinE"])
                G(lambda e: e.tensor_copy(out=Vsel[:, t, :, 0:64], in_=kvt[b][:, 768:1024].rearrange("p (g d) -> p g d", d=64)), r=["kvt%d" % b], w=["Vsel"])
                G(lambda e: e.tensor_copy(out=Vwin[:, t, :, 0:64], in_=kvt[b][:, 1280:1536].rearrange("p (g d) -> p g d", d=64)), r=["kvt%d" % b], w=["Vwin"])
            for kvi in range(2):
                for g in range(4):
                    for l in range(32):
                        P(lambda e: e.matmul(ph[:, 0:NCB], lhsT=W1[:, kvi, l, :], rhs=KcT[:, kvi, g, l:l + 16 * (NCB - 1) + 1:16],
                                             start=(l == 0), stop=(l == 31)), r=["naW1", "KcT"], w=["ph"])
                    V(lambda e: e.tensor_scalar(out=pre[:, 0:NCB], in0=ph[:, 0:NCB], scalar1=cv[:, kvi:kvi + 1], scalar2=None, op0=ALU.add),
                      r=["ph", "cv"], w=["pre"])
                    gelu_tanh(K, pre[:, 0:NCB], "pre", tmpg[:, 0:NCB], "tmpg", hidT[:, 255:0:-1], "hidT")
                    if kvi == 0:
                        P(lambda e: e.matmul(pk2[0:64, 0:256], lhsT=W2[:, 0, :], rhs=hidT[:], start=True, stop=True), r=["naW2", "hidT"], w=["pk2"])
                        V(lambda e: e.tensor_copy(out=kcT[:, g, :], in_=pk2[0:64, 0:256]), r=["pk2"], w=["kcT"])
                    else:
                        for nt in range(2):
                            P(lambda e: e.matmul(pk2[:, 64 * nt:64 * nt + 64], lhsT=hidT[:, nt * 128:(nt + 1) * 128], rhs=W2[:, 1, :], start=True, stop=True),
                              r=["naW2", "hidT"], w=["pk2"])
                        V(lambda e: e.tensor_copy(out=vca[:, :, g, 0:64], in_=pk2[:, 0:128].rearrange("p (n f) -> p n f", f=64)), r=["pk2"], w=["vca"])
        K.S.barrier()

        with ExitStack() as qes:
            wo = K.sb(qes, "na_wo", [128, 8, D], BF16)
            biasD = K.sb(qes, "na_biasD", [128, 16, 256], F32)
            load_w_bf16(K, wo, I["nsa_w_o"], D)
            with ExitStack() as hes:
                hk = K.sb(hes, "na_hk", [128, 16, 256], F32)
                Jm = K.sb(hes, "na_J", [128, 128], F32)
                pJ = K.ps(hes, "na_pJ", [128, 512], F32)
                dma(hk[:], AP(tensor=O["fpad_scr"].tensor, offset=0, ap=[[1, 128], [384, 16], [1, 256]]), r=["fpad_scr"], w=["hk"])
                G(lambda e: e.memset(Jm[:], 1.0), w=["Jm"])
                G(lambda e: e.affine_select(out=Jm[:], in_=Jm[:], pattern=[[1, 128]], compare_op=ALU.is_equal, fill=0.0, base=-127, channel_multiplier=1),
                  r=["Jm"], w=["Jm"])
                hkf = hk[:].rearrange("p h x -> p (h x)")
                bDf = biasD[:].rearrange("p h x -> p (h x)")
                for c in range(8):
                    P(lambda e: e.matmul(pJ[:], lhsT=Jm[:], rhs=hkf[:, c * 512:(c + 1) * 512], start=True, stop=True), r=["Jm", "hk"], w=["pJ"])
                    V(lambda e: e.tensor_copy(out=bDf[:, c * 512:(c + 1) * 512], in_=pJ[:]), r=["pJ"], w=["biasD"])
            K.S.barrier()
            qt_sb = [K.sb(qes, "na_q%d" % i, [128, D], BF16) for i in range(2)]
            gate = [K.sb(qes, "na_gate%d" % i, [128, 48], F32) for i in range(2)]
            yres = [K.sb(qes, "na_y%d" % i, [128, D], F32) for i in range(2)]
            bC = [[K.sb(qes, "na_bC%d_%d" % (i, n), [128, 16, 128], F32) for n in range(2)] for i in range(2)]
            QSel = K.sb(qes, "na_QSel", [128, 16, 128], BF16)
            QWin = K.sb(qes, "na_QWin", [128, 16, 128], BF16)
            NBUF = 3
            sc = [K.sb(qes, "na_sc%d" % i, [128, 4, 128], F32) for i in range(NBUF)]
            PT = [K.sb(qes, "na_PT%d" % i, [128, 4, 128], BF16) for i in range(NBUF)]
            rc = [K.sb(qes, "na_rc%d" % i, [128, 3, 4], F32) for i in range(2)]
            ocs = [K.sb(qes, "na_ocs%d" % i, [128, 4, 64], F32) for i in range(2)]
            imp = K.sb(qes, "na_imp", [128, 64], F32)
            wk1 = K.sb(qes, "na_wk1", [128, 64], F32)
            m8 = K.sb(qes, "na_m8", [128, 16], F32)
            selm = K.sb(qes, "na_selm", [128, 64], F32)
            tmpm = K.sb(qes, "na_tmpm", [128, 64], F32)
            mbw = K.sb(qes, "na_mbw", [128, 4, 128], BF16)
            otile = [K.sb(qes, "na_o%d" % i, [128, D], BF16) for i in range(2)]
            oacc = K.sb(qes, "na_oacc", [128, 64], F32)
            oT = K.sb(qes, "na_oT", [128, 8, 128], BF16)
            psb = [K.ps(qes, "na_ps%d" % i, [128, 4, 128], F32) for i in range(NBUF)]
            posel = K.ps(qes, "na_posel", [128, 512], F32)
            powin = K.ps(qes, "na_powin", [128, 512], F32)
            poc = K.ps(qes, "na_poc", [128, 512], F32)
            pimp = K.ps(qes, "na_pimp", [128, 4, 128], F32)
            pmisc = K.ps(qes, "na_pmisc", [128, 8, 128], BF16)
            posel_v = posel[:, 0:260].rearrange("p (h c) -> p h c", c=65)
            powin_v = powin[:, 0:260].rearrange("p (h c) -> p h c", c=65)
            poc_v = poc[:, 0:260].rearrange("p (h c) -> p h c", c=65)

            G(lambda e: e.memset(mbw[:], 0.0), w=["mbw"])
            V(lambda e: e.tensor_copy(out=QWin[64:128, :, :], in_=bc(chb[64:128, :].unsqueeze(2), [64, 16, 128])), r=["chb"],
              w=["QWin0", "QWin1", "QWin2", "QWin3"])
            sidx = [0]

            def emit_qk(blk):
                lhsT_full, g, kt, qrhs, qkey = blk["lhsT"], blk["g"], blk["kt"], blk["qrhs"], blk["qkey"]
                i = sidx[0] % NBUF
                sidx[0] += 1
                blk["i"] = i
                ks = slice(kt * 128, (kt + 1) * 128)
                P(lambda e: e.matmul(psb[i][:], lhsT=lhsT_full[:, g, ks], rhs=qrhs[:, 4 * g:4 * g + 4, :], start=True, stop=True),
                  r=["KselE", "KwinE", qkey], w=["psb%d" % i])

            def emit_exp_pv(blk):
                i, g, kt = blk["i"], blk["g"], blk["kt"]
                bias_ap = blk["bias"]
                if bias_ap is not None:
                    V(lambda e: e.tensor_tensor(out=sc[i][:], in0=psb[i][:], in1=bias_ap, op=ALU.add), r=["psb%d" % i, "biasD", "W4"], w=["sc%d" % i])
                    A(lambda e: e.activation(out=PT[i][:], in_=sc[i][:], func=AF.Exp), r=["sc%d" % i], w=["PT%d" % i])
                else:
                    A(lambda e: e.activation(out=PT[i][:], in_=psb[i][:], func=AF.Exp), r=[], w=["PT%d" % i, "psb%d" % i])
                for h in range(4):
                    P(lambda e: e.matmul(blk["po_v"][:, h, :], lhsT=PT[i][:, h, :], rhs=blk["vaug"][:, kt, g, :], start=(blk["first"] and h == 0),
                                         stop=blk["last"], skip_group_check=True), r=["PT%d" % i, "Vsel", "Vwin"], w=[blk["pokey"]])

            def tile_loads(qt):
                b = qt % 2
                sx = str(b)
                rs = slice(qt * 128, (qt + 1) * 128)
                dma(qt_sb[b][:], O["q_scr"][rs, :], r=["q_scr"], w=["qsb" + sx])
                dma(gate[b][:], O["gate_scr"][rs, :], r=["gate_scr"], w=["gate" + sx])
                dma(yres[b][:], O["y2_scr"][rs, :], r=["y2_scr"], w=["yres" + sx])
                nts = [1] if qt <= 15 else [0, 1]
                for nt in nts:
                    dma(bC[b][nt][:], AP(tensor=O["fc_scr"].tensor, offset=128 * qt + 2048 * nt, ap=[[16, 128], [8192, 16], [1, 128]]),
                        r=["fc_scr"], w=["bC%s_%d" % (sx, nt)])

            def phase_a(qt, g):
                b = qt % 2
                sx = str(b)
                gp = g % 2
                nts = [1] if qt <= 15 else [0, 1]
                qk, wk_ = "QSel%d" % g, "QWin%d" % g
                for h in range(4):
                    hh = 4 * g + h
                    P(lambda e: e.transpose(out=pmisc[0:64, h, :], in_=qt_sb[b][:, hh * 64:(hh + 1) * 64], identity=idb[:]), r=["qsb" + sx, "idb"], w=["pmisc"])
                A(lambda e: e.copy(out=QSel[0:64, 4 * g:4 * g + 4, :], in_=pmisc[0:64, 0:4, :]), r=["pmisc"], w=[qk])
                V(lambda e: e.tensor_copy(out=QWin[0:64, 4 * g:4 * g + 4, :], in_=QSel[0:64, 4 * g:4 * g + 4, :]), r=[qk], w=[wk_])
                for ni, nt in enumerate(nts):
                    i = sidx[0] % NBUF
                    sidx[0] += 1
                    P(lambda e: e.matmul(psb[i][:], lhsT=kcT[:, g, nt * 128:(nt + 1) * 128], rhs=QSel[0:64, 4 * g:4 * g + 4, :], start=True, stop=True),
                      r=["kcT", qk], w=["psb%d" % i])
                    V(lambda e: e.tensor_tensor(out=sc[i][:], in0=psb[i][:], in1=bC[b][nt][:, 4 * g:4 * g + 4, :], op=ALU.add),
                      r=["psb%d" % i, "bC%s_%d" % (sx, nt)], w=["sc%d" % i])
                    A(lambda e: e.activation(out=PT[i][:], in_=sc[i][:], func=AF.Exp), r=["sc%d" % i], w=["PT%d" % i])
                    for h in range(4):
                        P(lambda e: e.matmul(poc_v[:, h, :], lhsT=PT[i][:, h, :], rhs=vca[:, nt, g, 0:65], start=(ni == 0 and h == 0),
                                             stop=(ni == len(nts) - 1), skip_group_check=True), r=["PT%d" % i, "vca"], w=["poc"])
                        P(lambda e: e.matmul(pimp[:, h, 0:64], lhsT=PT[i][:, h, :], rhs=vca[:, nt, g, 65:129], start=(ni == 0 and h == 0),
                                             stop=(ni == len(nts) - 1), skip_group_check=True), r=["PT%d" % i, "vca"], w=["pimp"])
                rk = "rc%d" % gp
                V(lambda e: e.tensor_scalar(out=rc[gp][:, 0, :], in0=poc_v[:, :, 64], scalar1=1e-30, scalar2=None, op0=ALU.add), r=["poc"], w=[rk])
                V(lambda e: e.reciprocal(out=rc[gp][:, 0, :], in_=rc[gp][:, 0, :]), r=[rk], w=[rk])
                V(lambda e: e.tensor_scalar(out=imp[:], in0=pimp[:, 0, 0:64], scalar1=rc[gp][:, 0, 0:1], scalar2=None, op0=ALU.mult), r=["pimp", rk], w=["imp"])
                for h in range(1, 4):
                    V(lambda e: e.scalar_tensor_tensor(out=imp[:], in0=pimp[:, h, 0:64], scalar=rc[gp][:, 0, h:h + 1], in1=imp[:], op0=ALU.mult, op1=ALU.add),
                      r=["pimp", rk, "imp"], w=["imp"])
                V(lambda e: e.tensor_tensor(out=ocs[gp][:], in0=poc_v[:, :, 0:64], in1=bc(rc[gp][:, 0, :].unsqueeze(2), [128, 4, 64]), op=ALU.mult),
                  r=["poc", rk], w=["ocs%d" % gp])
                V(lambda e: e.memset(imp[:, 0:1], 1e4), r=["imp"], w=["imp"])
                V(lambda e: e.memset(imp[:, 2 * qt:2 * qt + 1], 1e4), r=["imp"], w=["imp"])
                if qt > 0:
                    V(lambda e: e.memset(imp[0:64, 2 * qt - 1:2 * qt], 1e4), r=["imp"], w=["imp"])
                V(lambda e: e.memset(imp[64:128, 2 * qt + 1:2 * qt + 2], 1e4), r=["imp"], w=["imp"])
                V(lambda e: e.memset(imp[0:64, 2 * qt + 1:2 * qt + 2], -1e30), r=["imp"], w=["imp"])
                if 2 * qt + 2 < 64:
                    V(lambda e: e.memset(imp[:, 2 * qt + 2:64], -1e30), r=["imp"], w=["imp"])
                V(lambda e: e.max(out=m8[:, 0:8], in_=imp[:]), r=["imp"], w=["m8"])
                V(lambda e: e.match_replace(out=wk1[:], in_to_replace=m8[:, 0:8], in_values=imp[:], imm_value=-3e38), r=["imp", "m8"], w=["wk1"])
                V(lambda e: e.max(out=m8[:, 8:16], in_=wk1[:]), r=["wk1"], w=["m8"])
                V(lambda e: e.tensor_scalar(out=selm[:], in0=imp[:], scalar1=m8[:, 15:16], scalar2=None, op0=ALU.is_ge), r=["imp", "m8"], w=["selm"])
                V(lambda e: e.tensor_scalar(out=tmpm[:], in0=selm[:], scalar1=-NEG, scalar2=NEG, op0=ALU.mult, op1=ALU.add), r=["selm"], w=["tmpm"])
                for h in range(4):
                    V(lambda e: e.scalar_tensor_tensor(out=mbw[:, h, 64:128], in0=selm[:], scalar=chb[:, 4 * g + h:4 * g + h + 1], in1=tmpm[:],
                                                       op0=ALU.mult, op1=ALU.add), r=["selm", "tmpm", "chb"], w=["mbw"])
                for h in range(4):
                    P(lambda e: e.transpose(out=pmisc[:, h, :], in_=mbw[:, h, :], identity=idb[:]), r=["mbw", "idb"], w=["pmisc"])
                A(lambda e: e.copy(out=QSel[64:128, 4 * g:4 * g + 4, :], in_=pmisc[64:128, 0:4, :]), r=["pmisc"], w=[qk])

            def phase_b(qt, g):
                b = qt % 2
                sx = str(b)
                gp = g % 2
                rk = "rc%d" % gp
                blocks = []
                for kt in range(qt + 1):
                    d = qt - kt
                    bias_ap = biasD[:, 4 * g:4 * g + 4, 128 * d:128 * d + 128] if d <= 1 else None
                    blocks.append(dict(lhsT=KselE, g=g, kt=kt, qrhs=QSel, qkey="QSel%d" % g, bias=bias_ap, vaug=Vsel, po_v=posel_v, pokey="posel",
                                       first=(kt == 0), last=(kt == qt)))
                k0 = max(0, qt - 4)
                for kt in range(k0, qt + 1):
                    d = qt - kt
                    if d <= 1:
                        bias_ap = biasD[:, 4 * g:4 * g + 4, 128 * d:128 * d + 128]
                    elif d == 4:
                        bias_ap = bc(W4[:].unsqueeze(1), [128, 4, 128])
                    else:
                        bias_ap = None
                    blocks.append(dict(lhsT=KwinE, g=g, kt=kt, qrhs=QWin, qkey="QWin%d" % g, bias=bias_ap, vaug=Vwin, po_v=powin_v, pokey="powin",
                                       first=(kt == k0), last=(kt == qt)))
                n = len(blocks)
                LA = NBUF - 1
                for i in range(n + LA):
                    if i < n:
                        emit_qk(blocks[i])
                    if i >= LA:
                        emit_exp_pv(blocks[i - LA])
                V(lambda e: e.tensor_scalar(out=rc[gp][:, 1, :], in0=posel_v[:, :, 64], scalar1=1e-30, scalar2=None, op0=ALU.add), r=["posel"], w=[rk])
                V(lambda e: e.tensor_scalar(out=rc[gp][:, 2, :], in0=powin_v[:, :, 64], scalar1=1e-30, scalar2=None, op0=ALU.add), r=["powin"], w=[rk])
                V(lambda e: e.reciprocal(out=rc[gp][:, 1:3, :], in_=rc[gp][:, 1:3, :]), r=[rk], w=[rk])
                V(lambda e: e.memset(rc[gp][:, 0, :], 1.0), r=[rk, "ocs%d" % gp], w=[rk])
                V(lambda e: e.tensor_tensor(out=rc[gp][:], in0=rc[gp][:], in1=gate[b][:].rearrange("p (r h) -> p r h", h=16)[:, :, 4 * g:4 * g + 4], op=ALU.mult),
                  r=[rk, "gate" + sx], w=[rk])
                for h in range(4):
                    hh = 4 * g + h
                    V(lambda e: e.tensor_scalar(out=oacc[:], in0=ocs[gp][:, h, :], scalar1=rc[gp][:, 0, h:h + 1], scalar2=None, op0=ALU.mult),
                      r=["ocs%d" % gp, rk], w=["oacc"])
                    V(lambda e: e.scalar_tensor_tensor(out=oacc[:], in0=posel_v[:, h, 0:64], scalar=rc[gp][:, 1, h:h + 1], in1=oacc[:], op0=ALU.mult, op1=ALU.add),
                      r=["posel", rk, "oacc"], w=["oacc"])
                    V(lambda e: e.scalar_tensor_tensor(out=otile[b][:, hh * 64:(hh + 1) * 64], in0=powin_v[:, h, 0:64], scalar=rc[gp][:, 2, h:h + 1], in1=oacc[:],
                                                       op0=ALU.mult, op1=ALU.add), r=["powin", rk, "oacc"], w=["otile" + sx])

            def tile_end(qt):
                b = qt % 2
                sx = str(b)
                rs = slice(qt * 128, (qt + 1) * 128)
                for k in range(8):
                    P(lambda e: e.transpose(out=pmisc[:, k, :], in_=otile[b][:, k * 128:(k + 1) * 128], identity=idb[:]), r=["otile" + sx, "idb"], w=["pmisc"])
                A(lambda e: e.copy(out=oT[:], in_=pmisc[:]), r=["pmisc"], w=["oT"])
                for c in range(2):
                    pso = psb[c][:].rearrange("p h n -> p (h n)")
                    for k in range(8):
                        P(lambda e: e.matmul(pso, lhsT=oT[:, k, :], rhs=wo[:, k, c * 512:(c + 1) * 512], start=(k == 0), stop=(k == 7)),
                          r=["oT", "w_na_wo"], w=["psb%d" % c])
                    V(lambda e: e.tensor_tensor(out=yres[b][:, c * 512:(c + 1) * 512], in0=pso, in1=yres[b][:, c * 512:(c + 1) * 512], op=ALU.add),
                      r=["psb%d" % c, "yres" + sx], w=["yres" + sx])
                dma(O["y3_scr"][rs, :], yres[b][:], r=["yres" + sx], w=["y3_scr"], add_writer=True)

            items = [(qt, g) for qt in range(NTILE) for g in range(4)]
            tile_loads(0)
            phase_a(*items[0])
            for ii, (qt, g) in enumerate(items):
                if ii + 1 < len(items):
                    nqt, ng = items[ii + 1]
                    if ng == 0:
                        tile_loads(nqt)
                    phase_a(nqt, ng)
                phase_b(qt, g)
                if g == 3:
                    tile_end(qt)
    K.S.barrier()


NKT_S = 17
NWT_S = 5
OFFC = 4111


def nsa_attn_sample(K, C, I, O):
    idf, idb = C["idf"], C["idb"]
    V, A, G, P, dma = K.V, K.A, K.G, K.P, K.dma
    N = NS
    with ExitStack() as es:
        W1 = K.sb(es, "sa_W1", [64, 2, 32, 128], BF16)
        W2 = K.sb(es, "sa_W2", [128, 2, 64], BF16)
        cv = K.sb(es, "sa_cv", [128, 2], F32)
        biasC = K.sb(es, "sa_bC", [128, 16], F32)
        biasS = K.sb(es, "sa_bS", [128, NKT_S, 16], F32)
        biasW = K.sb(es, "sa_bW", [128, NWT_S, 16], F32)
        QT = K.sb(es, "sa_QT", [128, 16, N], BF16)
        idx = K.sb(es, "sa_idx", [128, N * 16], I32)
        covs = K.sb(es, "sa_cov", [128, 64], BF16)
        Erow = K.sb(es, "sa_E", [128, NKT_S * 128], BF16)
        ones4 = K.sb(es, "sa_ones4", [1, 4], F32)
        for kvi in range(2):
            K.S.dma("pool", W1[:, kvi, :, :], I["nsa_phi_w1"][kvi].rearrange("l d e -> d l e"), (), ["saW1"], add_writer=True)
            K.S.dma("pool", W2[:, kvi, :], I["nsa_phi_w2"][kvi], (), ["saW2"], add_writer=True)
        K.S.dma("pool", covs[:], I["c_cover_s"], (), ["covs"])
        K.S.dma("pool", Erow[64:128, :], I["c_E"][:, 0:NKT_S * 128], (), ["Erow"])
        V(lambda e: e.memset(ones4[:], 1.0), w=["ones4"])
        with ExitStack() as bes:
            pe_f = K.sb(bes, "sa_pef", [32, 2, 64], F32)
            pe_b = K.sb(bes, "sa_peb", [32, 2, 64], BF16)
            peT = K.sb(bes, "sa_peT", [64, 2, 32], BF16)
            hk = K.sb(bes, "sa_hk", [128, NKT_S + NWT_S, 16], F32)
            Jm = K.sb(bes, "sa_J", [128, 128], F32)
            pti = K.sb(bes, "sa_pti", [128, N * 16], I32)
            ptf = K.sb(bes, "sa_ptf", [128, N * 16], F32)
            iot = K.sb(bes, "sa_iot", [128, 1], F32)
            qs = K.sb(bes, "sa_qs", [N, D], BF16)
            pA = K.ps(bes, "sa_pA", [128, 1024], BF16)
            pB = K.ps(bes, "sa_pB", [128, 512], F32)
            dma(pe_f[:], I["nsa_phi_pe"].rearrange("k l d -> l k d"), w=["pe_f"])
            V(lambda e: e.tensor_copy(out=pe_b[:], in_=pe_f[:]), r=["pe_f"], w=["pe_b"])
            for kvi in range(2):
                P(lambda e: e.transpose(out=pA[0:64, kvi * 32:kvi * 32 + 32], in_=pe_b[:, kvi, :], identity=idb[0:32, 0:32]), r=["pe_b", "idb"], w=["pA"])
            V(lambda e: e.tensor_copy(out=peT[:].rearrange("p k l -> p (k l)"), in_=pA[0:64, 0:64]), r=["pA"], w=["peT"])
            for kvi in range(2):
                for l in range(32):
                    P(lambda e: e.matmul(pB[:, kvi:kvi + 1], lhsT=W1[:, kvi, l, :], rhs=peT[:, kvi, l:l + 1], start=(l == 0), stop=(l == 31)),
                      r=["saW1", "peT"], w=["pB"])
            V(lambda e: e.tensor_copy(out=cv[:], in_=pB[:, 0:2]), r=["pB"], w=["cv"])
            bC2 = K.sb(bes, "sa_bC2", [128, 16, 2], F32)
            dma(bC2[:], AP(tensor=O["fc_scr"].tensor, offset=OFFC + 2017 - 16 * 127, ap=[[16, 128], [8192, 16], [1, 2]]), r=["fc_scr"], w=["bC2"])
            V(lambda e: e.tensor_copy(out=biasC[:], in_=bC2[:, :, 0]), r=["bC2"], w=["biasC"])
            hk2 = K.sb(bes, "sa_hk2", [128, NKT_S + NWT_S, 16, 2], F32)
            for kt in range(NKT_S):
                dma(hk2[:, kt, :, :], AP(tensor=O["fc_scr"].tensor, offset=OFFC + 2048 - 128 * kt - 127, ap=[[1, 128], [8192, 16], [1, 2]]),
                    r=["fc_scr"], w=["hk2"], add_writer=True)
            for wt in range(NWT_S):
                dma(hk2[:, NKT_S + wt, :, :], AP(tensor=O["fc_scr"].tensor, offset=OFFC + 512 - 128 * wt - 127, ap=[[1, 128], [8192, 16], [1, 2]]),
                    r=["fc_scr"], w=["hk2"], add_writer=True)
            V(lambda e: e.tensor_copy(out=hk[:], in_=hk2[:, :, :, 0]), r=["hk2"], w=["hk"])
            G(lambda e: e.memset(Jm[:], 1.0), w=["Jm"])
            G(lambda e: e.affine_select(out=Jm[:], in_=Jm[:], pattern=[[1, 128]], compare_op=ALU.is_equal, fill=0.0, base=-127, channel_multiplier=1),
              r=["Jm"], w=["Jm"])
            ncol = (NKT_S + NWT_S) * 16
            P(lambda e: e.matmul(pB[:, 0:ncol], lhsT=Jm[:], rhs=hk[:].rearrange("p t h -> p (t h)"), start=True, stop=True), r=["Jm", "hk", "cv"], w=["pB"])
            V(lambda e: e.tensor_copy(out=biasS[:].rearrange("p t h -> p (t h)"), in_=pB[:, 0:NKT_S * 16]), r=["pB"], w=["biasS"])
            V(lambda e: e.tensor_copy(out=biasW[:].rearrange("p t h -> p (t h)"), in_=pB[:, NKT_S * 16:ncol]), r=["pB"], w=["biasW"])
            dma(pti[:], AP(tensor=I["ptab"].tensor, offset=0, ap=[[0, 128], [1, N * 16]]), w=["pti"])
            G(lambda e: e.iota(out=iot[:], pattern=[[0, 1]], base=0, channel_multiplier=1, allow_small_or_imprecise_dtypes=True), w=["iot"])
            V(lambda e: e.tensor_copy(out=ptf[:], in_=pti[:]), r=["pti"], w=["ptf"])
            V(lambda e: e.tensor_scalar(out=ptf[:], in0=ptf[:], scalar1=128.0, scalar2=iot[:, 0:1], op0=ALU.mult, op1=ALU.add), r=["ptf", "iot"], w=["ptf"])
            V(lambda e: e.tensor_copy(out=idx[:], in_=ptf[:]), r=["ptf"], w=["idx"])
            dma(qs[:], O["qs_scr"], r=["q_scr"], w=["sqs"])
            for hh in range(16):
                P(lambda e: e.transpose(out=pA[0:64, 64 + hh * N:64 + (hh + 1) * N], in_=qs[:, hh * 64:(hh + 1) * 64], identity=idb[0:N, 0:N]),
                  r=["sqs", "idb", "peT"], w=["pA"])
            V(lambda e: e.tensor_copy(out=QT[0:64, :, :].rearrange("p h n -> p (h n)"), in_=pA[0:64, 64:64 + 16 * N]), r=["pA"], w=["QT"])
        K.S.barrier()

        with ExitStack() as tes:
            pg = [K.sb(tes, "sa_pg%d" % i, [128, D], F32) for i in range(2)]
            wn = K.sb(tes, "sa_wn", [128, 4, 512], F32)
            newf = K.sb(tes, "sa_newf", [N, 1536], BF16)
            KcT = K.sb(tes, "sa_KcT", [64, 2, 4, 2048], BF16)
            KsE = K.sb(tes, "sa_KsE", [128, 4, NKT_S * 128], BF16)
            KwE = K.sb(tes, "sa_KwE", [64, 4, NWT_S * 128], BF16)
            Vs = K.sb(tes, "sa_Vs", [128, NKT_S, 4, 65], BF16)
            Vw = K.sb(tes, "sa_Vw", [128, NWT_S, 4, 65], BF16)
            kcT = K.sb(tes, "sa_kcT", [64, 4, 128], BF16)
            vca = K.sb(tes, "sa_vca", [128, 4, 129], BF16)
            pre = [K.sb(tes, "sa_pre%d" % i, [128, 128], F32) for i in range(2)]
            tmpg = [K.sb(tes, "sa_tmpg%d" % i, [128, 128], F32) for i in range(2)]
            hidT = [K.sb(tes, "sa_hidT%d" % i, [128, 128], BF16) for i in range(2)]
            sc = K.sb(tes, "sa_sc", [128, NKT_S, 4], F32)
            PTs = K.sb(tes, "sa_PT", [128, NKT_S, 4], BF16)
            oc = K.sb(tes, "sa_oc", [4, 129], F32)
            osw = K.sb(tes, "sa_osw", [4, 2, 65], F32)
            rcc = K.sb(tes, "sa_rcc", [4, 1], F32)
            impr = K.sb(tes, "sa_impr", [1, 64], F32)
            wk1 = K.sb(tes, "sa_wk1", [1, 64], F32)
            m8 = K.sb(tes, "sa_m8", [1, 16], F32)
            mbr = K.sb(tes, "sa_mbr", [1, 128], F32)
            ptA = K.ps(tes, "sa_ptA", [64, 4, 128], F32)
            ptB = K.ps(tes, "sa_ptB", [64, 4, 128], F32)
            ph = [K.ps(tes, "sa_ph%d" % i, [128, 512], F32) for i in range(2)]
            pk2 = K.ps(tes, "sa_pk2", [128, 512], F32)
            psS = K.ps(tes, "sa_psS", [128, 512], F32)
            po = K.ps(tes, "sa_po", [128, 512], F32)
            ptN = K.ps(tes, "sa_ptN", [128, 1024], BF16)

            dma(newf[:], O["kvbs_scr"], r=["kvb_scr"], w=["newf"])
            G(lambda e: e.memset(Vs[:, :, :, 64:65], 1.0), w=["sVs"])
            G(lambda e: e.memset(Vw[:, :, :, 64:65], 1.0), w=["sVw"])
            G(lambda e: e.memset(vca[:, :, 64:65], 1.0), w=["svca"])
            for g in range(4):
                V(lambda e: e.tensor_copy(out=vca[:, g, 65:129], in_=covs[:]), r=["covs", "svca"], w=["svca"])
                V(lambda e: e.tensor_copy(out=KsE[64:128, g, :], in_=Erow[64:128, :]), r=["Erow"], w=["sKsE"])
            G(lambda e: e.memset(KsE[0:64, :, 16 * 128:17 * 128], 0.0), r=["sKsE"], w=["sKsE"])
            G(lambda e: e.memset(KwE[:, :, 4 * 128:5 * 128], 0.0), w=["sKwE"])
            G(lambda e: e.memset(Vs[:, 16, :, 0:64], 0.0), r=["sVs"], w=["sVs"])
            G(lambda e: e.memset(Vw[:, 4, :, 0:64], 0.0), r=["sVw"], w=["sVw"])
            for i_ in range(2):
                G(lambda e: e.memset(hidT[i_][:], 0.0), w=["shidT%d" % i_])
            G(lambda e: e.memset(mbr[:], 0.0), w=["mbr"])
            for j in range(4):
                P(lambda e: e.transpose(out=ptN[0:64, j * N:(j + 1) * N], in_=newf[:, 512 + j * 64:512 + (j + 1) * 64], identity=idb[0:N, 0:N]),
                  r=["newf", "idb"], w=["ptN"])
                P(lambda e: e.transpose(out=ptN[0:64, (4 + j) * N:(5 + j) * N], in_=newf[:, 1024 + j * 64:1024 + (j + 1) * 64], identity=idb[0:N, 0:N]),
                  r=["newf", "idb"], w=["ptN"])
            newT = K.sb(tes, "sa_newT", [64, 8, N], BF16)
            V(lambda e: e.tensor_copy(out=newT[:].rearrange("p j n -> p (j n)"), in_=ptN[0:64, 0:8 * N]), r=["ptN"], w=["newT"])
            newrow = K.sb(tes, "sa_newrow", [1, 1536], BF16)

            for tk in range(N):
                dma(wn[:], I["cwin"][tk].rearrange("(w r) c -> r w c", r=128), w=["wn"])
                for wt in range(4):
                    for g in range(4):
                        P(lambda e: e.transpose(out=ptA[:, g, :], in_=wn[:, wt, g * 64:(g + 1) * 64], identity=idf[:]), r=["wn", "idf"], w=["sptA"])
                    A(lambda e: e.copy(out=KwE[:, :, wt * 128:(wt + 1) * 128], in_=ptA[:]), r=["sptA"], w=["sKwE"])
                    V(lambda e: e.tensor_copy(out=Vw[:, wt, :, 0:64], in_=wn[:, wt, 256:512].rearrange("p (g d) -> p g d", d=64)), r=["wn", "sVw"], w=["sVw"])
                V(lambda e: e.tensor_copy(out=KsE[0:64, :, 16 * 128:16 * 128 + 1], in_=newT[:, 0:4, tk:tk + 1]), r=["newT", "sKsE"], w=["sKsE"])
                V(lambda e: e.tensor_copy(out=KwE[:, :, 4 * 128:4 * 128 + 1], in_=newT[:, 4:8, tk:tk + 1]), r=["newT", "sKwE"], w=["sKwE"])
                dma(newrow[:], O["kvbs_scr"][tk:tk + 1, :], r=["kvb_scr"], w=["newrow"])
                V(lambda e: e.tensor_copy(out=Vs[0:1, 16, :, 0:64], in_=newrow[:, 768:1024].rearrange("p (g d) -> p g d", d=64)), r=["newrow", "sVs"], w=["sVs"])
                V(lambda e: e.tensor_copy(out=Vw[0:1, 4, :, 0:64], in_=newrow[:, 1280:1536].rearrange("p (g d) -> p g d", d=64)), r=["newrow", "sVw"], w=["sVw"])
                for pgi in range(16):
                    b = pgi % 2
                    K.S.idma(pg[b][:], I["ckv"], idx[:, tk * 16 + pgi:tk * 16 + pgi + 1], ["idx"], ["spg%d" % b])
                    cs = slice(pgi * 128, (pgi + 1) * 128)
                    for g in range(4):
                        P(lambda e: e.transpose(out=ptA[:, g, :], in_=pg[b][:, g * 64:(g + 1) * 64], identity=idf[:]), r=["spg%d" % b, "idf"], w=["sptA"])
                    A(lambda e: e.copy(out=KcT[:, 0, :, cs], in_=ptA[:]), r=["sptA"], w=["sKcT"])
                    for g in range(4):
                        P(lambda e: e.transpose(out=ptB[:, g, :], in_=pg[b][:, 256 + g * 64:256 + (g + 1) * 64], identity=idf[:]), r=["spg%d" % b, "idf"], w=["sptB"])
                    V(lambda e: e.tensor_copy(out=KcT[:, 1, :, cs], in_=ptB[:]), r=["sptB", "sKcT"], w=["sKcT"])
                    for g in range(4):
                        P(lambda e: e.transpose(out=ptA[:, g, :], in_=pg[b][:, 512 + g * 64:512 + (g + 1) * 64], identity=idf[:]), r=["spg%d" % b, "idf"], w=["sptA"])
                    A(lambda e: e.copy(out=KsE[0:64, :, cs], in_=ptA[:]), r=["sptA"], w=["sKsE"])
                    G(lambda e: e.tensor_copy(out=Vs[:, pgi, :, 0:64], in_=pg[b][:, 768:1024].rearrange("p (g d) -> p g d", d=64)), r=["spg%d" % b, "sVs"], w=["sVs"])
                for kvi in range(2):
                    for g in range(4):
                        cb = (kvi * 4 + g) % 2
                        cx = str(cb)
                        for l in range(32):
                            P(lambda e: e.matmul(ph[cb][:, 0:127], lhsT=W1[:, kvi, l, :], rhs=KcT[:, kvi, g, l:l + 16 * 126 + 1:16], start=(l == 0), stop=(l == 31)),
                              r=["saW1", "sKcT"], w=["sph" + cx])
                        V(lambda e: e.tensor_scalar(out=pre[cb][:, 0:127], in0=ph[cb][:, 0:127], scalar1=cv[:, kvi:kvi + 1], scalar2=None, op0=ALU.add),
                          r=["sph" + cx, "cv"], w=["spre" + cx])
                        gelu_tanh(K, pre[cb][:, 0:127], "spre" + cx, tmpg[cb][:, 0:127], "stmpg" + cx, hidT[cb][:, 127:0:-1], "shidT" + cx)
                        if kvi == 0:
                            P(lambda e: e.matmul(pk2[0:64, 0:128], lhsT=W2[:, 0, :], rhs=hidT[cb][:], start=True, stop=True), r=["saW2", "shidT" + cx], w=["spk2"])
                            V(lambda e: e.tensor_copy(out=kcT[:, g, :], in_=pk2[0:64, 0:128]), r=["spk2"], w=["skcT"])
                        else:
                            P(lambda e: e.matmul(pk2[:, 0:64], lhsT=hidT[cb][:], rhs=W2[:, 1, :], start=True, stop=True), r=["saW2", "shidT" + cx], w=["spk2"])
                            V(lambda e: e.tensor_copy(out=vca[:, g, 0:64], in_=pk2[:, 0:64]), r=["spk2", "svca"], w=["svca"])
                for g in range(4):
                    qcol = QT[:, 4 * g:4 * g + 4, tk]
                    P(lambda e: e.matmul(psS[:, 0:4], lhsT=kcT[:, g, :], rhs=QT[0:64, 4 * g:4 * g + 4, tk], start=True, stop=True), r=["skcT", "QT"], w=["spsS"])
                    V(lambda e: e.tensor_tensor(out=sc[:, 0, :], in0=psS[:, 0:4], in1=biasC[:, 4 * g:4 * g + 4], op=ALU.add), r=["spsS", "biasC"], w=["ssc"])
                    A(lambda e: e.activation(out=PTs[:, 0, :], in_=sc[:, 0, :], func=AF.Exp), r=["ssc"], w=["sPT"])
                    P(lambda e: e.matmul(po[0:4, 0:129], lhsT=PTs[:, 0, :], rhs=vca[:, g, :], start=True, stop=True), r=["sPT", "svca"], w=["spo"])
                    V(lambda e: e.tensor_copy(out=oc[:], in_=po[0:4, 0:129]), r=["spo"], w=["soc"])
                    V(lambda e: e.tensor_scalar(out=rcc[:], in0=oc[:, 64:65], scalar1=1e-30, scalar2=None, op0=ALU.add), r=["soc"], w=["srcc"])
                    V(lambda e: e.reciprocal(out=rcc[:], in_=rcc[:]), r=["srcc"], w=["srcc"])
                    dma(O["os_scr"][tk, 0, 4 * g:4 * g + 4, :], oc[:, 0:65], r=["soc"], w=["os_scr"], add_writer=True)
                    P(lambda e: e.matmul(po[0:1, 256:320], lhsT=rcc[:], rhs=oc[:, 65:129], start=True, stop=True), r=["srcc", "soc"], w=["spo"])
                    V(lambda e: e.tensor_copy(out=impr[:], in_=po[0:1, 256:320]), r=["spo"], w=["simpr"])
                    V(lambda e: e.memset(impr[:, 0:1], 1e4), r=["simpr"], w=["simpr"])
                    V(lambda e: e.memset(impr[:, 31:33], 1e4), r=["simpr"], w=["simpr"])
                    V(lambda e: e.memset(impr[:, 33:64], -1e30), r=["simpr"], w=["simpr"])
                    V(lambda e: e.max(out=m8[:, 0:8], in_=impr[:]), r=["simpr"], w=["sm8"])
                    V(lambda e: e.match_replace(out=wk1[:], in_to_replace=m8[:, 0:8], in_values=impr[:], imm_value=-3e38), r=["simpr", "sm8"], w=["swk1"])
                    V(lambda e: e.max(out=m8[:, 8:16], in_=wk1[:]), r=["swk1"], w=["sm8"])
                    V(lambda e: e.tensor_scalar(out=wk1[:], in0=impr[:], scalar1=m8[:, 15:16], scalar2=None, op0=ALU.is_ge), r=["simpr", "sm8", "swk1"], w=["swk1"])
                    V(lambda e: e.tensor_scalar(out=mbr[:, 64:128], in0=wk1[:], scalar1=-NEG, scalar2=NEG, op0=ALU.mult, op1=ALU.add), r=["swk1", "mbr"], w=["mbr"])
                    P(lambda e: e.matmul(po[:, 320:324], lhsT=mbr[:], rhs=ones4[:], start=True, stop=True), r=["mbr", "ones4", "simpr"], w=["spo"])
                    V(lambda e: e.tensor_copy(out=QT[64:128, 4 * g:4 * g + 4, tk], in_=po[64:128, 320:324]), r=["spo"], w=["QT"])
                    for kt in range(NKT_S):
                        P(lambda e: e.matmul(psS[:, 4 + 4 * kt:8 + 4 * kt], lhsT=KsE[:, g, kt * 128:(kt + 1) * 128], rhs=qcol, start=True, stop=True),
                          r=["sKsE", "QT"], w=["spsS"])
                    V(lambda e: e.tensor_tensor(out=sc[:], in0=psS[:, 4:4 + 4 * NKT_S].rearrange("p (t h) -> p t h", h=4), in1=biasS[:, :, 4 * g:4 * g + 4], op=ALU.add),
                      r=["spsS", "biasS"], w=["ssc"])
                    A(lambda e: e.activation(out=PTs[:], in_=sc[:], func=AF.Exp), r=["ssc"], w=["sPT"])
                    for kt in range(NKT_S):
                        P(lambda e: e.matmul(po[0:4, 0:65], lhsT=PTs[:, kt, :], rhs=Vs[:, kt, g, :], start=(kt == 0), stop=(kt == NKT_S - 1)),
                          r=["sPT", "sVs"], w=["spo"])
                    V(lambda e: e.tensor_copy(out=osw[:, 0, :], in_=po[0:4, 0:65]), r=["spo"], w=["sosw"])
                    for wt in range(NWT_S):
                        P(lambda e: e.matmul(psS[:, 4 * wt:4 * wt + 4], lhsT=KwE[:, g, wt * 128:(wt + 1) * 128], rhs=QT[0:64, 4 * g:4 * g + 4, tk], start=True, stop=True),
                          r=["sKwE", "QT"], w=["spsS"])
                    V(lambda e: e.tensor_tensor(out=sc[:, 0:NWT_S, :], in0=psS[:, 0:4 * NWT_S].rearrange("p (t h) -> p t h", h=4), in1=biasW[:, :, 4 * g:4 * g + 4], op=ALU.add),
                      r=["spsS", "biasW"], w=["ssc"])
                    A(lambda e: e.activation(out=PTs[:, 0:NWT_S, :], in_=sc[:, 0:NWT_S, :], func=AF.Exp), r=["ssc"], w=["sPT"])
                    for wt in range(NWT_S):
                        P(lambda e: e.matmul(po[0:4, 0:65], lhsT=PTs[:, wt, :], rhs=Vw[:, wt, g, :], start=(wt == 0), stop=(wt == NWT_S - 1)),
                          r=["sPT", "sVw"], w=["spo"])
                    V(lambda e: e.tensor_copy(out=osw[:, 1, :], in_=po[0:4, 0:65]), r=["spo", "sosw"], w=["sosw"])
                    dma(O["os_scr"][tk, 1, 4 * g:4 * g + 4, :], osw[:, 0, :], r=["sosw"], w=["os_scr"], add_writer=True)
                    dma(O["os_scr"][tk, 2, 4 * g:4 * g + 4, :], osw[:, 1, :], r=["sosw"], w=["os_scr"], add_writer=True)
        K.S.barrier()

        with ExitStack() as mes:
            osb = K.sb(mes, "sa_osb", [N, 3, 16, 65], F32)
            gt = K.sb(mes, "sa_gt", [N, 3, 16], F32)
            wgt = K.sb(mes, "sa_wgt", [N, 3, 16], F32)
            o1 = K.sb(mes, "sa_o1", [N, 16, 64], F32)
            o2 = K.sb(mes, "sa_o2", [N, 16, 64], F32)
            yr = K.sb(mes, "sa_yr", [N, D], F32)
            dma(osb[:], O["os_scr"], r=["os_scr"], w=["osb"])
            dma(gt[:], O["gates_scr"].rearrange("n (r h) -> n r h", h=16), r=["gate_scr"], w=["sgt"])
            dma(yr[:], O["y2s_scr"], r=["y2_scr"], w=["syr"])
            V(lambda e: e.tensor_scalar(out=wgt[:], in0=osb[:, :, :, 64], scalar1=1e-30, scalar2=None, op0=ALU.add), r=["osb"], w=["swgt"])
            V(lambda e: e.reciprocal(out=wgt[:], in_=wgt[:]), r=["swgt"], w=["swgt"])
            V(lambda e: e.tensor_tensor(out=wgt[:], in0=wgt[:], in1=gt[:], op=ALU.mult), r=["swgt", "sgt"], w=["swgt"])
            V(lambda e: e.tensor_tensor(out=o1[:], in0=osb[:, 0, :, 0:64], in1=bc(wgt[:, 0, :].unsqueeze(2), [N, 16, 64]), op=ALU.mult), r=["osb", "swgt"], w=["so1"])
            for br in (1, 2):
                V(lambda e: e.tensor_tensor(out=o2[:], in0=osb[:, br, :, 0:64], in1=bc(wgt[:, br, :].unsqueeze(2), [N, 16, 64]), op=ALU.mult), r=["osb", "swgt"], w=["so2"])
                V(lambda e: e.tensor_tensor(out=o1[:], in0=o1[:], in1=o2[:], op=ALU.add), r=["so1", "so2"], w=["so1"])
            L = Lin32(K, mes, C, 8, "o")
            oT32 = K.sb(mes, "sa_oT32", [128, 8, N], F32)
            L.transpose(o1[:].rearrange("n h d -> n (h d)"), "so1", 8, oT32, "soT32")

            def add_y(c0, cw, ps, pk):
                V(lambda e: e.tensor_tensor(out=yr[:, c0:c0 + cw], in0=ps, in1=yr[:, c0:c0 + cw], op=ALU.add), r=[pk, "syr"], w=["syr"])
            L.run(oT32, "soT32", 8, I["nsa_w_o"], D, add_y)
            dma(O["y3s_scr"], yr[:], r=["syr"], w=["y3s_scr"])
    K.S.barrier()


NMT = NTILE // 2 + 1


def moe_stage(K, C, I, O):
    idf, idb = C["idf"], C["idb"]
    V, A, G, P, dma = K.V, K.A, K.G, K.P, K.dma
    EF = 1408
    CW = 352
    with ExitStack() as es:
        acc = K.sb(es, "mo_acc", [128, NMT, D], F32)
        hT = K.sb(es, "mo_hT", [128, NMT, 8, 128], BF16)
        gts = K.sb(es, "mo_gts", [128, NMT, 8], F32)
        hT32s = K.sb(es, "mo_hT32s", [128, 8, NS], F32)
        par = K.sb(es, "mo_par", [128, 2], F32)
        gain = K.sb(es, "mo_gain", [128, D], F32)
        gfin = K.sb(es, "mo_gfin", [128, D], F32)
        rt = K.sb(es, "mo_rt", [128, 8, 8], F32)
        dma(par[:], I["par"], w=["par"])
        dma(gain[:], AP(tensor=I["norm_ffn"].tensor, offset=D, ap=[[0, 128], [1, D]]), w=["gain"])
        dma(gfin[:], AP(tensor=I["norm_final"].tensor, offset=0, ap=[[0, 128], [1, D]]), w=["gfin"])
        dma(rt[:], I["moe_router"].rearrange("(k p) e -> p k e", p=128), w=["rt"])
        with ExitStack() as es2:
            ya = K.sb(es2, "mo_ya", [128, D], F32)
            yb = K.sb(es2, "mo_yb", [128, D], F32)
            h32 = K.sb(es2, "mo_h32", [128, D], F32)
            hb = K.sb(es2, "mo_hb", [128, D], BF16)
            hT32 = K.sb(es2, "mo_hT32", [128, 8, 128], F32)
            junk = K.sb(es2, "mo_junk", [128, D], F32)
            ss = K.sb(es2, "mo_ss", [128, 1], F32)
            lg = K.sb(es2, "mo_lg", [128, 8], F32)
            ex = K.sb(es2, "mo_ex", [128, 8], F32)
            mk = K.sb(es2, "mo_mk", [128, 8], F32)
            m8 = K.sb(es2, "mo_m8", [128, 8], F32)
            sm = K.sb(es2, "mo_sm", [128, 1], F32)
            ptb = K.ps(es2, "mo_ptb", [128, 8, 128], BF16)
            pt32 = [K.ps(es2, "mo_pt32%d" % i, [128, 4, 128], F32) for i in range(2)]
            plg = K.ps(es2, "mo_plg", [128, 512], F32)
            for j in range(NMT):
                Pn = 128 if j < NMT - 1 else NS
                if j < NMT - 1:
                    dma(ya[:], O["y3_scr"][(2 * j) * 128:(2 * j + 1) * 128, :], r=["y3_scr"], w=["ya"])
                    dma(yb[:], O["y3_scr"][(2 * j + 1) * 128:(2 * j + 2) * 128, :], r=["y3_scr"], w=["yb"])
                    V(lambda e: e.tensor_scalar(out=acc[:, j, :], in0=ya[:], scalar1=par[:, 0:1], scalar2=None, op0=ALU.mult), r=["ya", "par"], w=["acc"])
                    V(lambda e: e.scalar_tensor_tensor(out=acc[:, j, :], in0=yb[:], scalar=par[:, 1:2], in1=acc[:, j, :], op0=ALU.mult, op1=ALU.add),
                      r=["yb", "par", "acc"], w=["acc"])
                else:
                    dma(acc[0:Pn, j, :], O["y3s_scr"], r=["y3s_scr"], w=["acc"])
                xin = acc[0:Pn, j, :]
                A(lambda e: e.activation(out=junk[0:Pn, :], in_=xin, func=AF.Square, accum_out=ss[0:Pn, 0:1]), r=["acc"], w=["junk", "ss"])
                V(lambda e: e.tensor_scalar(out=ss[0:Pn, :], in0=ss[0:Pn, :], scalar1=1.0 / D, scalar2=EPS, op0=ALU.mult, op1=ALU.add), r=["ss"], w=["ss"])
                A(lambda e: e.activation(out=ss[0:Pn, :], in_=ss[0:Pn, :], func=AF.Sqrt), r=["ss"], w=["ss"])
                V(lambda e: e.reciprocal(out=ss[0:Pn, :], in_=ss[0:Pn, :]), r=["ss"], w=["ss"])
                V(lambda e: e.scalar_tensor_tensor(out=h32[0:Pn, :], in0=xin, scalar=ss[0:Pn, 0:1], in1=gain[0:Pn, :], op0=ALU.mult, op1=ALU.mult),
                  r=["acc", "ss", "gain"], w=["h32"])
                V(lambda e: e.tensor_copy(out=hb[0:Pn, :], in_=h32[0:Pn, :]), r=["h32"], w=["hb"])
                for k in range(8):
                    P(lambda e: e.transpose(out=ptb[:, k, 0:Pn], in_=hb[0:Pn, k * 128:(k + 1) * 128], identity=idb[0:Pn, 0:Pn]), r=["hb", "idb"], w=["ptb"])
                A(lambda e: e.copy(out=hT[:, j, :, 0:Pn], in_=ptb[:, :, 0:Pn]), r=["ptb"], w=["hT"])
                for k in range(8):
                    P(lambda e: e.transpose(out=pt32[k // 4][:, k % 4, 0:Pn], in_=h32[0:Pn, k * 128:(k + 1) * 128], identity=idf[0:Pn, 0:Pn]),
                      r=["h32", "idf"], w=["pt32%d" % (k // 4)])
                for hh in range(2):
                    V(lambda e: e.tensor_copy(out=hT32[:, 4 * hh:4 * hh + 4, 0:Pn], in_=pt32[hh][:, :, 0:Pn]), r=["pt32%d" % hh], w=["hT32"])
                if j == NMT - 1:
                    V(lambda e: e.tensor_copy(out=hT32s[:], in_=hT32[:, :, 0:NS]), r=["hT32"], w=["hT32s"])
                for k in range(8):
                    P(lambda e: e.matmul(plg[0:Pn, 0:8], lhsT=hT32[:, k, 0:Pn], rhs=rt[:, k, :], start=(k == 0), stop=(k == 7)), r=["hT32", "rt"], w=["plg"])
                V(lambda e: e.tensor_copy(out=lg[0:Pn, :], in_=plg[0:Pn, 0:8]), r=["plg"], w=["lg"])
                V(lambda e: e.max(out=m8[0:Pn, :], in_=lg[0:Pn, :]), r=["lg"], w=["m8"])
                V(lambda e: e.tensor_scalar(out=ex[0:Pn, :], in0=lg[0:Pn, :], scalar1=m8[0:Pn, 0:1], scalar2=None, op0=ALU.subtract), r=["lg", "m8"], w=["ex"])
                A(lambda e: e.activation(out=ex[0:Pn, :], in_=ex[0:Pn, :], func=AF.Exp), r=["ex"], w=["ex"])
                V(lambda e: e.tensor_scalar(out=mk[0:Pn, :], in0=lg[0:Pn, :], scalar1=m8[0:Pn, 1:2], scalar2=None, op0=ALU.is_ge), r=["lg", "m8"], w=["mk"])
                V(lambda e: e.tensor_tensor(out=ex[0:Pn, :], in0=ex[0:Pn, :], in1=mk[0:Pn, :], op=ALU.mult), r=["ex", "mk"], w=["ex"])
                V(lambda e: e.reduce_sum(out=sm[0:Pn, :], in_=ex[0:Pn, :], axis=AX.X), r=["ex"], w=["sm"])
                V(lambda e: e.reciprocal(out=sm[0:Pn, :], in_=sm[0:Pn, :]), r=["sm"], w=["sm"])
                V(lambda e: e.tensor_scalar(out=gts[0:Pn, j, :], in0=ex[0:Pn, :], scalar1=sm[0:Pn, 0:1], scalar2=None, op0=ALU.mult), r=["ex", "sm"], w=["gts"])
        K.S.barrier()
        with ExitStack() as es3:
            w1 = K.sb(es3, "mo_w1", [128, 8, 2 * EF], BF16)
            w2 = K.sb(es3, "mo_w2", [128, 11, D], BF16)
            act = [K.sb(es3, "mo_act%d" % i, [128, EF], BF16) for i in range(2)]
            actT = [K.sb(es3, "mo_actT%d" % i, [128, 11, 128], BF16) for i in range(2)]
            sl_ = [K.sb(es3, "mo_s%d" % i, [128, CW], F32) for i in range(2)]
            ptb = K.ps(es3, "mo_ptb2", [128, 8, 128], BF16)
            pa = [K.ps(es3, "mo_pa%d" % i, [128, 512], F32) for i in range(2)]
            pb = [K.ps(es3, "mo_pb%d" % i, [128, 512], F32) for i in range(2)]
            po = [K.ps(es3, "mo_po%d" % i, [128, 512], F32) for i in range(2)]
            for ex_ in range(8):
                v1 = I["moe_w_in"][ex_].rearrange("(k p) n -> p k n", p=128)
                for c0 in range(0, 2 * EF, 704):
                    K.S.dma("pool", w1[:, :, c0:c0 + 704], v1[:, :, c0:c0 + 704], (), ["mo_w1"], add_writer=(c0 > 0))
                v2 = I["moe_w_out"][ex_].rearrange("(k p) n -> p k n", p=128)
                for c0 in range(0, D, 512):
                    K.S.dma("pool", w2[:, :, c0:c0 + 512], v2[:, :, c0:c0 + 512], (), ["mo_w2"], add_writer=(c0 > 0))
                for j in range(NMT - 1):
                    Pn = 128
                    b = j % 2
                    sx = str(b)
                    for jj in range(4):
                        jb = jj % 2
                        for k in range(8):
                            P(lambda e: e.matmul(pa[jb][0:Pn, 0:CW], lhsT=hT[:, j, k, 0:Pn], rhs=w1[:, k, CW * jj:CW * jj + CW], start=(k == 0), stop=(k == 7)),
                              r=["hT", "mo_w1"], w=["mpa%d" % jb])
                        for k in range(8):
                            P(lambda e: e.matmul(pb[jb][0:Pn, 0:CW], lhsT=hT[:, j, k, 0:Pn], rhs=w1[:, k, EF + CW * jj:EF + CW * jj + CW], start=(k == 0), stop=(k == 7)),
                              r=["hT", "mo_w1"], w=["mpb%d" % jb])
                        A(lambda e: e.activation(out=sl_[jb][0:Pn, :], in_=pa[jb][0:Pn, 0:CW], func=AF.Silu), r=["mpa%d" % jb], w=["msl%d" % jb])
                        V(lambda e: e.tensor_tensor(out=act[b][0:Pn, CW * jj:CW * jj + CW], in0=pb[jb][0:Pn, 0:CW], in1=sl_[jb][0:Pn, :], op=ALU.mult),
                          r=["mpb%d" % jb, "msl%d" % jb], w=["mact" + sx])
                    for k0 in range(0, 11, 8):
                        k1 = min(11, k0 + 8)
                        for k in range(k0, k1):
                            P(lambda e: e.transpose(out=ptb[:, k - k0, 0:Pn], in_=act[b][0:Pn, k * 128:(k + 1) * 128], identity=idb[0:Pn, 0:Pn]),
                              r=["mact" + sx, "idb"], w=["mptb"])
                        A(lambda e: e.copy(out=actT[b][:, k0:k1, 0:Pn], in_=ptb[:, 0:k1 - k0, 0:Pn]), r=["mptb"], w=["mactT" + sx])
                    for c in range(2):
                        for k in range(11):
                            P(lambda e: e.matmul(po[c][0:Pn, :], lhsT=actT[b][:, k, 0:Pn], rhs=w2[:, k, c * 512:(c + 1) * 512], start=(k == 0), stop=(k == 10)),
                              r=["mactT" + sx, "mo_w2"], w=["mpo%d" % c])
                        V(lambda e: e.scalar_tensor_tensor(out=acc[0:Pn, j, c * 512:(c + 1) * 512], in0=po[c][0:Pn, :], scalar=gts[0:Pn, j, ex_:ex_ + 1],
                                                           in1=acc[0:Pn, j, c * 512:(c + 1) * 512], op0=ALU.mult, op1=ALU.add),
                          r=["mpo%d" % c, "gts", "acc"], w=["acc"])
        K.S.barrier()
        with ExitStack() as es4:
            L = Lin32(K, es4, C, 11, "m")
            bigm = K.sb(es4, "mo_big", [NS, 2 * EF], F32)
            actm = K.sb(es4, "mo_actm", [NS, EF], F32)
            actTm = K.sb(es4, "mo_actTm", [128, 11, NS], F32)
            js = NMT - 1

            def to_bigm(c0, cw, ps, pk):
                A(lambda e: e.copy(out=bigm[:, c0:c0 + cw], in_=ps), r=[pk], w=["mo_big"])
            for ex_ in range(8):
                L.run(hT32s, "hT32s", 8, I["moe_w_in"][ex_], 2 * EF, to_bigm)
                A(lambda e: e.activation(out=actm[:], in_=bigm[:, 0:EF], func=AF.Silu), r=["mo_big"], w=["mo_actm"])
                V(lambda e: e.tensor_tensor(out=actm[:], in0=actm[:], in1=bigm[:, EF:2 * EF], op=ALU.mult), r=["mo_big", "mo_actm"], w=["mo_actm"])
                L.transpose(actm, "mo_actm", 11, actTm, "mo_actTm")

                def acc_add(c0, cw, ps, pk):
                    V(lambda e: e.scalar_tensor_tensor(out=acc[0:NS, js, c0:c0 + cw], in0=ps, scalar=gts[0:NS, js, ex_:ex_ + 1],
                                                       in1=acc[0:NS, js, c0:c0 + cw], op0=ALU.mult, op1=ALU.add), r=[pk, "gts", "acc"], w=["acc"])
                L.run(actTm, "mo_actTm", 11, I["moe_w_out"][ex_], D, acc_add)
        K.S.barrier()
        with ExitStack() as es3:
            junk2 = K.sb(es3, "mo_junk2", [128, D], F32)
            ss2 = K.sb(es3, "mo_ss2", [128, 1], F32)
            for j in range(NMT):
                Pn = 128 if j < NMT - 1 else NS
                xin = acc[0:Pn, j, :]
                A(lambda e: e.activation(out=junk2[0:Pn, :], in_=xin, func=AF.Square, accum_out=ss2[0:Pn, 0:1]), r=["acc"], w=["junk2", "ss2"])
                V(lambda e: e.tensor_scalar(out=ss2[0:Pn, :], in0=ss2[0:Pn, :], scalar1=1.0 / D, scalar2=EPS, op0=ALU.mult, op1=ALU.add), r=["ss2"], w=["ss2"])
                A(lambda e: e.activation(out=ss2[0:Pn, :], in_=ss2[0:Pn, :], func=AF.Sqrt), r=["ss2"], w=["ss2"])
                V(lambda e: e.reciprocal(out=ss2[0:Pn, :], in_=ss2[0:Pn, :]), r=["ss2"], w=["ss2"])
                V(lambda e: e.scalar_tensor_tensor(out=junk2[0:Pn, :], in0=xin, scalar=ss2[0:Pn, 0:1], in1=gfin[0:Pn, :], op0=ALU.mult, op1=ALU.mult),
                  r=["acc", "ss2", "gfin", "junk2"], w=["junk2"])
                if j < NMT - 1:
                    dma(O["y_p"][j * 128:(j + 1) * 128, :], junk2[:], r=["junk2"], w=["y_p"], add_writer=True)
                else:
                    dma(O["y_s"], junk2[0:Pn, :], r=["junk2"], w=["y_s"])
    K.S.barrier()


IN_SPECS = [("norm_mix", [2, D]), ("norm_ffn", [2, D]), ("s5_lam_re", [64, 64]), ("s5_lam_im", [64, 64]), ("s5_log_dt", [64]),
            ("s5_b_re", [64, 64, 16]), ("s5_b_im", [64, 64, 16]), ("s5_c_re", [64, 16, 64]), ("s5_c_im", [64, 16, 64]),
            ("s5_d", [D]), ("s5_w_glu", [D, 2 * D]), ("ffn_w_in", [D, 5632]), ("ffn_w_out", [2816, D]), ("nsa_w_in", [D, 2608]),
            ("rel_bias", [32, 16]), ("nsa_phi_pe", [2, 32, 64]), ("nsa_phi_w1", [2, 32, 64, 128]), ("nsa_phi_w2", [2, 128, 64]),
            ("nsa_w_o", [D, D]), ("norm_final", [D]), ("moe_router", [D, 8]), ("moe_w_in", [8, D, 2816]), ("moe_w_out", [8, 1408, D])]
CONST_SPECS = [("c_ohb", [32, 256]), ("c_cover", [256, 64]), ("c_E", [64, T]), ("c_cover_s", [128, 64])]


def t5_bucket_np(n):
    n = np.maximum(n, 0)
    logpart = 16 + (np.log(np.maximum(n, 1).astype(np.float32) / 16) / math.log(8) * 16).astype(np.int32)
    return np.where(n < 16, n, np.minimum(logpart, 31))


def make_consts():
    c = {}
    bk = t5_bucket_np(np.arange(256))
    ohb = np.zeros((32, 256), np.float32)
    ohb[bk, np.arange(256)] = 1.0
    c["c_ohb"] = ohb
    cover = np.zeros((256, 64), np.float32)
    off = (np.arange(4)[:, None] - np.arange(2)[None, :]).reshape(-1)
    for j in range(64):
        for o in off:
            n = 4 * j + o
            if 0 <= n < NCB:
                cover[n, j] += 1.0
    c["c_cover"] = np.ascontiguousarray(cover[::-1])
    E = np.zeros((64, T), np.float32)
    E[np.arange(T) // 64, np.arange(T)] = 1.0
    c["c_E"] = E
    cov_s = np.zeros((128, 64), np.float32)
    for j in range(33):
        for o in off:
            n = 4 * j + o
            if 0 <= n < 127:
                cov_s[127 - n, j] += 1.0
    c["c_cover_s"] = cov_s
    return c


def s5_stage_w(K, C, I, O):
    s5_stage(K, None, C, I, O)


def build(upto=99, only=None):
    nc = bass.Bass("TRN2", target_bir_lowering=False)
    with ExitStack() as es:
        K = KB(nc, es)
        I, O = {}, {}
        I["xp"] = K.dram("xp", [T, D], F32, "ExternalInput")
        for nm, shp in IN_SPECS + CONST_SPECS:
            I[nm] = K.dram(nm, shp, F32, "ExternalInput")
        O["s5_re_p"] = K.dram("s5_re_p", [64, 64], F32, "ExternalOutput")
        O["s5_im_p"] = K.dram("s5_im_p", [64, 64], F32, "ExternalOutput")
        O["kv_p"] = K.dram("kv_p", [T, 1024], F32, "ExternalOutput")
        O["win_p"] = K.dram("win_p", [512, 512], F32, "ExternalOutput")
        I["xs"] = K.dram("xs", [NS, D], F32, "ExternalInput")
        I["st_re"] = K.dram("st_re", [NS, 4096], F32, "ExternalInput")
        I["st_im"] = K.dram("st_im", [NS, 4096], F32, "ExternalInput")
        O["s5_re_s"] = K.dram("s5_re_s", [NS, 4096], F32, "ExternalOutput")
        O["s5_im_s"] = K.dram("s5_im_s", [NS, 4096], F32, "ExternalOutput")
        O["kv_s"] = K.dram("kv_s", [NS, 1024], F32, "ExternalOutput")
        O["win_s"] = K.dram("win_s", [NS, 512], F32, "ExternalOutput")
        dbg = "ExternalOutput" if (upto < 99 or only is not None) else "Internal"
        O["g_scr"] = K.dram("g_scr", [T, D], BF16, dbg)
        O["y1_scr"] = K.dram("y1_scr", [T, D], F32, dbg)
        O["y2_scr"] = K.dram("y2_scr", [T, D], F32, dbg)
        O["gs_scr"] = K.dram("gs_scr", [NS, D], BF16, dbg)
        O["gs32_scr"] = K.dram("gs32_scr", [NS, D], F32, dbg)
        O["y1s_scr"] = K.dram("y1s_scr", [NS, D], F32, dbg)
        O["y2s_scr"] = K.dram("y2s_scr", [NS, D], F32, dbg)
        O["q_scr"] = K.dram("q_scr", [T, 1024], BF16, dbg)
        O["qs_scr"] = K.dram("qs_scr", [NS, 1024], BF16, dbg)
        O["kvb_scr"] = K.dram("kvb_scr", [T, 1536], BF16, dbg)
        O["kvbs_scr"] = K.dram("kvbs_scr", [NS, 1536], BF16, dbg)
        O["gate_scr"] = K.dram("gate_scr", [T, 48], F32, dbg)
        O["gates_scr"] = K.dram("gates_scr", [NS, 48], F32, dbg)
        O["fpad_scr"] = K.dram("fpad_scr", [16, 384], F32, "Internal")
        O["fc_scr"] = K.dram("fc_scr", [16, 8192], F32, "Internal")
        O["y3_scr"] = K.dram("y3_scr", [T, D], F32, dbg)
        O["y3s_scr"] = K.dram("y3s_scr", [NS, D], F32, dbg)
        O["os_scr"] = K.dram("os_scr", [NS, 3, 16, 65], F32, dbg)
        if only is None or 6 in only:
            I["ckv"] = K.dram("ckv", [2560 * 128, 1024], F32, "ExternalInput")
        I["cwin"] = K.dram("cwin", [NS, 512, 512], F32, "ExternalInput")
        I["ptab"] = K.dram("ptab", [NS * 16], I32, "ExternalInput")
        I["par"] = K.dram("par", [128, 2], F32, "ExternalInput")
        O["y_p"] = K.dram("y_p", [T // 2, D], F32, "ExternalOutput")
        O["y_s"] = K.dram("y_s", [NS, D], F32, "ExternalOutput")
        C = build_consts(K, es)
        stages = [(1, s5_stage_w), (1.5, sample_l0_f32), (2, glu_stage), (3, ffn_stage), (4, nsa_proj_stage), (5, nsa_attn_prompt), (6, nsa_attn_sample), (7, moe_stage)]
        for idx, fn in stages:
            if (only is None and idx <= upto) or (only is not None and idx in only):
                fn(K, C, I, O)
        K.S.finish()
    return nc


def kernel(**inp):
    f32 = lambda a: np.ascontiguousarray(np.asarray(a, dtype=np.float32))
    nc = build()
    shared = {nm: f32(inp[nm]) for nm, _ in IN_SPECS}
    shared.update(make_consts())
    shared["ckv"] = f32(inp["cache_kv"]).reshape(2560 * 128, 1024)
    in_maps = []
    for c in range(8):
        m = dict(shared)
        m["xp"] = f32(inp["x_prompt"][c // 2])
        sl = slice(NS * c, NS * (c + 1))
        m["xs"] = f32(inp["x_sample"][sl, 0, :])
        m["st_re"] = f32(inp["state_s5_re"][sl]).reshape(NS, 4096)
        m["st_im"] = f32(inp["state_s5_im"][sl]).reshape(NS, 4096)
        m["cwin"] = f32(inp["cache_win"][sl]).reshape(NS, 512, 512)
        m["ptab"] = np.ascontiguousarray(np.asarray(inp["page_table"][sl], dtype=np.int32)).reshape(NS * 16)
        par = c % 2
        m["par"] = np.tile(np.array([[1.0 - par, float(par)]], np.float32), (128, 1))
        in_maps.append(m)
    res = run_bass_kernel_spmd(nc, in_maps, core_ids=list(range(8))).results
    B = 4
    cat = lambda nm: np.concatenate([np.asarray(res[c][nm], dtype=np.float32) for c in range(8)], axis=0)
    y_prompt = np.zeros((B, NTILE, 128, D), np.float32)
    for c in range(8):
        yp = np.asarray(res[c]["y_p"], dtype=np.float32).reshape(NTILE // 2, 128, D)
        y_prompt[c // 2, (c % 2)::2] = yp
    y_prompt = y_prompt.reshape(B, T, D)
    y_sample = cat("y_s").reshape(128, 1, D)
    s5_re_p = np.stack([res[2 * b]["s5_re_p"] for b in range(B)]).astype(np.float32)
    s5_im_p = np.stack([res[2 * b]["s5_im_p"] for b in range(B)]).astype(np.float32)
    kv_p = np.stack([res[2 * b]["kv_p"] for b in range(B)]).astype(np.float32).reshape(B, T, 4, 4, 64)
    win_p = np.stack([res[2 * b]["win_p"] for b in range(B)]).astype(np.float32).reshape(B, 512, 2, 4, 64)
    s5_re_s = cat("s5_re_s").reshape(128, 64, 64)
    s5_im_s = cat("s5_im_s").reshape(128, 64, 64)
    kv_s = cat("kv_s").reshape(128, 1, 4, 4, 64)
    win_s = cat("win_s").reshape(128, 1, 2, 4, 64)
    return (y_prompt, y_sample, s5_re_p, s5_im_p, kv_p, win_p, s5_re_s, s5_im_s, kv_s, win_s)
```

```python
import math
import os
import numpy as np
STOP = float(os.environ.get('S5STOP', '99'))
NOSELF = os.environ.get('NOSELF', '0') == '1'
from contextlib import ExitStack
import concourse.bass as bass
import concourse.mybir as mybir
from concourse.bass_types import AP
from concourse.bass_utils import run_bass_kernel_spmd

F32 = mybir.dt.float32
BF16 = mybir.dt.bfloat16
I32 = mybir.dt.int32
AF = mybir.ActivationFunctionType
ALU = mybir.AluOpType
AX = mybir.AxisListType

D = 1024
T = 4096
NS = 16
EPS = 1e-6
TWO_PI = 2.0 * math.pi


class Sched:
    def __init__(self, nc, es, ndma=10):
        self.nc = nc
        self.eng = {"pe": nc.tensor, "dve": nc.vector, "act": nc.scalar, "pool": nc.gpsimd, "sp": nc.sync}
        self.sem = {k: es.enter_context(nc.semaphore("s_" + k)) for k in self.eng}
        self.cnt = {k: 0 for k in self.eng}
        self.dsem = {k: [es.enter_context(nc.semaphore("d_%s%d" % (k, i))) for i in range(ndma)] for k in ("sp", "act", "pool")}
        self.dcnt = {k: [0] * ndma for k in self.dsem}
        self.drr = {k: 0 for k in self.dsem}
        self.waited = {k: {} for k in self.eng}
        self.semobj = {}
        self.wr = {}
        self.rd = {}

    def _sid(self, s):
        i = id(s)
        self.semobj[i] = s
        return i

    def _wait(self, e, deps):
        w = self.waited[e]
        for sid, val in deps.items():
            if w.get(sid, 0) >= val:
                continue
            self.eng[e].wait_ge(self.semobj[sid], val)
            w[sid] = val

    def _deps(self, reads, writes, own_sid=None, skip_own=False):
        deps = {}

        def add(d):
            for sid, val in d.items():
                if skip_own and sid == own_sid:
                    continue
                if deps.get(sid, 0) < val:
                    deps[sid] = val
        for k in reads:
            add(self.wr.get(k, {}))
        for k in writes:
            add(self.wr.get(k, {}))
            add(self.rd.get(k, {}))
        return deps

    def op(self, e, fn, reads=(), writes=()):
        s = self.sem[e]
        sid = self._sid(s)
        deps = self._deps(reads, writes, own_sid=sid, skip_own=(e == "pe" or NOSELF))
        self._wait(e, deps)
        inst = fn(self.eng[e])
        self.cnt[e] += 1
        inst.then_inc(s, 1)
        val = self.cnt[e]
        for k in reads:
            self.rd.setdefault(k, {})[sid] = val
        for k in writes:
            self.wr[k] = {sid: val}
            self.rd[k] = {}
        return inst

    def dma(self, q, out, in_, reads=(), writes=(), add_writer=False, **kw):
        pool = self.dsem[q]
        i = self.drr[q]
        self.drr[q] = (i + 1) % len(pool)
        s = pool[i]
        sid = self._sid(s)
        deps = self._deps(reads, writes)
        if self.dcnt[q][i] > 0:
            deps[sid] = max(deps.get(sid, 0), self.dcnt[q][i])
        self._wait(q, deps)
        inst = self.eng[q].dma_start(out=out, in_=in_, **kw)
        self.dcnt[q][i] += 16
        inst.then_inc(s, 16)
        val = self.dcnt[q][i]
        for k in reads:
            self.rd.setdefault(k, {})[sid] = val
        for k in writes:
            if add_writer and k in self.wr:
                self.wr[k][sid] = val
            else:
                self.wr[k] = {sid: val}
                self.rd[k] = {}
        return inst

    def idma(self, out, in_, idx_ap, reads=(), writes=()):
        q = "pool"
        pool = self.dsem[q]
        i = self.drr[q]
        self.drr[q] = (i + 1) % len(pool)
        sm = pool[i]
        sid = self._sid(sm)
        deps = self._deps(reads, writes)
        if self.dcnt[q][i] > 0:
            deps[sid] = max(deps.get(sid, 0), self.dcnt[q][i])
        self._wait(q, deps)
        inst = self.eng[q].indirect_dma_start(out=out, out_offset=None, in_=in_, in_offset=bass.IndirectOffsetOnAxis(ap=idx_ap, axis=0))
        self.dcnt[q][i] += 16
        inst.then_inc(sm, 16)
        val = self.dcnt[q][i]
        for k in reads:
            self.rd.setdefault(k, {})[sid] = val
        for k in writes:
            self.wr[k] = {sid: val}
            self.rd[k] = {}
        return inst

    def barrier(self):
        deps = {}
        for e in self.eng:
            if self.cnt[e] > 0:
                deps[self._sid(self.sem[e])] = self.cnt[e]
        for q in self.dsem:
            for i, sm in enumerate(self.dsem[q]):
                if self.dcnt[q][i] > 0:
                    deps[self._sid(sm)] = self.dcnt[q][i]
        for e in self.eng:
            self._wait(e, dict(deps))

    def finish(self):
        deps = {}
        for k in list(self.wr.keys()):
            for sid, val in self.wr[k].items():
                deps[sid] = max(deps.get(sid, 0), val)
        for q in self.dsem:
            for i, s in enumerate(self.dsem[q]):
                if self.dcnt[q][i] > 0:
                    sid = self._sid(s)
                    deps[sid] = max(deps.get(sid, 0), self.dcnt[q][i])
        self._wait("sp", deps)


class KB:
    def __init__(self, nc, es):
        self.nc = nc
        self.es = es
        self.S = Sched(nc, es)
        self.dr = {}
        self._q = 0

    def sb(self, es, name, shape, dt):
        return es.enter_context(self.nc.sbuf_tensor(name, shape, dt))

    def ps(self, es, name, shape, dt):
        return es.enter_context(self.nc.psum_tensor(name, shape, dt))

    def dram(self, name, shape, dt, kind):
        t = self.nc.dram_tensor(name, shape, dt, kind=kind).ap()
        self.dr[name] = t
        return t

    def V(self, fn, r=(), w=()):
        return self.S.op("dve", fn, r, w)

    def A(self, fn, r=(), w=()):
        return self.S.op("act", fn, r, w)

    def G(self, fn, r=(), w=()):
        return self.S.op("pool", fn, r, w)

    def P(self, fn, r=(), w=()):
        return self.S.op("pe", fn, r, w)

    def dma(self, out, in_, r=(), w=(), q=None, **kw):
        if q is None:
            q = ("sp", "act")[self._q % 2]
            self._q += 1
        return self.S.dma(q, out, in_, r, w, **kw)


def bc(ap, shape):
    return ap.broadcast_to(shape)


def build_consts(K, es):
    c = {}
    idf = K.sb(es, "ident_f", [128, 128], F32)
    idb = K.sb(es, "ident_b", [128, 128], BF16)
    K.G(lambda e: e.memset(idf[:], 1.0), w=["idf"])
    K.G(lambda e: e.affine_select(out=idf[:], in_=idf[:], pattern=[[-1, 128]], compare_op=ALU.is_equal,
                                  fill=0.0, base=0, channel_multiplier=1), r=["idf"], w=["idf"])
    K.V(lambda e: e.tensor_copy(out=idb[:], in_=idf[:]), r=["idf"], w=["idb"])
    c["idf"], c["idb"] = idf, idb
    return c


def s5_stage(K, es_outer, C, I, O):
    nc = K.nc
    idf, idb = C["idf"], C["idb"]
    V, A, G, P, dma = K.V, K.A, K.G, K.P, K.dma
    with ExitStack() as es:
        _s5_body(K, es, C, I, O)
    K.S.barrier()


def _s5_body(K, es, C, I, O):
    idf, idb = C["idf"], C["idb"]
    V, A, G, P, dma = K.V, K.A, K.G, K.P, K.dma

    Win = K.sb(es, "s5Win", [128, 32, 2, 2, 128], BF16)
    Wout = K.sb(es, "s5Wout", [128, 32, 2, 128], BF16)
    Mint = K.sb(es, "s5Mint", [128, 64, 128], BF16)
    CA = K.sb(es, "s5CA", [128, 2, 32], F32)
    CBn = K.sb(es, "s5CBn", [128, 32], F32)
    CBp = K.sb(es, "s5CBp", [128, 32], F32)
    A1 = K.sb(es, "s5A1", [128, 2, 32], F32)
    gain0 = K.sb(es, "gain0", [128, D], F32)
    dskip = K.sb(es, "dskip", [128, D], F32)
    dma(gain0[:], AP(tensor=I["norm_mix"].tensor, offset=0, ap=[[0, 128], [1, D]]), w=["gain0"])
    dma(dskip[:], AP(tensor=I["s5_d"].tensor, offset=0, ap=[[0, 128], [1, D]]), w=["dskip"])

    with ExitStack() as pes:
        raw = K.sb(pes, "s5raw", [64, 3, 2, 64], F32)
        PRM = K.sb(pes, "s5prm", [128, 3, 32], F32)
        Wk = K.sb(pes, "s5wk", [128, 24, 32], F32)
        Wi = K.sb(pes, "s5wi", [128, 32], I32)
        PW = K.sb(pes, "s5pw", [128, 16, 2, 32], F32)
        Bt = K.sb(pes, "s5bt", [128, 2, 32, 16], F32)
        Ct = K.sb(pes, "s5ct", [128, 2, 32, 16], F32)
        BB = K.sb(pes, "s5bb", [128, 2, 32, 16], F32)
        craw = K.sb(pes, "s5craw", [128, 2, 2, 64], F32)
        T3 = K.sb(pes, "s5t3", [128, 4, 32, 16], F32)
        Ubf = K.sb(pes, "s5U", [128, 32, 2, 128], BF16)
        Vpad = K.sb(pes, "s5V", [128, 32, 2, 2, 128], BF16)
        maskLT = K.sb(pes, "s5mask", [128, 128], F32)
        pp = K.ps(pes, "s5pp", [128, 8, 64], F32)
        pc = [K.ps(pes, "s5pc%d" % i, [128, 512], F32) for i in range(2)]
        pm = [K.ps(pes, "s5pm%d" % i, [128, 512], F32) for i in range(2)]
        pw = [K.ps(pes, "s5pw%d" % i, [128, 1024], BF16) for i in range(2)]

        for i, nm in enumerate(["s5_lam_re", "s5_lam_im"]):
            dma(raw[:, i, :, :], AP(tensor=I[nm].tensor, offset=0, ap=[[64, 64], [0, 2], [1, 64]]), w=["raw"], add_writer=True)
        ldt = K.sb(pes, "s5ldt", [64, 1], F32)
        dma(ldt[:], AP(tensor=I["s5_log_dt"].tensor, offset=0, ap=[[1, 64], [1, 1]]), w=["ldt"])
        V(lambda e: e.tensor_copy(out=raw[:, 2, :, :].rearrange("p a b -> p (a b)"), in_=bc(ldt[:, 0:1], [64, 128])), r=["ldt"], w=["raw2"])
        for i in range(3):
            P(lambda e: e.transpose(out=pp[:, i, :], in_=raw[:, i, :, :], identity=idf[0:64, 0:64]), r=["raw", "raw2", "idf"], w=["pp"])
        for gl in range(2):
            sl = slice(64 * gl, 64 * gl + 64)
            V(lambda e: e.tensor_copy(out=PRM[sl, :, :], in_=pp[sl, 0:3, :].rearrange("p i (pr gl) -> p i pr gl", gl=2)[:, :, :, gl]),
              r=["pp"], w=["PRM"])
        if STOP <= 1:
            return
        LR, LI, LD = PRM[:, 0, :], PRM[:, 1, :], PRM[:, 2, :]
        _n = [0]

        def wk():
            _n[0] += 1
            return Wk[:, _n[0] - 1, :]
        rk, wkk = ["PRM", "Wk"], ["Wk"]

        def vtt(out, a, b, op):
            V(lambda e: e.tensor_tensor(out=out, in0=a, in1=b, op=op), r=rk + ["PW", "Bt", "Ct", "BB"], w=wkk)

        def vts(out, a, s1, s2, op0, op1=None):
            if op1 is None:
                V(lambda e: e.tensor_scalar(out=out, in0=a, scalar1=s1, scalar2=None, op0=op0), r=rk, w=wkk)
            else:
                V(lambda e: e.tensor_scalar(out=out, in0=a, scalar1=s1, scalar2=s2, op0=op0, op1=op1), r=rk, w=wkk)

        def sin_turns(out, turns):
            tf, fr, m = wk(), wk(), wk()
            V(lambda e: e.tensor_copy(out=Wi[:], in_=turns), r=rk, w=["Wi"])
            V(lambda e: e.tensor_copy(out=tf, in_=Wi[:]), r=["Wi"], w=wkk)
            vtt(fr, turns, tf, ALU.subtract)
            vts(m, fr, 0.5, None, ALU.is_gt)
            vtt(fr, fr, m, ALU.subtract)
            vts(m, fr, -0.5, None, ALU.is_lt)
            vtt(fr, fr, m, ALU.add)
            A(lambda e: e.activation(out=out, in_=fr, func=AF.Sin, scale=TWO_PI), r=rk, w=wkk)

        dt_, th, turns, turnc, sn, cs, ld, mag = [wk() for _ in range(8)]
        A(lambda e: e.activation(out=dt_, in_=LD, func=AF.Exp), r=rk, w=wkk)
        vtt(th, LI, dt_, ALU.mult)
        vts(turns, th, 1.0 / TWO_PI, None, ALU.mult)
        vts(turnc, turns, 0.25, None, ALU.add)
        sin_turns(sn, turns)
        sin_turns(cs, turnc)
        vtt(ld, LR, dt_, ALU.mult)
        A(lambda e: e.activation(out=mag, in_=ld, func=AF.Exp), r=rk, w=wkk)
        ar, ai = PW[:, 8, 0, :], PW[:, 8, 1, :]
        V(lambda e: e.tensor_tensor(out=ar, in0=mag, in1=cs, op=ALU.mult), r=rk, w=["PW"])
        V(lambda e: e.tensor_tensor(out=ai, in0=mag, in1=sn, op=ALU.mult), r=rk, w=["PW"])
        _n[0] = 8
        den, t1, t2, nr, fre, fim, q = [wk() for _ in range(7)]
        vtt(den, LR, LR, ALU.mult)
        vtt(t1, LI, LI, ALU.mult)
        vtt(den, den, t1, ALU.add)
        V(lambda e: e.reciprocal(out=den, in_=den), r=rk, w=wkk)
        V(lambda e: e.tensor_scalar(out=nr, in0=ar, scalar1=-1.0, scalar2=None, op0=ALU.add), r=rk + ["PW"], w=wkk)
        vtt(t1, nr, LR, ALU.mult)
        vtt(t2, ai, LI, ALU.mult)
        vtt(t1, t1, t2, ALU.add)
        vtt(fre, t1, den, ALU.mult)
        vtt(t1, ai, LR, ALU.mult)
        vtt(t2, nr, LI, ALU.mult)
        vtt(t1, t1, t2, ALU.subtract)
        vtt(fim, t1, den, ALU.mult)
        A(lambda e: e.activation(out=q, in_=ld, func=AF.Exp, scale=-2.0), r=rk, w=wkk)
        V(lambda e: e.memset(PW[:, 7, 0, :], 1.0), w=["PW"])
        V(lambda e: e.memset(PW[:, 7, 1, :], 0.0), w=["PW"])
        V(lambda e: e.tensor_tensor(out=PW[:, 6, 0, :], in0=ar, in1=q, op=ALU.mult), r=rk + ["PW"], w=["PW"])
        V(lambda e: e.scalar_tensor_tensor(out=PW[:, 6, 1, :], in0=ai, scalar=-1.0, in1=q, op0=ALU.mult, op1=ALU.mult),
          r=rk + ["PW"], w=["PW"])

        def cmul(o, x, y):
            a_, b_ = wk(), wk()
            _n[0] -= 2
            V(lambda e: e.tensor_tensor(out=a_, in0=PW[:, x, 0, :], in1=PW[:, y, 0, :], op=ALU.mult), r=["PW", "Wk"], w=wkk)
            V(lambda e: e.tensor_tensor(out=b_, in0=PW[:, x, 1, :], in1=PW[:, y, 1, :], op=ALU.mult), r=["PW", "Wk"], w=wkk)
            V(lambda e: e.tensor_tensor(out=PW[:, o, 0, :], in0=a_, in1=b_, op=ALU.subtract), r=["PW", "Wk"], w=["PW"])
            V(lambda e: e.tensor_tensor(out=a_, in0=PW[:, x, 0, :], in1=PW[:, y, 1, :], op=ALU.mult), r=["PW", "Wk"], w=wkk)
            V(lambda e: e.tensor_tensor(out=b_, in0=PW[:, x, 1, :], in1=PW[:, y, 0, :], op=ALU.mult), r=["PW", "Wk"], w=wkk)
            V(lambda e: e.tensor_tensor(out=PW[:, o, 1, :], in0=a_, in1=b_, op=ALU.add), r=["PW", "Wk"], w=["PW"])
        for k in range(2, 9):
            cmul(7 + k, 7 + k - 1, 8)
        for k in range(2, 8):
            cmul(7 - k, 7 - k + 1, 6)
        V(lambda e: e.tensor_copy(out=CA[:, 0, :], in_=PW[:, 15, 0, :]), r=["PW"], w=["CA"])
        V(lambda e: e.tensor_copy(out=CA[:, 1, :], in_=PW[:, 15, 0, :]), r=["PW"], w=["CA"])
        V(lambda e: e.tensor_copy(out=CBp[:], in_=PW[:, 15, 1, :]), r=["PW"], w=["CBp"])
        V(lambda e: e.tensor_scalar(out=CBn[:], in0=PW[:, 15, 1, :], scalar1=-1.0, scalar2=None, op0=ALU.mult), r=["PW"], w=["CBn"])
        V(lambda e: e.tensor_copy(out=A1[:], in_=PW[:, 8, :, :]), r=["PW"], w=["A1"])

        if STOP <= 2:
            return
        for i, nm in enumerate(["s5_b_re", "s5_b_im"]):
            for q4 in range(8):
                dma(Bt[:, i, 4 * q4:4 * q4 + 4, :], I[nm].rearrange("(pr gl) p c -> (gl p) pr c", gl=2)[:, 4 * q4:4 * q4 + 4, :],
                    w=["Bt"], add_writer=True)
        fre_b = bc(fre.unsqueeze(2), [128, 32, 16])
        fim_b = bc(fim.unsqueeze(2), [128, 32, 16])
        vtt(T3[:, 0], Bt[:, 0], fre_b, ALU.mult)
        vtt(T3[:, 1], Bt[:, 1], fim_b, ALU.mult)
        V(lambda e: e.tensor_tensor(out=BB[:, 0], in0=T3[:, 0], in1=T3[:, 1], op=ALU.subtract), r=["Wk"], w=["BB"])
        vtt(T3[:, 0], Bt[:, 1], fre_b, ALU.mult)
        vtt(T3[:, 1], Bt[:, 0], fim_b, ALU.mult)
        V(lambda e: e.tensor_tensor(out=BB[:, 1], in0=T3[:, 0], in1=T3[:, 1], op=ALU.add), r=["Wk"], w=["BB"])

        if STOP <= 2.3:
            return
        for i, nm in enumerate(["s5_c_re", "s5_c_im"]):
            cflat = I[nm]
            for k in range(8):
                for hh in range(2):
                    dma(craw[:, k % 2, hh, :], AP(tensor=cflat.tensor, offset=k * 128 * 64, ap=[[64, 128], [1, 64]]),
                        w=["craw%d" % (k % 2)], add_writer=(hh == 1))
                P(lambda e: e.transpose(out=pc[k % 2][:, 0:128], in_=craw[:, k % 2, :, :], identity=idf[:]),
                  r=["craw%d" % (k % 2), "idf"], w=["pc%d" % (k % 2)])
                for gl in range(2):
                    sl = slice(64 * gl, 64 * gl + 64)
                    V(lambda e: e.tensor_copy(
                        out=Ct[sl, i, 4 * k:4 * k + 4, :],
                        in_=pc[k % 2][sl, 0:128].rearrange("p (pr gl c) -> p pr gl c", gl=2, c=16)[:, :, gl, :]),
                      r=["pc%d" % (k % 2)], w=["Ct"])

        if STOP <= 2.5:
            return
        Uv = Ubf[:].rearrange("p pr r (s c) -> p pr r s c", c=16)
        for s in range(8):
            pi = 14 - s
            Pr = bc(PW[:, pi, 0, :].unsqueeze(2), [128, 32, 16])
            Pi = bc(PW[:, pi, 1, :].unsqueeze(2), [128, 32, 16])
            vtt(T3[:, 0], BB[:, 0], Pr, ALU.mult)
            vtt(T3[:, 1], BB[:, 1], Pi, ALU.mult)
            V(lambda e: e.tensor_tensor(out=Uv[:, :, 0, s, :], in0=T3[:, 0], in1=T3[:, 1], op=ALU.subtract), r=["Wk"], w=["Ubf"])
            vtt(T3[:, 2], BB[:, 1], Pr, ALU.mult)
            vtt(T3[:, 3], BB[:, 0], Pi, ALU.mult)
            V(lambda e: e.tensor_tensor(out=Uv[:, :, 1, s, :], in0=T3[:, 2], in1=T3[:, 3], op=ALU.add), r=["Wk"], w=["Ubf"])

        if STOP <= 2.7:
            return
        G(lambda e: e.memset(Vpad[:], 0.0), w=["Vpad"])
        G(lambda e: e.memset(Win[:], 0.0), w=["Win"])
        Vv = Vpad[:].rearrange("p pr g r (s c) -> p pr g r s c", c=16)
        Wv = Wout[:].rearrange("p pr r (s c) -> p pr r s c", c=16)
        for s in range(8):
            for which in range(2):
                pi = s if which == 0 else s + 8
                Pr = bc(PW[:, pi, 0, :].unsqueeze(2), [128, 32, 16])
                Pi = bc(PW[:, pi, 1, :].unsqueeze(2), [128, 32, 16])
                vtt(T3[:, 0], Ct[:, 0], Pr, ALU.mult)
                vtt(T3[:, 1], Ct[:, 1], Pi, ALU.mult)
                vtt(T3[:, 0], T3[:, 0], T3[:, 1], ALU.subtract)
                vtt(T3[:, 2], Ct[:, 0], Pi, ALU.mult)
                vtt(T3[:, 3], Ct[:, 1], Pr, ALU.mult)
                vtt(T3[:, 2], T3[:, 2], T3[:, 3], ALU.add)
                if which == 0:
                    for gl in range(2):
                        sl = slice(64 * gl, 64 * gl + 64)
                        V(lambda e: e.tensor_copy(out=Vv[sl, :, gl, 0, s, :], in_=T3[sl, 0]), r=["Wk"], w=["Vpad"])
                        V(lambda e: e.tensor_scalar(out=Vv[sl, :, gl, 1, s, :], in0=T3[sl, 2], scalar1=-1.0, scalar2=None,
                                                    op0=ALU.mult), r=["Wk"], w=["Vpad"])
                else:
                    V(lambda e: e.tensor_copy(out=Wv[:, :, 0, s, :], in_=T3[:, 0]), r=["Wk"], w=["Wout"])
                    V(lambda e: e.tensor_scalar(out=Wv[:, :, 1, s, :], in0=T3[:, 2], scalar1=-1.0, scalar2=None, op0=ALU.mult),
                      r=["Wk"], w=["Wout"])

        if STOP <= 2.9:
            return
        G(lambda e: e.memset(maskLT[:], 1.0), w=["maskLT"])
        G(lambda e: e.affine_select(out=maskLT[:].rearrange("p (s c) -> p s c", c=16), in_=maskLT[:].rearrange("p (s c) -> p s c", c=16),
                                    pattern=[[16, 8], [0, 16]], compare_op=ALU.is_ge, fill=0.0, base=15, channel_multiplier=-1),
          r=["maskLT"], w=["maskLT"])

        if STOP <= 3:
            return
        for pr in range(32):
            for gl in range(2):
                g = 2 * pr + gl
                pmx = pm[g % 2]
                key = "pm%d" % (g % 2)
                P(lambda e: e.matmul(pmx[:, 0:128], lhsT=Ubf[:, pr, 0, :], rhs=Vpad[:, pr, gl, 0, :], start=True, stop=False),
                  r=["Ubf", "Vpad"], w=[key])
                P(lambda e: e.matmul(pmx[:, 0:128], lhsT=Ubf[:, pr, 1, :], rhs=Vpad[:, pr, gl, 1, :], start=False, stop=True),
                  r=["Ubf", "Vpad"], w=[key])
                V(lambda e: e.tensor_tensor(out=Mint[:, g, :], in0=pmx[:, 0:128], in1=maskLT[:], op=ALU.mult), r=[key, "maskLT"], w=["Mint"])
            for ri in range(2):
                pwx = pw[ri]
                key = "pw%d" % ri
                P(lambda e: e.transpose(out=pwx[:, 0:128], in_=Ubf[:, pr, ri, :], identity=idb[:]), r=["Ubf", "idb"], w=[key])
                A(lambda e: e.copy(out=Win[:, pr, 0, ri, 0:64], in_=pwx[:, 0:64]), r=[key], w=["Win"])
                A(lambda e: e.copy(out=Win[:, pr, 1, ri, 64:128], in_=pwx[:, 64:128]), r=[key], w=["Win"])

    if STOP <= 4:
        return
    with ExitStack() as ses:
        NCH = 64
        xh = K.sb(ses, "s5xh", [NCH, 4, D], F32)
        junk = K.sb(ses, "s5junk", [NCH, D], F32)
        useg = K.sb(ses, "s5useg", [NCH, 64, 8, 16], BF16)
        uT = K.sb(ses, "s5uT", [128, 64, NCH], BF16)
        X = K.sb(ses, "s5X", [128, 2, 32, NCH], F32)
        Hbf = K.sb(ses, "s5H", [128, 2, 32, NCH + 1], BF16)
        carry = K.sb(ses, "s5carry", [128, 2, 32], F32)
        t1 = K.sb(ses, "s5t1", [128, 2, 32], F32)
        t2 = K.sb(ses, "s5t2", [128, 2, 32], F32)
        gtok = K.sb(ses, "s5gtok", [NCH, 8, D], BF16)
        ysb = K.sb(ses, "s5ysb", [128, 8, NCH], F32)
        yc = K.sb(ses, "s5yc", [NCH, 8, 128], F32)
        yc2 = K.sb(ses, "s5yc2", [NCH, 8, 128], F32)
        ss = K.sb(ses, "s5ss", [NCH, 8], F32)
        ptr = [K.ps(ses, "s5ptr%d" % i, [128, 16, NCH], BF16) for i in range(2)]
        px = [K.ps(ses, "s5px%d" % i, [128, 4, 2, NCH], F32) for i in range(2)]
        py = K.ps(ses, "s5py", [128, 8, NCH], F32)
        pyt = [K.ps(ses, "s5pyt%d" % i, [NCH, 4, 128], F32) for i in range(2)]

        V(lambda e: e.memset(carry[:], 0.0), w=["carry"])
        xp = I["xp"]
        for seg in range(T // (8 * NCH)):
            rows = xp[seg * 8 * NCH:(seg + 1) * 8 * NCH, :].rearrange("(n s) d -> n s d", s=8)
            for h in range(2):
                dma(xh[:], rows[:, 4 * h:4 * h + 4, :], w=["xh"])
                for s in range(4):
                    A(lambda e: e.activation(out=junk[:], in_=xh[:, s, :], func=AF.Square, accum_out=ss[:, 4 * h + s:4 * h + s + 1]),
                      r=["xh"], w=["junk", "ss"])
                V(lambda e: e.tensor_scalar(out=ss[:, 4 * h:4 * h + 4], in0=ss[:, 4 * h:4 * h + 4], scalar1=1.0 / D, scalar2=EPS,
                                            op0=ALU.mult, op1=ALU.add), r=["ss"], w=["ss"])
                A(lambda e: e.activation(out=ss[:, 4 * h:4 * h + 4], in_=ss[:, 4 * h:4 * h + 4], func=AF.Sqrt), r=["ss"], w=["ss"])
                V(lambda e: e.reciprocal(out=ss[:, 4 * h:4 * h + 4], in_=ss[:, 4 * h:4 * h + 4]), r=["ss"], w=["ss"])
                for s in range(4):
                    V(lambda e: e.scalar_tensor_tensor(out=useg[:, :, 4 * h + s, :], in0=xh[:, s, :].rearrange("n (g c) -> n g c", c=16),
                                                       scalar=ss[:, 4 * h + s:4 * h + s + 1],
                                                       in1=gain0[0:NCH, :].rearrange("n (g c) -> n g c", c=16), op0=ALU.mult, op1=ALU.mult),
                      r=["xh", "ss", "gain0"], w=["useg"])
            if STOP <= 5:
                return
            for g8 in range(8):
                pt_ = ptr[g8 % 2]
                key = "ptr%d" % (g8 % 2)
                for j in range(8):
                    g = 8 * g8 + j
                    P(lambda e: e.transpose(out=pt_[:, j, :], in_=useg[:, g, :, :], identity=idb[0:NCH, 0:NCH]),
                      r=["useg", "idb"], w=[key])
                A(lambda e: e.copy(out=uT[:, 8 * g8:8 * g8 + 8, :], in_=pt_[:, 0:8, :]), r=[key], w=["uT"])
            if STOP <= 6:
                return
            for q in range(8):
                px_ = px[q % 2]
                key = "px%d" % (q % 2)
                for a in range(4):
                    pr = 4 * q + a
                    for ri in range(2):
                        P(lambda e: e.matmul(px_[:, a, ri, :], lhsT=Win[:, pr, 0, ri, :], rhs=uT[:, 2 * pr, :], start=True, stop=False),
                          r=["Win", "uT"], w=[key])
                        P(lambda e: e.matmul(px_[:, a, ri, :], lhsT=Win[:, pr, 1, ri, :], rhs=uT[:, 2 * pr + 1, :], start=False, stop=True),
                          r=["Win", "uT"], w=[key])
                A(lambda e: e.copy(out=X[:, :, 4 * q:4 * q + 4, :], in_=px_[:].rearrange("p a r n -> p r a n")), r=[key], w=["X"])
            if STOP <= 7:
                return
            V(lambda e: e.tensor_copy(out=Hbf[:, :, :, 0], in_=carry[:]), r=["carry"], w=["Hbf"])
            for n in range(NCH):
                Sp = carry[:] if n == 0 else X[:, :, :, n - 1]
                V(lambda e: e.tensor_tensor(out=t1[:], in0=Sp, in1=CA[:], op=ALU.mult), r=["X", "carry", "CA"], w=["t1"])
                V(lambda e: e.tensor_tensor(out=t2[:, 0, :], in0=Sp[:, 1, :], in1=CBn[:], op=ALU.mult), r=["X", "carry", "CBn"], w=["t2"])
                V(lambda e: e.tensor_tensor(out=t2[:, 1, :], in0=Sp[:, 0, :], in1=CBp[:], op=ALU.mult), r=["X", "carry", "CBp"], w=["t2"])
                V(lambda e: e.tensor_tensor(out=t1[:], in0=t1[:], in1=t2[:], op=ALU.add), r=["t1", "t2"], w=["t1"])
                V(lambda e: e.tensor_tensor(out=X[:, :, :, n], in0=X[:, :, :, n], in1=t1[:], op=ALU.add), r=["t1", "X"], w=["X"])
            V(lambda e: e.tensor_copy(out=carry[:], in_=X[:, :, :, NCH - 1]), r=["X"], w=["carry"])
            A(lambda e: e.copy(out=Hbf[:, :, :, 1:NCH + 1], in_=X[:]), r=["X"], w=["Hbf"])
            if STOP <= 8:
                return
            for g8 in range(8):
                for j in range(8):
                    g = 8 * g8 + j
                    pr, gl = g // 2, g % 2
                    sl = slice(64 * gl, 64 * gl + 64)
                    P(lambda e: e.matmul(py[:, j, :], lhsT=Mint[:, g, :], rhs=uT[:, g, :], start=True, stop=False),
                      r=["Mint", "uT"], w=["py"])
                    P(lambda e: e.matmul(py[:, j, :], lhsT=Wout[sl, pr, 0, :], rhs=Hbf[sl, 0, pr, 0:NCH], start=False, stop=False),
                      r=["Wout", "Hbf"], w=["py"])
                    P(lambda e: e.matmul(py[:, j, :], lhsT=Wout[sl, pr, 1, :], rhs=Hbf[sl, 1, pr, 0:NCH], start=False, stop=True),
                      r=["Wout", "Hbf"], w=["py"])
                A(lambda e: e.copy(out=ysb[:], in_=py[:]), r=["py"], w=["ysb"])
                for j in range(8):
                    pyt_ = pyt[j // 4]
                    P(lambda e: e.transpose(out=pyt_[:, j % 4, :], in_=ysb[:, j, :], identity=idf[:]), r=["ysb", "idf"], w=["pyt%d" % (j // 4)])
                cs_ = slice(128 * g8, 128 * g8 + 128)
                for hh in range(2):
                    V(lambda e: e.tensor_copy(
                        out=yc[:, :, 64 * hh:64 * hh + 64].rearrange("n s (j c) -> n s j c", c=16),
                        in_=pyt[hh][:].rearrange("n j (s c) -> n s j c", c=16)), r=["pyt%d" % hh], w=["yc"])
                V(lambda e: e.tensor_tensor(out=yc2[:].rearrange("n s (j c) -> n s j c", c=16),
                                            in0=useg[:, 8 * g8:8 * g8 + 8, :, :].rearrange("n j s c -> n s j c"),
                                            in1=bc(dskip[0:NCH, cs_].unsqueeze(1), [NCH, 8, 128]).rearrange("n s (j c) -> n s j c", c=16), op=ALU.mult),
                  r=["useg", "dskip"], w=["yc2"])
                V(lambda e: e.tensor_tensor(out=yc[:], in0=yc[:], in1=yc2[:], op=ALU.add), r=["yc", "yc2"], w=["yc"])
                V(lambda e: e.tensor_tensor(out=yc2[:], in0=yc[:], in1=yc[:], op=ALU.mult), r=["yc"], w=["yc2"])
                V(lambda e: e.tensor_scalar(out=yc2[:], in0=yc2[:], scalar1=0.044715, scalar2=1.0, op0=ALU.mult, op1=ALU.add), r=["yc2"], w=["yc2"])
                V(lambda e: e.tensor_tensor(out=yc2[:], in0=yc2[:], in1=yc[:], op=ALU.mult), r=["yc", "yc2"], w=["yc2"])
                A(lambda e: e.activation(out=yc2[:], in_=yc2[:], func=AF.Sigmoid, scale=1.5957691216057308), r=["yc2"], w=["yc2"])
                V(lambda e: e.tensor_tensor(out=gtok[:, :, cs_], in0=yc[:], in1=yc2[:], op=ALU.mult), r=["yc", "yc2"], w=["gtok"])
            dma(O["g_scr"][seg * 8 * NCH:(seg + 1) * 8 * NCH, :].rearrange("(n s) d -> n s d", s=8), gtok[:], r=["gtok"], w=["g_scr"],
                add_writer=True)
        stT = K.sb(ses, "s5stT", [32, 2, 128], F32)
        for ri, nm in enumerate(["s5_re_p", "s5_im_p"]):
            P(lambda e: e.transpose(out=py[0:32, ri, :].rearrange("p n -> p n") if False else pyt[0][0:32, ri, :], in_=carry[:, ri, :], identity=idf[:]),
              r=["carry", "idf"], w=["pyt0"])
            V(lambda e: e.tensor_copy(out=stT[:, ri, :], in_=pyt[0][0:32, ri, :]), r=["pyt0"], w=["stT"])
            dma(O[nm].rearrange("(pr gl) p -> pr (gl p)", gl=2), stT[:, ri, :], r=["stT"], w=[nm])


    K.S.barrier()
    _s5_sample(K, C, I, O, Win, Wout, Mint, A1, gain0, dskip)


def gelu_tanh(K, y, ykey, tmp, tkey, out, okey):
    V, A = K.V, K.A
    V(lambda e: e.tensor_tensor(out=tmp, in0=y, in1=y, op=ALU.mult), r=[ykey], w=[tkey])
    V(lambda e: e.tensor_scalar(out=tmp, in0=tmp, scalar1=0.044715, scalar2=1.0, op0=ALU.mult, op1=ALU.add), r=[tkey], w=[tkey])
    V(lambda e: e.tensor_tensor(out=tmp, in0=tmp, in1=y, op=ALU.mult), r=[ykey, tkey], w=[tkey])
    A(lambda e: e.activation(out=tmp, in_=tmp, func=AF.Sigmoid, scale=1.5957691216057308), r=[tkey], w=[tkey])
    V(lambda e: e.tensor_tensor(out=out, in0=y, in1=tmp, op=ALU.mult), r=[ykey, tkey], w=[okey])


def _s5_sample(K, C, I, O, Win, Wout, Mint, A1, gain0, dskip):
    idf, idb = C["idf"], C["idb"]
    V, A, G, P, dma = K.V, K.A, K.G, K.P, K.dma
    N = NS
    with ExitStack() as es:
        xs = K.sb(es, "ss_x", [N, D], F32)
        ss = K.sb(es, "ss_ss", [N, 1], F32)
        us = K.sb(es, "ss_us", [N, D], F32)
        u0 = K.sb(es, "ss_u0", [N, 64, 8, 16], BF16)
        uT0 = K.sb(es, "ss_uT0", [128, 64, N], BF16)
        uT7 = K.sb(es, "ss_uT7", [128, 64, N], BF16)
        st = K.sb(es, "ss_st", [N, 2, 4096], F32)
        stO = st
        H0 = K.sb(es, "ss_H0", [128, 2, 32, N], F32)
        H0b = K.sb(es, "ss_H0b", [128, 2, 32, N], BF16)
        X7 = K.sb(es, "ss_X7", [128, 2, 32, N], F32)
        H1 = K.sb(es, "ss_H1", [128, 2, 32, N], F32)
        t1 = K.sb(es, "ss_t1", [128, 2, 32, N], F32)
        t2 = K.sb(es, "ss_t2", [128, 2, 32, N], F32)
        ys = K.sb(es, "ss_ys", [N, D], F32)
        y2 = K.sb(es, "ss_y2", [N, D], F32)
        junk = y2
        gsb = K.sb(es, "ss_g", [N, D], BF16)
        ptrA = K.ps(es, "ss_ptrA", [128, 64, N], BF16)
        ptrB = K.ps(es, "ss_ptrB", [128, 64, N], BF16)
        pxs = [K.ps(es, "ss_px%d" % i, [128, 16, 2, N], F32) for i in range(2)]
        ph = [K.ps(es, "ss_ph%d" % i, [128, 32, N], F32) for i in range(2)]
        pys = [K.ps(es, "ss_py%d" % i, [N, 32, 16], F32) for i in range(2)]

        dma(xs[:], I["xs"], w=["sxs"])
        dma(st[:, 0, :], I["st_re"], w=["sst"])
        dma(st[:, 1, :], I["st_im"], w=["sst"], add_writer=True)
        A(lambda e: e.activation(out=junk[:], in_=xs[:], func=AF.Square, accum_out=ss[:, 0:1]), r=["sxs"], w=["sy2", "sss"])
        V(lambda e: e.tensor_scalar(out=ss[:], in0=ss[:], scalar1=1.0 / D, scalar2=EPS, op0=ALU.mult, op1=ALU.add), r=["sss"], w=["sss"])
        A(lambda e: e.activation(out=ss[:], in_=ss[:], func=AF.Sqrt), r=["sss"], w=["sss"])
        V(lambda e: e.reciprocal(out=ss[:], in_=ss[:]), r=["sss"], w=["sss"])
        V(lambda e: e.scalar_tensor_tensor(out=us[:], in0=xs[:], scalar=ss[:, 0:1], in1=gain0[0:N, :], op0=ALU.mult, op1=ALU.mult),
          r=["sxs", "sss", "gain0"], w=["sus"])
        usv = us[:].rearrange("n (g c) -> n g c", c=16)
        G(lambda e: e.memset(u0[:], 0.0), w=["su0"])
        V(lambda e: e.tensor_copy(out=u0[:, :, 0, :], in_=usv), r=["sus", "su0"], w=["su0"])
        for g in range(64):
            P(lambda e: e.transpose(out=ptrA[:, g, :], in_=u0[:, g, :, :], identity=idb[0:N, 0:N]), r=["su0", "idb"], w=["sptrA"])
        G(lambda e: e.memset(u0[:], 0.0), w=["su0"])
        V(lambda e: e.tensor_copy(out=u0[:, :, 7, :], in_=usv), r=["sus", "su0"], w=["su0"])
        for g in range(64):
            P(lambda e: e.transpose(out=ptrB[:, g, :], in_=u0[:, g, :, :], identity=idb[0:N, 0:N]), r=["su0", "idb"], w=["sptrB"])
        A(lambda e: e.copy(out=uT0[:], in_=ptrA[:]), r=["sptrA"], w=["suT0"])
        A(lambda e: e.copy(out=uT7[:], in_=ptrB[:]), r=["sptrB"], w=["suT7"])
        for pr in range(32):
            h_, a_ = pr // 16, pr % 16
            for ri in range(2):
                P(lambda e: e.matmul(pxs[h_][:, a_, ri, :], lhsT=Win[:, pr, 0, ri, :], rhs=uT7[:, 2 * pr, :], start=True, stop=False),
                  r=["Win", "suT7"], w=["spx%d" % h_])
                P(lambda e: e.matmul(pxs[h_][:, a_, ri, :], lhsT=Win[:, pr, 1, ri, :], rhs=uT7[:, 2 * pr + 1, :], start=False, stop=True),
                  r=["Win", "suT7"], w=["spx%d" % h_])
        for h_ in range(2):
            A(lambda e: e.copy(out=X7[:, :, 16 * h_:16 * h_ + 16, :], in_=pxs[h_][:].rearrange("p a r n -> p r a n")), r=["spx%d" % h_], w=["sX7"])
        for ri in range(2):
            for pr in range(32):
                P(lambda e: e.transpose(out=ph[ri][:, pr, :], in_=st[:, ri, pr * 128:(pr + 1) * 128], identity=idf[0:N, 0:N]),
                  r=["sst", "idf"], w=["sph%d" % ri])
            V(lambda e: e.tensor_copy(out=H0[:, ri, :, :], in_=ph[ri][:]), r=["sph%d" % ri], w=["sH0"])
        A(lambda e: e.copy(out=H0b[:], in_=H0[:]), r=["sH0"], w=["sH0b"])
        a1r = bc(A1[:, 0, :].unsqueeze(2), [128, 32, N])
        a1i = bc(A1[:, 1, :].unsqueeze(2), [128, 32, N])
        for ri in range(2):
            V(lambda e: e.tensor_tensor(out=t1[:, ri], in0=H0[:, ri], in1=a1r, op=ALU.mult), r=["sH0", "A1"], w=["st1"])
            V(lambda e: e.tensor_tensor(out=t2[:, ri], in0=H0[:, 1 - ri], in1=a1i, op=ALU.mult), r=["sH0", "A1"], w=["st2"])
        V(lambda e: e.tensor_tensor(out=H1[:, 0], in0=t1[:, 0], in1=t2[:, 0], op=ALU.subtract), r=["st1", "st2"], w=["sH1"])
        V(lambda e: e.tensor_tensor(out=H1[:, 1], in0=t1[:, 1], in1=t2[:, 1], op=ALU.add), r=["st1", "st2"], w=["sH1"])
        V(lambda e: e.tensor_tensor(out=H1[:], in0=H1[:], in1=X7[:], op=ALU.add), r=["sH1", "sX7"], w=["sH1"])
        idx = 0
        for ri in range(2):
            for q in range(8):
                b_ = idx % 2
                idx += 1
                pv = pxs[b_][0:N].rearrange("p a r n -> p (a r n)")
                for j in range(4):
                    pr = 4 * q + j
                    P(lambda e: e.transpose(out=pv[:, j * 128:(j + 1) * 128], in_=H1[:, ri, pr, :], identity=idf[:]),
                      r=["sH1", "idf"], w=["spx%d" % b_])
                V(lambda e: e.tensor_copy(out=stO[:, ri, 512 * q:512 * q + 512], in_=pv[:, 0:512]), r=["spx%d" % b_], w=["sst"])
        dma(O["s5_re_s"], stO[:, 0, :], r=["sst"], w=["s5_re_s"])
        dma(O["s5_im_s"], stO[:, 1, :], r=["sst"], w=["s5_im_s"])
        for g in range(64):
            pr, gl = g // 2, g % 2
            sl = slice(64 * gl, 64 * gl + 64)
            h_ = g // 32
            P(lambda e: e.matmul(pys[h_][:, g % 32, :], lhsT=uT0[:, g, :], rhs=Mint[:, g, 0:16], start=True, stop=False),
              r=["suT0", "Mint"], w=["spy%d" % h_])
            P(lambda e: e.matmul(pys[h_][:, g % 32, :], lhsT=H0b[sl, 0, pr, :], rhs=Wout[sl, pr, 0, 0:16], start=False, stop=False),
              r=["sH0b", "Wout"], w=["spy%d" % h_])
            P(lambda e: e.matmul(pys[h_][:, g % 32, :], lhsT=H0b[sl, 1, pr, :], rhs=Wout[sl, pr, 1, 0:16], start=False, stop=True),
              r=["sH0b", "Wout"], w=["spy%d" % h_])
        V(lambda e: e.tensor_tensor(out=y2[:], in0=us[:], in1=dskip[0:N, :], op=ALU.mult), r=["sus", "dskip"], w=["sy2"])
        for h_ in range(2):
            V(lambda e: e.tensor_tensor(out=ys[:, 512 * h_:512 * h_ + 512], in0=pys[h_][:].rearrange("n g c -> n (g c)"),
                                        in1=y2[:, 512 * h_:512 * h_ + 512], op=ALU.add), r=["spy%d" % h_, "sy2"], w=["sys"])
        gelu_tanh(K, ys[:], "sys", y2[:], "sy2", gsb[:], "sgsb")
        dma(O["gs_scr"], gsb[:], r=["sgsb"], w=["gs_scr"])
        V(lambda e: e.tensor_tensor(out=ys[:], in0=ys[:], in1=y2[:], op=ALU.mult), r=["sys", "sy2", "sgsb"], w=["sys"])
        dma(O["gs32_scr"], ys[:], r=["sys"], w=["gs32_scr"])

def load_w_bf16(K, wt, src, ncols, step=1024):
    v = src.rearrange("(k p) n -> p k n", p=128)
    for c0 in range(0, ncols, step):
        c1 = min(ncols, c0 + step)
        K.S.dma("pool", wt[:, :, c0:c1], v[:, :, c0:c1], (), ["w_" + wt.name], add_writer=(c0 > 0))


def rms_to_bf16(K, x_t, xkey, gain, h_t, hkey, junk, ss, sfx, Pn=128):
    V, A = K.V, K.A
    A(lambda e: e.activation(out=junk[0:Pn, :], in_=x_t[0:Pn, :], func=AF.Square, accum_out=ss[0:Pn, 0:1]), r=[xkey], w=["junk" + sfx, "ss" + sfx])
    V(lambda e: e.tensor_scalar(out=ss[0:Pn, 0:1], in0=ss[0:Pn, 0:1], scalar1=1.0 / D, scalar2=EPS, op0=ALU.mult, op1=ALU.add), r=["ss" + sfx], w=["ss" + sfx])
    A(lambda e: e.activation(out=ss[0:Pn, 0:1], in_=ss[0:Pn, 0:1], func=AF.Sqrt), r=["ss" + sfx], w=["ss" + sfx])
    V(lambda e: e.reciprocal(out=ss[0:Pn, 0:1], in_=ss[0:Pn, 0:1]), r=["ss" + sfx], w=["ss" + sfx])
    V(lambda e: e.scalar_tensor_tensor(out=h_t[0:Pn, :], in0=x_t[0:Pn, :], scalar=ss[0:Pn, 0:1], in1=gain[0:Pn, :], op0=ALU.mult, op1=ALU.mult),
      r=[xkey, "ss" + sfx, "gain"], w=[hkey])


def transpose_tiles(K, src, skey, nk, dstT, dkey, ptb, idb, Pn=128):
    for k0 in range(0, nk, 8):
        k1 = min(nk, k0 + 8)
        for k in range(k0, k1):
            K.P(lambda e: e.transpose(out=ptb[:, k - k0, 0:Pn], in_=src[0:Pn, k * 128:(k + 1) * 128], identity=idb[0:Pn, 0:Pn]),
                r=[skey, "idb"], w=["ptb"])
        K.A(lambda e: e.copy(out=dstT[:, k0:k1, 0:Pn], in_=ptb[:, 0:k1 - k0, 0:Pn]), r=["ptb"], w=[dkey])


NTILE = T // 128


def tile_rows(t):
    return (128, slice(t * 128, (t + 1) * 128)) if t < NTILE else (NS, None)


def pick(big, small, t):
    Pn, rs = tile_rows(t)
    return big[rs, :] if rs is not None else small


def glu_stage(K, C, I, O):
    idb = C["idb"]
    V, A, G, P, dma = K.V, K.A, K.G, K.P, K.dma
    with ExitStack() as es:
        wg = K.sb(es, "wglu", [128, 8, 2048], BF16)
        load_w_bf16(K, wg, I["s5_w_glu"], 2048)
        gt = [K.sb(es, "glu_g%d" % i, [128, D], BF16) for i in range(2)]
        xt = [K.sb(es, "glu_x%d" % i, [128, D], F32) for i in range(2)]
        gT = [K.sb(es, "glu_gT%d" % i, [128, 8, 128], BF16) for i in range(2)]
        sg = [K.sb(es, "glu_sg%d" % i, [128, D], F32) for i in range(2)]
        ptb = K.ps(es, "glu_ptb", [128, 8, 128], BF16)
        pa = [K.ps(es, "glu_pa%d" % i, [128, 512], F32) for i in range(4)]
        for t in range(NTILE):
            b = t % 2
            sx = str(b)
            Pn, _ = tile_rows(t)
            dma(gt[b][0:Pn, :], pick(O["g_scr"], O["gs_scr"], t), r=["g_scr", "gs_scr"], w=["gt" + sx])
            dma(xt[b][0:Pn, :], pick(I["xp"], I["xs"], t), w=["xt" + sx])
            transpose_tiles(K, gt[b], "gt" + sx, 8, gT[b], "gT" + sx, ptb, idb, Pn)
            for c in range(4):
                for k in range(8):
                    P(lambda e: e.matmul(pa[c][0:Pn, :], lhsT=gT[b][:, k, 0:Pn], rhs=wg[:, k, c * 512:(c + 1) * 512], start=(k == 0), stop=(k == 7)),
                      r=["gT" + sx, "w_wglu"], w=["pa%d" % c])
            for hh in range(2):
                cs = slice(512 * hh, 512 * hh + 512)
                A(lambda e: e.activation(out=sg[b][0:Pn, cs], in_=pa[2 + hh][0:Pn, :], func=AF.Sigmoid), r=["pa%d" % (2 + hh)], w=["sg" + sx])
                V(lambda e: e.tensor_tensor(out=sg[b][0:Pn, cs], in0=pa[hh][0:Pn, :], in1=sg[b][0:Pn, cs], op=ALU.mult), r=["pa%d" % hh, "sg" + sx], w=["sg" + sx])
            V(lambda e: e.tensor_tensor(out=xt[b][0:Pn, :], in0=xt[b][0:Pn, :], in1=sg[b][0:Pn, :], op=ALU.add), r=["xt" + sx, "sg" + sx], w=["xt" + sx])
            dma(pick(O["y1_scr"], O["y1s_scr"], t), xt[b][0:Pn, :], r=["xt" + sx], w=["y1_scr"], add_writer=True)
    K.S.barrier()


def ffn_stage(K, C, I, O):
    idb = C["idb"]
    V, A, G, P, dma = K.V, K.A, K.G, K.P, K.dma
    FF = 2816
    CW = 352
    with ExitStack() as es:
        w1 = K.sb(es, "ffw1", [128, 8, 2 * FF], BF16)
        w2 = K.sb(es, "ffw2", [128, 22, D], BF16)
        load_w_bf16(K, w1, I["ffn_w_in"], 2 * FF)
        load_w_bf16(K, w2, I["ffn_w_out"], D)
        gain = K.sb(es, "ff_gain", [128, D], F32)
        dma(gain[:], AP(tensor=I["norm_ffn"].tensor, offset=0, ap=[[0, 128], [1, D]]), w=["gain"])
        yt = [K.sb(es, "ff_y%d" % i, [128, D], F32) for i in range(2)]
        hb = [K.sb(es, "ff_h%d" % i, [128, D], BF16) for i in range(2)]
        hT = [K.sb(es, "ff_hT%d" % i, [128, 8, 128], BF16) for i in range(2)]
        act = [K.sb(es, "ff_act%d" % i, [128, FF], BF16) for i in range(2)]
        actT = [K.sb(es, "ff_actT%d" % i, [128, 22, 128], BF16) for i in range(2)]
        sl_ = [K.sb(es, "ff_s%d" % i, [128, CW], F32) for i in range(2)]
        junk = K.sb(es, "ff_junk", [128, D], F32)
        ss = [K.sb(es, "ff_ss%d" % i, [128, 1], F32) for i in range(2)]
        ptb = K.ps(es, "ff_ptb", [128, 8, 128], BF16)
        pa = [K.ps(es, "ff_pa%d" % i, [128, 512], F32) for i in range(2)]
        pb = [K.ps(es, "ff_pb%d" % i, [128, 512], F32) for i in range(2)]
        po = [K.ps(es, "ff_po%d" % i, [128, 512], F32) for i in range(2)]
        for t in range(NTILE):
            b = t % 2
            sx = str(b)
            Pn, _ = tile_rows(t)
            dma(yt[b][0:Pn, :], pick(O["y1_scr"], O["y1s_scr"], t), r=["y1_scr"], w=["yt" + sx])
            rms_to_bf16(K, yt[b], "yt" + sx, gain, hb[b], "hb" + sx, junk, ss[b], sx, Pn)
            transpose_tiles(K, hb[b], "hb" + sx, 8, hT[b], "hT" + sx, ptb, idb, Pn)
            for j in range(8):
                jb = j % 2
                for k in range(8):
                    P(lambda e: e.matmul(pa[jb][0:Pn, 0:CW], lhsT=hT[b][:, k, 0:Pn], rhs=w1[:, k, CW * j:CW * j + CW], start=(k == 0), stop=(k == 7)),
                      r=["hT" + sx, "w_ffw1"], w=["fpa%d" % jb])
                for k in range(8):
                    P(lambda e: e.matmul(pb[jb][0:Pn, 0:CW], lhsT=hT[b][:, k, 0:Pn], rhs=w1[:, k, FF + CW * j:FF + CW * j + CW], start=(k == 0), stop=(k == 7)),
                      r=["hT" + sx, "w_ffw1"], w=["fpb%d" % jb])
                A(lambda e: e.activation(out=sl_[jb][0:Pn, :], in_=pa[jb][0:Pn, 0:CW], func=AF.Silu), r=["fpa%d" % jb], w=["fsl%d" % jb])
                V(lambda e: e.tensor_tensor(out=act[b][0:Pn, CW * j:CW * j + CW], in0=pb[jb][0:Pn, 0:CW], in1=sl_[jb][0:Pn, :], op=ALU.mult),
                  r=["fpb%d" % jb, "fsl%d" % jb], w=["act" + sx])
            transpose_tiles(K, act[b], "act" + sx, 22, actT[b], "actT" + sx, ptb, idb, Pn)
            for c in range(2):
                for k in range(22):
                    P(lambda e: e.matmul(po[c][0:Pn, :], lhsT=actT[b][:, k, 0:Pn], rhs=w2[:, k, c * 512:(c + 1) * 512], start=(k == 0), stop=(k == 21)),
                      r=["actT" + sx, "w_ffw2"], w=["fpo%d" % c])
                V(lambda e: e.tensor_tensor(out=yt[b][0:Pn, c * 512:(c + 1) * 512], in0=po[c][0:Pn, :], in1=yt[b][0:Pn, c * 512:(c + 1) * 512], op=ALU.add),
                  r=["fpo%d" % c, "yt" + sx], w=["yt" + sx])
            dma(pick(O["y2_scr"], O["y2s_scr"], t), yt[b][0:Pn, :], r=["yt" + sx], w=["y2_scr"], add_writer=True)
    K.S.barrier()


def nsa_proj_stage(K, C, I, O):
    idb = C["idb"]
    V, A, G, P, dma = K.V, K.A, K.G, K.P, K.dma
    NCOL = 2608
    with ExitStack() as es:
        wn = K.sb(es, "nsw", [128, 8, NCOL], BF16)
        load_w_bf16(K, wn, I["nsa_w_in"], NCOL)
        gain = K.sb(es, "ns_gain", [128, D], F32)
        dma(gain[:], AP(tensor=I["norm_mix"].tensor, offset=D, ap=[[0, 128], [1, D]]), w=["gain"])
        yt = [K.sb(es, "ns_y%d" % i, [128, D], F32) for i in range(2)]
        hb = [K.sb(es, "ns_h%d" % i, [128, D], BF16) for i in range(2)]
        hT = [K.sb(es, "ns_hT%d" % i, [128, 8, 128], BF16) for i in range(2)]
        kv = [K.sb(es, "ns_kv%d" % i, [128, 1536], F32) for i in range(2)]
        kvb = [K.sb(es, "ns_kvb%d" % i, [128, 1536], BF16) for i in range(2)]
        qb = [K.sb(es, "ns_qb%d" % i, [128, 1024], BF16) for i in range(2)]
        gt_ = [K.sb(es, "ns_gt%d" % i, [128, 48], F32) for i in range(2)]
        junk = K.sb(es, "ns_junk", [128, D], F32)
        ss = [K.sb(es, "ns_ss%d" % i, [128, 1], F32) for i in range(2)]
        ptb = K.ps(es, "ns_ptb", [128, 8, 128], BF16)
        pk = [K.ps(es, "ns_pk%d" % i, [128, 512], F32) for i in range(6)]
        for t in range(NTILE):
            b = t % 2
            sx = str(b)
            Pn, rs = tile_rows(t)
            dma(yt[b][0:Pn, :], pick(O["y2_scr"], O["y2s_scr"], t), r=["y2_scr"], w=["yt" + sx])
            rms_to_bf16(K, yt[b], "yt" + sx, gain, hb[b], "hb" + sx, junk, ss[b], sx, Pn)
            transpose_tiles(K, hb[b], "hb" + sx, 8, hT[b], "hT" + sx, ptb, idb, Pn)
            for c in range(6):
                c0 = 512 * c
                cw = min(512, NCOL - c0)
                for k in range(8):
                    P(lambda e: e.matmul(pk[c][0:Pn, 0:cw], lhsT=hT[b][:, k, 0:Pn], rhs=wn[:, k, c0:c0 + cw], start=(k == 0), stop=(k == 7)),
                      r=["hT" + sx, "w_nsw"], w=["npk%d" % c])
            for c in range(2):
                A(lambda e: e.activation(out=qb[b][0:Pn, 512 * c:512 * c + 512], in_=pk[c][0:Pn, :], func=AF.Identity, scale=0.125),
                  r=["npk%d" % c], w=["qb" + sx])
            for c in range(3):
                A(lambda e: e.copy(out=kv[b][0:Pn, 512 * c:512 * c + 512], in_=pk[2 + c][0:Pn, :]), r=["npk%d" % (2 + c)], w=["kv" + sx])
                V(lambda e: e.tensor_copy(out=kvb[b][0:Pn, 512 * c:512 * c + 512], in_=kv[b][0:Pn, 512 * c:512 * c + 512]), r=["kv" + sx], w=["kvb" + sx])
            A(lambda e: e.activation(out=gt_[b][0:Pn, :], in_=pk[5][0:Pn, 0:48], func=AF.Sigmoid), r=["npk5"], w=["gt_" + sx])
            dma(pick(O["kv_p"], O["kv_s"], t), kv[b][0:Pn, 0:1024], r=["kv" + sx], w=["kv_p"], add_writer=True)
            NSDBG = int(os.environ.get("NSDBG", "0"))
            if not NSDBG & 1:
                dma(pick(O["q_scr"], O["qs_scr"], t), qb[b][0:Pn, :], r=["qb" + sx], w=["q_scr"], add_writer=True)
            if not NSDBG & 2:
                dma(pick(O["kvb_scr"], O["kvbs_scr"], t), kvb[b][0:Pn, :], r=["kvb" + sx], w=["kvb_scr"], add_writer=True)
            if not NSDBG & 4:
                dma(pick(O["gate_scr"], O["gates_scr"], t), gt_[b][0:Pn, :], r=["gt_" + sx], w=["gate_scr"], add_writer=True)
            if rs is None:
                dma(O["win_s"], kv[b][0:Pn, 1024:1536], r=["kv" + sx], w=["win_p"], add_writer=True)
            elif t >= NTILE - 4:
                tt = t - (NTILE - 4)
                dma(O["win_p"][tt * 128:(tt + 1) * 128, :], kv[b][0:Pn, 1024:1536], r=["kv" + sx], w=["win_p"], add_writer=True)
    K.S.barrier()


class Lin32:
    def __init__(self, K, es, C, kcmax, tag):
        self.K, self.C, self.tag = K, C, tag
        self.wch = [K.sb(es, "l32w%s%d" % (tag, i), [128, kcmax, 512], F32) for i in range(2)]
        self.ps = [K.ps(es, "l32p%s%d" % (tag, i), [128, 512], F32) for i in range(2)]
        self.pt = K.ps(es, "l32t%s" % tag, [128, 32, NS], F32)
        self.i = 0

    def transpose(self, src, skey, kc, dstT, dkey):
        K, idf = self.K, self.C["idf"]
        for k in range(kc):
            K.P(lambda e: e.transpose(out=self.pt[:, k, :], in_=src[:, k * 128:(k + 1) * 128], identity=idf[0:NS, 0:NS]),
                r=[skey, "idf"], w=["l32t" + self.tag])
        K.V(lambda e: e.tensor_copy(out=dstT[:, 0:kc, :], in_=self.pt[:, 0:kc, :]), r=["l32t" + self.tag], w=[dkey])

    def run(self, xT, xkey, kc, Wd, ncols, evac):
        K = self.K
        Wv = Wd.rearrange("(k p) n -> p k n", p=128)
        for c0 in range(0, ncols, 512):
            cw = min(512, ncols - c0)
            b = self.i % 2
            self.i += 1
            wk = "l32w%s%d" % (self.tag, b)
            pk = "l32p%s%d" % (self.tag, b)
            K.dma(self.wch[b][:, 0:kc, 0:cw], Wv[:, :, c0:c0 + cw], w=[wk])
            for k in range(kc):
                K.P(lambda e: e.matmul(self.ps[b][0:NS, 0:cw], lhsT=xT[:, k, :], rhs=self.wch[b][:, k, 0:cw], start=(k == 0), stop=(k == kc - 1)),
                    r=[xkey, wk], w=[pk])
            evac(c0, cw, self.ps[b][0:NS, 0:cw], pk)


def rms32(K, x, xkey, gain, out, okey, junk, ss, tag):
    V, A = K.V, K.A
    N = NS
    A(lambda e: e.activation(out=junk[0:N, :], in_=x, func=AF.Square, accum_out=ss[0:N, 0:1]), r=[xkey], w=["junk" + tag, "ss" + tag])
    V(lambda e: e.tensor_scalar(out=ss[0:N, :], in0=ss[0:N, :], scalar1=1.0 / D, scalar2=EPS, op0=ALU.mult, op1=ALU.add), r=["ss" + tag], w=["ss" + tag])
    A(lambda e: e.activation(out=ss[0:N, :], in_=ss[0:N, :], func=AF.Sqrt), r=["ss" + tag], w=["ss" + tag])
    V(lambda e: e.reciprocal(out=ss[0:N, :], in_=ss[0:N, :]), r=["ss" + tag], w=["ss" + tag])
    V(lambda e: e.scalar_tensor_tensor(out=out, in0=x, scalar=ss[0:N, 0:1], in1=gain[0:N, :], op0=ALU.mult, op1=ALU.mult),
      r=[xkey, "ss" + tag, "gain" + tag], w=[okey])


def sample_l0_f32(K, C, I, O):
    V, A, G, P, dma = K.V, K.A, K.G, K.P, K.dma
    N = NS
    FF = 2816
    with ExitStack() as es:
        L = Lin32(K, es, C, 22, "a")
        x = K.sb(es, "f_x", [N, D], F32)
        g32 = K.sb(es, "f_g", [N, D], F32)
        xT = K.sb(es, "f_xT", [128, 22, N], F32)
        big = K.sb(es, "f_big", [N, 2 * FF], F32)
        act = K.sb(es, "f_act", [N, FF], F32)
        h = K.sb(es, "f_h", [N, D], F32)
        junk = K.sb(es, "f_junk", [N, D], F32)
        ss = K.sb(es, "f_ss", [N, 1], F32)
        gain_f = K.sb(es, "f_gainf", [N, D], F32)
        gain_m = K.sb(es, "f_gainm", [N, D], F32)
        qb = K.sb(es, "f_qb", [N, D], BF16)
        kvb = K.sb(es, "f_kvb", [N, 1536], BF16)
        dma(x[:], I["xs"], w=["f_x"])
        dma(g32[:], O["gs32_scr"], r=["gs32_scr"], w=["f_g"])
        dma(gain_f[:], AP(tensor=I["norm_ffn"].tensor, offset=0, ap=[[0, N], [1, D]]), w=["gainf"])
        dma(gain_m[:], AP(tensor=I["norm_mix"].tensor, offset=D, ap=[[0, N], [1, D]]), w=["gainm"])

        def to_big(c0, cw, ps, pk):
            A(lambda e: e.copy(out=big[:, c0:c0 + cw], in_=ps), r=[pk], w=["f_big"])
        L.transpose(g32, "f_g", 8, xT, "f_xT")
        L.run(xT, "f_xT", 8, I["s5_w_glu"], 2048, to_big)
        A(lambda e: e.activation(out=big[:, 1024:2048], in_=big[:, 1024:2048], func=AF.Sigmoid), r=["f_big"], w=["f_big"])
        V(lambda e: e.tensor_tensor(out=big[:, 0:1024], in0=big[:, 0:1024], in1=big[:, 1024:2048], op=ALU.mult), r=["f_big"], w=["f_big"])
        V(lambda e: e.tensor_tensor(out=x[:], in0=x[:], in1=big[:, 0:1024], op=ALU.add), r=["f_big", "f_x"], w=["f_x"])
        rms32(K, x[:], "f_x", gain_f, h[:], "f_h", junk, ss, "f")
        L.transpose(h, "f_h", 8, xT, "f_xT")
        L.run(xT, "f_xT", 8, I["ffn_w_in"], 2 * FF, to_big)
        A(lambda e: e.activation(out=act[:], in_=big[:, 0:FF], func=AF.Silu), r=["f_big"], w=["f_act"])
        V(lambda e: e.tensor_tensor(out=act[:], in0=act[:], in1=big[:, FF:2 * FF], op=ALU.mult), r=["f_big", "f_act"], w=["f_act"])
        L.transpose(act, "f_act", 22, xT, "f_xT")

        def add_x(c0, cw, ps, pk):
            V(lambda e: e.tensor_tensor(out=x[:, c0:c0 + cw], in0=ps, in1=x[:, c0:c0 + cw], op=ALU.add), r=[pk, "f_x"], w=["f_x"])
        L.run(xT, "f_xT", 22, I["ffn_w_out"], D, add_x)
        dma(O["y2s_scr"], x[:], r=["f_x"], w=["y2_scr"], add_writer=True)
        rms32(K, x[:], "f_x", gain_m, h[:], "f_h", junk, ss, "m")
        L.transpose(h, "f_h", 8, xT, "f_xT")
        L.run(xT, "f_xT", 8, I["nsa_w_in"], 2608, to_big)
        A(lambda e: e.activation(out=qb[:], in_=big[:, 0:1024], func=AF.Identity, scale=0.125), r=["f_big"], w=["f_qb"])
        V(lambda e: e.tensor_copy(out=kvb[:], in_=big[:, 1024:2560]), r=["f_big"], w=["f_kvb"])
        A(lambda e: e.activation(out=act[:, 0:48], in_=big[:, 2560:2608], func=AF.Sigmoid), r=["f_big", "f_act"], w=["f_act"])
        dma(O["kv_s"], big[:, 1024:2048], r=["f_big"], w=["kv_p"], add_writer=True)
        dma(O["win_s"], big[:, 2048:2560], r=["f_big"], w=["win_p"], add_writer=True)
        dma(O["qs_scr"], qb[:], r=["f_qb"], w=["q_scr"], add_writer=True)
        dma(O["kvbs_scr"], kvb[:], r=["f_kvb"], w=["kvb_scr"], add_writer=True)
        dma(O["gates_scr"], act[:, 0:48], r=["f_act"], w=["gate_scr"], add_writer=True)
    K.S.barrier()


NEG = -30000.0
NCB = 255


def nsa_attn_prompt(K, C, I, O):
    idf, idb = C["idf"], C["idb"]
    V, A, G, P, dma = K.V, K.A, K.G, K.P, K.dma
    nc = K.nc
    with ExitStack() as es:
        KselE = K.sb(es, "na_KselE", [128, 4, T], BF16)
        KwinE = K.sb(es, "na_KwinE", [128, 4, T], BF16)
        Vsel = K.sb(es, "na_Vsel", [128, NTILE, 4, 65], BF16)
        Vwin = K.sb(es, "na_Vwin", [128, NTILE, 4, 65], BF16)
        kcT = K.sb(es, "na_kcT", [64, 4, 256], BF16)
        vca = K.sb(es, "na_vca", [128, 2, 4, 129], BF16)
        chb = K.sb(es, "na_chb", [128, 16], F32)
        W4 = K.sb(es, "na_W4", [128, 128], F32)
        for g in range(4):
            K.S.dma("pool", KselE[64:128, g, :], I["c_E"], (), ["KselE"], add_writer=True)
            K.S.dma("pool", KwinE[64:128, g, :], I["c_E"], (), ["KwinE"], add_writer=True)
        G(lambda e: e.memset(Vsel[:, :, :, 64:65], 1.0), w=["Vsel"])
        G(lambda e: e.memset(Vwin[:, :, :, 64:65], 1.0), w=["Vwin"])
        G(lambda e: e.memset(vca[:, :, :, 64:65], 1.0), w=["vca"])
        for nt in range(2):
            for g in range(4):
                K.S.dma("pool", vca[:, nt, g, 65:129], I["c_cover"][nt * 128:(nt + 1) * 128, :], (), ["vca"], add_writer=True)
        dma(chb[:], AP(tensor=I["rel_bias"].tensor, offset=31 * 16, ap=[[0, 128], [1, 16]]), w=["chb"])
        G(lambda e: e.memset(W4[:], 0.0), w=["W4"])
        G(lambda e: e.affine_select(out=W4[:], in_=W4[:], pattern=[[-1, 128]], compare_op=ALU.is_ge, fill=NEG, base=0, channel_multiplier=1),
          r=["W4"], w=["W4"])

        with ExitStack() as bes:
            tabs = K.sb(bes, "na_tab", [32, 16], F32)
            ohb = K.sb(bes, "na_ohb", [32, 256], F32)
            Fraw = K.sb(bes, "na_Fraw", [16, 256], F32)
            Fpad = K.sb(bes, "na_Fpad", [16, 384], F32)
            FC = K.sb(bes, "na_FC", [16, 8192], F32)
            pF = K.ps(bes, "na_pF", [128, 512], F32)
            dma(tabs[:], I["rel_bias"], w=["tabs"])
            dma(ohb[:], I["c_ohb"], w=["ohb"])
            P(lambda e: e.matmul(pF[0:16, 0:256], lhsT=tabs[:], rhs=ohb[:], start=True, stop=True), r=["tabs", "ohb"], w=["pF"])
            V(lambda e: e.tensor_copy(out=Fraw[:], in_=pF[0:16, 0:256]), r=["pF"], w=["Fraw"])
            G(lambda e: e.memset(Fpad[:], NEG), w=["Fpad"])
            V(lambda e: e.tensor_scalar(out=Fpad[:, 127:383], in0=Fraw[:], scalar1=Fraw[:, 255:256], scalar2=None, op0=ALU.subtract),
              r=["Fraw", "Fpad"], w=["Fpad"])
            dma(O["fpad_scr"], Fpad[:], r=["Fpad"], w=["fpad_scr"])
            G(lambda e: e.memset(FC[:, 0:4111], NEG), w=["FC"])
            V(lambda e: e.tensor_copy(out=FC[:, 4111:4367], in_=Fraw[:]), r=["Fraw", "FC"], w=["FC"])
            V(lambda e: e.tensor_copy(out=FC[:, 4367:8192], in_=bc(Fraw[:, 255:256], [16, 8192 - 4367])), r=["Fraw", "FC"], w=["FC"])
            dma(O["fc_scr"], FC[:], r=["FC"], w=["fc_scr"])

        K.S.barrier()
        with ExitStack() as ces:
            KcT = K.sb(ces, "na_KcT", [64, 2, 4, T], BF16)
            kvt = [K.sb(ces, "na_kvt%d" % i, [128, 1536], BF16) for i in range(2)]
            W1 = K.sb(ces, "na_W1", [64, 2, 32, 128], BF16)
            W2 = K.sb(ces, "na_W2", [128, 2, 64], BF16)
            pe_f = K.sb(ces, "na_pef", [32, 2, 64], F32)
            pe_b = K.sb(ces, "na_peb", [32, 2, 64], BF16)
            peT = K.sb(ces, "na_peT", [64, 2, 32], BF16)
            cv = K.sb(ces, "na_cv", [128, 2], F32)
            pre = K.sb(ces, "na_pre", [128, 256], F32)
            tmpg = K.sb(ces, "na_tmpg", [128, 256], F32)
            hidT = K.sb(ces, "na_hidT", [128, 256], BF16)
            ptk = [K.ps(ces, "na_ptk%d" % i, [64, 8, 128], BF16) for i in range(2)]
            ph = K.ps(ces, "na_ph", [128, 512], F32)
            pk2 = K.ps(ces, "na_pk2", [128, 512], F32)
            for kvi in range(2):
                K.S.dma("pool", W1[:, kvi, :, :], I["nsa_phi_w1"][kvi].rearrange("l d e -> d l e"), (), ["naW1"], add_writer=True)
                K.S.dma("pool", W2[:, kvi, :], I["nsa_phi_w2"][kvi], (), ["naW2"], add_writer=True)
            dma(pe_f[:], I["nsa_phi_pe"].rearrange("k l d -> l k d"), w=["pe_f"])
            V(lambda e: e.tensor_copy(out=pe_b[:], in_=pe_f[:]), r=["pe_f"], w=["pe_b"])
            for kvi in range(2):
                P(lambda e: e.transpose(out=ptk[0][:, kvi, 0:32], in_=pe_b[:, kvi, :], identity=idb[0:32, 0:32]), r=["pe_b", "idb"], w=["ptk0"])
            V(lambda e: e.tensor_copy(out=peT[:], in_=ptk[0][:, 0:2, 0:32]), r=["ptk0"], w=["peT"])
            for kvi in range(2):
                for l in range(32):
                    P(lambda e: e.matmul(ph[:, kvi:kvi + 1], lhsT=W1[:, kvi, l, :], rhs=peT[:, kvi, l:l + 1], start=(l == 0), stop=(l == 31)),
                      r=["naW1", "peT"], w=["ph"])
            V(lambda e: e.tensor_copy(out=cv[:], in_=ph[:, 0:2]), r=["ph"], w=["cv"])
            G(lambda e: e.memset(hidT[:], 0.0), w=["hidT"])
            for t in range(NTILE):
                b = t % 2
                cs = slice(t * 128, (t + 1) * 128)
                dma(kvt[b][:], O["kvb_scr"][cs, :], r=["kvb_scr"], w=["kvt%d" % b])
                for j in range(8):
                    P(lambda e: e.transpose(out=ptk[0][:, j, :], in_=kvt[b][:, j * 64:(j + 1) * 64], identity=idb[:]), r=["kvt%d" % b, "idb"], w=["ptk0"])
                A(lambda e: e.copy(out=KcT[:, :, :, cs], in_=ptk[0][:].rearrange("p (k g) n -> p k g n", g=4)), r=["ptk0"], w=["KcT"])
                for j in range(4):
                    P(lambda e: e.transpose(out=ptk[1][:, j, :], in_=kvt[b][:, 512 + j * 64:512 + (j + 1) * 64], identity=idb[:]), r=["kvt%d" % b, "idb"], w=["ptk1"])
                    P(lambda e: e.transpose(out=ptk[1][:, 4 + j, :], in_=kvt[b][:, 1024 + j * 64:1024 + (j + 1) * 64], identity=idb[:]), r=["kvt%d" % b, "idb"], w=["ptk1"])
                V(lambda e: e.tensor_copy(out=KselE[0:64, :, cs], in_=ptk[1][:, 0:4, :]), r=["ptk1"], w=["KselE"])
                V(lambda e: e.tensor_copy(out=KwinE[0:64, :, cs], in_=ptk[1][:, 4:8, :]), r=["ptk1", "KselE"], w=["KwinE"])
                G(lambda e: e.tensor_copy(out=Vsel[:, t, :, 0:64], in_=kvt[b][:, 768:1024].rearrange("p (g d) -> p g d", d=64)), r=["kvt%d" % b], w=["Vsel"])
                G(lambda e: e.tensor_copy(out=Vwin[:, t, :, 0:64], in_=kvt[b][:, 1280:1536].rearrange("p (g d) -> p g d", d=64)), r=["kvt%d" % b], w=["Vwin"])
            for kvi in range(2):
                for g in range(4):
                    for l in range(32):
                        P(lambda e: e.matmul(ph[:, 0:NCB], lhsT=W1[:, kvi, l, :], rhs=KcT[:, kvi, g, l:l + 16 * (NCB - 1) + 1:16],
                                             start=(l == 0), stop=(l == 31)), r=["naW1", "KcT"], w=["ph"])
                    V(lambda e: e.tensor_scalar(out=pre[:, 0:NCB], in0=ph[:, 0:NCB], scalar1=cv[:, kvi:kvi + 1], scalar2=None, op0=ALU.add),
                      r=["ph", "cv"], w=["pre"])
                    gelu_tanh(K, pre[:, 0:NCB], "pre", tmpg[:, 0:NCB], "tmpg", hidT[:, 255:0:-1], "hidT")
                    if kvi == 0:
                        P(lambda e: e.matmul(pk2[0:64, 0:256], lhsT=W2[:, 0, :], rhs=hidT[:], start=True, stop=True), r=["naW2", "hidT"], w=["pk2"])
                        V(lambda e: e.tensor_copy(out=kcT[:, g, :], in_=pk2[0:64, 0:256]), r=["pk2"], w=["kcT"])
                    else:
                        for nt in range(2):
                            P(lambda e: e.matmul(pk2[:, 64 * nt:64 * nt + 64], lhsT=hidT[:, nt * 128:(nt + 1) * 128], rhs=W2[:, 1, :], start=True, stop=True),
                              r=["naW2", "hidT"], w=["pk2"])
                        V(lambda e: e.tensor_copy(out=vca[:, :, g, 0:64], in_=pk2[:, 0:128].rearrange("p (n f) -> p n f", f=64)), r=["pk2"], w=["vca"])
        K.S.barrier()

        with ExitStack() as qes:
            wo = K.sb(qes, "na_wo", [128, 8, D], BF16)
            parT = K.sb(qes, "na_par", [128, 2], F32)
            biasT = K.sb(qes, "na_biasT", [128, 3, 16, 128], BF16)
            TW = K.sb(qes, "na_TW", [128, 2, 128], F32)
            dma(parT[:], I["par"], w=["parT"])
            p0, p1 = parT[:, 0:1], parT[:, 1:2]
            load_w_bf16(K, wo, I["nsa_w_o"], D)
            with ExitStack() as hes:
                hk = K.sb(hes, "na_hk", [128, 16, 256], F32)
                biasD = K.sb(hes, "na_biasD", [128, 16, 256], F32)
                negt = K.sb(hes, "na_negt", [128, 4, 128], F32)
                Jm = K.sb(hes, "na_J", [128, 128], F32)
                pJ = K.ps(hes, "na_pJ", [128, 512], F32)
                dma(hk[:], AP(tensor=O["fpad_scr"].tensor, offset=0, ap=[[1, 128], [384, 16], [1, 256]]), r=["fpad_scr"], w=["hk"])
                G(lambda e: e.memset(Jm[:], 1.0), w=["Jm"])
                G(lambda e: e.affine_select(out=Jm[:], in_=Jm[:], pattern=[[1, 128]], compare_op=ALU.is_equal, fill=0.0, base=-127, channel_multiplier=1),
                  r=["Jm"], w=["Jm"])
                hkf = hk[:].rearrange("p h x -> p (h x)")
                bDf = biasD[:].rearrange("p h x -> p (h x)")
                for c in range(8):
                    P(lambda e: e.matmul(pJ[:], lhsT=Jm[:], rhs=hkf[:, c * 512:(c + 1) * 512], start=True, stop=True), r=["Jm", "hk"], w=["pJ"])
                    V(lambda e: e.tensor_copy(out=bDf[:, c * 512:(c + 1) * 512], in_=pJ[:]), r=["pJ"], w=["biasD"])
                bD0 = biasD[:, :, 0:128]
                bD1 = biasD[:, :, 128:256]
                V(lambda e: e.tensor_scalar(out=biasT[:, 0], in0=bD0, scalar1=p1, scalar2=None, op0=ALU.mult), r=["biasD", "parT"], w=["biasT"])
                G(lambda e: e.memset(negt[:], NEG), w=["negt"])
                for hq in range(4):
                    V(lambda e: e.scalar_tensor_tensor(out=biasT[:, 0, 4 * hq:4 * hq + 4, :], in0=negt[:], scalar=p0, in1=biasT[:, 0, 4 * hq:4 * hq + 4, :],
                                                       op0=ALU.mult, op1=ALU.add), r=["negt", "parT", "biasT"], w=["biasT"])
                V(lambda e: e.tensor_scalar(out=biasT[:, 1], in0=bD1, scalar1=p1, scalar2=None, op0=ALU.mult), r=["biasD", "parT", "biasT"], w=["biasT"])
                V(lambda e: e.scalar_tensor_tensor(out=biasT[:, 1], in0=bD0, scalar=p0, in1=biasT[:, 1], op0=ALU.mult, op1=ALU.add), r=["biasD", "parT", "biasT"], w=["biasT"])
                V(lambda e: e.tensor_scalar(out=biasT[:, 2], in0=bD1, scalar1=p0, scalar2=None, op0=ALU.mult), r=["biasD", "parT", "biasT"], w=["biasT"])
                V(lambda e: e.tensor_scalar(out=TW[:, 0, :], in0=W4[:], scalar1=p0, scalar2=None, op0=ALU.mult), r=["W4", "parT"], w=["TW"])
                V(lambda e: e.scalar_tensor_tensor(out=TW[:, 0, :], in0=negt[:, 0, :], scalar=p1, in1=TW[:, 0, :], op0=ALU.mult, op1=ALU.add), r=["negt", "parT", "TW"], w=["TW"])
                V(lambda e: e.tensor_scalar(out=TW[:, 1, :], in0=W4[:], scalar1=p1, scalar2=None, op0=ALU.mult), r=["W4", "parT", "TW"], w=["TW"])


            K.S.barrier()
            NJ = NTILE // 2
            qt_sb = [K.sb(qes, "na_q%d" % i, [128, D], BF16) for i in range(2)]
            gate = [K.sb(qes, "na_gate%d" % i, [128, 48], F32) for i in range(2)]
            yres = [K.sb(qes, "na_y%d" % i, [128, D], F32) for i in range(2)]
            bW1 = K.sb(qes, "na_bW", [128, 16, 256], F32)
            bW = [bW1, bW1]
            bC1 = [K.sb(qes, "na_bC_%d" % n, [128, 16, 128], BF16) for n in range(2)]
            bC = [bC1, bC1]
            ka = [K.sb(qes, "na_ka%d" % i, [128, 2, 64], F32) for i in range(2)]
            qidx = K.sb(qes, "na_qidx", [128, NJ], I32)
            QSel = K.sb(qes, "na_QSel", [128, 16, 128], BF16)
            QWin = K.sb(qes, "na_QWin", [128, 16, 128], BF16)
            NBUF = 3
            sc = [K.sb(qes, "na_sc%d" % i, [128, 4, 128], F32) for i in range(NBUF)]
            PT = [K.sb(qes, "na_PT%d" % i, [128, 4, 128], BF16) for i in range(NBUF)]
            rc = [K.sb(qes, "na_rc%d" % i, [128, 3, 4], F32) for i in range(2)]
            ocs = [K.sb(qes, "na_ocs%d" % i, [128, 4, 64], F32) for i in range(2)]
            imp = K.sb(qes, "na_imp", [128, 64], F32)
            wk1 = K.sb(qes, "na_wk1", [128, 64], F32)
            m8 = K.sb(qes, "na_m8", [128, 16], F32)
            selm = K.sb(qes, "na_selm", [128, 64], F32)
            tmpm = K.sb(qes, "na_tmpm", [128, 64], F32)
            mbw = K.sb(qes, "na_mbw", [128, 4, 128], BF16)
            otile = [K.sb(qes, "na_o%d" % i, [128, D], BF16) for i in range(2)]
            oacc = K.sb(qes, "na_oacc", [128, 64], F32)
            oT = K.sb(qes, "na_oT", [128, 8, 128], BF16)
            psb = [K.ps(qes, "na_ps%d" % i, [128, 4, 128], F32) for i in range(NBUF)]
            posel = K.ps(qes, "na_posel", [128, 512], F32)
            powin = K.ps(qes, "na_powin", [128, 512], F32)
            poc = K.ps(qes, "na_poc", [128, 512], F32)
            pimp = K.ps(qes, "na_pimp", [128, 4, 128], F32)
            pmisc = K.ps(qes, "na_pmisc", [128, 8, 128], BF16)
            posel_v = posel[:, 0:260].rearrange("p (h c) -> p h c", c=65)
            powin_v = powin[:, 0:260].rearrange("p (h c) -> p h c", c=65)
            poc_v = poc[:, 0:260].rearrange("p (h c) -> p h c", c=65)

            dma(qidx[:], I["c_qidx"], w=["qidx"])
            G(lambda e: e.memset(mbw[:], 0.0), w=["mbw"])
            V(lambda e: e.tensor_copy(out=QWin[64:128, :, :], in_=bc(chb[64:128, :].unsqueeze(2), [64, 16, 128])), r=["chb"],
              w=["QWin0", "QWin1", "QWin2", "QWin3"])
            sidx = [0]

            def emit_qk(blk):
                lhsT_full, g, kt, qrhs, qkey = blk["lhsT"], blk["g"], blk["kt"], blk["qrhs"], blk["qkey"]
                i = sidx[0] % NBUF
                sidx[0] += 1
                blk["i"] = i
                ks = slice(kt * 128, (kt + 1) * 128)
                P(lambda e: e.matmul(psb[i][:], lhsT=lhsT_full[:, g, ks], rhs=qrhs[:, 4 * g:4 * g + 4, :], start=True, stop=True),
                  r=["KselE", "KwinE", qkey], w=["psb%d" % i])

            def emit_exp_pv(blk):
                i, g, kt = blk["i"], blk["g"], blk["kt"]
                bias_ap = blk["bias"]
                if bias_ap is not None:
                    V(lambda e: e.tensor_tensor(out=sc[i][:], in0=psb[i][:], in1=bias_ap, op=ALU.add), r=["psb%d" % i, "biasT", "TW"], w=["sc%d" % i])
                    A(lambda e: e.activation(out=PT[i][:], in_=sc[i][:], func=AF.Exp), r=["sc%d" % i], w=["PT%d" % i])
                else:
                    A(lambda e: e.activation(out=PT[i][:], in_=psb[i][:], func=AF.Exp), r=[], w=["PT%d" % i, "psb%d" % i])
                for h in range(4):
                    P(lambda e: e.matmul(blk["po_v"][:, h, :], lhsT=PT[i][:, h, :], rhs=blk["vaug"][:, kt, g, :], start=(blk["first"] and h == 0),
                                         stop=blk["last"], skip_group_check=True), r=["PT%d" % i, "Vsel", "Vwin"], w=[blk["pokey"]])

            def nts_of(j):
                return [1] if j <= 7 else [0, 1]

            def tile_loads(j):
                b = j % 2
                sx = str(b)
                K.S.idma(qt_sb[b][:], O["q_scr"], qidx[:, j:j + 1], ["qidx", "q_scr"], ["qsb" + sx])
                K.S.idma(gate[b][:], O["gate_scr"], qidx[:, j:j + 1], ["qidx", "gate_scr"], ["gate" + sx])
                K.S.idma(yres[b][:], O["y2_scr"], qidx[:, j:j + 1], ["qidx", "y2_scr"], ["yres" + sx])
                dma(ka[b][:], I["c_ka"][j], w=["ka" + sx])
                for nt in nts_of(j):
                    dma(bW[b][:], AP(tensor=O["fc_scr"].tensor, offset=256 * j + 2048 * nt, ap=[[16, 128], [8192, 16], [1, 256]]),
                        r=["fc_scr"], w=["bW"])
                    G(lambda e: e.tensor_scalar(out=bC[b][nt][:], in0=bW[b][:, :, 0:128], scalar1=p0, scalar2=None, op0=ALU.mult),
                      r=["bW", "parT"], w=["bC_%d" % nt])
                    V(lambda e: e.scalar_tensor_tensor(out=bC[b][nt][:], in0=bW[b][:, :, 128:256], scalar=p1, in1=bC[b][nt][:], op0=ALU.mult, op1=ALU.add),
                      r=["bW", "parT", "bC_%d" % nt], w=["bC_%d" % nt])

            def phase_a(j, g):
                b = j % 2
                sx = str(b)
                gp = g % 2
                nts = nts_of(j)
                qk, wk_ = "QSel%d" % g, "QWin%d" % g
                for h in range(4):
                    hh = 4 * g + h
                    P(lambda e: e.transpose(out=pmisc[0:64, h, :], in_=qt_sb[b][:, hh * 64:(hh + 1) * 64], identity=idb[:]), r=["qsb" + sx, "idb"], w=["pmisc"])
                A(lambda e: e.copy(out=QSel[0:64, 4 * g:4 * g + 4, :], in_=pmisc[0:64, 0:4, :]), r=["pmisc"], w=[qk])
                V(lambda e: e.tensor_copy(out=QWin[0:64, 4 * g:4 * g + 4, :], in_=QSel[0:64, 4 * g:4 * g + 4, :]), r=[qk], w=[wk_])
                for ni, nt in enumerate(nts):
                    i = sidx[0] % NBUF
                    sidx[0] += 1
                    P(lambda e: e.matmul(psb[i][:], lhsT=kcT[:, g, nt * 128:(nt + 1) * 128], rhs=QSel[0:64, 4 * g:4 * g + 4, :], start=True, stop=True),
                      r=["kcT", qk], w=["psb%d" % i])
                    V(lambda e: e.tensor_tensor(out=sc[i][:], in0=psb[i][:], in1=bC[b][nt][:, 4 * g:4 * g + 4, :], op=ALU.add),
                      r=["psb%d" % i, "bC_%d" % nt], w=["sc%d" % i])
                    A(lambda e: e.activation(out=PT[i][:], in_=sc[i][:], func=AF.Exp), r=["sc%d" % i], w=["PT%d" % i])
                    for h in range(4):
                        P(lambda e: e.matmul(poc_v[:, h, :], lhsT=PT[i][:, h, :], rhs=vca[:, nt, g, 0:65], start=(ni == 0 and h == 0),
                                             stop=(ni == len(nts) - 1), skip_group_check=True), r=["PT%d" % i, "vca"], w=["poc"])
                        P(lambda e: e.matmul(pimp[:, h, 0:64], lhsT=PT[i][:, h, :], rhs=vca[:, nt, g, 65:129], start=(ni == 0 and h == 0),
                                             stop=(ni == len(nts) - 1), skip_group_check=True), r=["PT%d" % i, "vca"], w=["pimp"])
                rk = "rc%d" % gp
                V(lambda e: e.tensor_scalar(out=rc[gp][:, 0, :], in0=poc_v[:, :, 64], scalar1=1e-30, scalar2=None, op0=ALU.add), r=["poc"], w=[rk])
                V(lambda e: e.reciprocal(out=rc[gp][:, 0, :], in_=rc[gp][:, 0, :]), r=[rk], w=[rk])
                V(lambda e: e.tensor_scalar(out=imp[:], in0=pimp[:, 0, 0:64], scalar1=rc[gp][:, 0, 0:1], scalar2=None, op0=ALU.mult), r=["pimp", rk], w=["imp"])
                for h in range(1, 4):
                    V(lambda e: e.scalar_tensor_tensor(out=imp[:], in0=pimp[:, h, 0:64], scalar=rc[gp][:, 0, h:h + 1], in1=imp[:], op0=ALU.mult, op1=ALU.add),
                      r=["pimp", rk, "imp"], w=["imp"])
                V(lambda e: e.tensor_tensor(out=ocs[gp][:], in0=poc_v[:, :, 0:64], in1=bc(rc[gp][:, 0, :].unsqueeze(2), [128, 4, 64]), op=ALU.mult),
                  r=["poc", rk], w=["ocs%d" % gp])
                V(lambda e: e.tensor_tensor(out=imp[:], in0=imp[:], in1=ka[b][:, 0, :], op=ALU.mult), r=["imp", "ka" + sx], w=["imp"])
                V(lambda e: e.tensor_tensor(out=imp[:], in0=imp[:], in1=ka[b][:, 1, :], op=ALU.add), r=["imp", "ka" + sx], w=["imp"])
                V(lambda e: e.max(out=m8[:, 0:8], in_=imp[:]), r=["imp"], w=["m8"])
                V(lambda e: e.match_replace(out=wk1[:], in_to_replace=m8[:, 0:8], in_values=imp[:], imm_value=-3e38), r=["imp", "m8"], w=["wk1"])
                V(lambda e: e.max(out=m8[:, 8:16], in_=wk1[:]), r=["wk1"], w=["m8"])
                V(lambda e: e.tensor_scalar(out=selm[:], in0=imp[:], scalar1=m8[:, 15:16], scalar2=None, op0=ALU.is_ge), r=["imp", "m8"], w=["selm"])
                V(lambda e: e.tensor_scalar(out=tmpm[:], in0=selm[:], scalar1=-NEG, scalar2=NEG, op0=ALU.mult, op1=ALU.add), r=["selm"], w=["tmpm"])
                for h in range(4):
                    V(lambda e: e.scalar_tensor_tensor(out=mbw[:, h, 64:128], in0=selm[:], scalar=chb[:, 4 * g + h:4 * g + h + 1], in1=tmpm[:],
                                                       op0=ALU.mult, op1=ALU.add), r=["selm", "tmpm", "chb"], w=["mbw"])
                for h in range(4):
                    P(lambda e: e.transpose(out=pmisc[:, h, :], in_=mbw[:, h, :], identity=idb[:]), r=["mbw", "idb"], w=["pmisc"])
                A(lambda e: e.copy(out=QSel[64:128, 4 * g:4 * g + 4, :], in_=pmisc[64:128, 0:4, :]), r=["pmisc"], w=[qk])

            def phase_b(j, g):
                b = j % 2
                sx = str(b)
                gp = g % 2
                rk = "rc%d" % gp
                ktop = 2 * j + 1
                blocks = []
                for kt in range(ktop + 1):
                    kr = ktop - kt
                    bias_ap = biasT[:, kr, 4 * g:4 * g + 4, :] if kr <= 2 else None
                    blocks.append(dict(lhsT=KselE, g=g, kt=kt, qrhs=QSel, qkey="QSel%d" % g, bias=bias_ap, vaug=Vsel, po_v=posel_v, pokey="posel",
                                       first=(kt == 0), last=(kt == ktop)))
                k0 = max(0, ktop - 5)
                for kt in range(k0, ktop + 1):
                    kr = ktop - kt
                    if kr <= 2:
                        bias_ap = biasT[:, kr, 4 * g:4 * g + 4, :]
                    elif kr == 5:
                        bias_ap = bc(TW[:, 0, :].unsqueeze(1), [128, 4, 128])
                    elif kr == 4:
                        bias_ap = bc(TW[:, 1, :].unsqueeze(1), [128, 4, 128])
                    else:
                        bias_ap = None
                    blocks.append(dict(lhsT=KwinE, g=g, kt=kt, qrhs=QWin, qkey="QWin%d" % g, bias=bias_ap, vaug=Vwin, po_v=powin_v, pokey="powin",
                                       first=(kt == k0), last=(kt == ktop)))
                n = len(blocks)
                LA = NBUF - 1
                for i in range(n + LA):
                    if i < n:
                        emit_qk(blocks[i])
                    if i >= LA:
                        emit_exp_pv(blocks[i - LA])
                V(lambda e: e.tensor_scalar(out=rc[gp][:, 1, :], in0=posel_v[:, :, 64], scalar1=1e-30, scalar2=None, op0=ALU.add), r=["posel"], w=[rk])
                V(lambda e: e.tensor_scalar(out=rc[gp][:, 2, :], in0=powin_v[:, :, 64], scalar1=1e-30, scalar2=None, op0=ALU.add), r=["powin"], w=[rk])
                V(lambda e: e.reciprocal(out=rc[gp][:, 1:3, :], in_=rc[gp][:, 1:3, :]), r=[rk], w=[rk])
                V(lambda e: e.memset(rc[gp][:, 0, :], 1.0), r=[rk, "ocs%d" % gp], w=[rk])
                V(lambda e: e.tensor_tensor(out=rc[gp][:], in0=rc[gp][:], in1=gate[b][:].rearrange("p (r h) -> p r h", h=16)[:, :, 4 * g:4 * g + 4], op=ALU.mult),
                  r=[rk, "gate" + sx], w=[rk])
                for h in range(4):
                    hh = 4 * g + h
                    V(lambda e: e.tensor_scalar(out=oacc[:], in0=ocs[gp][:, h, :], scalar1=rc[gp][:, 0, h:h + 1], scalar2=None, op0=ALU.mult),
                      r=["ocs%d" % gp, rk], w=["oacc"])
                    V(lambda e: e.scalar_tensor_tensor(out=oacc[:], in0=posel_v[:, h, 0:64], scalar=rc[gp][:, 1, h:h + 1], in1=oacc[:], op0=ALU.mult, op1=ALU.add),
                      r=["posel", rk, "oacc"], w=["oacc"])
                    V(lambda e: e.scalar_tensor_tensor(out=otile[b][:, hh * 64:(hh + 1) * 64], in0=powin_v[:, h, 0:64], scalar=rc[gp][:, 2, h:h + 1], in1=oacc[:],
                                                       op0=ALU.mult, op1=ALU.add), r=["powin", rk, "oacc"], w=["otile" + sx])

            def tile_end(j):
                b = j % 2
                sx = str(b)
                rs = slice(j * 128, (j + 1) * 128)
                for k in range(8):
                    P(lambda e: e.transpose(out=pmisc[:, k, :], in_=otile[b][:, k * 128:(k + 1) * 128], identity=idb[:]), r=["otile" + sx, "idb"], w=["pmisc"])
                A(lambda e: e.copy(out=oT[:], in_=pmisc[:]), r=["pmisc"], w=["oT"])
                for c in range(2):
                    pso = psb[c][:].rearrange("p h n -> p (h n)")
                    for k in range(8):
                        P(lambda e: e.matmul(pso, lhsT=oT[:, k, :], rhs=wo[:, k, c * 512:(c + 1) * 512], start=(k == 0), stop=(k == 7)),
                          r=["oT", "w_na_wo"], w=["psb%d" % c])
                    V(lambda e: e.tensor_tensor(out=yres[b][:, c * 512:(c + 1) * 512], in0=pso, in1=yres[b][:, c * 512:(c + 1) * 512], op=ALU.add),
                      r=["psb%d" % c, "yres" + sx], w=["yres" + sx])
                dma(O["y3_scr"][rs, :], yres[b][:], r=["yres" + sx], w=["y3_scr"], add_writer=True)

            items = [(j, g) for j in range(NJ) for g in range(4)]
            tile_loads(0)
            phase_a(*items[0])
            for ii, (j, g) in enumerate(items):
                if ii + 1 < len(items):
                    nj, ng = items[ii + 1]
                    if ng == 0:
                        tile_loads(nj)
                    phase_a(nj, ng)
                phase_b(j, g)
                if g == 3:
                    tile_end(j)
    K.S.barrier()


NKT_S = 17
NWT_S = 5
OFFC = 4111


def nsa_attn_sample(K, C, I, O):
    idf, idb = C["idf"], C["idb"]
    V, A, G, P, dma = K.V, K.A, K.G, K.P, K.dma
    N = NS
    with ExitStack() as es:
        W1 = K.sb(es, "sa_W1", [64, 2, 32, 128], BF16)
        W2 = K.sb(es, "sa_W2", [128, 2, 64], BF16)
        cv = K.sb(es, "sa_cv", [128, 2], F32)
        biasC = K.sb(es, "sa_bC", [128, 16], F32)
        biasS = K.sb(es, "sa_bS", [128, NKT_S, 16], F32)
        biasW = K.sb(es, "sa_bW", [128, NWT_S, 16], F32)
        QT = K.sb(es, "sa_QT", [128, 16, N], BF16)
        idx = K.sb(es, "sa_idx", [128, N * 16], I32)
        covs = K.sb(es, "sa_cov", [128, 64], BF16)
        Erow = K.sb(es, "sa_E", [128, NKT_S * 128], BF16)
        ones4 = K.sb(es, "sa_ones4", [1, 4], F32)
        for kvi in range(2):
            K.S.dma("pool", W1[:, kvi, :, :], I["nsa_phi_w1"][kvi].rearrange("l d e -> d l e"), (), ["saW1"], add_writer=True)
            K.S.dma("pool", W2[:, kvi, :], I["nsa_phi_w2"][kvi], (), ["saW2"], add_writer=True)
        K.S.dma("pool", covs[:], I["c_cover_s"], (), ["covs"])
        K.S.dma("pool", Erow[64:128, :], I["c_E"][:, 0:NKT_S * 128], (), ["Erow"])
        V(lambda e: e.memset(ones4[:], 1.0), w=["ones4"])
        with ExitStack() as bes:
            pe_f = K.sb(bes, "sa_pef", [32, 2, 64], F32)
            pe_b = K.sb(bes, "sa_peb", [32, 2, 64], BF16)
            peT = K.sb(bes, "sa_peT", [64, 2, 32], BF16)
            hk = K.sb(bes, "sa_hk", [128, NKT_S + NWT_S, 16], F32)
            Jm = K.sb(bes, "sa_J", [128, 128], F32)
            pti = K.sb(bes, "sa_pti", [128, N * 16], I32)
            ptf = K.sb(bes, "sa_ptf", [128, N * 16], F32)
            iot = K.sb(bes, "sa_iot", [128, 1], F32)
            qs = K.sb(bes, "sa_qs", [N, D], BF16)
            pA = K.ps(bes, "sa_pA", [128, 1024], BF16)
            pB = K.ps(bes, "sa_pB", [128, 512], F32)
            dma(pe_f[:], I["nsa_phi_pe"].rearrange("k l d -> l k d"), w=["pe_f"])
            V(lambda e: e.tensor_copy(out=pe_b[:], in_=pe_f[:]), r=["pe_f"], w=["pe_b"])
            for kvi in range(2):
                P(lambda e: e.transpose(out=pA[0:64, kvi * 32:kvi * 32 + 32], in_=pe_b[:, kvi, :], identity=idb[0:32, 0:32]), r=["pe_b", "idb"], w=["pA"])
            V(lambda e: e.tensor_copy(out=peT[:].rearrange("p k l -> p (k l)"), in_=pA[0:64, 0:64]), r=["pA"], w=["peT"])
            for kvi in range(2):
                for l in range(32):
                    P(lambda e: e.matmul(pB[:, kvi:kvi + 1], lhsT=W1[:, kvi, l, :], rhs=peT[:, kvi, l:l + 1], start=(l == 0), stop=(l == 31)),
                      r=["saW1", "peT"], w=["pB"])
            V(lambda e: e.tensor_copy(out=cv[:], in_=pB[:, 0:2]), r=["pB"], w=["cv"])
            bC2 = K.sb(bes, "sa_bC2", [128, 16, 2], F32)
            dma(bC2[:], AP(tensor=O["fc_scr"].tensor, offset=OFFC + 2017 - 16 * 127, ap=[[16, 128], [8192, 16], [1, 2]]), r=["fc_scr"], w=["bC2"])
            V(lambda e: e.tensor_copy(out=biasC[:], in_=bC2[:, :, 0]), r=["bC2"], w=["biasC"])
            hk2 = K.sb(bes, "sa_hk2", [128, NKT_S + NWT_S, 16, 2], F32)
            for kt in range(NKT_S):
                dma(hk2[:, kt, :, :], AP(tensor=O["fc_scr"].tensor, offset=OFFC + 2048 - 128 * kt - 127, ap=[[1, 128], [8192, 16], [1, 2]]),
                    r=["fc_scr"], w=["hk2"], add_writer=True)
            for wt in range(NWT_S):
                dma(hk2[:, NKT_S + wt, :, :], AP(tensor=O["fc_scr"].tensor, offset=OFFC + 512 - 128 * wt - 127, ap=[[1, 128], [8192, 16], [1, 2]]),
                    r=["fc_scr"], w=["hk2"], add_writer=True)
            V(lambda e: e.tensor_copy(out=hk[:], in_=hk2[:, :, :, 0]), r=["hk2"], w=["hk"])
            G(lambda e: e.memset(Jm[:], 1.0), w=["Jm"])
            G(lambda e: e.affine_select(out=Jm[:], in_=Jm[:], pattern=[[1, 128]], compare_op=ALU.is_equal, fill=0.0, base=-127, channel_multiplier=1),
              r=["Jm"], w=["Jm"])
            ncol = (NKT_S + NWT_S) * 16
            P(lambda e: e.matmul(pB[:, 0:ncol], lhsT=Jm[:], rhs=hk[:].rearrange("p t h -> p (t h)"), start=True, stop=True), r=["Jm", "hk", "cv"], w=["pB"])
            V(lambda e: e.tensor_copy(out=biasS[:].rearrange("p t h -> p (t h)"), in_=pB[:, 0:NKT_S * 16]), r=["pB"], w=["biasS"])
            V(lambda e: e.tensor_copy(out=biasW[:].rearrange("p t h -> p (t h)"), in_=pB[:, NKT_S * 16:ncol]), r=["pB"], w=["biasW"])
            dma(pti[:], AP(tensor=I["ptab"].tensor, offset=0, ap=[[0, 128], [1, N * 16]]), w=["pti"])
            G(lambda e: e.iota(out=iot[:], pattern=[[0, 1]], base=0, channel_multiplier=1, allow_small_or_imprecise_dtypes=True), w=["iot"])
            V(lambda e: e.tensor_copy(out=ptf[:], in_=pti[:]), r=["pti"], w=["ptf"])
            V(lambda e: e.tensor_scalar(out=ptf[:], in0=ptf[:], scalar1=128.0, scalar2=iot[:, 0:1], op0=ALU.mult, op1=ALU.add), r=["ptf", "iot"], w=["ptf"])
            V(lambda e: e.tensor_copy(out=idx[:], in_=ptf[:]), r=["ptf"], w=["idx"])
            dma(qs[:], O["qs_scr"], r=["q_scr"], w=["sqs"])
            for hh in range(16):
                P(lambda e: e.transpose(out=pA[0:64, 64 + hh * N:64 + (hh + 1) * N], in_=qs[:, hh * 64:(hh + 1) * 64], identity=idb[0:N, 0:N]),
                  r=["sqs", "idb", "peT"], w=["pA"])
            V(lambda e: e.tensor_copy(out=QT[0:64, :, :].rearrange("p h n -> p (h n)"), in_=pA[0:64, 64:64 + 16 * N]), r=["pA"], w=["QT"])
        K.S.barrier()

        with ExitStack() as tes:
            pg = [K.sb(tes, "sa_pg%d" % i, [128, D], F32) for i in range(2)]
            wn = K.sb(tes, "sa_wn", [128, 4, 512], F32)
            newf = K.sb(tes, "sa_newf", [N, 1536], BF16)
            KcT = K.sb(tes, "sa_KcT", [64, 2, 4, 2048], BF16)
            KsE = K.sb(tes, "sa_KsE", [128, 4, NKT_S * 128], BF16)
            KwE = K.sb(tes, "sa_KwE", [64, 4, NWT_S * 128], BF16)
            Vs = K.sb(tes, "sa_Vs", [128, NKT_S, 4, 65], BF16)
            Vw = K.sb(tes, "sa_Vw", [128, NWT_S, 4, 65], BF16)
            kcT = K.sb(tes, "sa_kcT", [64, 4, 128], BF16)
            vca = K.sb(tes, "sa_vca", [128, 4, 129], BF16)
            pre = [K.sb(tes, "sa_pre%d" % i, [128, 128], F32) for i in range(2)]
            tmpg = [K.sb(tes, "sa_tmpg%d" % i, [128, 128], F32) for i in range(2)]
            hidT = [K.sb(tes, "sa_hidT%d" % i, [128, 128], BF16) for i in range(2)]
            sc = K.sb(tes, "sa_sc", [128, NKT_S, 4], F32)
            PTs = K.sb(tes, "sa_PT", [128, NKT_S, 4], BF16)
            oc = K.sb(tes, "sa_oc", [4, 129], F32)
            osw = K.sb(tes, "sa_osw", [4, 2, 65], F32)
            rcc = K.sb(tes, "sa_rcc", [4, 1], F32)
            impr = K.sb(tes, "sa_impr", [1, 64], F32)
            wk1 = K.sb(tes, "sa_wk1", [1, 64], F32)
            m8 = K.sb(tes, "sa_m8", [1, 16], F32)
            mbr = K.sb(tes, "sa_mbr", [1, 128], F32)
            ptA = K.ps(tes, "sa_ptA", [64, 4, 128], F32)
            ptB = K.ps(tes, "sa_ptB", [64, 4, 128], F32)
            ph = [K.ps(tes, "sa_ph%d" % i, [128, 512], F32) for i in range(2)]
            pk2 = K.ps(tes, "sa_pk2", [128, 512], F32)
            psS = K.ps(tes, "sa_psS", [128, 512], F32)
            po = K.ps(tes, "sa_po", [128, 512], F32)
            ptN = K.ps(tes, "sa_ptN", [128, 1024], BF16)

            dma(newf[:], O["kvbs_scr"], r=["kvb_scr"], w=["newf"])
            G(lambda e: e.memset(Vs[:, :, :, 64:65], 1.0), w=["sVs"])
            G(lambda e: e.memset(Vw[:, :, :, 64:65], 1.0), w=["sVw"])
            G(lambda e: e.memset(vca[:, :, 64:65], 1.0), w=["svca"])
            for g in range(4):
                V(lambda e: e.tensor_copy(out=vca[:, g, 65:129], in_=covs[:]), r=["covs", "svca"], w=["svca"])
                V(lambda e: e.tensor_copy(out=KsE[64:128, g, :], in_=Erow[64:128, :]), r=["Erow"], w=["sKsE"])
            G(lambda e: e.memset(KsE[0:64, :, 16 * 128:17 * 128], 0.0), r=["sKsE"], w=["sKsE"])
            G(lambda e: e.memset(KwE[:, :, 4 * 128:5 * 128], 0.0), w=["sKwE"])
            G(lambda e: e.memset(Vs[:, 16, :, 0:64], 0.0), r=["sVs"], w=["sVs"])
            G(lambda e: e.memset(Vw[:, 4, :, 0:64], 0.0), r=["sVw"], w=["sVw"])
            for i_ in range(2):
                G(lambda e: e.memset(hidT[i_][:], 0.0), w=["shidT%d" % i_])
            G(lambda e: e.memset(mbr[:], 0.0), w=["mbr"])
            for j in range(4):
                P(lambda e: e.transpose(out=ptN[0:64, j * N:(j + 1) * N], in_=newf[:, 512 + j * 64:512 + (j + 1) * 64], identity=idb[0:N, 0:N]),
                  r=["newf", "idb"], w=["ptN"])
                P(lambda e: e.transpose(out=ptN[0:64, (4 + j) * N:(5 + j) * N], in_=newf[:, 1024 + j * 64:1024 + (j + 1) * 64], identity=idb[0:N, 0:N]),
                  r=["newf", "idb"], w=["ptN"])
            newT = K.sb(tes, "sa_newT", [64, 8, N], BF16)
            V(lambda e: e.tensor_copy(out=newT[:].rearrange("p j n -> p (j n)"), in_=ptN[0:64, 0:8 * N]), r=["ptN"], w=["newT"])
            newrow = K.sb(tes, "sa_newrow", [1, 1536], BF16)

            for tk in range(N):
                dma(wn[:], I["cwin"][tk].rearrange("(w r) c -> r w c", r=128), w=["wn"])
                for wt in range(4):
                    for g in range(4):
                        P(lambda e: e.transpose(out=ptA[:, g, :], in_=wn[:, wt, g * 64:(g + 1) * 64], identity=idf[:]), r=["wn", "idf"], w=["sptA"])
                    A(lambda e: e.copy(out=KwE[:, :, wt * 128:(wt + 1) * 128], in_=ptA[:]), r=["sptA"], w=["sKwE"])
                    V(lambda e: e.tensor_copy(out=Vw[:, wt, :, 0:64], in_=wn[:, wt, 256:512].rearrange("p (g d) -> p g d", d=64)), r=["wn", "sVw"], w=["sVw"])
                V(lambda e: e.tensor_copy(out=KsE[0:64, :, 16 * 128:16 * 128 + 1], in_=newT[:, 0:4, tk:tk + 1]), r=["newT", "sKsE"], w=["sKsE"])
                V(lambda e: e.tensor_copy(out=KwE[:, :, 4 * 128:4 * 128 + 1], in_=newT[:, 4:8, tk:tk + 1]), r=["newT", "sKwE"], w=["sKwE"])
                dma(newrow[:], O["kvbs_scr"][tk:tk + 1, :], r=["kvb_scr"], w=["newrow"])
                V(lambda e: e.tensor_copy(out=Vs[0:1, 16, :, 0:64], in_=newrow[:, 768:1024].rearrange("p (g d) -> p g d", d=64)), r=["newrow", "sVs"], w=["sVs"])
                V(lambda e: e.tensor_copy(out=Vw[0:1, 4, :, 0:64], in_=newrow[:, 1280:1536].rearrange("p (g d) -> p g d", d=64)), r=["newrow", "sVw"], w=["sVw"])
                for pgi in range(16):
                    b = pgi % 2
                    K.S.idma(pg[b][:], I["ckv"], idx[:, tk * 16 + pgi:tk * 16 + pgi + 1], ["idx"], ["spg%d" % b])
                    cs = slice(pgi * 128, (pgi + 1) * 128)
                    for g in range(4):
                        P(lambda e: e.transpose(out=ptA[:, g, :], in_=pg[b][:, g * 64:(g + 1) * 64], identity=idf[:]), r=["spg%d" % b, "idf"], w=["sptA"])
                    A(lambda e: e.copy(out=KcT[:, 0, :, cs], in_=ptA[:]), r=["sptA"], w=["sKcT"])
                    for g in range(4):
                        P(lambda e: e.transpose(out=ptB[:, g, :], in_=pg[b][:, 256 + g * 64:256 + (g + 1) * 64], identity=idf[:]), r=["spg%d" % b, "idf"], w=["sptB"])
                    V(lambda e: e.tensor_copy(out=KcT[:, 1, :, cs], in_=ptB[:]), r=["sptB", "sKcT"], w=["sKcT"])
                    for g in range(4):
                        P(lambda e: e.transpose(out=ptA[:, g, :], in_=pg[b][:, 512 + g * 64:512 + (g + 1) * 64], identity=idf[:]), r=["spg%d" % b, "idf"], w=["sptA"])
                    A(lambda e: e.copy(out=KsE[0:64, :, cs], in_=ptA[:]), r=["sptA"], w=["sKsE"])
                    G(lambda e: e.tensor_copy(out=Vs[:, pgi, :, 0:64], in_=pg[b][:, 768:1024].rearrange("p (g d) -> p g d", d=64)), r=["spg%d" % b, "sVs"], w=["sVs"])
                for kvi in range(2):
                    for g in range(4):
                        cb = (kvi * 4 + g) % 2
                        cx = str(cb)
                        for l in range(32):
                            P(lambda e: e.matmul(ph[cb][:, 0:127], lhsT=W1[:, kvi, l, :], rhs=KcT[:, kvi, g, l:l + 16 * 126 + 1:16], start=(l == 0), stop=(l == 31)),
                              r=["saW1", "sKcT"], w=["sph" + cx])
                        V(lambda e: e.tensor_scalar(out=pre[cb][:, 0:127], in0=ph[cb][:, 0:127], scalar1=cv[:, kvi:kvi + 1], scalar2=None, op0=ALU.add),
                          r=["sph" + cx, "cv"], w=["spre" + cx])
                        gelu_tanh(K, pre[cb][:, 0:127], "spre" + cx, tmpg[cb][:, 0:127], "stmpg" + cx, hidT[cb][:, 127:0:-1], "shidT" + cx)
                        if kvi == 0:
                            P(lambda e: e.matmul(pk2[0:64, 0:128], lhsT=W2[:, 0, :], rhs=hidT[cb][:], start=True, stop=True), r=["saW2", "shidT" + cx], w=["spk2"])
                            V(lambda e: e.tensor_copy(out=kcT[:, g, :], in_=pk2[0:64, 0:128]), r=["spk2"], w=["skcT"])
                        else:
                            P(lambda e: e.matmul(pk2[:, 0:64], lhsT=hidT[cb][:], rhs=W2[:, 1, :], start=True, stop=True), r=["saW2", "shidT" + cx], w=["spk2"])
                            V(lambda e: e.tensor_copy(out=vca[:, g, 0:64], in_=pk2[:, 0:64]), r=["spk2", "svca"], w=["svca"])
                for g in range(4):
                    qcol = QT[:, 4 * g:4 * g + 4, tk]
                    P(lambda e: e.matmul(psS[:, 0:4], lhsT=kcT[:, g, :], rhs=QT[0:64, 4 * g:4 * g + 4, tk], start=True, stop=True), r=["skcT", "QT"], w=["spsS"])
                    V(lambda e: e.tensor_tensor(out=sc[:, 0, :], in0=psS[:, 0:4], in1=biasC[:, 4 * g:4 * g + 4], op=ALU.add), r=["spsS", "biasC"], w=["ssc"])
                    A(lambda e: e.activation(out=PTs[:, 0, :], in_=sc[:, 0, :], func=AF.Exp), r=["ssc"], w=["sPT"])
                    P(lambda e: e.matmul(po[0:4, 0:129], lhsT=PTs[:, 0, :], rhs=vca[:, g, :], start=True, stop=True), r=["sPT", "svca"], w=["spo"])
                    V(lambda e: e.tensor_copy(out=oc[:], in_=po[0:4, 0:129]), r=["spo"], w=["soc"])
                    V(lambda e: e.tensor_scalar(out=rcc[:], in0=oc[:, 64:65], scalar1=1e-30, scalar2=None, op0=ALU.add), r=["soc"], w=["srcc"])
                    V(lambda e: e.reciprocal(out=rcc[:], in_=rcc[:]), r=["srcc"], w=["srcc"])
                    dma(O["os_scr"][tk, 0, 4 * g:4 * g + 4, :], oc[:, 0:65], r=["soc"], w=["os_scr"], add_writer=True)
                    P(lambda e: e.matmul(po[0:1, 256:320], lhsT=rcc[:], rhs=oc[:, 65:129], start=True, stop=True), r=["srcc", "soc"], w=["spo"])
                    V(lambda e: e.tensor_copy(out=impr[:], in_=po[0:1, 256:320]), r=["spo"], w=["simpr"])
                    V(lambda e: e.memset(impr[:, 0:1], 1e4), r=["simpr"], w=["simpr"])
                    V(lambda e: e.memset(impr[:, 31:33], 1e4), r=["simpr"], w=["simpr"])
                    V(lambda e: e.memset(impr[:, 33:64], -1e30), r=["simpr"], w=["simpr"])
                    V(lambda e: e.max(out=m8[:, 0:8], in_=impr[:]), r=["simpr"], w=["sm8"])
                    V(lambda e: e.match_replace(out=wk1[:], in_to_replace=m8[:, 0:8], in_values=impr[:], imm_value=-3e38), r=["simpr", "sm8"], w=["swk1"])
                    V(lambda e: e.max(out=m8[:, 8:16], in_=wk1[:]), r=["swk1"], w=["sm8"])
                    V(lambda e: e.tensor_scalar(out=wk1[:], in0=impr[:], scalar1=m8[:, 15:16], scalar2=None, op0=ALU.is_ge), r=["simpr", "sm8", "swk1"], w=["swk1"])
                    V(lambda e: e.tensor_scalar(out=mbr[:, 64:128], in0=wk1[:], scalar1=-NEG, scalar2=NEG, op0=ALU.mult, op1=ALU.add), r=["swk1", "mbr"], w=["mbr"])
                    P(lambda e: e.matmul(po[:, 320:324], lhsT=mbr[:], rhs=ones4[:], start=True, stop=True), r=["mbr", "ones4", "simpr"], w=["spo"])
                    V(lambda e: e.tensor_copy(out=QT[64:128, 4 * g:4 * g + 4, tk], in_=po[64:128, 320:324]), r=["spo"], w=["QT"])
                    for kt in range(NKT_S):
                        P(lambda e: e.matmul(psS[:, 4 + 4 * kt:8 + 4 * kt], lhsT=KsE[:, g, kt * 128:(kt + 1) * 128], rhs=qcol, start=True, stop=True),
                          r=["sKsE", "QT"], w=["spsS"])
                    V(lambda e: e.tensor_tensor(out=sc[:], in0=psS[:, 4:4 + 4 * NKT_S].rearrange("p (t h) -> p t h", h=4), in1=biasS[:, :, 4 * g:4 * g + 4], op=ALU.add),
                      r=["spsS", "biasS"], w=["ssc"])
                    A(lambda e: e.activation(out=PTs[:], in_=sc[:], func=AF.Exp), r=["ssc"], w=["sPT"])
                    for kt in range(NKT_S):
                        P(lambda e: e.matmul(po[0:4, 0:65], lhsT=PTs[:, kt, :], rhs=Vs[:, kt, g, :], start=(kt == 0), stop=(kt == NKT_S - 1)),
                          r=["sPT", "sVs"], w=["spo"])
                    V(lambda e: e.tensor_copy(out=osw[:, 0, :], in_=po[0:4, 0:65]), r=["spo"], w=["sosw"])
                    for wt in range(NWT_S):
                        P(lambda e: e.matmul(psS[:, 4 * wt:4 * wt + 4], lhsT=KwE[:, g, wt * 128:(wt + 1) * 128], rhs=QT[0:64, 4 * g:4 * g + 4, tk], start=True, stop=True),
                          r=["sKwE", "QT"], w=["spsS"])
                    V(lambda e: e.tensor_tensor(out=sc[:, 0:NWT_S, :], in0=psS[:, 0:4 * NWT_S].rearrange("p (t h) -> p t h", h=4), in1=biasW[:, :, 4 * g:4 * g + 4], op=ALU.add),
                      r=["spsS", "biasW"], w=["ssc"])
                    A(lambda e: e.activation(out=PTs[:, 0:NWT_S, :], in_=sc[:, 0:NWT_S, :], func=AF.Exp), r=["ssc"], w=["sPT"])
                    for wt in range(NWT_S):
                        P(lambda e: e.matmul(po[0:4, 0:65], lhsT=PTs[:, wt, :], rhs=Vw[:, wt, g, :], start=(wt == 0), stop=(wt == NWT_S - 1)),
                          r=["sPT", "sVw"], w=["spo"])
                    V(lambda e: e.tensor_copy(out=osw[:, 1, :], in_=po[0:4, 0:65]), r=["spo", "sosw"], w=["sosw"])
                    dma(O["os_scr"][tk, 1, 4 * g:4 * g + 4, :], osw[:, 0, :], r=["sosw"], w=["os_scr"], add_writer=True)
                    dma(O["os_scr"][tk, 2, 4 * g:4 * g + 4, :], osw[:, 1, :], r=["sosw"], w=["os_scr"], add_writer=True)
        K.S.barrier()

        with ExitStack() as mes:
            osb = K.sb(mes, "sa_osb", [N, 3, 16, 65], F32)
            gt = K.sb(mes, "sa_gt", [N, 3, 16], F32)
            wgt = K.sb(mes, "sa_wgt", [N, 3, 16], F32)
            o1 = K.sb(mes, "sa_o1", [N, 16, 64], F32)
            o2 = K.sb(mes, "sa_o2", [N, 16, 64], F32)
            yr = K.sb(mes, "sa_yr", [N, D], F32)
            dma(osb[:], O["os_scr"], r=["os_scr"], w=["osb"])
            dma(gt[:], O["gates_scr"].rearrange("n (r h) -> n r h", h=16), r=["gate_scr"], w=["sgt"])
            dma(yr[:], O["y2s_scr"], r=["y2_scr"], w=["syr"])
            V(lambda e: e.tensor_scalar(out=wgt[:], in0=osb[:, :, :, 64], scalar1=1e-30, scalar2=None, op0=ALU.add), r=["osb"], w=["swgt"])
            V(lambda e: e.reciprocal(out=wgt[:], in_=wgt[:]), r=["swgt"], w=["swgt"])
            V(lambda e: e.tensor_tensor(out=wgt[:], in0=wgt[:], in1=gt[:], op=ALU.mult), r=["swgt", "sgt"], w=["swgt"])
            V(lambda e: e.tensor_tensor(out=o1[:], in0=osb[:, 0, :, 0:64], in1=bc(wgt[:, 0, :].unsqueeze(2), [N, 16, 64]), op=ALU.mult), r=["osb", "swgt"], w=["so1"])
            for br in (1, 2):
                V(lambda e: e.tensor_tensor(out=o2[:], in0=osb[:, br, :, 0:64], in1=bc(wgt[:, br, :].unsqueeze(2), [N, 16, 64]), op=ALU.mult), r=["osb", "swgt"], w=["so2"])
                V(lambda e: e.tensor_tensor(out=o1[:], in0=o1[:], in1=o2[:], op=ALU.add), r=["so1", "so2"], w=["so1"])
            L = Lin32(K, mes, C, 8, "o")
            oT32 = K.sb(mes, "sa_oT32", [128, 8, N], F32)
            L.transpose(o1[:].rearrange("n h d -> n (h d)"), "so1", 8, oT32, "soT32")

            def add_y(c0, cw, ps, pk):
                V(lambda e: e.tensor_tensor(out=yr[:, c0:c0 + cw], in0=ps, in1=yr[:, c0:c0 + cw], op=ALU.add), r=[pk, "syr"], w=["syr"])
            L.run(oT32, "soT32", 8, I["nsa_w_o"], D, add_y)
            dma(O["y3s_scr"], yr[:], r=["syr"], w=["y3s_scr"])
    K.S.barrier()


NMT = NTILE // 2 + 1


def moe_stage(K, C, I, O):
    idf, idb = C["idf"], C["idb"]
    V, A, G, P, dma = K.V, K.A, K.G, K.P, K.dma
    EF = 1408
    CW = 352
    with ExitStack() as es:
        acc = K.sb(es, "mo_acc", [128, NMT, D], F32)
        hT = K.sb(es, "mo_hT", [128, NMT, 8, 128], BF16)
        gts = K.sb(es, "mo_gts", [128, NMT, 8], F32)
        hT32s = K.sb(es, "mo_hT32s", [128, 8, NS], F32)
        par = K.sb(es, "mo_par", [128, 2], F32)
        gain = K.sb(es, "mo_gain", [128, D], F32)
        gfin = K.sb(es, "mo_gfin", [128, D], F32)
        rt = K.sb(es, "mo_rt", [128, 8, 8], F32)
        dma(par[:], I["par"], w=["par"])
        dma(gain[:], AP(tensor=I["norm_ffn"].tensor, offset=D, ap=[[0, 128], [1, D]]), w=["gain"])
        dma(gfin[:], AP(tensor=I["norm_final"].tensor, offset=0, ap=[[0, 128], [1, D]]), w=["gfin"])
        dma(rt[:], I["moe_router"].rearrange("(k p) e -> p k e", p=128), w=["rt"])
        with ExitStack() as es2:
            ya = K.sb(es2, "mo_ya", [128, D], F32)
            yb = K.sb(es2, "mo_yb", [128, D], F32)
            h32 = K.sb(es2, "mo_h32", [128, D], F32)
            hb = K.sb(es2, "mo_hb", [128, D], BF16)
            hT32 = K.sb(es2, "mo_hT32", [128, 8, 128], F32)
            junk = K.sb(es2, "mo_junk", [128, D], F32)
            ss = K.sb(es2, "mo_ss", [128, 1], F32)
            lg = K.sb(es2, "mo_lg", [128, 8], F32)
            ex = K.sb(es2, "mo_ex", [128, 8], F32)
            mk = K.sb(es2, "mo_mk", [128, 8], F32)
            m8 = K.sb(es2, "mo_m8", [128, 8], F32)
            sm = K.sb(es2, "mo_sm", [128, 1], F32)
            ptb = K.ps(es2, "mo_ptb", [128, 8, 128], BF16)
            pt32 = [K.ps(es2, "mo_pt32%d" % i, [128, 4, 128], F32) for i in range(2)]
            plg = K.ps(es2, "mo_plg", [128, 512], F32)
            for j in range(NMT):
                Pn = 128 if j < NMT - 1 else NS
                if j < NMT - 1:
                    dma(acc[:, j, :], O["y3_scr"][j * 128:(j + 1) * 128, :], r=["y3_scr"], w=["acc"])
                else:
                    dma(acc[0:Pn, j, :], O["y3s_scr"], r=["y3s_scr"], w=["acc"])
                xin = acc[0:Pn, j, :]
                A(lambda e: e.activation(out=junk[0:Pn, :], in_=xin, func=AF.Square, accum_out=ss[0:Pn, 0:1]), r=["acc"], w=["junk", "ss"])
                V(lambda e: e.tensor_scalar(out=ss[0:Pn, :], in0=ss[0:Pn, :], scalar1=1.0 / D, scalar2=EPS, op0=ALU.mult, op1=ALU.add), r=["ss"], w=["ss"])
                A(lambda e: e.activation(out=ss[0:Pn, :], in_=ss[0:Pn, :], func=AF.Sqrt), r=["ss"], w=["ss"])
                V(lambda e: e.reciprocal(out=ss[0:Pn, :], in_=ss[0:Pn, :]), r=["ss"], w=["ss"])
                V(lambda e: e.scalar_tensor_tensor(out=h32[0:Pn, :], in0=xin, scalar=ss[0:Pn, 0:1], in1=gain[0:Pn, :], op0=ALU.mult, op1=ALU.mult),
                  r=["acc", "ss", "gain"], w=["h32"])
                V(lambda e: e.tensor_copy(out=hb[0:Pn, :], in_=h32[0:Pn, :]), r=["h32"], w=["hb"])
                for k in range(8):
                    P(lambda e: e.transpose(out=ptb[:, k, 0:Pn], in_=hb[0:Pn, k * 128:(k + 1) * 128], identity=idb[0:Pn, 0:Pn]), r=["hb", "idb"], w=["ptb"])
                A(lambda e: e.copy(out=hT[:, j, :, 0:Pn], in_=ptb[:, :, 0:Pn]), r=["ptb"], w=["hT"])
                for k in range(8):
                    P(lambda e: e.transpose(out=pt32[k // 4][:, k % 4, 0:Pn], in_=h32[0:Pn, k * 128:(k + 1) * 128], identity=idf[0:Pn, 0:Pn]),
                      r=["h32", "idf"], w=["pt32%d" % (k // 4)])
                for hh in range(2):
                    V(lambda e: e.tensor_copy(out=hT32[:, 4 * hh:4 * hh + 4, 0:Pn], in_=pt32[hh][:, :, 0:Pn]), r=["pt32%d" % hh], w=["hT32"])
                if j == NMT - 1:
                    V(lambda e: e.tensor_copy(out=hT32s[:], in_=hT32[:, :, 0:NS]), r=["hT32"], w=["hT32s"])
                for k in range(8):
                    P(lambda e: e.matmul(plg[0:Pn, 0:8], lhsT=hT32[:, k, 0:Pn], rhs=rt[:, k, :], start=(k == 0), stop=(k == 7)), r=["hT32", "rt"], w=["plg"])
                V(lambda e: e.tensor_copy(out=lg[0:Pn, :], in_=plg[0:Pn, 0:8]), r=["plg"], w=["lg"])
                V(lambda e: e.max(out=m8[0:Pn, :], in_=lg[0:Pn, :]), r=["lg"], w=["m8"])
                V(lambda e: e.tensor_scalar(out=ex[0:Pn, :], in0=lg[0:Pn, :], scalar1=m8[0:Pn, 0:1], scalar2=None, op0=ALU.subtract), r=["lg", "m8"], w=["ex"])
                A(lambda e: e.activation(out=ex[0:Pn, :], in_=ex[0:Pn, :], func=AF.Exp), r=["ex"], w=["ex"])
                V(lambda e: e.tensor_scalar(out=mk[0:Pn, :], in0=lg[0:Pn, :], scalar1=m8[0:Pn, 1:2], scalar2=None, op0=ALU.is_ge), r=["lg", "m8"], w=["mk"])
                V(lambda e: e.tensor_tensor(out=ex[0:Pn, :], in0=ex[0:Pn, :], in1=mk[0:Pn, :], op=ALU.mult), r=["ex", "mk"], w=["ex"])
                V(lambda e: e.reduce_sum(out=sm[0:Pn, :], in_=ex[0:Pn, :], axis=AX.X), r=["ex"], w=["sm"])
                V(lambda e: e.reciprocal(out=sm[0:Pn, :], in_=sm[0:Pn, :]), r=["sm"], w=["sm"])
                V(lambda e: e.tensor_scalar(out=gts[0:Pn, j, :], in0=ex[0:Pn, :], scalar1=sm[0:Pn, 0:1], scalar2=None, op0=ALU.mult), r=["ex", "sm"], w=["gts"])
        K.S.barrier()
        with ExitStack() as es3:
            w1 = K.sb(es3, "mo_w1", [128, 8, 2 * EF], BF16)
            w2 = K.sb(es3, "mo_w2", [128, 11, D], BF16)
            act = [K.sb(es3, "mo_act%d" % i, [128, EF], BF16) for i in range(2)]
            actT = [K.sb(es3, "mo_actT%d" % i, [128, 11, 128], BF16) for i in range(2)]
            sl_ = [K.sb(es3, "mo_s%d" % i, [128, CW], F32) for i in range(2)]
            ptb = K.ps(es3, "mo_ptb2", [128, 8, 128], BF16)
            pa = [K.ps(es3, "mo_pa%d" % i, [128, 512], F32) for i in range(2)]
            pb = [K.ps(es3, "mo_pb%d" % i, [128, 512], F32) for i in range(2)]
            po = [K.ps(es3, "mo_po%d" % i, [128, 512], F32) for i in range(2)]
            for ex_ in range(8):
                v1 = I["moe_w_in"][ex_].rearrange("(k p) n -> p k n", p=128)
                for c0 in range(0, 2 * EF, 704):
                    K.S.dma("pool", w1[:, :, c0:c0 + 704], v1[:, :, c0:c0 + 704], (), ["mo_w1"], add_writer=(c0 > 0))
                v2 = I["moe_w_out"][ex_].rearrange("(k p) n -> p k n", p=128)
                for c0 in range(0, D, 512):
                    K.S.dma("pool", w2[:, :, c0:c0 + 512], v2[:, :, c0:c0 + 512], (), ["mo_w2"], add_writer=(c0 > 0))
                for j in range(NMT - 1):
                    Pn = 128
                    b = j % 2
                    sx = str(b)
                    for jj in range(4):
                        jb = jj % 2
                        for k in range(8):
                            P(lambda e: e.matmul(pa[jb][0:Pn, 0:CW], lhsT=hT[:, j, k, 0:Pn], rhs=w1[:, k, CW * jj:CW * jj + CW], start=(k == 0), stop=(k == 7)),
                              r=["hT", "mo_w1"], w=["mpa%d" % jb])
                        for k in range(8):
                            P(lambda e: e.matmul(pb[jb][0:Pn, 0:CW], lhsT=hT[:, j, k, 0:Pn], rhs=w1[:, k, EF + CW * jj:EF + CW * jj + CW], start=(k == 0), stop=(k == 7)),
                              r=["hT", "mo_w1"], w=["mpb%d" % jb])
                        A(lambda e: e.activation(out=sl_[jb][0:Pn, :], in_=pa[jb][0:Pn, 0:CW], func=AF.Silu), r=["mpa%d" % jb], w=["msl%d" % jb])
                        V(lambda e: e.tensor_tensor(out=act[b][0:Pn, CW * jj:CW * jj + CW], in0=pb[jb][0:Pn, 0:CW], in1=sl_[jb][0:Pn, :], op=ALU.mult),
                          r=["mpb%d" % jb, "msl%d" % jb], w=["mact" + sx])
                    for k0 in range(0, 11, 8):
                        k1 = min(11, k0 + 8)
                        for k in range(k0, k1):
                            P(lambda e: e.transpose(out=ptb[:, k - k0, 0:Pn], in_=act[b][0:Pn, k * 128:(k + 1) * 128], identity=idb[0:Pn, 0:Pn]),
                              r=["mact" + sx, "idb"], w=["mptb"])
                        A(lambda e: e.copy(out=actT[b][:, k0:k1, 0:Pn], in_=ptb[:, 0:k1 - k0, 0:Pn]), r=["mptb"], w=["mactT" + sx])
                    for c in range(2):
                        for k in range(11):
                            P(lambda e: e.matmul(po[c][0:Pn, :], lhsT=actT[b][:, k, 0:Pn], rhs=w2[:, k, c * 512:(c + 1) * 512], start=(k == 0), stop=(k == 10)),
                              r=["mactT" + sx, "mo_w2"], w=["mpo%d" % c])
                        V(lambda e: e.scalar_tensor_tensor(out=acc[0:Pn, j, c * 512:(c + 1) * 512], in0=po[c][0:Pn, :], scalar=gts[0:Pn, j, ex_:ex_ + 1],
                                                           in1=acc[0:Pn, j, c * 512:(c + 1) * 512], op0=ALU.mult, op1=ALU.add),
                          r=["mpo%d" % c, "gts", "acc"], w=["acc"])
        K.S.barrier()
        with ExitStack() as es4:
            L = Lin32(K, es4, C, 11, "m")
            bigm = K.sb(es4, "mo_big", [NS, 2 * EF], F32)
            actm = K.sb(es4, "mo_actm", [NS, EF], F32)
            actTm = K.sb(es4, "mo_actTm", [128, 11, NS], F32)
            js = NMT - 1

            def to_bigm(c0, cw, ps, pk):
                A(lambda e: e.copy(out=bigm[:, c0:c0 + cw], in_=ps), r=[pk], w=["mo_big"])
            for ex_ in range(8):
                L.run(hT32s, "hT32s", 8, I["moe_w_in"][ex_], 2 * EF, to_bigm)
                A(lambda e: e.activation(out=actm[:], in_=bigm[:, 0:EF], func=AF.Silu), r=["mo_big"], w=["mo_actm"])
                V(lambda e: e.tensor_tensor(out=actm[:], in0=actm[:], in1=bigm[:, EF:2 * EF], op=ALU.mult), r=["mo_big", "mo_actm"], w=["mo_actm"])
                L.transpose(actm, "mo_actm", 11, actTm, "mo_actTm")

                def acc_add(c0, cw, ps, pk):
                    V(lambda e: e.scalar_tensor_tensor(out=acc[0:NS, js, c0:c0 + cw], in0=ps, scalar=gts[0:NS, js, ex_:ex_ + 1],
                                                       in1=acc[0:NS, js, c0:c0 + cw], op0=ALU.mult, op1=ALU.add), r=[pk, "gts", "acc"], w=["acc"])
                L.run(actTm, "mo_actTm", 11, I["moe_w_out"][ex_], D, acc_add)
        K.S.barrier()
        with ExitStack() as es3:
            junk2 = K.sb(es3, "mo_junk2", [128, D], F32)
            ss2 = K.sb(es3, "mo_ss2", [128, 1], F32)
            for j in range(NMT):
                Pn = 128 if j < NMT - 1 else NS
                xin = acc[0:Pn, j, :]
                A(lambda e: e.activation(out=junk2[0:Pn, :], in_=xin, func=AF.Square, accum_out=ss2[0:Pn, 0:1]), r=["acc"], w=["junk2", "ss2"])
                V(lambda e: e.tensor_scalar(out=ss2[0:Pn, :], in0=ss2[0:Pn, :], scalar1=1.0 / D, scalar2=EPS, op0=ALU.mult, op1=ALU.add), r=["ss2"], w=["ss2"])
                A(lambda e: e.activation(out=ss2[0:Pn, :], in_=ss2[0:Pn, :], func=AF.Sqrt), r=["ss2"], w=["ss2"])
                V(lambda e: e.reciprocal(out=ss2[0:Pn, :], in_=ss2[0:Pn, :]), r=["ss2"], w=["ss2"])
                V(lambda e: e.scalar_tensor_tensor(out=junk2[0:Pn, :], in0=xin, scalar=ss2[0:Pn, 0:1], in1=gfin[0:Pn, :], op0=ALU.mult, op1=ALU.mult),
                  r=["acc", "ss2", "gfin", "junk2"], w=["junk2"])
                if j < NMT - 1:
                    dma(O["y_p"][j * 128:(j + 1) * 128, :], junk2[:], r=["junk2"], w=["y_p"], add_writer=True)
                else:
                    dma(O["y_s"], junk2[0:Pn, :], r=["junk2"], w=["y_s"])
    K.S.barrier()


IN_SPECS = [("norm_mix", [2, D]), ("norm_ffn", [2, D]), ("s5_lam_re", [64, 64]), ("s5_lam_im", [64, 64]), ("s5_log_dt", [64]),
            ("s5_b_re", [64, 64, 16]), ("s5_b_im", [64, 64, 16]), ("s5_c_re", [64, 16, 64]), ("s5_c_im", [64, 16, 64]),
            ("s5_d", [D]), ("s5_w_glu", [D, 2 * D]), ("ffn_w_in", [D, 5632]), ("ffn_w_out", [2816, D]), ("nsa_w_in", [D, 2608]),
            ("rel_bias", [32, 16]), ("nsa_phi_pe", [2, 32, 64]), ("nsa_phi_w1", [2, 32, 64, 128]), ("nsa_phi_w2", [2, 128, 64]),
            ("nsa_w_o", [D, D]), ("norm_final", [D]), ("moe_router", [D, 8]), ("moe_w_in", [8, D, 2816]), ("moe_w_out", [8, 1408, D])]
CONST_SPECS = [("c_ohb", [32, 256]), ("c_cover", [256, 64]), ("c_E", [64, T]), ("c_cover_s", [128, 64])]


def t5_bucket_np(n):
    n = np.maximum(n, 0)
    logpart = 16 + (np.log(np.maximum(n, 1).astype(np.float32) / 16) / math.log(8) * 16).astype(np.int32)
    return np.where(n < 16, n, np.minimum(logpart, 31))


def make_core_consts(par):
    nj = NTILE // 2
    qidx = np.zeros((128, nj), np.int32)
    ka = np.zeros((nj, 128, 2, 64), np.float32)
    jj = np.arange(64)
    for j in range(nj):
        qt = 2 * j + par
        qpos = qt * 128 + np.arange(128)
        qidx[:, j] = qpos
        jt = qpos // 64
        forced = (jj[None, :] == 0) | (jj[None, :] == jt[:, None]) | (jj[None, :] == jt[:, None] - 1)
        vis = (jj[None, :] * 64) <= qpos[:, None]
        keep = (vis & ~forced).astype(np.float32)
        add = np.where(vis, np.where(forced, 1e4, 0.0), -1e30).astype(np.float32)
        ka[j, :, 0, :] = keep
        ka[j, :, 1, :] = add
    return {"c_qidx": qidx, "c_ka": ka, "par": np.tile(np.array([[1.0 - par, float(par)]], np.float32), (128, 1))}


def make_consts():
    c = {}
    bk = t5_bucket_np(np.arange(256))
    ohb = np.zeros((32, 256), np.float32)
    ohb[bk, np.arange(256)] = 1.0
    c["c_ohb"] = ohb
    cover = np.zeros((256, 64), np.float32)
    off = (np.arange(4)[:, None] - np.arange(2)[None, :]).reshape(-1)
    for j in range(64):
        for o in off:
            n = 4 * j + o
            if 0 <= n < NCB:
                cover[n, j] += 1.0
    c["c_cover"] = np.ascontiguousarray(cover[::-1])
    E = np.zeros((64, T), np.float32)
    E[np.arange(T) // 64, np.arange(T)] = 1.0
    c["c_E"] = E
    cov_s = np.zeros((128, 64), np.float32)
    for j in range(33):
        for o in off:
            n = 4 * j + o
            if 0 <= n < 127:
                cov_s[127 - n, j] += 1.0
    c["c_cover_s"] = cov_s
    return c


def s5_stage_w(K, C, I, O):
    s5_stage(K, None, C, I, O)


def build(upto=99, only=None):
    nc = bass.Bass("TRN2", target_bir_lowering=False)
    with ExitStack() as es:
        K = KB(nc, es)
        I, O = {}, {}
        I["xp"] = K.dram("xp", [T, D], F32, "ExternalInput")
        for nm, shp in IN_SPECS + CONST_SPECS:
            I[nm] = K.dram(nm, shp, F32, "ExternalInput")
        O["s5_re_p"] = K.dram("s5_re_p", [64, 64], F32, "ExternalOutput")
        O["s5_im_p"] = K.dram("s5_im_p", [64, 64], F32, "ExternalOutput")
        O["kv_p"] = K.dram("kv_p", [T, 1024], F32, "ExternalOutput")
        O["win_p"] = K.dram("win_p", [512, 512], F32, "ExternalOutput")
        I["xs"] = K.dram("xs", [NS, D], F32, "ExternalInput")
        I["st_re"] = K.dram("st_re", [NS, 4096], F32, "ExternalInput")
        I["st_im"] = K.dram("st_im", [NS, 4096], F32, "ExternalInput")
        O["s5_re_s"] = K.dram("s5_re_s", [NS, 4096], F32, "ExternalOutput")
        O["s5_im_s"] = K.dram("s5_im_s", [NS, 4096], F32, "ExternalOutput")
        O["kv_s"] = K.dram("kv_s", [NS, 1024], F32, "ExternalOutput")
        O["win_s"] = K.dram("win_s", [NS, 512], F32, "ExternalOutput")
        dbg = "ExternalOutput" if (upto < 99 or only is not None) else "Internal"
        O["g_scr"] = K.dram("g_scr", [T, D], BF16, dbg)
        O["y1_scr"] = K.dram("y1_scr", [T, D], F32, dbg)
        O["y2_scr"] = K.dram("y2_scr", [T, D], F32, dbg)
        O["gs_scr"] = K.dram("gs_scr", [NS, D], BF16, dbg)
        O["gs32_scr"] = K.dram("gs32_scr", [NS, D], F32, dbg)
        O["y1s_scr"] = K.dram("y1s_scr", [NS, D], F32, dbg)
        O["y2s_scr"] = K.dram("y2s_scr", [NS, D], F32, dbg)
        O["q_scr"] = K.dram("q_scr", [T, 1024], BF16, dbg)
        O["qs_scr"] = K.dram("qs_scr", [NS, 1024], BF16, dbg)
        O["kvb_scr"] = K.dram("kvb_scr", [T, 1536], BF16, dbg)
        O["kvbs_scr"] = K.dram("kvbs_scr", [NS, 1536], BF16, dbg)
        O["gate_scr"] = K.dram("gate_scr", [T, 48], F32, dbg)
        O["gates_scr"] = K.dram("gates_scr", [NS, 48], F32, dbg)
        O["fpad_scr"] = K.dram("fpad_scr", [16, 384], F32, "Internal")
        O["fc_scr"] = K.dram("fc_scr", [16, 8192], F32, "Internal")
        O["y3_scr"] = K.dram("y3_scr", [T // 2, D], F32, dbg)
        O["y3s_scr"] = K.dram("y3s_scr", [NS, D], F32, dbg)
        O["os_scr"] = K.dram("os_scr", [NS, 3, 16, 65], F32, dbg)
        if only is None or 6 in only:
            I["ckv"] = K.dram("ckv", [2560 * 128, 1024], F32, "ExternalInput")
        I["cwin"] = K.dram("cwin", [NS, 512, 512], F32, "ExternalInput")
        I["ptab"] = K.dram("ptab", [NS * 16], I32, "ExternalInput")
        I["par"] = K.dram("par", [128, 2], F32, "ExternalInput")
        I["c_qidx"] = K.dram("c_qidx", [128, NTILE // 2], I32, "ExternalInput")
        I["c_ka"] = K.dram("c_ka", [NTILE // 2, 128, 2, 64], F32, "ExternalInput")
        O["y_p"] = K.dram("y_p", [T // 2, D], F32, "ExternalOutput")
        O["y_s"] = K.dram("y_s", [NS, D], F32, "ExternalOutput")
        C = build_consts(K, es)
        stages = [(1, s5_stage_w), (1.5, sample_l0_f32), (2, glu_stage), (3, ffn_stage), (4, nsa_proj_stage), (5, nsa_attn_prompt), (6, nsa_attn_sample), (7, moe_stage)]
        for idx, fn in stages:
            if (only is None and idx <= upto) or (only is not None and idx in only):
                fn(K, C, I, O)
        K.S.finish()
    return nc


def kernel(**inp):
    f32 = lambda a: np.ascontiguousarray(np.asarray(a, dtype=np.float32))
    nc = build()
    shared = {nm: f32(inp[nm]) for nm, _ in IN_SPECS}
    shared.update(make_consts())
    shared["ckv"] = f32(inp["cache_kv"]).reshape(2560 * 128, 1024)
    in_maps = []
    for c in range(8):
        m = dict(shared)
        m["xp"] = f32(inp["x_prompt"][c // 2])
        sl = slice(NS * c, NS * (c + 1))
        m["xs"] = f32(inp["x_sample"][sl, 0, :])
        m["st_re"] = f32(inp["state_s5_re"][sl]).reshape(NS, 4096)
        m["st_im"] = f32(inp["state_s5_im"][sl]).reshape(NS, 4096)
        m["cwin"] = f32(inp["cache_win"][sl]).reshape(NS, 512, 512)
        m["ptab"] = np.ascontiguousarray(np.asarray(inp["page_table"][sl], dtype=np.int32)).reshape(NS * 16)
        m.update(make_core_consts(c % 2))
        in_maps.append(m)
    res = run_bass_kernel_spmd(nc, in_maps, core_ids=list(range(8))).results
    B = 4
    cat = lambda nm: np.concatenate([np.asarray(res[c][nm], dtype=np.float32) for c in range(8)], axis=0)
    y_prompt = np.zeros((B, NTILE, 128, D), np.float32)
    for c in range(8):
        yp = np.asarray(res[c]["y_p"], dtype=np.float32).reshape(NTILE // 2, 128, D)
        y_prompt[c // 2, (c % 2)::2] = yp
    y_prompt = y_prompt.reshape(B, T, D)
    y_sample = cat("y_s").reshape(128, 1, D)
    s5_re_p = np.stack([res[2 * b]["s5_re_p"] for b in range(B)]).astype(np.float32)
    s5_im_p = np.stack([res[2 * b]["s5_im_p"] for b in range(B)]).astype(np.float32)
    kv_p = np.stack([res[2 * b]["kv_p"] for b in range(B)]).astype(np.float32).reshape(B, T, 4, 4, 64)
    win_p = np.stack([res[2 * b]["win_p"] for b in range(B)]).astype(np.float32).reshape(B, 512, 2, 4, 64)
    s5_re_s = cat("s5_re_s").reshape(128, 64, 64)
    s5_im_s = cat("s5_im_s").reshape(128, 64, 64)
    kv_s = cat("kv_s").reshape(128, 1, 4, 4, 64)
    win_s = cat("win_s").reshape(128, 1, 2, 4, 64)
    return (y_prompt, y_sample, s5_re_p, s5_im_p, kv_p, win_p, s5_re_s, s5_im_s, kv_s, win_s)
```

```python
import math
import os
import numpy as np
STOP = float(os.environ.get('S5STOP', '99'))
NOSELF = os.environ.get('NOSELF', '0') == '1'
from contextlib import ExitStack
import concourse.bass as bass
import concourse.mybir as mybir
from concourse.bass_types import AP
from concourse.bass_utils import run_bass_kernel_spmd

F32 = mybir.dt.float32
BF16 = mybir.dt.bfloat16
I32 = mybir.dt.int32
AF = mybir.ActivationFunctionType
ALU = mybir.AluOpType
AX = mybir.AxisListType

D = 1024
T = 4096
NS = 16
EPS = 1e-6
TWO_PI = 2.0 * math.pi


class Sched:
    def __init__(self, nc, es, ndma=10):
        self.nc = nc
        self.eng = {"pe": nc.tensor, "dve": nc.vector, "act": nc.scalar, "pool": nc.gpsimd, "sp": nc.sync}
        self.sem = {k: es.enter_context(nc.semaphore("s_" + k)) for k in self.eng}
        self.cnt = {k: 0 for k in self.eng}
        self.dsem = {k: [es.enter_context(nc.semaphore("d_%s%d" % (k, i))) for i in range(ndma)] for k in ("sp", "act", "pool")}
        self.dcnt = {k: [0] * ndma for k in self.dsem}
        self.drr = {k: 0 for k in self.dsem}
        self.waited = {k: {} for k in self.eng}
        self.semobj = {}
        self.wr = {}
        self.rd = {}

    def _sid(self, s):
        i = id(s)
        self.semobj[i] = s
        return i

    def _wait(self, e, deps):
        w = self.waited[e]
        for sid, val in deps.items():
            if w.get(sid, 0) >= val:
                continue
            self.eng[e].wait_ge(self.semobj[sid], val)
            w[sid] = val

    def _deps(self, reads, writes, own_sid=None, skip_own=False):
        deps = {}

        def add(d):
            for sid, val in d.items():
                if skip_own and sid == own_sid:
                    continue
                if deps.get(sid, 0) < val:
                    deps[sid] = val
        for k in reads:
            add(self.wr.get(k, {}))
        for k in writes:
            add(self.wr.get(k, {}))
            add(self.rd.get(k, {}))
        return deps

    def op(self, e, fn, reads=(), writes=()):
        s = self.sem[e]
        sid = self._sid(s)
        deps = self._deps(reads, writes, own_sid=sid, skip_own=(e == "pe" or NOSELF))
        self._wait(e, deps)
        inst = fn(self.eng[e])
        self.cnt[e] += 1
        inst.then_inc(s, 1)
        val = self.cnt[e]
        for k in reads:
            self.rd.setdefault(k, {})[sid] = val
        for k in writes:
            self.wr[k] = {sid: val}
            self.rd[k] = {}
        return inst

    def dma(self, q, out, in_, reads=(), writes=(), add_writer=False, **kw):
        pool = self.dsem[q]
        i = self.drr[q]
        self.drr[q] = (i + 1) % len(pool)
        s = pool[i]
        sid = self._sid(s)
        deps = self._deps(reads, writes)
        if self.dcnt[q][i] > 0:
            deps[sid] = max(deps.get(sid, 0), self.dcnt[q][i])
        self._wait(q, deps)
        inst = self.eng[q].dma_start(out=out, in_=in_, **kw)
        self.dcnt[q][i] += 16
        inst.then_inc(s, 16)
        val = self.dcnt[q][i]
        for k in reads:
            self.rd.setdefault(k, {})[sid] = val
        for k in writes:
            if add_writer and k in self.wr:
                self.wr[k][sid] = val
            else:
                self.wr[k] = {sid: val}
                self.rd[k] = {}
        return inst

    def idma(self, out, in_, idx_ap, reads=(), writes=()):
        q = "pool"
        pool = self.dsem[q]
        i = self.drr[q]
        self.drr[q] = (i + 1) % len(pool)
        sm = pool[i]
        sid = self._sid(sm)
        deps = self._deps(reads, writes)
        if self.dcnt[q][i] > 0:
            deps[sid] = max(deps.get(sid, 0), self.dcnt[q][i])
        self._wait(q, deps)
        inst = self.eng[q].indirect_dma_start(out=out, out_offset=None, in_=in_, in_offset=bass.IndirectOffsetOnAxis(ap=idx_ap, axis=0))
        self.dcnt[q][i] += 16
        inst.then_inc(sm, 16)
        val = self.dcnt[q][i]
        for k in reads:
            self.rd.setdefault(k, {})[sid] = val
        for k in writes:
            self.wr[k] = {sid: val}
            self.rd[k] = {}
        return inst

    def barrier(self):
        deps = {}
        for e in self.eng:
            if self.cnt[e] > 0:
                deps[self._sid(self.sem[e])] = self.cnt[e]
        for q in self.dsem:
            for i, sm in enumerate(self.dsem[q]):
                if self.dcnt[q][i] > 0:
                    deps[self._sid(sm)] = self.dcnt[q][i]
        for e in self.eng:
            self._wait(e, dict(deps))

    def finish(self):
        deps = {}
        for k in list(self.wr.keys()):
            for sid, val in self.wr[k].items():
                deps[sid] = max(deps.get(sid, 0), val)
        for q in self.dsem:
            for i, s in enumerate(self.dsem[q]):
                if self.dcnt[q][i] > 0:
                    sid = self._sid(s)
                    deps[sid] = max(deps.get(sid, 0), self.dcnt[q][i])
        self._wait("sp", deps)


class KB:
    def __init__(self, nc, es):
        self.nc = nc
        self.es = es
        self.S = Sched(nc, es)
        self.dr = {}
        self._q = 0

    def sb(self, es, name, shape, dt):
        return es.enter_context(self.nc.sbuf_tensor(name, shape, dt))

    def ps(self, es, name, shape, dt):
        return es.enter_context(self.nc.psum_tensor(name, shape, dt))

    def dram(self, name, shape, dt, kind):
        t = self.nc.dram_tensor(name, shape, dt, kind=kind).ap()
        self.dr[name] = t
        return t

    def V(self, fn, r=(), w=()):
        return self.S.op("dve", fn, r, w)

    def A(self, fn, r=(), w=()):
        return self.S.op("act", fn, r, w)

    def G(self, fn, r=(), w=()):
        return self.S.op("pool", fn, r, w)

    def P(self, fn, r=(), w=()):
        return self.S.op("pe", fn, r, w)

    def dma(self, out, in_, r=(), w=(), q=None, **kw):
        if q is None:
            q = ("sp", "act")[self._q % 2]
            self._q += 1
        return self.S.dma(q, out, in_, r, w, **kw)


def bc(ap, shape):
    return ap.broadcast_to(shape)


def build_consts(K, es):
    c = {}
    idf = K.sb(es, "ident_f", [128, 128], F32)
    idb = K.sb(es, "ident_b", [128, 128], BF16)
    K.G(lambda e: e.memset(idf[:], 1.0), w=["idf"])
    K.G(lambda e: e.affine_select(out=idf[:], in_=idf[:], pattern=[[-1, 128]], compare_op=ALU.is_equal,
                                  fill=0.0, base=0, channel_multiplier=1), r=["idf"], w=["idf"])
    K.V(lambda e: e.tensor_copy(out=idb[:], in_=idf[:]), r=["idf"], w=["idb"])
    c["idf"], c["idb"] = idf, idb
    return c


def s5_stage(K, es_outer, C, I, O):
    nc = K.nc
    idf, idb = C["idf"], C["idb"]
    V, A, G, P, dma = K.V, K.A, K.G, K.P, K.dma
    with ExitStack() as es:
        _s5_body(K, es, C, I, O)
    K.S.barrier()


def _s5_body(K, es, C, I, O):
    idf, idb = C["idf"], C["idb"]
    V, A, G, P, dma = K.V, K.A, K.G, K.P, K.dma

    Win = K.sb(es, "s5Win", [128, 32, 2, 2, 128], BF16)
    Wout = K.sb(es, "s5Wout", [128, 32, 2, 128], BF16)
    Mint = K.sb(es, "s5Mint", [128, 64, 128], BF16)
    CA = K.sb(es, "s5CA", [128, 2, 32], F32)
    CBn = K.sb(es, "s5CBn", [128, 32], F32)
    CBp = K.sb(es, "s5CBp", [128, 32], F32)
    A1 = K.sb(es, "s5A1", [128, 2, 32], F32)
    gain0 = K.sb(es, "gain0", [128, D], F32)
    dskip = K.sb(es, "dskip", [128, D], F32)
    dma(gain0[:], AP(tensor=I["norm_mix"].tensor, offset=0, ap=[[0, 128], [1, D]]), w=["gain0"])
    dma(dskip[:], AP(tensor=I["s5_d"].tensor, offset=0, ap=[[0, 128], [1, D]]), w=["dskip"])

    with ExitStack() as pes:
        raw = K.sb(pes, "s5raw", [64, 3, 2, 64], F32)
        PRM = K.sb(pes, "s5prm", [128, 3, 32], F32)
        Wk = K.sb(pes, "s5wk", [128, 24, 32], F32)
        Wi = K.sb(pes, "s5wi", [128, 32], I32)
        PW = K.sb(pes, "s5pw", [128, 16, 2, 32], F32)
        Bt = K.sb(pes, "s5bt", [128, 2, 32, 16], F32)
        Ct = K.sb(pes, "s5ct", [128, 2, 32, 16], F32)
        BB = K.sb(pes, "s5bb", [128, 2, 32, 16], F32)
        craw = K.sb(pes, "s5craw", [128, 2, 2, 64], F32)
        T3 = K.sb(pes, "s5t3", [128, 4, 32, 16], F32)
        Ubf = K.sb(pes, "s5U", [128, 32, 2, 128], BF16)
        Vpad = K.sb(pes, "s5V", [128, 32, 2, 2, 128], BF16)
        maskLT = K.sb(pes, "s5mask", [128, 128], F32)
        pp = K.ps(pes, "s5pp", [128, 8, 64], F32)
        pc = [K.ps(pes, "s5pc%d" % i, [128, 512], F32) for i in range(2)]
        pm = [K.ps(pes, "s5pm%d" % i, [128, 512], F32) for i in range(2)]
        pw = [K.ps(pes, "s5pw%d" % i, [128, 1024], BF16) for i in range(2)]

        for i, nm in enumerate(["s5_lam_re", "s5_lam_im"]):
            dma(raw[:, i, :, :], AP(tensor=I[nm].tensor, offset=0, ap=[[64, 64], [0, 2], [1, 64]]), w=["raw"], add_writer=True)
        ldt = K.sb(pes, "s5ldt", [64, 1], F32)
        dma(ldt[:], AP(tensor=I["s5_log_dt"].tensor, offset=0, ap=[[1, 64], [1, 1]]), w=["ldt"])
        V(lambda e: e.tensor_copy(out=raw[:, 2, :, :].rearrange("p a b -> p (a b)"), in_=bc(ldt[:, 0:1], [64, 128])), r=["ldt"], w=["raw2"])
        for i in range(3):
            P(lambda e: e.transpose(out=pp[:, i, :], in_=raw[:, i, :, :], identity=idf[0:64, 0:64]), r=["raw", "raw2", "idf"], w=["pp"])
        for gl in range(2):
            sl = slice(64 * gl, 64 * gl + 64)
            V(lambda e: e.tensor_copy(out=PRM[sl, :, :], in_=pp[sl, 0:3, :].rearrange("p i (pr gl) -> p i pr gl", gl=2)[:, :, :, gl]),
              r=["pp"], w=["PRM"])
        if STOP <= 1:
            return
        LR, LI, LD = PRM[:, 0, :], PRM[:, 1, :], PRM[:, 2, :]
        _n = [0]

        def wk():
            _n[0] += 1
            return Wk[:, _n[0] - 1, :]
        rk, wkk = ["PRM", "Wk"], ["Wk"]

        def vtt(out, a, b, op):
            V(lambda e: e.tensor_tensor(out=out, in0=a, in1=b, op=op), r=rk + ["PW", "Bt", "Ct", "BB"], w=wkk)

        def vts(out, a, s1, s2, op0, op1=None):
            if op1 is None:
                V(lambda e: e.tensor_scalar(out=out, in0=a, scalar1=s1, scalar2=None, op0=op0), r=rk, w=wkk)
            else:
                V(lambda e: e.tensor_scalar(out=out, in0=a, scalar1=s1, scalar2=s2, op0=op0, op1=op1), r=rk, w=wkk)

        def sin_turns(out, turns):
            tf, fr, m = wk(), wk(), wk()
            V(lambda e: e.tensor_copy(out=Wi[:], in_=turns), r=rk, w=["Wi"])
            V(lambda e: e.tensor_copy(out=tf, in_=Wi[:]), r=["Wi"], w=wkk)
            vtt(fr, turns, tf, ALU.subtract)
            vts(m, fr, 0.5, None, ALU.is_gt)
            vtt(fr, fr, m, ALU.subtract)
            vts(m, fr, -0.5, None, ALU.is_lt)
            vtt(fr, fr, m, ALU.add)
            A(lambda e: e.activation(out=out, in_=fr, func=AF.Sin, scale=TWO_PI), r=rk, w=wkk)

        dt_, th, turns, turnc, sn, cs, ld, mag = [wk() for _ in range(8)]
        A(lambda e: e.activation(out=dt_, in_=LD, func=AF.Exp), r=rk, w=wkk)
        vtt(th, LI, dt_, ALU.mult)
        vts(turns, th, 1.0 / TWO_PI, None, ALU.mult)
        vts(turnc, turns, 0.25, None, ALU.add)
        sin_turns(sn, turns)
        sin_turns(cs, turnc)
        vtt(ld, LR, dt_, ALU.mult)
        A(lambda e: e.activation(out=mag, in_=ld, func=AF.Exp), r=rk, w=wkk)
        ar, ai = PW[:, 8, 0, :], PW[:, 8, 1, :]
        V(lambda e: e.tensor_tensor(out=ar, in0=mag, in1=cs, op=ALU.mult), r=rk, w=["PW"])
        V(lambda e: e.tensor_tensor(out=ai, in0=mag, in1=sn, op=ALU.mult), r=rk, w=["PW"])
        _n[0] = 8
        den, t1, t2, nr, fre, fim, q = [wk() for _ in range(7)]
        vtt(den, LR, LR, ALU.mult)
        vtt(t1, LI, LI, ALU.mult)
        vtt(den, den, t1, ALU.add)
        V(lambda e: e.reciprocal(out=den, in_=den), r=rk, w=wkk)
        V(lambda e: e.tensor_scalar(out=nr, in0=ar, scalar1=-1.0, scalar2=None, op0=ALU.add), r=rk + ["PW"], w=wkk)
        vtt(t1, nr, LR, ALU.mult)
        vtt(t2, ai, LI, ALU.mult)
        vtt(t1, t1, t2, ALU.add)
        vtt(fre, t1, den, ALU.mult)
        vtt(t1, ai, LR, ALU.mult)
        vtt(t2, nr, LI, ALU.mult)
        vtt(t1, t1, t2, ALU.subtract)
        vtt(fim, t1, den, ALU.mult)
        A(lambda e: e.activation(out=q, in_=ld, func=AF.Exp, scale=-2.0), r=rk, w=wkk)
        V(lambda e: e.memset(PW[:, 7, 0, :], 1.0), w=["PW"])
        V(lambda e: e.memset(PW[:, 7, 1, :], 0.0), w=["PW"])
        V(lambda e: e.tensor_tensor(out=PW[:, 6, 0, :], in0=ar, in1=q, op=ALU.mult), r=rk + ["PW"], w=["PW"])
        V(lambda e: e.scalar_tensor_tensor(out=PW[:, 6, 1, :], in0=ai, scalar=-1.0, in1=q, op0=ALU.mult, op1=ALU.mult),
          r=rk + ["PW"], w=["PW"])

        def cmul(o, x, y):
            a_, b_ = wk(), wk()
            _n[0] -= 2
            V(lambda e: e.tensor_tensor(out=a_, in0=PW[:, x, 0, :], in1=PW[:, y, 0, :], op=ALU.mult), r=["PW", "Wk"], w=wkk)
            V(lambda e: e.tensor_tensor(out=b_, in0=PW[:, x, 1, :], in1=PW[:, y, 1, :], op=ALU.mult), r=["PW", "Wk"], w=wkk)
            V(lambda e: e.tensor_tensor(out=PW[:, o, 0, :], in0=a_, in1=b_, op=ALU.subtract), r=["PW", "Wk"], w=["PW"])
            V(lambda e: e.tensor_tensor(out=a_, in0=PW[:, x, 0, :], in1=PW[:, y, 1, :], op=ALU.mult), r=["PW", "Wk"], w=wkk)
            V(lambda e: e.tensor_tensor(out=b_, in0=PW[:, x, 1, :], in1=PW[:, y, 0, :], op=ALU.mult), r=["PW", "Wk"], w=wkk)
            V(lambda e: e.tensor_tensor(out=PW[:, o, 1, :], in0=a_, in1=b_, op=ALU.add), r=["PW", "Wk"], w=["PW"])
        for k in range(2, 9):
            cmul(7 + k, 7 + k - 1, 8)
        for k in range(2, 8):
            cmul(7 - k, 7 - k + 1, 6)
        V(lambda e: e.tensor_copy(out=CA[:, 0, :], in_=PW[:, 15, 0, :]), r=["PW"], w=["CA"])
        V(lambda e: e.tensor_copy(out=CA[:, 1, :], in_=PW[:, 15, 0, :]), r=["PW"], w=["CA"])
        V(lambda e: e.tensor_copy(out=CBp[:], in_=PW[:, 15, 1, :]), r=["PW"], w=["CBp"])
        V(lambda e: e.tensor_scalar(out=CBn[:], in0=PW[:, 15, 1, :], scalar1=-1.0, scalar2=None, op0=ALU.mult), r=["PW"], w=["CBn"])
        V(lambda e: e.tensor_copy(out=A1[:], in_=PW[:, 8, :, :]), r=["PW"], w=["A1"])

        if STOP <= 2:
            return
        for i, nm in enumerate(["s5_b_re", "s5_b_im"]):
            for q4 in range(8):
                dma(Bt[:, i, 4 * q4:4 * q4 + 4, :], I[nm].rearrange("(pr gl) p c -> (gl p) pr c", gl=2)[:, 4 * q4:4 * q4 + 4, :],
                    w=["Bt"], add_writer=True)
        fre_b = bc(fre.unsqueeze(2), [128, 32, 16])
        fim_b = bc(fim.unsqueeze(2), [128, 32, 16])
        vtt(T3[:, 0], Bt[:, 0], fre_b, ALU.mult)
        vtt(T3[:, 1], Bt[:, 1], fim_b, ALU.mult)
        V(lambda e: e.tensor_tensor(out=BB[:, 0], in0=T3[:, 0], in1=T3[:, 1], op=ALU.subtract), r=["Wk"], w=["BB"])
        vtt(T3[:, 0], Bt[:, 1], fre_b, ALU.mult)
        vtt(T3[:, 1], Bt[:, 0], fim_b, ALU.mult)
        V(lambda e: e.tensor_tensor(out=BB[:, 1], in0=T3[:, 0], in1=T3[:, 1], op=ALU.add), r=["Wk"], w=["BB"])

        if STOP <= 2.3:
            return
        for i, nm in enumerate(["s5_c_re", "s5_c_im"]):
            cflat = I[nm]
            for k in range(8):
                for hh in range(2):
                    dma(craw[:, k % 2, hh, :], AP(tensor=cflat.tensor, offset=k * 128 * 64, ap=[[64, 128], [1, 64]]),
                        w=["craw%d" % (k % 2)], add_writer=(hh == 1))
                P(lambda e: e.transpose(out=pc[k % 2][:, 0:128], in_=craw[:, k % 2, :, :], identity=idf[:]),
                  r=["craw%d" % (k % 2), "idf"], w=["pc%d" % (k % 2)])
                for gl in range(2):
                    sl = slice(64 * gl, 64 * gl + 64)
                    V(lambda e: e.tensor_copy(
                        out=Ct[sl, i, 4 * k:4 * k + 4, :],
                        in_=pc[k % 2][sl, 0:128].rearrange("p (pr gl c) -> p pr gl c", gl=2, c=16)[:, :, gl, :]),
                      r=["pc%d" % (k % 2)], w=["Ct"])

        if STOP <= 2.5:
            return
        Uv = Ubf[:].rearrange("p pr r (s c) -> p pr r s c", c=16)
        for s in range(8):
            pi = 14 - s
            Pr = bc(PW[:, pi, 0, :].unsqueeze(2), [128, 32, 16])
            Pi = bc(PW[:, pi, 1, :].unsqueeze(2), [128, 32, 16])
            vtt(T3[:, 0], BB[:, 0], Pr, ALU.mult)
            vtt(T3[:, 1], BB[:, 1], Pi, ALU.mult)
            V(lambda e: e.tensor_tensor(out=Uv[:, :, 0, s, :], in0=T3[:, 0], in1=T3[:, 1], op=ALU.subtract), r=["Wk"], w=["Ubf"])
            vtt(T3[:, 2], BB[:, 1], Pr, ALU.mult)
            vtt(T3[:, 3], BB[:, 0], Pi, ALU.mult)
            V(lambda e: e.tensor_tensor(out=Uv[:, :, 1, s, :], in0=T3[:, 2], in1=T3[:, 3], op=ALU.add), r=["Wk"], w=["Ubf"])

        if STOP <= 2.7:
            return
        G(lambda e: e.memset(Vpad[:], 0.0), w=["Vpad"])
        G(lambda e: e.memset(Win[:], 0.0), w=["Win"])
        Vv = Vpad[:].rearrange("p pr g r (s c) -> p pr g r s c", c=16)
        Wv = Wout[:].rearrange("p pr r (s c) -> p pr r s c", c=16)
        for s in range(8):
            for which in range(2):
                pi = s if which == 0 else s + 8
                Pr = bc(PW[:, pi, 0, :].unsqueeze(2), [128, 32, 16])
                Pi = bc(PW[:, pi, 1, :].unsqueeze(2), [128, 32, 16])
                vtt(T3[:, 0], Ct[:, 0], Pr, ALU.mult)
                vtt(T3[:, 1], Ct[:, 1], Pi, ALU.mult)
                vtt(T3[:, 0], T3[:, 0], T3[:, 1], ALU.subtract)
                vtt(T3[:, 2], Ct[:, 0], Pi, ALU.mult)
                vtt(T3[:, 3], Ct[:, 1], Pr, ALU.mult)
                vtt(T3[:, 2], T3[:, 2], T3[:, 3], ALU.add)
                if which == 0:
                    for gl in range(2):
                        sl = slice(64 * gl, 64 * gl + 64)
                        V(lambda e: e.tensor_copy(out=Vv[sl, :, gl, 0, s, :], in_=T3[sl, 0]), r=["Wk"], w=["Vpad"])
                        V(lambda e: e.tensor_scalar(out=Vv[sl, :, gl, 1, s, :], in0=T3[sl, 2], scalar1=-1.0, scalar2=None,
                                                    op0=ALU.mult), r=["Wk"], w=["Vpad"])
                else:
                    V(lambda e: e.tensor_copy(out=Wv[:, :, 0, s, :], in_=T3[:, 0]), r=["Wk"], w=["Wout"])
                    V(lambda e: e.tensor_scalar(out=Wv[:, :, 1, s, :], in0=T3[:, 2], scalar1=-1.0, scalar2=None, op0=ALU.mult),
                      r=["Wk"], w=["Wout"])

        if STOP <= 2.9:
            return
        G(lambda e: e.memset(maskLT[:], 1.0), w=["maskLT"])
        G(lambda e: e.affine_select(out=maskLT[:].rearrange("p (s c) -> p s c", c=16), in_=maskLT[:].rearrange("p (s c) -> p s c", c=16),
                                    pattern=[[16, 8], [0, 16]], compare_op=ALU.is_ge, fill=0.0, base=15, channel_multiplier=-1),
          r=["maskLT"], w=["maskLT"])

        if STOP <= 3:
            return
        for pr in range(32):
            for gl in range(2):
                g = 2 * pr + gl
                pmx = pm[g % 2]
                key = "pm%d" % (g % 2)
                P(lambda e: e.matmul(pmx[:, 0:128], lhsT=Ubf[:, pr, 0, :], rhs=Vpad[:, pr, gl, 0, :], start=True, stop=False),
                  r=["Ubf", "Vpad"], w=[key])
                P(lambda e: e.matmul(pmx[:, 0:128], lhsT=Ubf[:, pr, 1, :], rhs=Vpad[:, pr, gl, 1, :], start=False, stop=True),
                  r=["Ubf", "Vpad"], w=[key])
                V(lambda e: e.tensor_tensor(out=Mint[:, g, :], in0=pmx[:, 0:128], in1=maskLT[:], op=ALU.mult), r=[key, "maskLT"], w=["Mint"])
            for ri in range(2):
                pwx = pw[ri]
                key = "pw%d" % ri
                P(lambda e: e.transpose(out=pwx[:, 0:128], in_=Ubf[:, pr, ri, :], identity=idb[:]), r=["Ubf", "idb"], w=[key])
                A(lambda e: e.copy(out=Win[:, pr, 0, ri, 0:64], in_=pwx[:, 0:64]), r=[key], w=["Win"])
                A(lambda e: e.copy(out=Win[:, pr, 1, ri, 64:128], in_=pwx[:, 64:128]), r=[key], w=["Win"])

    if STOP <= 4:
        return
    with ExitStack() as ses:
        NCH = 64
        xh = K.sb(ses, "s5xh", [NCH, 4, D], F32)
        junk = K.sb(ses, "s5junk", [NCH, D], F32)
        useg = K.sb(ses, "s5useg", [NCH, 64, 8, 16], BF16)
        uT = K.sb(ses, "s5uT", [128, 64, NCH], BF16)
        X = K.sb(ses, "s5X", [128, 2, 32, NCH], F32)
        Hbf = K.sb(ses, "s5H", [128, 2, 32, NCH + 1], BF16)
        carry = K.sb(ses, "s5carry", [128, 2, 32], F32)
        t1 = K.sb(ses, "s5t1", [128, 2, 32], F32)
        t2 = K.sb(ses, "s5t2", [128, 2, 32], F32)
        gtok = K.sb(ses, "s5gtok", [NCH, 8, D], BF16)
        ysb = K.sb(ses, "s5ysb", [128, 8, NCH], F32)
        yc = K.sb(ses, "s5yc", [NCH, 8, 128], F32)
        yc2 = K.sb(ses, "s5yc2", [NCH, 8, 128], F32)
        ss = K.sb(ses, "s5ss", [NCH, 8], F32)
        ptr = [K.ps(ses, "s5ptr%d" % i, [128, 16, NCH], BF16) for i in range(2)]
        px = [K.ps(ses, "s5px%d" % i, [128, 4, 2, NCH], F32) for i in range(2)]
        py = K.ps(ses, "s5py", [128, 8, NCH], F32)
        pyt = [K.ps(ses, "s5pyt%d" % i, [NCH, 4, 128], F32) for i in range(2)]

        V(lambda e: e.memset(carry[:], 0.0), w=["carry"])
        xp = I["xp"]
        for seg in range(T // (8 * NCH)):
            rows = xp[seg * 8 * NCH:(seg + 1) * 8 * NCH, :].rearrange("(n s) d -> n s d", s=8)
            for h in range(2):
                dma(xh[:], rows[:, 4 * h:4 * h + 4, :], w=["xh"])
                for s in range(4):
                    A(lambda e: e.activation(out=junk[:], in_=xh[:, s, :], func=AF.Square, accum_out=ss[:, 4 * h + s:4 * h + s + 1]),
                      r=["xh"], w=["junk", "ss"])
                V(lambda e: e.tensor_scalar(out=ss[:, 4 * h:4 * h + 4], in0=ss[:, 4 * h:4 * h + 4], scalar1=1.0 / D, scalar2=EPS,
                                            op0=ALU.mult, op1=ALU.add), r=["ss"], w=["ss"])
                A(lambda e: e.activation(out=ss[:, 4 * h:4 * h + 4], in_=ss[:, 4 * h:4 * h + 4], func=AF.Sqrt), r=["ss"], w=["ss"])
                V(lambda e: e.reciprocal(out=ss[:, 4 * h:4 * h + 4], in_=ss[:, 4 * h:4 * h + 4]), r=["ss"], w=["ss"])
                for s in range(4):
                    V(lambda e: e.scalar_tensor_tensor(out=useg[:, :, 4 * h + s, :], in0=xh[:, s, :].rearrange("n (g c) -> n g c", c=16),
                                                       scalar=ss[:, 4 * h + s:4 * h + s + 1],
                                                       in1=gain0[0:NCH, :].rearrange("n (g c) -> n g c", c=16), op0=ALU.mult, op1=ALU.mult),
                      r=["xh", "ss", "gain0"], w=["useg"])
            if STOP <= 5:
                return
            for g8 in range(8):
                pt_ = ptr[g8 % 2]
                key = "ptr%d" % (g8 % 2)
                for j in range(8):
                    g = 8 * g8 + j
                    P(lambda e: e.transpose(out=pt_[:, j, :], in_=useg[:, g, :, :], identity=idb[0:NCH, 0:NCH]),
                      r=["useg", "idb"], w=[key])
                A(lambda e: e.copy(out=uT[:, 8 * g8:8 * g8 + 8, :], in_=pt_[:, 0:8, :]), r=[key], w=["uT"])
            if STOP <= 6:
                return
            for q in range(8):
                px_ = px[q % 2]
                key = "px%d" % (q % 2)
                for a in range(4):
                    pr = 4 * q + a
                    for ri in range(2):
                        P(lambda e: e.matmul(px_[:, a, ri, :], lhsT=Win[:, pr, 0, ri, :], rhs=uT[:, 2 * pr, :], start=True, stop=False),
                          r=["Win", "uT"], w=[key])
                        P(lambda e: e.matmul(px_[:, a, ri, :], lhsT=Win[:, pr, 1, ri, :], rhs=uT[:, 2 * pr + 1, :], start=False, stop=True),
                          r=["Win", "uT"], w=[key])
                A(lambda e: e.copy(out=X[:, :, 4 * q:4 * q + 4, :], in_=px_[:].rearrange("p a r n -> p r a n")), r=[key], w=["X"])
            if STOP <= 7:
                return
            V(lambda e: e.tensor_copy(out=Hbf[:, :, :, 0], in_=carry[:]), r=["carry"], w=["Hbf"])
            for n in range(NCH):
                Sp = carry[:] if n == 0 else X[:, :, :, n - 1]
                V(lambda e: e.tensor_tensor(out=t1[:], in0=Sp, in1=CA[:], op=ALU.mult), r=["X", "carry", "CA"], w=["t1"])
                V(lambda e: e.tensor_tensor(out=t2[:, 0, :], in0=Sp[:, 1, :], in1=CBn[:], op=ALU.mult), r=["X", "carry", "CBn"], w=["t2"])
                V(lambda e: e.tensor_tensor(out=t2[:, 1, :], in0=Sp[:, 0, :], in1=CBp[:], op=ALU.mult), r=["X", "carry", "CBp"], w=["t2"])
                V(lambda e: e.tensor_tensor(out=t1[:], in0=t1[:], in1=t2[:], op=ALU.add), r=["t1", "t2"], w=["t1"])
                V(lambda e: e.tensor_tensor(out=X[:, :, :, n], in0=X[:, :, :, n], in1=t1[:], op=ALU.add), r=["t1", "X"], w=["X"])
            V(lambda e: e.tensor_copy(out=carry[:], in_=X[:, :, :, NCH - 1]), r=["X"], w=["carry"])
            A(lambda e: e.copy(out=Hbf[:, :, :, 1:NCH + 1], in_=X[:]), r=["X"], w=["Hbf"])
            if STOP <= 8:
                return
            for g8 in range(8):
                for j in range(8):
                    g = 8 * g8 + j
                    pr, gl = g // 2, g % 2
                    sl = slice(64 * gl, 64 * gl + 64)
                    P(lambda e: e.matmul(py[:, j, :], lhsT=Mint[:, g, :], rhs=uT[:, g, :], start=True, stop=False),
                      r=["Mint", "uT"], w=["py"])
                    P(lambda e: e.matmul(py[:, j, :], lhsT=Wout[sl, pr, 0, :], rhs=Hbf[sl, 0, pr, 0:NCH], start=False, stop=False),
                      r=["Wout", "Hbf"], w=["py"])
                    P(lambda e: e.matmul(py[:, j, :], lhsT=Wout[sl, pr, 1, :], rhs=Hbf[sl, 1, pr, 0:NCH], start=False, stop=True),
                      r=["Wout", "Hbf"], w=["py"])
                A(lambda e: e.copy(out=ysb[:], in_=py[:]), r=["py"], w=["ysb"])
                for j in range(8):
                    pyt_ = pyt[j // 4]
                    P(lambda e: e.transpose(out=pyt_[:, j % 4, :], in_=ysb[:, j, :], identity=idf[:]), r=["ysb", "idf"], w=["pyt%d" % (j // 4)])
                cs_ = slice(128 * g8, 128 * g8 + 128)
                for hh in range(2):
                    V(lambda e: e.tensor_copy(
                        out=yc[:, :, 64 * hh:64 * hh + 64].rearrange("n s (j c) -> n s j c", c=16),
                        in_=pyt[hh][:].rearrange("n j (s c) -> n s j c", c=16)), r=["pyt%d" % hh], w=["yc"])
                V(lambda e: e.tensor_tensor(out=yc2[:].rearrange("n s (j c) -> n s j c", c=16),
                                            in0=useg[:, 8 * g8:8 * g8 + 8, :, :].rearrange("n j s c -> n s j c"),
                                            in1=bc(dskip[0:NCH, cs_].unsqueeze(1), [NCH, 8, 128]).rearrange("n s (j c) -> n s j c", c=16), op=ALU.mult),
                  r=["useg", "dskip"], w=["yc2"])
                V(lambda e: e.tensor_tensor(out=yc[:], in0=yc[:], in1=yc2[:], op=ALU.add), r=["yc", "yc2"], w=["yc"])
                V(lambda e: e.tensor_tensor(out=yc2[:], in0=yc[:], in1=yc[:], op=ALU.mult), r=["yc"], w=["yc2"])
                V(lambda e: e.tensor_scalar(out=yc2[:], in0=yc2[:], scalar1=0.044715, scalar2=1.0, op0=ALU.mult, op1=ALU.add), r=["yc2"], w=["yc2"])
                V(lambda e: e.tensor_tensor(out=yc2[:], in0=yc2[:], in1=yc[:], op=ALU.mult), r=["yc", "yc2"], w=["yc2"])
                A(lambda e: e.activation(out=yc2[:], in_=yc2[:], func=AF.Sigmoid, scale=1.5957691216057308), r=["yc2"], w=["yc2"])
                V(lambda e: e.tensor_tensor(out=gtok[:, :, cs_], in0=yc[:], in1=yc2[:], op=ALU.mult), r=["yc", "yc2"], w=["gtok"])
            dma(O["g_scr"][seg * 8 * NCH:(seg + 1) * 8 * NCH, :].rearrange("(n s) d -> n s d", s=8), gtok[:], r=["gtok"], w=["g_scr"],
                add_writer=True)
        stT = K.sb(ses, "s5stT", [32, 2, 128], F32)
        for ri, nm in enumerate(["s5_re_p", "s5_im_p"]):
            P(lambda e: e.transpose(out=py[0:32, ri, :].rearrange("p n -> p n") if False else pyt[0][0:32, ri, :], in_=carry[:, ri, :], identity=idf[:]),
              r=["carry", "idf"], w=["pyt0"])
            V(lambda e: e.tensor_copy(out=stT[:, ri, :], in_=pyt[0][0:32, ri, :]), r=["pyt0"], w=["stT"])
            dma(O[nm].rearrange("(pr gl) p -> pr (gl p)", gl=2), stT[:, ri, :], r=["stT"], w=[nm])


    K.S.barrier()
    _s5_sample(K, C, I, O, Win, Wout, Mint, A1, gain0, dskip)


def gelu_tanh(K, y, ykey, tmp, tkey, out, okey):
    V, A = K.V, K.A
    V(lambda e: e.tensor_tensor(out=tmp, in0=y, in1=y, op=ALU.mult), r=[ykey], w=[tkey])
    V(lambda e: e.tensor_scalar(out=tmp, in0=tmp, scalar1=0.044715, scalar2=1.0, op0=ALU.mult, op1=ALU.add), r=[tkey], w=[tkey])
    V(lambda e: e.tensor_tensor(out=tmp, in0=tmp, in1=y, op=ALU.mult), r=[ykey, tkey], w=[tkey])
    A(lambda e: e.activation(out=tmp, in_=tmp, func=AF.Sigmoid, scale=1.5957691216057308), r=[tkey], w=[tkey])
    V(lambda e: e.tensor_tensor(out=out, in0=y, in1=tmp, op=ALU.mult), r=[ykey, tkey], w=[okey])


def _s5_sample(K, C, I, O, Win, Wout, Mint, A1, gain0, dskip):
    idf, idb = C["idf"], C["idb"]
    V, A, G, P, dma = K.V, K.A, K.G, K.P, K.dma
    N = NS
    with ExitStack() as es:
        xs = K.sb(es, "ss_x", [N, D], F32)
        ss = K.sb(es, "ss_ss", [N, 1], F32)
        us = K.sb(es, "ss_us", [N, D], F32)
        u0 = K.sb(es, "ss_u0", [N, 64, 8, 16], BF16)
        uT0 = K.sb(es, "ss_uT0", [128, 64, N], BF16)
        uT7 = K.sb(es, "ss_uT7", [128, 64, N], BF16)
        st = K.sb(es, "ss_st", [N, 2, 4096], F32)
        stO = st
        H0 = K.sb(es, "ss_H0", [128, 2, 32, N], F32)
        H0b = K.sb(es, "ss_H0b", [128, 2, 32, N], BF16)
        X7 = K.sb(es, "ss_X7", [128, 2, 32, N], F32)
        H1 = K.sb(es, "ss_H1", [128, 2, 32, N], F32)
        t1 = K.sb(es, "ss_t1", [128, 2, 32, N], F32)
        t2 = K.sb(es, "ss_t2", [128, 2, 32, N], F32)
        ys = K.sb(es, "ss_ys", [N, D], F32)
        y2 = K.sb(es, "ss_y2", [N, D], F32)
        junk = y2
        gsb = K.sb(es, "ss_g", [N, D], BF16)
        ptrA = K.ps(es, "ss_ptrA", [128, 64, N], BF16)
        ptrB = K.ps(es, "ss_ptrB", [128, 64, N], BF16)
        pxs = [K.ps(es, "ss_px%d" % i, [128, 16, 2, N], F32) for i in range(2)]
        ph = [K.ps(es, "ss_ph%d" % i, [128, 32, N], F32) for i in range(2)]
        pys = [K.ps(es, "ss_py%d" % i, [N, 32, 16], F32) for i in range(2)]

        dma(xs[:], I["xs"], w=["sxs"])
        dma(st[:, 0, :], I["st_re"], w=["sst"])
        dma(st[:, 1, :], I["st_im"], w=["sst"], add_writer=True)
        A(lambda e: e.activation(out=junk[:], in_=xs[:], func=AF.Square, accum_out=ss[:, 0:1]), r=["sxs"], w=["sy2", "sss"])
        V(lambda e: e.tensor_scalar(out=ss[:], in0=ss[:], scalar1=1.0 / D, scalar2=EPS, op0=ALU.mult, op1=ALU.add), r=["sss"], w=["sss"])
        A(lambda e: e.activation(out=ss[:], in_=ss[:], func=AF.Sqrt), r=["sss"], w=["sss"])
        V(lambda e: e.reciprocal(out=ss[:], in_=ss[:]), r=["sss"], w=["sss"])
        V(lambda e: e.scalar_tensor_tensor(out=us[:], in0=xs[:], scalar=ss[:, 0:1], in1=gain0[0:N, :], op0=ALU.mult, op1=ALU.mult),
          r=["sxs", "sss", "gain0"], w=["sus"])
        usv = us[:].rearrange("n (g c) -> n g c", c=16)
        G(lambda e: e.memset(u0[:], 0.0), w=["su0"])
        V(lambda e: e.tensor_copy(out=u0[:, :, 0, :], in_=usv), r=["sus", "su0"], w=["su0"])
        for g in range(64):
            P(lambda e: e.transpose(out=ptrA[:, g, :], in_=u0[:, g, :, :], identity=idb[0:N, 0:N]), r=["su0", "idb"], w=["sptrA"])
        G(lambda e: e.memset(u0[:], 0.0), w=["su0"])
        V(lambda e: e.tensor_copy(out=u0[:, :, 7, :], in_=usv), r=["sus", "su0"], w=["su0"])
        for g in range(64):
            P(lambda e: e.transpose(out=ptrB[:, g, :], in_=u0[:, g, :, :], identity=idb[0:N, 0:N]), r=["su0", "idb"], w=["sptrB"])
        A(lambda e: e.copy(out=uT0[:], in_=ptrA[:]), r=["sptrA"], w=["suT0"])
        A(lambda e: e.copy(out=uT7[:], in_=ptrB[:]), r=["sptrB"], w=["suT7"])
        for pr in range(32):
            h_, a_ = pr // 16, pr % 16
            for ri in range(2):
                P(lambda e: e.matmul(pxs[h_][:, a_, ri, :], lhsT=Win[:, pr, 0, ri, :], rhs=uT7[:, 2 * pr, :], start=True, stop=False),
                  r=["Win", "suT7"], w=["spx%d" % h_])
                P(lambda e: e.matmul(pxs[h_][:, a_, ri, :], lhsT=Win[:, pr, 1, ri, :], rhs=uT7[:, 2 * pr + 1, :], start=False, stop=True),
                  r=["Win", "suT7"], w=["spx%d" % h_])
        for h_ in range(2):
            A(lambda e: e.copy(out=X7[:, :, 16 * h_:16 * h_ + 16, :], in_=pxs[h_][:].rearrange("p a r n -> p r a n")), r=["spx%d" % h_], w=["sX7"])
        for ri in range(2):
            for pr in range(32):
                P(lambda e: e.transpose(out=ph[ri][:, pr, :], in_=st[:, ri, pr * 128:(pr + 1) * 128], identity=idf[0:N, 0:N]),
                  r=["sst", "idf"], w=["sph%d" % ri])
            V(lambda e: e.tensor_copy(out=H0[:, ri, :, :], in_=ph[ri][:]), r=["sph%d" % ri], w=["sH0"])
        A(lambda e: e.copy(out=H0b[:], in_=H0[:]), r=["sH0"], w=["sH0b"])
        a1r = bc(A1[:, 0, :].unsqueeze(2), [128, 32, N])
        a1i = bc(A1[:, 1, :].unsqueeze(2), [128, 32, N])
        for ri in range(2):
            V(lambda e: e.tensor_tensor(out=t1[:, ri], in0=H0[:, ri], in1=a1r, op=ALU.mult), r=["sH0", "A1"], w=["st1"])
            V(lambda e: e.tensor_tensor(out=t2[:, ri], in0=H0[:, 1 - ri], in1=a1i, op=ALU.mult), r=["sH0", "A1"], w=["st2"])
        V(lambda e: e.tensor_tensor(out=H1[:, 0], in0=t1[:, 0], in1=t2[:, 0], op=ALU.subtract), r=["st1", "st2"], w=["sH1"])
        V(lambda e: e.tensor_tensor(out=H1[:, 1], in0=t1[:, 1], in1=t2[:, 1], op=ALU.add), r=["st1", "st2"], w=["sH1"])
        V(lambda e: e.tensor_tensor(out=H1[:], in0=H1[:], in1=X7[:], op=ALU.add), r=["sH1", "sX7"], w=["sH1"])
        idx = 0
        for ri in range(2):
            for q in range(8):
                b_ = idx % 2
                idx += 1
                pv = pxs[b_][0:N].rearrange("p a r n -> p (a r n)")
                for j in range(4):
                    pr = 4 * q + j
                    P(lambda e: e.transpose(out=pv[:, j * 128:(j + 1) * 128], in_=H1[:, ri, pr, :], identity=idf[:]),
                      r=["sH1", "idf"], w=["spx%d" % b_])
                V(lambda e: e.tensor_copy(out=stO[:, ri, 512 * q:512 * q + 512], in_=pv[:, 0:512]), r=["spx%d" % b_], w=["sst"])
        dma(O["s5_re_s"], stO[:, 0, :], r=["sst"], w=["s5_re_s"])
        dma(O["s5_im_s"], stO[:, 1, :], r=["sst"], w=["s5_im_s"])
        for g in range(64):
            pr, gl = g // 2, g % 2
            sl = slice(64 * gl, 64 * gl + 64)
            h_ = g // 32
            P(lambda e: e.matmul(pys[h_][:, g % 32, :], lhsT=uT0[:, g, :], rhs=Mint[:, g, 0:16], start=True, stop=False),
              r=["suT0", "Mint"], w=["spy%d" % h_])
            P(lambda e: e.matmul(pys[h_][:, g % 32, :], lhsT=H0b[sl, 0, pr, :], rhs=Wout[sl, pr, 0, 0:16], start=False, stop=False),
              r=["sH0b", "Wout"], w=["spy%d" % h_])
            P(lambda e: e.matmul(pys[h_][:, g % 32, :], lhsT=H0b[sl, 1, pr, :], rhs=Wout[sl, pr, 1, 0:16], start=False, stop=True),
              r=["sH0b", "Wout"], w=["spy%d" % h_])
        V(lambda e: e.tensor_tensor(out=y2[:], in0=us[:], in1=dskip[0:N, :], op=ALU.mult), r=["sus", "dskip"], w=["sy2"])
        for h_ in range(2):
            V(lambda e: e.tensor_tensor(out=ys[:, 512 * h_:512 * h_ + 512], in0=pys[h_][:].rearrange("n g c -> n (g c)"),
                                        in1=y2[:, 512 * h_:512 * h_ + 512], op=ALU.add), r=["spy%d" % h_, "sy2"], w=["sys"])
        gelu_tanh(K, ys[:], "sys", y2[:], "sy2", gsb[:], "sgsb")
        dma(O["gs_scr"], gsb[:], r=["sgsb"], w=["gs_scr"])
        V(lambda e: e.tensor_tensor(out=ys[:], in0=ys[:], in1=y2[:], op=ALU.mult), r=["sys", "sy2", "sgsb"], w=["sys"])
        dma(O["gs32_scr"], ys[:], r=["sys"], w=["gs32_scr"])

def load_w_bf16(K, wt, src, ncols, step=1024):
    v = src.rearrange("(k p) n -> p k n", p=128)
    for c0 in range(0, ncols, step):
        c1 = min(ncols, c0 + step)
        K.S.dma("pool", wt[:, :, c0:c1], v[:, :, c0:c1], (), ["w_" + wt.name], add_writer=(c0 > 0))


def rms_to_bf16(K, x_t, xkey, gain, h_t, hkey, junk, ss, sfx, Pn=128):
    V, A = K.V, K.A
    A(lambda e: e.activation(out=junk[0:Pn, :], in_=x_t[0:Pn, :], func=AF.Square, accum_out=ss[0:Pn, 0:1]), r=[xkey], w=["junk" + sfx, "ss" + sfx])
    V(lambda e: e.tensor_scalar(out=ss[0:Pn, 0:1], in0=ss[0:Pn, 0:1], scalar1=1.0 / D, scalar2=EPS, op0=ALU.mult, op1=ALU.add), r=["ss" + sfx], w=["ss" + sfx])
    A(lambda e: e.activation(out=ss[0:Pn, 0:1], in_=ss[0:Pn, 0:1], func=AF.Sqrt), r=["ss" + sfx], w=["ss" + sfx])
    V(lambda e: e.reciprocal(out=ss[0:Pn, 0:1], in_=ss[0:Pn, 0:1]), r=["ss" + sfx], w=["ss" + sfx])
    V(lambda e: e.scalar_tensor_tensor(out=h_t[0:Pn, :], in0=x_t[0:Pn, :], scalar=ss[0:Pn, 0:1], in1=gain[0:Pn, :], op0=ALU.mult, op1=ALU.mult),
      r=[xkey, "ss" + sfx, "gain"], w=[hkey])


def transpose_tiles(K, src, skey, nk, dstT, dkey, ptb, idb, Pn=128):
    for k0 in range(0, nk, 8):
        k1 = min(nk, k0 + 8)
        for k in range(k0, k1):
            K.P(lambda e: e.transpose(out=ptb[:, k - k0, 0:Pn], in_=src[0:Pn, k * 128:(k + 1) * 128], identity=idb[0:Pn, 0:Pn]),
                r=[skey, "idb"], w=["ptb"])
        K.A(lambda e: e.copy(out=dstT[:, k0:k1, 0:Pn], in_=ptb[:, 0:k1 - k0, 0:Pn]), r=["ptb"], w=[dkey])


NTILE = T // 128


def tile_rows(t):
    return (128, slice(t * 128, (t + 1) * 128)) if t < NTILE else (NS, None)


def pick(big, small, t):
    Pn, rs = tile_rows(t)
    return big[rs, :] if rs is not None else small


def glu_stage(K, C, I, O):
    idb = C["idb"]
    V, A, G, P, dma = K.V, K.A, K.G, K.P, K.dma
    with ExitStack() as es:
        wg = K.sb(es, "wglu", [128, 8, 2048], BF16)
        load_w_bf16(K, wg, I["s5_w_glu"], 2048)
        gt = [K.sb(es, "glu_g%d" % i, [128, D], BF16) for i in range(2)]
        xt = [K.sb(es, "glu_x%d" % i, [128, D], F32) for i in range(2)]
        gT = [K.sb(es, "glu_gT%d" % i, [128, 8, 128], BF16) for i in range(2)]
        sg = [K.sb(es, "glu_sg%d" % i, [128, D], F32) for i in range(2)]
        ptb = K.ps(es, "glu_ptb", [128, 8, 128], BF16)
        pa = [K.ps(es, "glu_pa%d" % i, [128, 512], F32) for i in range(4)]
        for t in range(NTILE):
            b = t % 2
            sx = str(b)
            Pn, _ = tile_rows(t)
            dma(gt[b][0:Pn, :], pick(O["g_scr"], O["gs_scr"], t), r=["g_scr", "gs_scr"], w=["gt" + sx])
            dma(xt[b][0:Pn, :], pick(I["xp"], I["xs"], t), w=["xt" + sx])
            transpose_tiles(K, gt[b], "gt" + sx, 8, gT[b], "gT" + sx, ptb, idb, Pn)
            for c in range(4):
                for k in range(8):
                    P(lambda e: e.matmul(pa[c][0:Pn, :], lhsT=gT[b][:, k, 0:Pn], rhs=wg[:, k, c * 512:(c + 1) * 512], start=(k == 0), stop=(k == 7)),
                      r=["gT" + sx, "w_wglu"], w=["pa%d" % c])
            for hh in range(2):
                cs = slice(512 * hh, 512 * hh + 512)
                A(lambda e: e.activation(out=sg[b][0:Pn, cs], in_=pa[2 + hh][0:Pn, :], func=AF.Sigmoid), r=["pa%d" % (2 + hh)], w=["sg" + sx])
                V(lambda e: e.tensor_tensor(out=sg[b][0:Pn, cs], in0=pa[hh][0:Pn, :], in1=sg[b][0:Pn, cs], op=ALU.mult), r=["pa%d" % hh, "sg" + sx], w=["sg" + sx])
            V(lambda e: e.tensor_tensor(out=xt[b][0:Pn, :], in0=xt[b][0:Pn, :], in1=sg[b][0:Pn, :], op=ALU.add), r=["xt" + sx, "sg" + sx], w=["xt" + sx])
            dma(pick(O["y1_scr"], O["y1s_scr"], t), xt[b][0:Pn, :], r=["xt" + sx], w=["y1_scr"], add_writer=True)
    K.S.barrier()


def ffn_stage(K, C, I, O):
    idb = C["idb"]
    V, A, G, P, dma = K.V, K.A, K.G, K.P, K.dma
    FF = 2816
    CW = 352
    with ExitStack() as es:
        w1 = K.sb(es, "ffw1", [128, 8, 2 * FF], BF16)
        w2 = K.sb(es, "ffw2", [128, 22, D], BF16)
        load_w_bf16(K, w1, I["ffn_w_in"], 2 * FF)
        load_w_bf16(K, w2, I["ffn_w_out"], D)
        gain = K.sb(es, "ff_gain", [128, D], F32)
        dma(gain[:], AP(tensor=I["norm_ffn"].tensor, offset=0, ap=[[0, 128], [1, D]]), w=["gain"])
        yt = [K.sb(es, "ff_y%d" % i, [128, D], F32) for i in range(2)]
        hb = [K.sb(es, "ff_h%d" % i, [128, D], BF16) for i in range(2)]
        hT = [K.sb(es, "ff_hT%d" % i, [128, 8, 128], BF16) for i in range(2)]
        act = [K.sb(es, "ff_act%d" % i, [128, FF], BF16) for i in range(2)]
        actT = [K.sb(es, "ff_actT%d" % i, [128, 22, 128], BF16) for i in range(2)]
        sl_ = [K.sb(es, "ff_s%d" % i, [128, CW], F32) for i in range(2)]
        junk = K.sb(es, "ff_junk", [128, D], F32)
        ss = [K.sb(es, "ff_ss%d" % i, [128, 1], F32) for i in range(2)]
        ptb = K.ps(es, "ff_ptb", [128, 8, 128], BF16)
        pa = [K.ps(es, "ff_pa%d" % i, [128, 512], F32) for i in range(2)]
        pb = [K.ps(es, "ff_pb%d" % i, [128, 512], F32) for i in range(2)]
        po = [K.ps(es, "ff_po%d" % i, [128, 512], F32) for i in range(2)]
        for t in range(NTILE):
            b = t % 2
            sx = str(b)
            Pn, _ = tile_rows(t)
            dma(yt[b][0:Pn, :], pick(O["y1_scr"], O["y1s_scr"], t), r=["y1_scr"], w=["yt" + sx])
            rms_to_bf16(K, yt[b], "yt" + sx, gain, hb[b], "hb" + sx, junk, ss[b], sx, Pn)
            transpose_tiles(K, hb[b], "hb" + sx, 8, hT[b], "hT" + sx, ptb, idb, Pn)
            for j in range(8):
                jb = j % 2
                for k in range(8):
                    P(lambda e: e.matmul(pa[jb][0:Pn, 0:CW], lhsT=hT[b][:, k, 0:Pn], rhs=w1[:, k, CW * j:CW * j + CW], start=(k == 0), stop=(k == 7)),
                      r=["hT" + sx, "w_ffw1"], w=["fpa%d" % jb])
                for k in range(8):
                    P(lambda e: e.matmul(pb[jb][0:Pn, 0:CW], lhsT=hT[b][:, k, 0:Pn], rhs=w1[:, k, FF + CW * j:FF + CW * j + CW], start=(k == 0), stop=(k == 7)),
                      r=["hT" + sx, "w_ffw1"], w=["fpb%d" % jb])
                A(lambda e: e.activation(out=sl_[jb][0:Pn, :], in_=pa[jb][0:Pn, 0:CW], func=AF.Silu), r=["fpa%d" % jb], w=["fsl%d" % jb])
                V(lambda e: e.tensor_tensor(out=act[b][0:Pn, CW * j:CW * j + CW], in0=pb[jb][0:Pn, 0:CW], in1=sl_[jb][0:Pn, :], op=ALU.mult),
                  r=["fpb%d" % jb, "fsl%d" % jb], w=["act" + sx])
            transpose_tiles(K, act[b], "act" + sx, 22, actT[b], "actT" + sx, ptb, idb, Pn)
            for c in range(2):
                for k in range(22):
                    P(lambda e: e.matmul(po[c][0:Pn, :], lhsT=actT[b][:, k, 0:Pn], rhs=w2[:, k, c * 512:(c + 1) * 512], start=(k == 0), stop=(k == 21)),
                      r=["actT" + sx, "w_ffw2"], w=["fpo%d" % c])
                V(lambda e: e.tensor_tensor(out=yt[b][0:Pn, c * 512:(c + 1) * 512], in0=po[c][0:Pn, :], in1=yt[b][0:Pn, c * 512:(c + 1) * 512], op=ALU.add),
                  r=["fpo%d" % c, "yt" + sx], w=["yt" + sx])
            dma(pick(O["y2_scr"], O["y2s_scr"], t), yt[b][0:Pn, :], r=["yt" + sx], w=["y2_scr"], add_writer=True)
    K.S.barrier()


def nsa_proj_stage(K, C, I, O):
    idb = C["idb"]
    V, A, G, P, dma = K.V, K.A, K.G, K.P, K.dma
    NCOL = 2608
    with ExitStack() as es:
        wn = K.sb(es, "nsw", [128, 8, NCOL], BF16)
        load_w_bf16(K, wn, I["nsa_w_in"], NCOL)
        gain = K.sb(es, "ns_gain", [128, D], F32)
        dma(gain[:], AP(tensor=I["norm_mix"].tensor, offset=D, ap=[[0, 128], [1, D]]), w=["gain"])
        yt = [K.sb(es, "ns_y%d" % i, [128, D], F32) for i in range(2)]
        hb = [K.sb(es, "ns_h%d" % i, [128, D], BF16) for i in range(2)]
        hT = [K.sb(es, "ns_hT%d" % i, [128, 8, 128], BF16) for i in range(2)]
        kv = [K.sb(es, "ns_kv%d" % i, [128, 1536], F32) for i in range(2)]
        kvb = [K.sb(es, "ns_kvb%d" % i, [128, 1536], BF16) for i in range(2)]
        qb = [K.sb(es, "ns_qb%d" % i, [128, 1024], BF16) for i in range(2)]
        gt_ = [K.sb(es, "ns_gt%d" % i, [128, 48], F32) for i in range(2)]
        junk = K.sb(es, "ns_junk", [128, D], F32)
        ss = [K.sb(es, "ns_ss%d" % i, [128, 1], F32) for i in range(2)]
        ptb = K.ps(es, "ns_ptb", [128, 8, 128], BF16)
        pk = [K.ps(es, "ns_pk%d" % i, [128, 512], F32) for i in range(6)]
        for t in range(NTILE):
            b = t % 2
            sx = str(b)
            Pn, rs = tile_rows(t)
            dma(yt[b][0:Pn, :], pick(O["y2_scr"], O["y2s_scr"], t), r=["y2_scr"], w=["yt" + sx])
            rms_to_bf16(K, yt[b], "yt" + sx, gain, hb[b], "hb" + sx, junk, ss[b], sx, Pn)
            transpose_tiles(K, hb[b], "hb" + sx, 8, hT[b], "hT" + sx, ptb, idb, Pn)
            for c in range(6):
                c0 = 512 * c
                cw = min(512, NCOL - c0)
                for k in range(8):
                    P(lambda e: e.matmul(pk[c][0:Pn, 0:cw], lhsT=hT[b][:, k, 0:Pn], rhs=wn[:, k, c0:c0 + cw], start=(k == 0), stop=(k == 7)),
                      r=["hT" + sx, "w_nsw"], w=["npk%d" % c])
            for c in range(2):
                A(lambda e: e.activation(out=qb[b][0:Pn, 512 * c:512 * c + 512], in_=pk[c][0:Pn, :], func=AF.Identity, scale=0.125),
                  r=["npk%d" % c], w=["qb" + sx])
            for c in range(3):
                A(lambda e: e.copy(out=kv[b][0:Pn, 512 * c:512 * c + 512], in_=pk[2 + c][0:Pn, :]), r=["npk%d" % (2 + c)], w=["kv" + sx])
                V(lambda e: e.tensor_copy(out=kvb[b][0:Pn, 512 * c:512 * c + 512], in_=kv[b][0:Pn, 512 * c:512 * c + 512]), r=["kv" + sx], w=["kvb" + sx])
            A(lambda e: e.activation(out=gt_[b][0:Pn, :], in_=pk[5][0:Pn, 0:48], func=AF.Sigmoid), r=["npk5"], w=["gt_" + sx])
            dma(pick(O["kv_p"], O["kv_s"], t), kv[b][0:Pn, 0:1024], r=["kv" + sx], w=["kv_p"], add_writer=True)
            NSDBG = int(os.environ.get("NSDBG", "0"))
            if not NSDBG & 1:
                dma(pick(O["q_scr"], O["qs_scr"], t), qb[b][0:Pn, :], r=["qb" + sx], w=["q_scr"], add_writer=True)
            if not NSDBG & 2:
                dma(pick(O["kvb_scr"], O["kvbs_scr"], t), kvb[b][0:Pn, :], r=["kvb" + sx], w=["kvb_scr"], add_writer=True)
            if not NSDBG & 4:
                dma(pick(O["gate_scr"], O["gates_scr"], t), gt_[b][0:Pn, :], r=["gt_" + sx], w=["gate_scr"], add_writer=True)
            if rs is None:
                dma(O["win_s"], kv[b][0:Pn, 1024:1536], r=["kv" + sx], w=["win_p"], add_writer=True)
            elif t >= NTILE - 4:
                tt = t - (NTILE - 4)
                dma(O["win_p"][tt * 128:(tt + 1) * 128, :], kv[b][0:Pn, 1024:1536], r=["kv" + sx], w=["win_p"], add_writer=True)
    K.S.barrier()


class Lin32:
    def __init__(self, K, es, C, kcmax, tag):
        self.K, self.C, self.tag = K, C, tag
        self.wch = [K.sb(es, "l32w%s%d" % (tag, i), [128, kcmax, 512], F32) for i in range(2)]
        self.ps = [K.ps(es, "l32p%s%d" % (tag, i), [128, 512], F32) for i in range(2)]
        self.pt = K.ps(es, "l32t%s" % tag, [128, 32, NS], F32)
        self.i = 0

    def transpose(self, src, skey, kc, dstT, dkey):
        K, idf = self.K, self.C["idf"]
        for k in range(kc):
            K.P(lambda e: e.transpose(out=self.pt[:, k, :], in_=src[:, k * 128:(k + 1) * 128], identity=idf[0:NS, 0:NS]),
                r=[skey, "idf"], w=["l32t" + self.tag])
        K.V(lambda e: e.tensor_copy(out=dstT[:, 0:kc, :], in_=self.pt[:, 0:kc, :]), r=["l32t" + self.tag], w=[dkey])

    def run(self, xT, xkey, kc, Wd, ncols, evac):
        K = self.K
        Wv = Wd.rearrange("(k p) n -> p k n", p=128)
        for c0 in range(0, ncols, 512):
            cw = min(512, ncols - c0)
            b = self.i % 2
            self.i += 1
            wk = "l32w%s%d" % (self.tag, b)
            pk = "l32p%s%d" % (self.tag, b)
            K.dma(self.wch[b][:, 0:kc, 0:cw], Wv[:, :, c0:c0 + cw], w=[wk])
            for k in range(kc):
                K.P(lambda e: e.matmul(self.ps[b][0:NS, 0:cw], lhsT=xT[:, k, :], rhs=self.wch[b][:, k, 0:cw], start=(k == 0), stop=(k == kc - 1)),
                    r=[xkey, wk], w=[pk])
            evac(c0, cw, self.ps[b][0:NS, 0:cw], pk)


def rms32(K, x, xkey, gain, out, okey, junk, ss, tag):
    V, A = K.V, K.A
    N = NS
    A(lambda e: e.activation(out=junk[0:N, :], in_=x, func=AF.Square, accum_out=ss[0:N, 0:1]), r=[xkey], w=["junk" + tag, "ss" + tag])
    V(lambda e: e.tensor_scalar(out=ss[0:N, :], in0=ss[0:N, :], scalar1=1.0 / D, scalar2=EPS, op0=ALU.mult, op1=ALU.add), r=["ss" + tag], w=["ss" + tag])
    A(lambda e: e.activation(out=ss[0:N, :], in_=ss[0:N, :], func=AF.Sqrt), r=["ss" + tag], w=["ss" + tag])
    V(lambda e: e.reciprocal(out=ss[0:N, :], in_=ss[0:N, :]), r=["ss" + tag], w=["ss" + tag])
    V(lambda e: e.scalar_tensor_tensor(out=out, in0=x, scalar=ss[0:N, 0:1], in1=gain[0:N, :], op0=ALU.mult, op1=ALU.mult),
      r=[xkey, "ss" + tag, "gain" + tag], w=[okey])


def sample_l0_f32(K, C, I, O):
    V, A, G, P, dma = K.V, K.A, K.G, K.P, K.dma
    N = NS
    FF = 2816
    with ExitStack() as es:
        L = Lin32(K, es, C, 22, "a")
        x = K.sb(es, "f_x", [N, D], F32)
        g32 = K.sb(es, "f_g", [N, D], F32)
        xT = K.sb(es, "f_xT", [128, 22, N], F32)
        big = K.sb(es, "f_big", [N, 2 * FF], F32)
        act = K.sb(es, "f_act", [N, FF], F32)
        h = K.sb(es, "f_h", [N, D], F32)
        junk = K.sb(es, "f_junk", [N, D], F32)
        ss = K.sb(es, "f_ss", [N, 1], F32)
        gain_f = K.sb(es, "f_gainf", [N, D], F32)
        gain_m = K.sb(es, "f_gainm", [N, D], F32)
        qb = K.sb(es, "f_qb", [N, D], BF16)
        kvb = K.sb(es, "f_kvb", [N, 1536], BF16)
        dma(x[:], I["xs"], w=["f_x"])
        dma(g32[:], O["gs32_scr"], r=["gs32_scr"], w=["f_g"])
        dma(gain_f[:], AP(tensor=I["norm_ffn"].tensor, offset=0, ap=[[0, N], [1, D]]), w=["gainf"])
        dma(gain_m[:], AP(tensor=I["norm_mix"].tensor, offset=D, ap=[[0, N], [1, D]]), w=["gainm"])

        def to_big(c0, cw, ps, pk):
            A(lambda e: e.copy(out=big[:, c0:c0 + cw], in_=ps), r=[pk], w=["f_big"])
        L.transpose(g32, "f_g", 8, xT, "f_xT")
        L.run(xT, "f_xT", 8, I["s5_w_glu"], 2048, to_big)
        A(lambda e: e.activation(out=big[:, 1024:2048], in_=big[:, 1024:2048], func=AF.Sigmoid), r=["f_big"], w=["f_big"])
        V(lambda e: e.tensor_tensor(out=big[:, 0:1024], in0=big[:, 0:1024], in1=big[:, 1024:2048], op=ALU.mult), r=["f_big"], w=["f_big"])
        V(lambda e: e.tensor_tensor(out=x[:], in0=x[:], in1=big[:, 0:1024], op=ALU.add), r=["f_big", "f_x"], w=["f_x"])
        rms32(K, x[:], "f_x", gain_f, h[:], "f_h", junk, ss, "f")
        L.transpose(h, "f_h", 8, xT, "f_xT")
        L.run(xT, "f_xT", 8, I["ffn_w_in"], 2 * FF, to_big)
        A(lambda e: e.activation(out=act[:], in_=big[:, 0:FF], func=AF.Silu), r=["f_big"], w=["f_act"])
        V(lambda e: e.tensor_tensor(out=act[:], in0=act[:], in1=big[:, FF:2 * FF], op=ALU.mult), r=["f_big", "f_act"], w=["f_act"])
        L.transpose(act, "f_act", 22, xT, "f_xT")

        def add_x(c0, cw, ps, pk):
            V(lambda e: e.tensor_tensor(out=x[:, c0:c0 + cw], in0=ps, in1=x[:, c0:c0 + cw], op=ALU.add), r=[pk, "f_x"], w=["f_x"])
        L.run(xT, "f_xT", 22, I["ffn_w_out"], D, add_x)
        dma(O["y2s_scr"], x[:], r=["f_x"], w=["y2_scr"], add_writer=True)
        rms32(K, x[:], "f_x", gain_m, h[:], "f_h", junk, ss, "m")
        L.transpose(h, "f_h", 8, xT, "f_xT")
        L.run(xT, "f_xT", 8, I["nsa_w_in"], 2608, to_big)
        A(lambda e: e.activation(out=qb[:], in_=big[:, 0:1024], func=AF.Identity, scale=0.125), r=["f_big"], w=["f_qb"])
        V(lambda e: e.tensor_copy(out=kvb[:], in_=big[:, 1024:2560]), r=["f_big"], w=["f_kvb"])
        A(lambda e: e.activation(out=act[:, 0:48], in_=big[:, 2560:2608], func=AF.Sigmoid), r=["f_big", "f_act"], w=["f_act"])
        dma(O["kv_s"], big[:, 1024:2048], r=["f_big"], w=["kv_p"], add_writer=True)
        dma(O["win_s"], big[:, 2048:2560], r=["f_big"], w=["win_p"], add_writer=True)
        dma(O["qs_scr"], qb[:], r=["f_qb"], w=["q_scr"], add_writer=True)
        dma(O["kvbs_scr"], kvb[:], r=["f_kvb"], w=["kvb_scr"], add_writer=True)
        dma(O["gates_scr"], act[:, 0:48], r=["f_act"], w=["gate_scr"], add_writer=True)
    K.S.barrier()


NEG = -30000.0
NCB = 255


def nsa_attn_prompt(K, C, I, O):
    idf, idb = C["idf"], C["idb"]
    V, A, G, P, dma = K.V, K.A, K.G, K.P, K.dma
    nc = K.nc
    with ExitStack() as es:
        KselE = K.sb(es, "na_KselE", [128, 4, T], BF16)
        KwinE = K.sb(es, "na_KwinE", [128, 4, T], BF16)
        Vsel = K.sb(es, "na_Vsel", [128, NTILE, 4, 65], BF16)
        Vwin = K.sb(es, "na_Vwin", [128, NTILE, 4, 65], BF16)
        kcT = K.sb(es, "na_kcT", [64, 4, 256], BF16)
        vca = K.sb(es, "na_vca", [128, 2, 4, 129], BF16)
        chb = K.sb(es, "na_chb", [128, 16], F32)
        W4 = K.sb(es, "na_W4", [128, 128], F32)
        for g in range(4):
            K.S.dma("pool", KselE[64:128, g, :], I["c_E"], (), ["KselE"], add_writer=True)
            K.S.dma("pool", KwinE[64:128, g, :], I["c_E"], (), ["KwinE"], add_writer=True)
        G(lambda e: e.memset(Vsel[:, :, :, 64:65], 1.0), w=["Vsel"])
        G(lambda e: e.memset(Vwin[:, :, :, 64:65], 1.0), w=["Vwin"])
        G(lambda e: e.memset(vca[:, :, :, 64:65], 1.0), w=["vca"])
        for nt in range(2):
            for g in range(4):
                K.S.dma("pool", vca[:, nt, g, 65:129], I["c_cover"][nt * 128:(nt + 1) * 128, :], (), ["vca"], add_writer=True)
        dma(chb[:], AP(tensor=I["rel_bias"].tensor, offset=31 * 16, ap=[[0, 128], [1, 16]]), w=["chb"])
        G(lambda e: e.memset(W4[:], 0.0), w=["W4"])
        G(lambda e: e.affine_select(out=W4[:], in_=W4[:], pattern=[[-1, 128]], compare_op=ALU.is_ge, fill=NEG, base=0, channel_multiplier=1),
          r=["W4"], w=["W4"])

        with ExitStack() as bes:
            tabs = K.sb(bes, "na_tab", [32, 16], F32)
            ohb = K.sb(bes, "na_ohb", [32, 256], F32)
            Fraw = K.sb(bes, "na_Fraw", [16, 256], F32)
            Fpad = K.sb(bes, "na_Fpad", [16, 384], F32)
            FC = K.sb(bes, "na_FC", [16, 8192], F32)
            pF = K.ps(bes, "na_pF", [128, 512], F32)
            dma(tabs[:], I["rel_bias"], w=["tabs"])
            dma(ohb[:], I["c_ohb"], w=["ohb"])
            P(lambda e: e.matmul(pF[0:16, 0:256], lhsT=tabs[:], rhs=ohb[:], start=True, stop=True), r=["tabs", "ohb"], w=["pF"])
            V(lambda e: e.tensor_copy(out=Fraw[:], in_=pF[0:16, 0:256]), r=["pF"], w=["Fraw"])
            G(lambda e: e.memset(Fpad[:], NEG), w=["Fpad"])
            V(lambda e: e.tensor_scalar(out=Fpad[:, 127:383], in0=Fraw[:], scalar1=Fraw[:, 255:256], scalar2=None, op0=ALU.subtract),
              r=["Fraw", "Fpad"], w=["Fpad"])
            dma(O["fpad_scr"], Fpad[:], r=["Fpad"], w=["fpad_scr"])
            G(lambda e: e.memset(FC[:, 0:4111], NEG), w=["FC"])
            V(lambda e: e.tensor_copy(out=FC[:, 4111:4367], in_=Fraw[:]), r=["Fraw", "FC"], w=["FC"])
            V(lambda e: e.tensor_copy(out=FC[:, 4367:8192], in_=bc(Fraw[:, 255:256], [16, 8192 - 4367])), r=["Fraw", "FC"], w=["FC"])
            dma(O["fc_scr"], FC[:], r=["FC"], w=["fc_scr"])

        K.S.barrier()
        with ExitStack() as ces:
            KcT = K.sb(ces, "na_KcT", [64, 2, 4, T], BF16)
            kvt = [K.sb(ces, "na_kvt%d" % i, [128, 1536], BF16) for i in range(2)]
            W1 = K.sb(ces, "na_W1", [64, 2, 32, 128], BF16)
            W2 = K.sb(ces, "na_W2", [128, 2, 64], BF16)
            pe_f = K.sb(ces, "na_pef", [32, 2, 64], F32)
            pe_b = K.sb(ces, "na_peb", [32, 2, 64], BF16)
            peT = K.sb(ces, "na_peT", [64, 2, 32], BF16)
            cv = K.sb(ces, "na_cv", [128, 2], F32)
            pre = K.sb(ces, "na_pre", [128, 256], F32)
            tmpg = K.sb(ces, "na_tmpg", [128, 256], F32)
            hidT = K.sb(ces, "na_hidT", [128, 256], BF16)
            ptk = [K.ps(ces, "na_ptk%d" % i, [64, 8, 128], BF16) for i in range(2)]
            ph = K.ps(ces, "na_ph", [128, 512], F32)
            pk2 = K.ps(ces, "na_pk2", [128, 512], F32)
            for kvi in range(2):
                K.S.dma("pool", W1[:, kvi, :, :], I["nsa_phi_w1"][kvi].rearrange("l d e -> d l e"), (), ["naW1"], add_writer=True)
                K.S.dma("pool", W2[:, kvi, :], I["nsa_phi_w2"][kvi], (), ["naW2"], add_writer=True)
            dma(pe_f[:], I["nsa_phi_pe"].rearrange("k l d -> l k d"), w=["pe_f"])
            V(lambda e: e.tensor_copy(out=pe_b[:], in_=pe_f[:]), r=["pe_f"], w=["pe_b"])
            for kvi in range(2):
                P(lambda e: e.transpose(out=ptk[0][:, kvi, 0:32], in_=pe_b[:, kvi, :], identity=idb[0:32, 0:32]), r=["pe_b", "idb"], w=["ptk0"])
            V(lambda e: e.tensor_copy(out=peT[:], in_=ptk[0][:, 0:2, 0:32]), r=["ptk0"], w=["peT"])
            for kvi in range(2):
                for l in range(32):
                    P(lambda e: e.matmul(ph[:, kvi:kvi + 1], lhsT=W1[:, kvi, l, :], rhs=peT[:, kvi, l:l + 1], start=(l == 0), stop=(l == 31)),
                      r=["naW1", "peT"], w=["ph"])
            V(lambda e: e.tensor_copy(out=cv[:], in_=ph[:, 0:2]), r=["ph"], w=["cv"])
            G(lambda e: e.memset(hidT[:], 0.0), w=["hidT"])
            for t in range(NTILE):
                b = t % 2
                cs = slice(t * 128, (t + 1) * 128)
                dma(kvt[b][:], O["kvb_scr"][cs, :], r=["kvb_scr"], w=["kvt%d" % b])
                for j in range(8):
                    P(lambda e: e.transpose(out=ptk[0][:, j, :], in_=kvt[b][:, j * 64:(j + 1) * 64], identity=idb[:]), r=["kvt%d" % b, "idb"], w=["ptk0"])
                A(lambda e: e.copy(out=KcT[:, :, :, cs], in_=ptk[0][:].rearrange("p (k g) n -> p k g n", g=4)), r=["ptk0"], w=["KcT"])
                for j in range(4):
                    P(lambda e: e.transpose(out=ptk[1][:, j, :], in_=kvt[b][:, 512 + j * 64:512 + (j + 1) * 64], identity=idb[:]), r=["kvt%d" % b, "idb"], w=["ptk1"])
                    P(lambda e: e.transpose(out=ptk[1][:, 4 + j, :], in_=kvt[b][:, 1024 + j * 64:1024 + (j + 1) * 64], identity=idb[:]), r=["kvt%d" % b, "idb"], w=["ptk1"])
                V(lambda e: e.tensor_copy(out=KselE[0:64, :, cs], in_=ptk[1][:, 0:4, :]), r=["ptk1"], w=["KselE"])
                V(lambda e: e.tensor_copy(out=KwinE[0:64, :, cs], in_=ptk[1][:, 4:8, :]), r=["ptk1", "KselE"], w=["KwinE"])
                G(lambda e: e.tensor_copy(out=Vsel[:, t, :, 0:64], in_=kvt[b][:, 768:1024].rearrange("p (g d) -> p g d", d=64)), r=["kvt%d" % b], w=["Vsel"])
                G(lambda e: e.tensor_copy(out=Vwin[:, t, :, 0:64], in_=kvt[b][:, 1280:1536].rearrange("p (g d) -> p g d", d=64)), r=["kvt%d" % b], w=["Vwin"])
            for kvi in range(2):
                for g in range(4):
                    for l in range(32):
                        P(lambda e: e.matmul(ph[:, 0:NCB], lhsT=W1[:, kvi, l, :], rhs=KcT[:, kvi, g, l:l + 16 * (NCB - 1) + 1:16],
                                             start=(l == 0), stop=(l == 31)), r=["naW1", "KcT"], w=["ph"])
                    V(lambda e: e.tensor_scalar(out=pre[:, 0:NCB], in0=ph[:, 0:NCB], scalar1=cv[:, kvi:kvi + 1], scalar2=None, op0=ALU.add),
                      r=["ph", "cv"], w=["pre"])
                    gelu_tanh(K, pre[:, 0:NCB], "pre", tmpg[:, 0:NCB], "tmpg", hidT[:, 255:0:-1], "hidT")
                    if kvi == 0:
                        P(lambda e: e.matmul(pk2[0:64, 0:256], lhsT=W2[:, 0, :], rhs=hidT[:], start=True, stop=True), r=["naW2", "hidT"], w=["pk2"])
                        V(lambda e: e.tensor_copy(out=kcT[:, g, :], in_=pk2[0:64, 0:256]), r=["pk2"], w=["kcT"])
                    else:
                        for nt in range(2):
                            P(lambda e: e.matmul(pk2[:, 64 * nt:64 * nt + 64], lhsT=hidT[:, nt * 128:(nt + 1) * 128], rhs=W2[:, 1, :], start=True, stop=True),
                              r=["naW2", "hidT"], w=["pk2"])
                        V(lambda e: e.tensor_copy(out=vca[:, :, g, 0:64], in_=pk2[:, 0:128].rearrange("p (n f) -> p n f", f=64)), r=["pk2"], w=["vca"])
        K.S.barrier()

        with ExitStack() as qes:
            wo = K.sb(qes, "na_wo", [128, 8, D], BF16)
            parT = K.sb(qes, "na_par", [128, 2], F32)
            biasT = K.sb(qes, "na_biasT", [128, 3, 16, 128], BF16)
            TW = K.sb(qes, "na_TW", [128, 2, 128], F32)
            dma(parT[:], I["par"], w=["parT"])
            p0, p1 = parT[:, 0:1], parT[:, 1:2]
            load_w_bf16(K, wo, I["nsa_w_o"], D)
            with ExitStack() as hes:
                hk = K.sb(hes, "na_hk", [128, 16, 256], F32)
                biasD = K.sb(hes, "na_biasD", [128, 16, 256], F32)
                negt = K.sb(hes, "na_negt", [128, 4, 128], F32)
                Jm = K.sb(hes, "na_J", [128, 128], F32)
                pJ = K.ps(hes, "na_pJ", [128, 512], F32)
                dma(hk[:], AP(tensor=O["fpad_scr"].tensor, offset=0, ap=[[1, 128], [384, 16], [1, 256]]), r=["fpad_scr"], w=["hk"])
                G(lambda e: e.memset(Jm[:], 1.0), w=["Jm"])
                G(lambda e: e.affine_select(out=Jm[:], in_=Jm[:], pattern=[[1, 128]], compare_op=ALU.is_equal, fill=0.0, base=-127, channel_multiplier=1),
                  r=["Jm"], w=["Jm"])
                hkf = hk[:].rearrange("p h x -> p (h x)")
                bDf = biasD[:].rearrange("p h x -> p (h x)")
                for c in range(8):
                    P(lambda e: e.matmul(pJ[:], lhsT=Jm[:], rhs=hkf[:, c * 512:(c + 1) * 512], start=True, stop=True), r=["Jm", "hk"], w=["pJ"])
                    V(lambda e: e.tensor_copy(out=bDf[:, c * 512:(c + 1) * 512], in_=pJ[:]), r=["pJ"], w=["biasD"])
                bD0 = biasD[:, :, 0:128]
                bD1 = biasD[:, :, 128:256]
                V(lambda e: e.tensor_scalar(out=biasT[:, 0], in0=bD0, scalar1=p1, scalar2=None, op0=ALU.mult), r=["biasD", "parT"], w=["biasT"])
                G(lambda e: e.memset(negt[:], NEG), w=["negt"])
                for hq in range(4):
                    V(lambda e: e.scalar_tensor_tensor(out=biasT[:, 0, 4 * hq:4 * hq + 4, :], in0=negt[:], scalar=p0, in1=biasT[:, 0, 4 * hq:4 * hq + 4, :],
                                                       op0=ALU.mult, op1=ALU.add), r=["negt", "parT", "biasT"], w=["biasT"])
                V(lambda e: e.tensor_scalar(out=biasT[:, 1], in0=bD1, scalar1=p1, scalar2=None, op0=ALU.mult), r=["biasD", "parT", "biasT"], w=["biasT"])
                V(lambda e: e.scalar_tensor_tensor(out=biasT[:, 1], in0=bD0, scalar=p0, in1=biasT[:, 1], op0=ALU.mult, op1=ALU.add), r=["biasD", "parT", "biasT"], w=["biasT"])
                V(lambda e: e.tensor_scalar(out=biasT[:, 2], in0=bD1, scalar1=p0, scalar2=None, op0=ALU.mult), r=["biasD", "parT", "biasT"], w=["biasT"])
                V(lambda e: e.tensor_scalar(out=TW[:, 0, :], in0=W4[:], scalar1=p0, scalar2=None, op0=ALU.mult), r=["W4", "parT"], w=["TW"])
                V(lambda e: e.scalar_tensor_tensor(out=TW[:, 0, :], in0=negt[:, 0, :], scalar=p1, in1=TW[:, 0, :], op0=ALU.mult, op1=ALU.add), r=["negt", "parT", "TW"], w=["TW"])
                V(lambda e: e.tensor_scalar(out=TW[:, 1, :], in0=W4[:], scalar1=p1, scalar2=None, op0=ALU.mult), r=["W4", "parT", "TW"], w=["TW"])


            K.S.barrier()
            NJ = NTILE // 2
            qt_sb = [K.sb(qes, "na_q%d" % i, [128, D], BF16) for i in range(2)]
            gate = [K.sb(qes, "na_gate%d" % i, [128, 48], F32) for i in range(2)]
            yres = [K.sb(qes, "na_y%d" % i, [128, D], F32) for i in range(2)]
            bW1 = K.sb(qes, "na_bW", [128, 16, 256], F32)
            bW = [bW1, bW1]
            bC1 = [K.sb(qes, "na_bC_%d" % n, [128, 16, 128], BF16) for n in range(2)]
            bC = [bC1, bC1]
            ka = [K.sb(qes, "na_ka%d" % i, [128, 2, 64], F32) for i in range(2)]
            qidx = K.sb(qes, "na_qidx", [128, NJ], I32)
            QSel = K.sb(qes, "na_QSel", [128, 16, 128], BF16)
            QWin = K.sb(qes, "na_QWin", [128, 16, 128], BF16)
            NBUF = 3
            sc = [K.sb(qes, "na_sc%d" % i, [128, 4, 128], F32) for i in range(NBUF)]
            PT = [K.sb(qes, "na_PT%d" % i, [128, 4, 128], BF16) for i in range(NBUF)]
            rc = [K.sb(qes, "na_rc%d" % i, [128, 3, 4], F32) for i in range(2)]
            ocs = [K.sb(qes, "na_ocs%d" % i, [128, 4, 64], F32) for i in range(2)]
            imp = K.sb(qes, "na_imp", [128, 64], F32)
            wk1 = K.sb(qes, "na_wk1", [128, 64], F32)
            m8 = K.sb(qes, "na_m8", [128, 16], F32)
            selm = K.sb(qes, "na_selm", [128, 64], F32)
            tmpm = K.sb(qes, "na_tmpm", [128, 64], F32)
            mbw = [K.sb(qes, "na_mbw%d" % i, [128, 4, 128], BF16) for i in range(2)]
            otile = [K.sb(qes, "na_o%d" % i, [128, D], BF16) for i in range(2)]
            oacc = K.sb(qes, "na_oacc", [128, 64], F32)
            oT = K.sb(qes, "na_oT", [128, 8, 128], BF16)
            psb = [K.ps(qes, "na_ps%d" % i, [128, 4, 128], F32) for i in range(NBUF)]
            posel = K.ps(qes, "na_posel", [128, 512], F32)
            powin = K.ps(qes, "na_powin", [128, 512], F32)
            poc = K.ps(qes, "na_poc", [128, 512], F32)
            pimp = K.ps(qes, "na_pimp", [128, 4, 128], F32)
            pmisc = K.ps(qes, "na_pmisc", [128, 8, 128], BF16)
            posel_v = posel[:, 0:260].rearrange("p (h c) -> p h c", c=65)
            powin_v = powin[:, 0:260].rearrange("p (h c) -> p h c", c=65)
            poc_v = poc[:, 0:260].rearrange("p (h c) -> p h c", c=65)

            dma(qidx[:], I["c_qidx"], w=["qidx"])
            for i_ in range(2):
                G(lambda e: e.memset(mbw[i_][:], 0.0), w=["mbw%d" % i_])
            V(lambda e: e.tensor_copy(out=QWin[64:128, :, :], in_=bc(chb[64:128, :].unsqueeze(2), [64, 16, 128])), r=["chb"],
              w=["QWin0", "QWin1", "QWin2", "QWin3"])
            sidx = [0]

            def emit_qk(blk):
                lhsT_full, g, kt, qrhs, qkey = blk["lhsT"], blk["g"], blk["kt"], blk["qrhs"], blk["qkey"]
                i = sidx[0] % NBUF
                sidx[0] += 1
                blk["i"] = i
                ks = slice(kt * 128, (kt + 1) * 128)
                P(lambda e: e.matmul(psb[i][:], lhsT=lhsT_full[:, g, ks], rhs=qrhs[:, 4 * g:4 * g + 4, :], start=True, stop=True),
                  r=["KselE", "KwinE", qkey], w=["psb%d" % i])

            def emit_exp_pv(blk):
                i, g, kt = blk["i"], blk["g"], blk["kt"]
                bias_ap = blk["bias"]
                if bias_ap is not None:
                    V(lambda e: e.tensor_tensor(out=sc[i][:], in0=psb[i][:], in1=bias_ap, op=ALU.add), r=["psb%d" % i, "biasT", "TW"], w=["sc%d" % i])
                    A(lambda e: e.activation(out=PT[i][:], in_=sc[i][:], func=AF.Exp), r=["sc%d" % i], w=["PT%d" % i])
                else:
                    A(lambda e: e.activation(out=PT[i][:], in_=psb[i][:], func=AF.Exp), r=[], w=["PT%d" % i, "psb%d" % i])
                for h in range(4):
                    P(lambda e: e.matmul(blk["po_v"][:, h, :], lhsT=PT[i][:, h, :], rhs=blk["vaug"][:, kt, g, :], start=(blk["first"] and h == 0),
                                         stop=blk["last"], skip_group_check=True), r=["PT%d" % i, "Vsel", "Vwin"], w=[blk["pokey"]])

            def nts_of(j):
                return [1] if j <= 7 else [0, 1]

            def tile_loads(j):
                b = j % 2
                sx = str(b)
                K.S.idma(qt_sb[b][:], O["q_scr"], qidx[:, j:j + 1], ["qidx", "q_scr"], ["qsb" + sx])
                K.S.idma(gate[b][:], O["gate_scr"], qidx[:, j:j + 1], ["qidx", "gate_scr"], ["gate" + sx])
                K.S.idma(yres[b][:], O["y2_scr"], qidx[:, j:j + 1], ["qidx", "y2_scr"], ["yres" + sx])
                dma(ka[b][:], I["c_ka"][j], w=["ka" + sx])
                for nt in nts_of(j):
                    dma(bW[b][:], AP(tensor=O["fc_scr"].tensor, offset=256 * j + 2048 * nt, ap=[[16, 128], [8192, 16], [1, 256]]),
                        r=["fc_scr"], w=["bW"])
                    G(lambda e: e.tensor_scalar(out=bC[b][nt][:], in0=bW[b][:, :, 0:128], scalar1=p0, scalar2=None, op0=ALU.mult),
                      r=["bW", "parT"], w=["bC_%d" % nt])
                    V(lambda e: e.scalar_tensor_tensor(out=bC[b][nt][:], in0=bW[b][:, :, 128:256], scalar=p1, in1=bC[b][nt][:], op0=ALU.mult, op1=ALU.add),
                      r=["bW", "parT", "bC_%d" % nt], w=["bC_%d" % nt])

            def phase_a(j, g):
                b = j % 2
                sx = str(b)
                gp = g % 2
                nts = nts_of(j)
                qk, wk_ = "QSel%d" % g, "QWin%d" % g
                for h in range(4):
                    hh = 4 * g + h
                    P(lambda e: e.transpose(out=pmisc[0:64, h, :], in_=qt_sb[b][:, hh * 64:(hh + 1) * 64], identity=idb[:]), r=["qsb" + sx, "idb"], w=["pmisc"])
                A(lambda e: e.copy(out=QSel[0:64, 4 * g:4 * g + 4, :], in_=pmisc[0:64, 0:4, :]), r=["pmisc"], w=[qk])
                V(lambda e: e.tensor_copy(out=QWin[0:64, 4 * g:4 * g + 4, :], in_=QSel[0:64, 4 * g:4 * g + 4, :]), r=[qk], w=[wk_])
                for ni, nt in enumerate(nts):
                    i = sidx[0] % NBUF
                    sidx[0] += 1
                    P(lambda e: e.matmul(psb[i][:], lhsT=kcT[:, g, nt * 128:(nt + 1) * 128], rhs=QSel[0:64, 4 * g:4 * g + 4, :], start=True, stop=True),
                      r=["kcT", qk], w=["psb%d" % i])
                    V(lambda e: e.tensor_tensor(out=sc[i][:], in0=psb[i][:], in1=bC[b][nt][:, 4 * g:4 * g + 4, :], op=ALU.add),
                      r=["psb%d" % i, "bC_%d" % nt], w=["sc%d" % i])
                    A(lambda e: e.activation(out=PT[i][:], in_=sc[i][:], func=AF.Exp), r=["sc%d" % i], w=["PT%d" % i])
                    for h in range(4):
                        P(lambda e: e.matmul(poc_v[:, h, :], lhsT=PT[i][:, h, :], rhs=vca[:, nt, g, 0:65], start=(ni == 0 and h == 0),
                                             stop=(ni == len(nts) - 1), skip_group_check=True), r=["PT%d" % i, "vca"], w=["poc"])
                        P(lambda e: e.matmul(pimp[:, h, 0:64], lhsT=PT[i][:, h, :], rhs=vca[:, nt, g, 65:129], start=(ni == 0 and h == 0),
                                             stop=(ni == len(nts) - 1), skip_group_check=True), r=["PT%d" % i, "vca"], w=["pimp"])
                rk = "rc%d" % gp
                V(lambda e: e.tensor_scalar(out=rc[gp][:, 0, :], in0=poc_v[:, :, 64], scalar1=1e-30, scalar2=None, op0=ALU.add), r=["poc"], w=[rk])
                V(lambda e: e.reciprocal(out=rc[gp][:, 0, :], in_=rc[gp][:, 0, :]), r=[rk], w=[rk])
                V(lambda e: e.tensor_scalar(out=imp[:], in0=pimp[:, 0, 0:64], scalar1=rc[gp][:, 0, 0:1], scalar2=None, op0=ALU.mult), r=["pimp", rk], w=["imp"])
                for h in range(1, 4):
                    V(lambda e: e.scalar_tensor_tensor(out=imp[:], in0=pimp[:, h, 0:64], scalar=rc[gp][:, 0, h:h + 1], in1=imp[:], op0=ALU.mult, op1=ALU.add),
                      r=["pimp", rk, "imp"], w=["imp"])
                V(lambda e: e.tensor_tensor(out=ocs[gp][:], in0=poc_v[:, :, 0:64], in1=bc(rc[gp][:, 0, :].unsqueeze(2), [128, 4, 64]), op=ALU.mult),
                  r=["poc", rk], w=["ocs%d" % gp])
                V(lambda e: e.tensor_tensor(out=imp[:], in0=imp[:], in1=ka[b][:, 0, :], op=ALU.mult), r=["imp", "ka" + sx], w=["imp"])
                V(lambda e: e.tensor_tensor(out=imp[:], in0=imp[:], in1=ka[b][:, 1, :], op=ALU.add), r=["imp", "ka" + sx], w=["imp"])
                V(lambda e: e.max(out=m8[:, 0:8], in_=imp[:]), r=["imp"], w=["m8"])
                V(lambda e: e.match_replace(out=wk1[:], in_to_replace=m8[:, 0:8], in_values=imp[:], imm_value=-3e38), r=["imp", "m8"], w=["wk1"])
                V(lambda e: e.max(out=m8[:, 8:16], in_=wk1[:]), r=["wk1"], w=["m8"])
                V(lambda e: e.tensor_scalar(out=selm[:], in0=imp[:], scalar1=m8[:, 15:16], scalar2=None, op0=ALU.is_ge), r=["imp", "m8"], w=["selm"])
                V(lambda e: e.tensor_scalar(out=tmpm[:], in0=selm[:], scalar1=-NEG, scalar2=NEG, op0=ALU.mult, op1=ALU.add), r=["selm"], w=["tmpm"])
                for h in range(4):
                    V(lambda e: e.scalar_tensor_tensor(out=mbw[g % 2][:, h, 64:128], in0=selm[:], scalar=chb[:, 4 * g + h:4 * g + h + 1], in1=tmpm[:],
                                                       op0=ALU.mult, op1=ALU.add), r=["selm", "tmpm", "chb"], w=["mbw%d" % (g % 2)])

            def phase_a2(j, g):
                qk = "QSel%d" % g
                for h in range(4):
                    P(lambda e: e.transpose(out=pmisc[:, h, :], in_=mbw[g % 2][:, h, :], identity=idb[:]), r=["mbw%d" % (g % 2), "idb"], w=["pmisc"])
                A(lambda e: e.copy(out=QSel[64:128, 4 * g:4 * g + 4, :], in_=pmisc[64:128, 0:4, :]), r=["pmisc"], w=[qk])

            def phase_b(j, g):
                b = j % 2
                sx = str(b)
                gp = g % 2
                rk = "rc%d" % gp
                ktop = 2 * j + 1
                blocks = []
                for kt in range(ktop + 1):
                    kr = ktop - kt
                    bias_ap = biasT[:, kr, 4 * g:4 * g + 4, :] if kr <= 2 else None
                    blocks.append(dict(lhsT=KselE, g=g, kt=kt, qrhs=QSel, qkey="QSel%d" % g, bias=bias_ap, vaug=Vsel, po_v=posel_v, pokey="posel",
                                       first=(kt == 0), last=(kt == ktop)))
                k0 = max(0, ktop - 5)
                for kt in range(k0, ktop + 1):
                    kr = ktop - kt
                    if kr <= 2:
                        bias_ap = biasT[:, kr, 4 * g:4 * g + 4, :]
                    elif kr == 5:
                        bias_ap = bc(TW[:, 0, :].unsqueeze(1), [128, 4, 128])
                    elif kr == 4:
                        bias_ap = bc(TW[:, 1, :].unsqueeze(1), [128, 4, 128])
                    else:
                        bias_ap = None
                    blocks.append(dict(lhsT=KwinE, g=g, kt=kt, qrhs=QWin, qkey="QWin%d" % g, bias=bias_ap, vaug=Vwin, po_v=powin_v, pokey="powin",
                                       first=(kt == k0), last=(kt == ktop)))
                n = len(blocks)
                LA = NBUF - 1
                for i in range(n + LA):
                    if i < n:
                        emit_qk(blocks[i])
                    if i >= LA:
                        emit_exp_pv(blocks[i - LA])
                V(lambda e: e.tensor_scalar(out=rc[gp][:, 1, :], in0=posel_v[:, :, 64], scalar1=1e-30, scalar2=None, op0=ALU.add), r=["posel"], w=[rk])
                V(lambda e: e.tensor_scalar(out=rc[gp][:, 2, :], in0=powin_v[:, :, 64], scalar1=1e-30, scalar2=None, op0=ALU.add), r=["powin"], w=[rk])
                V(lambda e: e.reciprocal(out=rc[gp][:, 1:3, :], in_=rc[gp][:, 1:3, :]), r=[rk], w=[rk])
                V(lambda e: e.memset(rc[gp][:, 0, :], 1.0), r=[rk, "ocs%d" % gp], w=[rk])
                V(lambda e: e.tensor_tensor(out=rc[gp][:], in0=rc[gp][:], in1=gate[b][:].rearrange("p (r h) -> p r h", h=16)[:, :, 4 * g:4 * g + 4], op=ALU.mult),
                  r=[rk, "gate" + sx], w=[rk])
                for h in range(4):
                    hh = 4 * g + h
                    V(lambda e: e.tensor_scalar(out=oacc[:], in0=ocs[gp][:, h, :], scalar1=rc[gp][:, 0, h:h + 1], scalar2=None, op0=ALU.mult),
                      r=["ocs%d" % gp, rk], w=["oacc"])
                    V(lambda e: e.scalar_tensor_tensor(out=oacc[:], in0=posel_v[:, h, 0:64], scalar=rc[gp][:, 1, h:h + 1], in1=oacc[:], op0=ALU.mult, op1=ALU.add),
                      r=["posel", rk, "oacc"], w=["oacc"])
                    V(lambda e: e.scalar_tensor_tensor(out=otile[b][:, hh * 64:(hh + 1) * 64], in0=powin_v[:, h, 0:64], scalar=rc[gp][:, 2, h:h + 1], in1=oacc[:],
                                                       op0=ALU.mult, op1=ALU.add), r=["powin", rk, "oacc"], w=["otile" + sx])

            def tile_end(j):
                b = j % 2
                sx = str(b)
                rs = slice(j * 128, (j + 1) * 128)
                for k in range(8):
                    P(lambda e: e.transpose(out=pmisc[:, k, :], in_=otile[b][:, k * 128:(k + 1) * 128], identity=idb[:]), r=["otile" + sx, "idb"], w=["pmisc"])
                A(lambda e: e.copy(out=oT[:], in_=pmisc[:]), r=["pmisc"], w=["oT"])
                for c in range(2):
                    pso = psb[c][:].rearrange("p h n -> p (h n)")
                    for k in range(8):
                        P(lambda e: e.matmul(pso, lhsT=oT[:, k, :], rhs=wo[:, k, c * 512:(c + 1) * 512], start=(k == 0), stop=(k == 7)),
                          r=["oT", "w_na_wo"], w=["psb%d" % c])
                    V(lambda e: e.tensor_tensor(out=yres[b][:, c * 512:(c + 1) * 512], in0=pso, in1=yres[b][:, c * 512:(c + 1) * 512], op=ALU.add),
                      r=["psb%d" % c, "yres" + sx], w=["yres" + sx])
                dma(O["y3_scr"][rs, :], yres[b][:], r=["yres" + sx], w=["y3_scr"], add_writer=True)

            items = [(j, g) for j in range(NJ) for g in range(4)]
            tile_loads(0)
            phase_a(*items[0])
            phase_a2(*items[0])
            for ii, (j, g) in enumerate(items):
                if ii + 1 < len(items):
                    nj, ng = items[ii + 1]
                    if ng == 0:
                        tile_loads(nj)
                    phase_a(nj, ng)
                phase_b(j, g)
                if ii + 1 < len(items):
                    phase_a2(*items[ii + 1])
                if g == 3:
                    tile_end(j)
    K.S.barrier()


NKT_S = 17
NWT_S = 5
OFFC = 4111


def nsa_attn_sample(K, C, I, O):
    idf, idb = C["idf"], C["idb"]
    V, A, G, P, dma = K.V, K.A, K.G, K.P, K.dma
    N = NS
    with ExitStack() as es:
        W1 = K.sb(es, "sa_W1", [64, 2, 32, 128], BF16)
        W2 = K.sb(es, "sa_W2", [128, 2, 64], BF16)
        cv = K.sb(es, "sa_cv", [128, 2], F32)
        biasC = K.sb(es, "sa_bC", [128, 16], F32)
        biasS = K.sb(es, "sa_bS", [128, NKT_S, 16], F32)
        biasW = K.sb(es, "sa_bW", [128, NWT_S, 16], F32)
        QT = K.sb(es, "sa_QT", [128, 16, N], BF16)
        idx = K.sb(es, "sa_idx", [128, N * 16], I32)
        covs = K.sb(es, "sa_cov", [128, 64], BF16)
        Erow = K.sb(es, "sa_E", [128, NKT_S * 128], BF16)
        ones4 = K.sb(es, "sa_ones4", [1, 4], F32)
        for kvi in range(2):
            K.S.dma("pool", W1[:, kvi, :, :], I["nsa_phi_w1"][kvi].rearrange("l d e -> d l e"), (), ["saW1"], add_writer=True)
            K.S.dma("pool", W2[:, kvi, :], I["nsa_phi_w2"][kvi], (), ["saW2"], add_writer=True)
        K.S.dma("pool", covs[:], I["c_cover_s"], (), ["covs"])
        K.S.dma("pool", Erow[64:128, :], I["c_E"][:, 0:NKT_S * 128], (), ["Erow"])
        V(lambda e: e.memset(ones4[:], 1.0), w=["ones4"])
        with ExitStack() as bes:
            pe_f = K.sb(bes, "sa_pef", [32, 2, 64], F32)
            pe_b = K.sb(bes, "sa_peb", [32, 2, 64], BF16)
            peT = K.sb(bes, "sa_peT", [64, 2, 32], BF16)
            hk = K.sb(bes, "sa_hk", [128, NKT_S + NWT_S, 16], F32)
            Jm = K.sb(bes, "sa_J", [128, 128], F32)
            pti = K.sb(bes, "sa_pti", [128, N * 16], I32)
            ptf = K.sb(bes, "sa_ptf", [128, N * 16], F32)
            iot = K.sb(bes, "sa_iot", [128, 1], F32)
            qs = K.sb(bes, "sa_qs", [N, D], BF16)
            pA = K.ps(bes, "sa_pA", [128, 1024], BF16)
            pB = K.ps(bes, "sa_pB", [128, 512], F32)
            dma(pe_f[:], I["nsa_phi_pe"].rearrange("k l d -> l k d"), w=["pe_f"])
            V(lambda e: e.tensor_copy(out=pe_b[:], in_=pe_f[:]), r=["pe_f"], w=["pe_b"])
            for kvi in range(2):
                P(lambda e: e.transpose(out=pA[0:64, kvi * 32:kvi * 32 + 32], in_=pe_b[:, kvi, :], identity=idb[0:32, 0:32]), r=["pe_b", "idb"], w=["pA"])
            V(lambda e: e.tensor_copy(out=peT[:].rearrange("p k l -> p (k l)"), in_=pA[0:64, 0:64]), r=["pA"], w=["peT"])
            for kvi in range(2):
                for l in range(32):
                    P(lambda e: e.matmul(pB[:, kvi:kvi + 1], lhsT=W1[:, kvi, l, :], rhs=peT[:, kvi, l:l + 1], start=(l == 0), stop=(l == 31)),
                      r=["saW1", "peT"], w=["pB"])
            V(lambda e: e.tensor_copy(out=cv[:], in_=pB[:, 0:2]), r=["pB"], w=["cv"])
            bC2 = K.sb(bes, "sa_bC2", [128, 16, 2], F32)
            dma(bC2[:], AP(tensor=O["fc_scr"].tensor, offset=OFFC + 2017 - 16 * 127, ap=[[16, 128], [8192, 16], [1, 2]]), r=["fc_scr"], w=["bC2"])
            V(lambda e: e.tensor_copy(out=biasC[:], in_=bC2[:, :, 0]), r=["bC2"], w=["biasC"])
            hk2 = K.sb(bes, "sa_hk2", [128, NKT_S + NWT_S, 16, 2], F32)
            for kt in range(NKT_S):
                dma(hk2[:, kt, :, :], AP(tensor=O["fc_scr"].tensor, offset=OFFC + 2048 - 128 * kt - 127, ap=[[1, 128], [8192, 16], [1, 2]]),
                    r=["fc_scr"], w=["hk2"], add_writer=True)
            for wt in range(NWT_S):
                dma(hk2[:, NKT_S + wt, :, :], AP(tensor=O["fc_scr"].tensor, offset=OFFC + 512 - 128 * wt - 127, ap=[[1, 128], [8192, 16], [1, 2]]),
                    r=["fc_scr"], w=["hk2"], add_writer=True)
            V(lambda e: e.tensor_copy(out=hk[:], in_=hk2[:, :, :, 0]), r=["hk2"], w=["hk"])
            G(lambda e: e.memset(Jm[:], 1.0), w=["Jm"])
            G(lambda e: e.affine_select(out=Jm[:], in_=Jm[:], pattern=[[1, 128]], compare_op=ALU.is_equal, fill=0.0, base=-127, channel_multiplier=1),
              r=["Jm"], w=["Jm"])
            ncol = (NKT_S + NWT_S) * 16
            P(lambda e: e.matmul(pB[:, 0:ncol], lhsT=Jm[:], rhs=hk[:].rearrange("p t h -> p (t h)"), start=True, stop=True), r=["Jm", "hk", "cv"], w=["pB"])
            V(lambda e: e.tensor_copy(out=biasS[:].rearrange("p t h -> p (t h)"), in_=pB[:, 0:NKT_S * 16]), r=["pB"], w=["biasS"])
            V(lambda e: e.tensor_copy(out=biasW[:].rearrange("p t h -> p (t h)"), in_=pB[:, NKT_S * 16:ncol]), r=["pB"], w=["biasW"])
            dma(pti[:], AP(tensor=I["ptab"].tensor, offset=0, ap=[[0, 128], [1, N * 16]]), w=["pti"])
            G(lambda e: e.iota(out=iot[:], pattern=[[0, 1]], base=0, channel_multiplier=1, allow_small_or_imprecise_dtypes=True), w=["iot"])
            V(lambda e: e.tensor_copy(out=ptf[:], in_=pti[:]), r=["pti"], w=["ptf"])
            V(lambda e: e.tensor_scalar(out=ptf[:], in0=ptf[:], scalar1=128.0, scalar2=iot[:, 0:1], op0=ALU.mult, op1=ALU.add), r=["ptf", "iot"], w=["ptf"])
            V(lambda e: e.tensor_copy(out=idx[:], in_=ptf[:]), r=["ptf"], w=["idx"])
            dma(qs[:], O["qs_scr"], r=["q_scr"], w=["sqs"])
            for hh in range(16):
                P(lambda e: e.transpose(out=pA[0:64, 64 + hh * N:64 + (hh + 1) * N], in_=qs[:, hh * 64:(hh + 1) * 64], identity=idb[0:N, 0:N]),
                  r=["sqs", "idb", "peT"], w=["pA"])
            V(lambda e: e.tensor_copy(out=QT[0:64, :, :].rearrange("p h n -> p (h n)"), in_=pA[0:64, 64:64 + 16 * N]), r=["pA"], w=["QT"])
        K.S.barrier()

        with ExitStack() as tes:
            pg = [K.sb(tes, "sa_pg%d" % i, [128, D], F32) for i in range(2)]
            wn = K.sb(tes, "sa_wn", [128, 4, 512], F32)
            newf = K.sb(tes, "sa_newf", [N, 1536], BF16)
            KcT = K.sb(tes, "sa_KcT", [64, 2, 4, 2048], BF16)
            KsE = K.sb(tes, "sa_KsE", [128, 4, NKT_S * 128], BF16)
            KwE = K.sb(tes, "sa_KwE", [64, 4, NWT_S * 128], BF16)
            Vs = K.sb(tes, "sa_Vs", [128, NKT_S, 4, 65], BF16)
            Vw = K.sb(tes, "sa_Vw", [128, NWT_S, 4, 65], BF16)
            kcT = K.sb(tes, "sa_kcT", [64, 4, 128], BF16)
            vca = K.sb(tes, "sa_vca", [128, 4, 129], BF16)
            pre = [K.sb(tes, "sa_pre%d" % i, [128, 128], F32) for i in range(2)]
            tmpg = [K.sb(tes, "sa_tmpg%d" % i, [128, 128], F32) for i in range(2)]
            hidT = [K.sb(tes, "sa_hidT%d" % i, [128, 128], BF16) for i in range(2)]
            sc = K.sb(tes, "sa_sc", [128, NKT_S, 4], F32)
            PTs = K.sb(tes, "sa_PT", [128, NKT_S, 4], BF16)
            oc = K.sb(tes, "sa_oc", [4, 129], F32)
            osw = K.sb(tes, "sa_osw", [4, 2, 65], F32)
            rcc = K.sb(tes, "sa_rcc", [4, 1], F32)
            impr = K.sb(tes, "sa_impr", [1, 64], F32)
            wk1 = K.sb(tes, "sa_wk1", [1, 64], F32)
            m8 = K.sb(tes, "sa_m8", [1, 16], F32)
            mbr = K.sb(tes, "sa_mbr", [1, 128], F32)
            ptA = K.ps(tes, "sa_ptA", [64, 4, 128], F32)
            ptB = K.ps(tes, "sa_ptB", [64, 4, 128], F32)
            ph = [K.ps(tes, "sa_ph%d" % i, [128, 512], F32) for i in range(2)]
            pk2 = K.ps(tes, "sa_pk2", [128, 512], F32)
            psS = K.ps(tes, "sa_psS", [128, 512], F32)
            po = K.ps(tes, "sa_po", [128, 512], F32)
            ptN = K.ps(tes, "sa_ptN", [128, 1024], BF16)

            dma(newf[:], O["kvbs_scr"], r=["kvb_scr"], w=["newf"])
            G(lambda e: e.memset(Vs[:, :, :, 64:65], 1.0), w=["sVs"])
            G(lambda e: e.memset(Vw[:, :, :, 64:65], 1.0), w=["sVw"])
            G(lambda e: e.memset(vca[:, :, 64:65], 1.0), w=["svca"])
            for g in range(4):
                V(lambda e: e.tensor_copy(out=vca[:, g, 65:129], in_=covs[:]), r=["covs", "svca"], w=["svca"])
                V(lambda e: e.tensor_copy(out=KsE[64:128, g, :], in_=Erow[64:128, :]), r=["Erow"], w=["sKsE"])
            G(lambda e: e.memset(KsE[0:64, :, 16 * 128:17 * 128], 0.0), r=["sKsE"], w=["sKsE"])
            G(lambda e: e.memset(KwE[:, :, 4 * 128:5 * 128], 0.0), w=["sKwE"])
            G(lambda e: e.memset(Vs[:, 16, :, 0:64], 0.0), r=["sVs"], w=["sVs"])
            G(lambda e: e.memset(Vw[:, 4, :, 0:64], 0.0), r=["sVw"], w=["sVw"])
            for i_ in range(2):
                G(lambda e: e.memset(hidT[i_][:], 0.0), w=["shidT%d" % i_])
            G(lambda e: e.memset(mbr[:], 0.0), w=["mbr"])
            for j in range(4):
                P(lambda e: e.transpose(out=ptN[0:64, j * N:(j + 1) * N], in_=newf[:, 512 + j * 64:512 + (j + 1) * 64], identity=idb[0:N, 0:N]),
                  r=["newf", "idb"], w=["ptN"])
                P(lambda e: e.transpose(out=ptN[0:64, (4 + j) * N:(5 + j) * N], in_=newf[:, 1024 + j * 64:1024 + (j + 1) * 64], identity=idb[0:N, 0:N]),
                  r=["newf", "idb"], w=["ptN"])
            newT = K.sb(tes, "sa_newT", [64, 8, N], BF16)
            V(lambda e: e.tensor_copy(out=newT[:].rearrange("p j n -> p (j n)"), in_=ptN[0:64, 0:8 * N]), r=["ptN"], w=["newT"])
            newrow = K.sb(tes, "sa_newrow", [1, 1536], BF16)

            for tk in range(N):
                dma(wn[:], I["cwin"][tk].rearrange("(w r) c -> r w c", r=128), w=["wn"])
                for wt in range(4):
                    for g in range(4):
                        P(lambda e: e.transpose(out=ptA[:, g, :], in_=wn[:, wt, g * 64:(g + 1) * 64], identity=idf[:]), r=["wn", "idf"], w=["sptA"])
                    A(lambda e: e.copy(out=KwE[:, :, wt * 128:(wt + 1) * 128], in_=ptA[:]), r=["sptA"], w=["sKwE"])
                    V(lambda e: e.tensor_copy(out=Vw[:, wt, :, 0:64], in_=wn[:, wt, 256:512].rearrange("p (g d) -> p g d", d=64)), r=["wn", "sVw"], w=["sVw"])
                V(lambda e: e.tensor_copy(out=KsE[0:64, :, 16 * 128:16 * 128 + 1], in_=newT[:, 0:4, tk:tk + 1]), r=["newT", "sKsE"], w=["sKsE"])
                V(lambda e: e.tensor_copy(out=KwE[:, :, 4 * 128:4 * 128 + 1], in_=newT[:, 4:8, tk:tk + 1]), r=["newT", "sKwE"], w=["sKwE"])
                dma(newrow[:], O["kvbs_scr"][tk:tk + 1, :], r=["kvb_scr"], w=["newrow"])
                V(lambda e: e.tensor_copy(out=Vs[0:1, 16, :, 0:64], in_=newrow[:, 768:1024].rearrange("p (g d) -> p g d", d=64)), r=["newrow", "sVs"], w=["sVs"])
                V(lambda e: e.tensor_copy(out=Vw[0:1, 4, :, 0:64], in_=newrow[:, 1280:1536].rearrange("p (g d) -> p g d", d=64)), r=["newrow", "sVw"], w=["sVw"])
                for pgi in range(16):
                    b = pgi % 2
                    K.S.idma(pg[b][:], I["ckv"], idx[:, tk * 16 + pgi:tk * 16 + pgi + 1], ["idx"], ["spg%d" % b])
                    cs = slice(pgi * 128, (pgi + 1) * 128)
                    for g in range(4):
                        P(lambda e: e.transpose(out=ptA[:, g, :], in_=pg[b][:, g * 64:(g + 1) * 64], identity=idf[:]), r=["spg%d" % b, "idf"], w=["sptA"])
                    A(lambda e: e.copy(out=KcT[:, 0, :, cs], in_=ptA[:]), r=["sptA"], w=["sKcT"])
                    for g in range(4):
                        P(lambda e: e.transpose(out=ptB[:, g, :], in_=pg[b][:, 256 + g * 64:256 + (g + 1) * 64], identity=idf[:]), r=["spg%d" % b, "idf"], w=["sptB"])
                    V(lambda e: e.tensor_copy(out=KcT[:, 1, :, cs], in_=ptB[:]), r=["sptB", "sKcT"], w=["sKcT"])
                    for g in range(4):
                        P(lambda e: e.transpose(out=ptA[:, g, :], in_=pg[b][:, 512 + g * 64:512 + (g + 1) * 64], identity=idf[:]), r=["spg%d" % b, "idf"], w=["sptA"])
                    A(lambda e: e.copy(out=KsE[0:64, :, cs], in_=ptA[:]), r=["sptA"], w=["sKsE"])
                    G(lambda e: e.tensor_copy(out=Vs[:, pgi, :, 0:64], in_=pg[b][:, 768:1024].rearrange("p (g d) -> p g d", d=64)), r=["spg%d" % b, "sVs"], w=["sVs"])
                for kvi in range(2):
                    for g in range(4):
                        cb = (kvi * 4 + g) % 2
                        cx = str(cb)
                        for l in range(32):
                            P(lambda e: e.matmul(ph[cb][:, 0:127], lhsT=W1[:, kvi, l, :], rhs=KcT[:, kvi, g, l:l + 16 * 126 + 1:16], start=(l == 0), stop=(l == 31)),
                              r=["saW1", "sKcT"], w=["sph" + cx])
                        V(lambda e: e.tensor_scalar(out=pre[cb][:, 0:127], in0=ph[cb][:, 0:127], scalar1=cv[:, kvi:kvi + 1], scalar2=None, op0=ALU.add),
                          r=["sph" + cx, "cv"], w=["spre" + cx])
                        gelu_tanh(K, pre[cb][:, 0:127], "spre" + cx, tmpg[cb][:, 0:127], "stmpg" + cx, hidT[cb][:, 127:0:-1], "shidT" + cx)
                        if kvi == 0:
                            P(lambda e: e.matmul(pk2[0:64, 0:128], lhsT=W2[:, 0, :], rhs=hidT[cb][:], start=True, stop=True), r=["saW2", "shidT" + cx], w=["spk2"])
                            V(lambda e: e.tensor_copy(out=kcT[:, g, :], in_=pk2[0:64, 0:128]), r=["spk2"], w=["skcT"])
                        else:
                            P(lambda e: e.matmul(pk2[:, 0:64], lhsT=hidT[cb][:], rhs=W2[:, 1, :], start=True, stop=True), r=["saW2", "shidT" + cx], w=["spk2"])
                            V(lambda e: e.tensor_copy(out=vca[:, g, 0:64], in_=pk2[:, 0:64]), r=["spk2", "svca"], w=["svca"])
                for g in range(4):
                    qcol = QT[:, 4 * g:4 * g + 4, tk]
                    P(lambda e: e.matmul(psS[:, 0:4], lhsT=kcT[:, g, :], rhs=QT[0:64, 4 * g:4 * g + 4, tk], start=True, stop=True), r=["skcT", "QT"], w=["spsS"])
                    V(lambda e: e.tensor_tensor(out=sc[:, 0, :], in0=psS[:, 0:4], in1=biasC[:, 4 * g:4 * g + 4], op=ALU.add), r=["spsS", "biasC"], w=["ssc"])
                    A(lambda e: e.activation(out=PTs[:, 0, :], in_=sc[:, 0, :], func=AF.Exp), r=["ssc"], w=["sPT"])
                    P(lambda e: e.matmul(po[0:4, 0:129], lhsT=PTs[:, 0, :], rhs=vca[:, g, :], start=True, stop=True), r=["sPT", "svca"], w=["spo"])
                    V(lambda e: e.tensor_copy(out=oc[:], in_=po[0:4, 0:129]), r=["spo"], w=["soc"])
                    V(lambda e: e.tensor_scalar(out=rcc[:], in0=oc[:, 64:65], scalar1=1e-30, scalar2=None, op0=ALU.add), r=["soc"], w=["srcc"])
                    V(lambda e: e.reciprocal(out=rcc[:], in_=rcc[:]), r=["srcc"], w=["srcc"])
                    dma(O["os_scr"][tk, 0, 4 * g:4 * g + 4, :], oc[:, 0:65], r=["soc"], w=["os_scr"], add_writer=True)
                    P(lambda e: e.matmul(po[0:1, 256:320], lhsT=rcc[:], rhs=oc[:, 65:129], start=True, stop=True), r=["srcc", "soc"], w=["spo"])
                    V(lambda e: e.tensor_copy(out=impr[:], in_=po[0:1, 256:320]), r=["spo"], w=["simpr"])
                    V(lambda e: e.memset(impr[:, 0:1], 1e4), r=["simpr"], w=["simpr"])
                    V(lambda e: e.memset(impr[:, 31:33], 1e4), r=["simpr"], w=["simpr"])
                    V(lambda e: e.memset(impr[:, 33:64], -1e30), r=["simpr"], w=["simpr"])
                    V(lambda e: e.max(out=m8[:, 0:8], in_=impr[:]), r=["simpr"], w=["sm8"])
                    V(lambda e: e.match_replace(out=wk1[:], in_to_replace=m8[:, 0:8], in_values=impr[:], imm_value=-3e38), r=["simpr", "sm8"], w=["swk1"])
                    V(lambda e: e.max(out=m8[:, 8:16], in_=wk1[:]), r=["swk1"], w=["sm8"])
                    V(lambda e: e.tensor_scalar(out=wk1[:], in0=impr[:], scalar1=m8[:, 15:16], scalar2=None, op0=ALU.is_ge), r=["simpr", "sm8", "swk1"], w=["swk1"])
                    V(lambda e: e.tensor_scalar(out=mbr[:, 64:128], in0=wk1[:], scalar1=-NEG, scalar2=NEG, op0=ALU.mult, op1=ALU.add), r=["swk1", "mbr"], w=["mbr"])
                    P(lambda e: e.matmul(po[:, 320:324], lhsT=mbr[:], rhs=ones4[:], start=True, stop=True), r=["mbr", "ones4", "simpr"], w=["spo"])
                    V(lambda e: e.tensor_copy(out=QT[64:128, 4 * g:4 * g + 4, tk], in_=po[64:128, 320:324]), r=["spo"], w=["QT"])
                    for kt in range(NKT_S):
                        P(lambda e: e.matmul(psS[:, 4 + 4 * kt:8 + 4 * kt], lhsT=KsE[:, g, kt * 128:(kt + 1) * 128], rhs=qcol, start=True, stop=True),
                          r=["sKsE", "QT"], w=["spsS"])
                    V(lambda e: e.tensor_tensor(out=sc[:], in0=psS[:, 4:4 + 4 * NKT_S].rearrange("p (t h) -> p t h", h=4), in1=biasS[:, :, 4 * g:4 * g + 4], op=ALU.add),
                      r=["spsS", "biasS"], w=["ssc"])
                    A(lambda e: e.activation(out=PTs[:], in_=sc[:], func=AF.Exp), r=["ssc"], w=["sPT"])
                    for kt in range(NKT_S):
                        P(lambda e: e.matmul(po[0:4, 0:65], lhsT=PTs[:, kt, :], rhs=Vs[:, kt, g, :], start=(kt == 0), stop=(kt == NKT_S - 1)),
                          r=["sPT", "sVs"], w=["spo"])
                    V(lambda e: e.tensor_copy(out=osw[:, 0, :], in_=po[0:4, 0:65]), r=["spo"], w=["sosw"])
                    for wt in range(NWT_S):
                        P(lambda e: e.matmul(psS[:, 4 * wt:4 * wt + 4], lhsT=KwE[:, g, wt * 128:(wt + 1) * 128], rhs=QT[0:64, 4 * g:4 * g + 4, tk], start=True, stop=True),
                          r=["sKwE", "QT"], w=["spsS"])
                    V(lambda e: e.tensor_tensor(out=sc[:, 0:NWT_S, :], in0=psS[:, 0:4 * NWT_S].rearrange("p (t h) -> p t h", h=4), in1=biasW[:, :, 4 * g:4 * g + 4], op=ALU.add),
                      r=["spsS", "biasW"], w=["ssc"])
                    A(lambda e: e.activation(out=PTs[:, 0:NWT_S, :], in_=sc[:, 0:NWT_S, :], func=AF.Exp), r=["ssc"], w=["sPT"])
                    for wt in range(NWT_S):
                        P(lambda e: e.matmul(po[0:4, 0:65], lhsT=PTs[:, wt, :], rhs=Vw[:, wt, g, :], start=(wt == 0), stop=(wt == NWT_S - 1)),
                          r=["sPT", "sVw"], w=["spo"])
                    V(lambda e: e.tensor_copy(out=osw[:, 1, :], in_=po[0:4, 0:65]), r=["spo", "sosw"], w=["sosw"])
                    dma(O["os_scr"][tk, 1, 4 * g:4 * g + 4, :], osw[:, 0, :], r=["sosw"], w=["os_scr"], add_writer=True)
                    dma(O["os_scr"][tk, 2, 4 * g:4 * g + 4, :], osw[:, 1, :], r=["sosw"], w=["os_scr"], add_writer=True)
        K.S.barrier()

        with ExitStack() as mes:
            osb = K.sb(mes, "sa_osb", [N, 3, 16, 65], F32)
            gt = K.sb(mes, "sa_gt", [N, 3, 16], F32)
            wgt = K.sb(mes, "sa_wgt", [N, 3, 16], F32)
            o1 = K.sb(mes, "sa_o1", [N, 16, 64], F32)
            o2 = K.sb(mes, "sa_o2", [N, 16, 64], F32)
            yr = K.sb(mes, "sa_yr", [N, D], F32)
            dma(osb[:], O["os_scr"], r=["os_scr"], w=["osb"])
            dma(gt[:], O["gates_scr"].rearrange("n (r h) -> n r h", h=16), r=["gate_scr"], w=["sgt"])
            dma(yr[:], O["y2s_scr"], r=["y2_scr"], w=["syr"])
            V(lambda e: e.tensor_scalar(out=wgt[:], in0=osb[:, :, :, 64], scalar1=1e-30, scalar2=None, op0=ALU.add), r=["osb"], w=["swgt"])
            V(lambda e: e.reciprocal(out=wgt[:], in_=wgt[:]), r=["swgt"], w=["swgt"])
            V(lambda e: e.tensor_tensor(out=wgt[:], in0=wgt[:], in1=gt[:], op=ALU.mult), r=["swgt", "sgt"], w=["swgt"])
            V(lambda e: e.tensor_tensor(out=o1[:], in0=osb[:, 0, :, 0:64], in1=bc(wgt[:, 0, :].unsqueeze(2), [N, 16, 64]), op=ALU.mult), r=["osb", "swgt"], w=["so1"])
            for br in (1, 2):
                V(lambda e: e.tensor_tensor(out=o2[:], in0=osb[:, br, :, 0:64], in1=bc(wgt[:, br, :].unsqueeze(2), [N, 16, 64]), op=ALU.mult), r=["osb", "swgt"], w=["so2"])
                V(lambda e: e.tensor_tensor(out=o1[:], in0=o1[:], in1=o2[:], op=ALU.add), r=["so1", "so2"], w=["so1"])
            L = Lin32(K, mes, C, 8, "o")
            oT32 = K.sb(mes, "sa_oT32", [128, 8, N], F32)
            L.transpose(o1[:].rearrange("n h d -> n (h d)"), "so1", 8, oT32, "soT32")

            def add_y(c0, cw, ps, pk):
                V(lambda e: e.tensor_tensor(out=yr[:, c0:c0 + cw], in0=ps, in1=yr[:, c0:c0 + cw], op=ALU.add), r=[pk, "syr"], w=["syr"])
            L.run(oT32, "soT32", 8, I["nsa_w_o"], D, add_y)
            dma(O["y3s_scr"], yr[:], r=["syr"], w=["y3s_scr"])
    K.S.barrier()


NMT = NTILE // 2 + 1


def moe_stage(K, C, I, O):
    idf, idb = C["idf"], C["idb"]
    V, A, G, P, dma = K.V, K.A, K.G, K.P, K.dma
    EF = 1408
    CW = 352
    with ExitStack() as es:
        acc = K.sb(es, "mo_acc", [128, NMT, D], F32)
        hT = K.sb(es, "mo_hT", [128, NMT, 8, 128], BF16)
        gts = K.sb(es, "mo_gts", [128, NMT, 8], F32)
        hT32s = K.sb(es, "mo_hT32s", [128, 8, NS], F32)
        par = K.sb(es, "mo_par", [128, 2], F32)
        gain = K.sb(es, "mo_gain", [128, D], F32)
        gfin = K.sb(es, "mo_gfin", [128, D], F32)
        rt = K.sb(es, "mo_rt", [128, 8, 8], F32)
        dma(par[:], I["par"], w=["par"])
        dma(gain[:], AP(tensor=I["norm_ffn"].tensor, offset=D, ap=[[0, 128], [1, D]]), w=["gain"])
        dma(gfin[:], AP(tensor=I["norm_final"].tensor, offset=0, ap=[[0, 128], [1, D]]), w=["gfin"])
        dma(rt[:], I["moe_router"].rearrange("(k p) e -> p k e", p=128), w=["rt"])
        with ExitStack() as es2:
            ya = K.sb(es2, "mo_ya", [128, D], F32)
            yb = K.sb(es2, "mo_yb", [128, D], F32)
            h32 = K.sb(es2, "mo_h32", [128, D], F32)
            hb = K.sb(es2, "mo_hb", [128, D], BF16)
            hT32 = K.sb(es2, "mo_hT32", [128, 8, 128], F32)
            junk = K.sb(es2, "mo_junk", [128, D], F32)
            ss = K.sb(es2, "mo_ss", [128, 1], F32)
            lg = K.sb(es2, "mo_lg", [128, 8], F32)
            ex = K.sb(es2, "mo_ex", [128, 8], F32)
            mk = K.sb(es2, "mo_mk", [128, 8], F32)
            m8 = K.sb(es2, "mo_m8", [128, 8], F32)
            sm = K.sb(es2, "mo_sm", [128, 1], F32)
            ptb = K.ps(es2, "mo_ptb", [128, 8, 128], BF16)
            pt32 = [K.ps(es2, "mo_pt32%d" % i, [128, 4, 128], F32) for i in range(2)]
            plg = K.ps(es2, "mo_plg", [128, 512], F32)
            for j in range(NMT):
                Pn = 128 if j < NMT - 1 else NS
                if j < NMT - 1:
                    dma(acc[:, j, :], O["y3_scr"][j * 128:(j + 1) * 128, :], r=["y3_scr"], w=["acc"])
                else:
                    dma(acc[0:Pn, j, :], O["y3s_scr"], r=["y3s_scr"], w=["acc"])
                xin = acc[0:Pn, j, :]
                A(lambda e: e.activation(out=junk[0:Pn, :], in_=xin, func=AF.Square, accum_out=ss[0:Pn, 0:1]), r=["acc"], w=["junk", "ss"])
                V(lambda e: e.tensor_scalar(out=ss[0:Pn, :], in0=ss[0:Pn, :], scalar1=1.0 / D, scalar2=EPS, op0=ALU.mult, op1=ALU.add), r=["ss"], w=["ss"])
                A(lambda e: e.activation(out=ss[0:Pn, :], in_=ss[0:Pn, :], func=AF.Sqrt), r=["ss"], w=["ss"])
                V(lambda e: e.reciprocal(out=ss[0:Pn, :], in_=ss[0:Pn, :]), r=["ss"], w=["ss"])
                V(lambda e: e.scalar_tensor_tensor(out=h32[0:Pn, :], in0=xin, scalar=ss[0:Pn, 0:1], in1=gain[0:Pn, :], op0=ALU.mult, op1=ALU.mult),
                  r=["acc", "ss", "gain"], w=["h32"])
                V(lambda e: e.tensor_copy(out=hb[0:Pn, :], in_=h32[0:Pn, :]), r=["h32"], w=["hb"])
                for k in range(8):
                    P(lambda e: e.transpose(out=ptb[:, k, 0:Pn], in_=hb[0:Pn, k * 128:(k + 1) * 128], identity=idb[0:Pn, 0:Pn]), r=["hb", "idb"], w=["ptb"])
                A(lambda e: e.copy(out=hT[:, j, :, 0:Pn], in_=ptb[:, :, 0:Pn]), r=["ptb"], w=["hT"])
                for k in range(8):
                    P(lambda e: e.transpose(out=pt32[k // 4][:, k % 4, 0:Pn], in_=h32[0:Pn, k * 128:(k + 1) * 128], identity=idf[0:Pn, 0:Pn]),
                      r=["h32", "idf"], w=["pt32%d" % (k // 4)])
                for hh in range(2):
                    V(lambda e: e.tensor_copy(out=hT32[:, 4 * hh:4 * hh + 4, 0:Pn], in_=pt32[hh][:, :, 0:Pn]), r=["pt32%d" % hh], w=["hT32"])
                if j == NMT - 1:
                    V(lambda e: e.tensor_copy(out=hT32s[:], in_=hT32[:, :, 0:NS]), r=["hT32"], w=["hT32s"])
                for k in range(8):
                    P(lambda e: e.matmul(plg[0:Pn, 0:8], lhsT=hT32[:, k, 0:Pn], rhs=rt[:, k, :], start=(k == 0), stop=(k == 7)), r=["hT32", "rt"], w=["plg"])
                V(lambda e: e.tensor_copy(out=lg[0:Pn, :], in_=plg[0:Pn, 0:8]), r=["plg"], w=["lg"])
                V(lambda e: e.max(out=m8[0:Pn, :], in_=lg[0:Pn, :]), r=["lg"], w=["m8"])
                V(lambda e: e.tensor_scalar(out=ex[0:Pn, :], in0=lg[0:Pn, :], scalar1=m8[0:Pn, 0:1], scalar2=None, op0=ALU.subtract), r=["lg", "m8"], w=["ex"])
                A(lambda e: e.activation(out=ex[0:Pn, :], in_=ex[0:Pn, :], func=AF.Exp), r=["ex"], w=["ex"])
                V(lambda e: e.tensor_scalar(out=mk[0:Pn, :], in0=lg[0:Pn, :], scalar1=m8[0:Pn, 1:2], scalar2=None, op0=ALU.is_ge), r=["lg", "m8"], w=["mk"])
                V(lambda e: e.tensor_tensor(out=ex[0:Pn, :], in0=ex[0:Pn, :], in1=mk[0:Pn, :], op=ALU.mult), r=["ex", "mk"], w=["ex"])
                V(lambda e: e.reduce_sum(out=sm[0:Pn, :], in_=ex[0:Pn, :], axis=AX.X), r=["ex"], w=["sm"])
                V(lambda e: e.reciprocal(out=sm[0:Pn, :], in_=sm[0:Pn, :]), r=["sm"], w=["sm"])
                V(lambda e: e.tensor_scalar(out=gts[0:Pn, j, :], in0=ex[0:Pn, :], scalar1=sm[0:Pn, 0:1], scalar2=None, op0=ALU.mult), r=["ex", "sm"], w=["gts"])
        K.S.barrier()
        with ExitStack() as es3:
            w1 = K.sb(es3, "mo_w1", [128, 8, 2 * EF], BF16)
            w2 = K.sb(es3, "mo_w2", [128, 11, D], BF16)
            act = [K.sb(es3, "mo_act%d" % i, [128, EF], BF16) for i in range(2)]
            actT = [K.sb(es3, "mo_actT%d" % i, [128, 11, 128], BF16) for i in range(2)]
            sl_ = [K.sb(es3, "mo_s%d" % i, [128, CW], F32) for i in range(2)]
            ptb = K.ps(es3, "mo_ptb2", [128, 8, 128], BF16)
            pa = [K.ps(es3, "mo_pa%d" % i, [128, 512], F32) for i in range(2)]
            pb = [K.ps(es3, "mo_pb%d" % i, [128, 512], F32) for i in range(2)]
            po = [K.ps(es3, "mo_po%d" % i, [128, 512], F32) for i in range(2)]
            for ex_ in range(8):
                v1 = I["moe_w_in"][ex_].rearrange("(k p) n -> p k n", p=128)
                for c0 in range(0, 2 * EF, 704):
                    K.S.dma("pool", w1[:, :, c0:c0 + 704], v1[:, :, c0:c0 + 704], (), ["mo_w1"], add_writer=(c0 > 0))
                v2 = I["moe_w_out"][ex_].rearrange("(k p) n -> p k n", p=128)
                for c0 in range(0, D, 512):
                    K.S.dma("pool", w2[:, :, c0:c0 + 512], v2[:, :, c0:c0 + 512], (), ["mo_w2"], add_writer=(c0 > 0))
                for j in range(NMT - 1):
                    Pn = 128
                    b = j % 2
                    sx = str(b)
                    for jj in range(4):
                        jb = jj % 2
                        for k in range(8):
                            P(lambda e: e.matmul(pa[jb][0:Pn, 0:CW], lhsT=hT[:, j, k, 0:Pn], rhs=w1[:, k, CW * jj:CW * jj + CW], start=(k == 0), stop=(k == 7)),
                              r=["hT", "mo_w1"], w=["mpa%d" % jb])
                        for k in range(8):
                            P(lambda e: e.matmul(pb[jb][0:Pn, 0:CW], lhsT=hT[:, j, k, 0:Pn], rhs=w1[:, k, EF + CW * jj:EF + CW * jj + CW], start=(k == 0), stop=(k == 7)),
                              r=["hT", "mo_w1"], w=["mpb%d" % jb])
                        A(lambda e: e.activation(out=sl_[jb][0:Pn, :], in_=pa[jb][0:Pn, 0:CW], func=AF.Silu), r=["mpa%d" % jb], w=["msl%d" % jb])
                        V(lambda e: e.tensor_tensor(out=act[b][0:Pn, CW * jj:CW * jj + CW], in0=pb[jb][0:Pn, 0:CW], in1=sl_[jb][0:Pn, :], op=ALU.mult),
                          r=["mpb%d" % jb, "msl%d" % jb], w=["mact" + sx])
                    for k0 in range(0, 11, 8):
                        k1 = min(11, k0 + 8)
                        for k in range(k0, k1):
                            P(lambda e: e.transpose(out=ptb[:, k - k0, 0:Pn], in_=act[b][0:Pn, k * 128:(k + 1) * 128], identity=idb[0:Pn, 0:Pn]),
                              r=["mact" + sx, "idb"], w=["mptb"])
                        A(lambda e: e.copy(out=actT[b][:, k0:k1, 0:Pn], in_=ptb[:, 0:k1 - k0, 0:Pn]), r=["mptb"], w=["mactT" + sx])
                    for c in range(2):
                        for k in range(11):
                            P(lambda e: e.matmul(po[c][0:Pn, :], lhsT=actT[b][:, k, 0:Pn], rhs=w2[:, k, c * 512:(c + 1) * 512], start=(k == 0), stop=(k == 10)),
                              r=["mactT" + sx, "mo_w2"], w=["mpo%d" % c])
                        V(lambda e: e.scalar_tensor_tensor(out=acc[0:Pn, j, c * 512:(c + 1) * 512], in0=po[c][0:Pn, :], scalar=gts[0:Pn, j, ex_:ex_ + 1],
                                                           in1=acc[0:Pn, j, c * 512:(c + 1) * 512], op0=ALU.mult, op1=ALU.add),
                          r=["mpo%d" % c, "gts", "acc"], w=["acc"])
        K.S.barrier()
        with ExitStack() as es4:
            L = Lin32(K, es4, C, 11, "m")
            bigm = K.sb(es4, "mo_big", [NS, 2 * EF], F32)
            actm = K.sb(es4, "mo_actm", [NS, EF], F32)
            actTm = K.sb(es4, "mo_actTm", [128, 11, NS], F32)
            js = NMT - 1

            def to_bigm(c0, cw, ps, pk):
                A(lambda e: e.copy(out=bigm[:, c0:c0 + cw], in_=ps), r=[pk], w=["mo_big"])
            for ex_ in range(8):
                L.run(hT32s, "hT32s", 8, I["moe_w_in"][ex_], 2 * EF, to_bigm)
                A(lambda e: e.activation(out=actm[:], in_=bigm[:, 0:EF], func=AF.Silu), r=["mo_big"], w=["mo_actm"])
                V(lambda e: e.tensor_tensor(out=actm[:], in0=actm[:], in1=bigm[:, EF:2 * EF], op=ALU.mult), r=["mo_big", "mo_actm"], w=["mo_actm"])
                L.transpose(actm, "mo_actm", 11, actTm, "mo_actTm")

                def acc_add(c0, cw, ps, pk):
                    V(lambda e: e.scalar_tensor_tensor(out=acc[0:NS, js, c0:c0 + cw], in0=ps, scalar=gts[0:NS, js, ex_:ex_ + 1],
                                                       in1=acc[0:NS, js, c0:c0 + cw], op0=ALU.mult, op1=ALU.add), r=[pk, "gts", "acc"], w=["acc"])
                L.run(actTm, "mo_actTm", 11, I["moe_w_out"][ex_], D, acc_add)
        K.S.barrier()
        with ExitStack() as es3:
            junk2 = K.sb(es3, "mo_junk2", [128, D], F32)
            ss2 = K.sb(es3, "mo_ss2", [128, 1], F32)
            for j in range(NMT):
                Pn = 128 if j < NMT - 1 else NS
                xin = acc[0:Pn, j, :]
                A(lambda e: e.activation(out=junk2[0:Pn, :], in_=xin, func=AF.Square, accum_out=ss2[0:Pn, 0:1]), r=["acc"], w=["junk2", "ss2"])
                V(lambda e: e.tensor_scalar(out=ss2[0:Pn, :], in0=ss2[0:Pn, :], scalar1=1.0 / D, scalar2=EPS, op0=ALU.mult, op1=ALU.add), r=["ss2"], w=["ss2"])
                A(lambda e: e.activation(out=ss2[0:Pn, :], in_=ss2[0:Pn, :], func=AF.Sqrt), r=["ss2"], w=["ss2"])
                V(lambda e: e.reciprocal(out=ss2[0:Pn, :], in_=ss2[0:Pn, :]), r=["ss2"], w=["ss2"])
                V(lambda e: e.scalar_tensor_tensor(out=junk2[0:Pn, :], in0=xin, scalar=ss2[0:Pn, 0:1], in1=gfin[0:Pn, :], op0=ALU.mult, op1=ALU.mult),
                  r=["acc", "ss2", "gfin", "junk2"], w=["junk2"])
                if j < NMT - 1:
                    dma(O["y_p"][j * 128:(j + 1) * 128, :], junk2[:], r=["junk2"], w=["y_p"], add_writer=True)
                else:
                    dma(O["y_s"], junk2[0:Pn, :], r=["junk2"], w=["y_s"])
    K.S.barrier()


IN_SPECS = [("norm_mix", [2, D]), ("norm_ffn", [2, D]), ("s5_lam_re", [64, 64]), ("s5_lam_im", [64, 64]), ("s5_log_dt", [64]),
            ("s5_b_re", [64, 64, 16]), ("s5_b_im", [64, 64, 16]), ("s5_c_re", [64, 16, 64]), ("s5_c_im", [64, 16, 64]),
            ("s5_d", [D]), ("s5_w_glu", [D, 2 * D]), ("ffn_w_in", [D, 5632]), ("ffn_w_out", [2816, D]), ("nsa_w_in", [D, 2608]),
            ("rel_bias", [32, 16]), ("nsa_phi_pe", [2, 32, 64]), ("nsa_phi_w1", [2, 32, 64, 128]), ("nsa_phi_w2", [2, 128, 64]),
            ("nsa_w_o", [D, D]), ("norm_final", [D]), ("moe_router", [D, 8]), ("moe_w_in", [8, D, 2816]), ("moe_w_out", [8, 1408, D])]
CONST_SPECS = [("c_ohb", [32, 256]), ("c_cover", [256, 64]), ("c_E", [64, T]), ("c_cover_s", [128, 64])]


def t5_bucket_np(n):
    n = np.maximum(n, 0)
    logpart = 16 + (np.log(np.maximum(n, 1).astype(np.float32) / 16) / math.log(8) * 16).astype(np.int32)
    return np.where(n < 16, n, np.minimum(logpart, 31))


def make_core_consts(par):
    nj = NTILE // 2
    qidx = np.zeros((128, nj), np.int32)
    ka = np.zeros((nj, 128, 2, 64), np.float32)
    jj = np.arange(64)
    for j in range(nj):
        qt = 2 * j + par
        qpos = qt * 128 + np.arange(128)
        qidx[:, j] = qpos
        jt = qpos // 64
        forced = (jj[None, :] == 0) | (jj[None, :] == jt[:, None]) | (jj[None, :] == jt[:, None] - 1)
        vis = (jj[None, :] * 64) <= qpos[:, None]
        keep = (vis & ~forced).astype(np.float32)
        add = np.where(vis, np.where(forced, 1e4, 0.0), -1e30).astype(np.float32)
        ka[j, :, 0, :] = keep
        ka[j, :, 1, :] = add
    return {"c_qidx": qidx, "c_ka": ka, "par": np.tile(np.array([[1.0 - par, float(par)]], np.float32), (128, 1))}


def make_consts():
    c = {}
    bk = t5_bucket_np(np.arange(256))
    ohb = np.zeros((32, 256), np.float32)
    ohb[bk, np.arange(256)] = 1.0
    c["c_ohb"] = ohb
    cover = np.zeros((256, 64), np.float32)
    off = (np.arange(4)[:, None] - np.arange(2)[None, :]).reshape(-1)
    for j in range(64):
        for o in off:
            n = 4 * j + o
            if 0 <= n < NCB:
                cover[n, j] += 1.0
    c["c_cover"] = np.ascontiguousarray(cover[::-1])
    E = np.zeros((64, T), np.float32)
    E[np.arange(T) // 64, np.arange(T)] = 1.0
    c["c_E"] = E
    cov_s = np.zeros((128, 64), np.float32)
    for j in range(33):
        for o in off:
            n = 4 * j + o
            if 0 <= n < 127:
                cov_s[127 - n, j] += 1.0
    c["c_cover_s"] = cov_s
    return c


def s5_stage_w(K, C, I, O):
    s5_stage(K, None, C, I, O)


def build(upto=99, only=None):
    nc = bass.Bass("TRN2", target_bir_lowering=False)
    with ExitStack() as es:
        K = KB(nc, es)
        I, O = {}, {}
        I["xp"] = K.dram("xp", [T, D], F32, "ExternalInput")
        for nm, shp in IN_SPECS + CONST_SPECS:
            I[nm] = K.dram(nm, shp, F32, "ExternalInput")
        O["s5_re_p"] = K.dram("s5_re_p", [64, 64], F32, "ExternalOutput")
        O["s5_im_p"] = K.dram("s5_im_p", [64, 64], F32, "ExternalOutput")
        O["kv_p"] = K.dram("kv_p", [T, 1024], F32, "ExternalOutput")
        O["win_p"] = K.dram("win_p", [512, 512], F32, "ExternalOutput")
        I["xs"] = K.dram("xs", [NS, D], F32, "ExternalInput")
        I["st_re"] = K.dram("st_re", [NS, 4096], F32, "ExternalInput")
        I["st_im"] = K.dram("st_im", [NS, 4096], F32, "ExternalInput")
        O["s5_re_s"] = K.dram("s5_re_s", [NS, 4096], F32, "ExternalOutput")
        O["s5_im_s"] = K.dram("s5_im_s", [NS, 4096], F32, "ExternalOutput")
        O["kv_s"] = K.dram("kv_s", [NS, 1024], F32, "ExternalOutput")
        O["win_s"] = K.dram("win_s", [NS, 512], F32, "ExternalOutput")
        dbg = "ExternalOutput" if (upto < 99 or only is not None) else "Internal"
        O["g_scr"] = K.dram("g_scr", [T, D], BF16, dbg)
        O["y1_scr"] = K.dram("y1_scr", [T, D], F32, dbg)
        O["y2_scr"] = K.dram("y2_scr", [T, D], F32, dbg)
        O["gs_scr"] = K.dram("gs_scr", [NS, D], BF16, dbg)
        O["gs32_scr"] = K.dram("gs32_scr", [NS, D], F32, dbg)
        O["y1s_scr"] = K.dram("y1s_scr", [NS, D], F32, dbg)
        O["y2s_scr"] = K.dram("y2s_scr", [NS, D], F32, dbg)
        O["q_scr"] = K.dram("q_scr", [T, 1024], BF16, dbg)
        O["qs_scr"] = K.dram("qs_scr", [NS, 1024], BF16, dbg)
        O["kvb_scr"] = K.dram("kvb_scr", [T, 1536], BF16, dbg)
        O["kvbs_scr"] = K.dram("kvbs_scr", [NS, 1536], BF16, dbg)
        O["gate_scr"] = K.dram("gate_scr", [T, 48], F32, dbg)
        O["gates_scr"] = K.dram("gates_scr", [NS, 48], F32, dbg)
        O["fpad_scr"] = K.dram("fpad_scr", [16, 384], F32, "Internal")
        O["fc_scr"] = K.dram("fc_scr", [16, 8192], F32, "Internal")
        O["y3_scr"] = K.dram("y3_scr", [T // 2, D], F32, dbg)
        O["y3s_scr"] = K.dram("y3s_scr", [NS, D], F32, dbg)
        O["os_scr"] = K.dram("os_scr", [NS, 3, 16, 65], F32, dbg)
        if only is None or 6 in only:
            I["ckv"] = K.dram("ckv", [2560 * 128, 1024], F32, "ExternalInput")
        I["cwin"] = K.dram("cwin", [NS, 512, 512], F32, "ExternalInput")
        I["ptab"] = K.dram("ptab", [NS * 16], I32, "ExternalInput")
        I["par"] = K.dram("par", [128, 2], F32, "ExternalInput")
        I["c_qidx"] = K.dram("c_qidx", [128, NTILE // 2], I32, "ExternalInput")
        I["c_ka"] = K.dram("c_ka", [NTILE // 2, 128, 2, 64], F32, "ExternalInput")
        O["y_p"] = K.dram("y_p", [T // 2, D], F32, "ExternalOutput")
        O["y_s"] = K.dram("y_s", [NS, D], F32, "ExternalOutput")
        C = build_consts(K, es)
        stages = [(1, s5_stage_w), (1.5, sample_l0_f32), (2, glu_stage), (3, ffn_stage), (4, nsa_proj_stage), (5, nsa_attn_prompt), (6, nsa_attn_sample), (7, moe_stage)]
        for idx, fn in stages:
            if (only is None and idx <= upto) or (only is not None and idx in only):
                fn(K, C, I, O)
        K.S.finish()
    return nc


def kernel(**inp):
    f32 = lambda a: np.ascontiguousarray(np.asarray(a, dtype=np.float32))
    nc = build()
    shared = {nm: f32(inp[nm]) for nm, _ in IN_SPECS}
    shared.update(make_consts())
    shared["ckv"] = f32(inp["cache_kv"]).reshape(2560 * 128, 1024)
    in_maps = []
    for c in range(8):
        m = dict(shared)
        m["xp"] = f32(inp["x_prompt"][c // 2])
        sl = slice(NS * c, NS * (c + 1))
        m["xs"] = f32(inp["x_sample"][sl, 0, :])
        m["st_re"] = f32(inp["state_s5_re"][sl]).reshape(NS, 4096)
        m["st_im"] = f32(inp["state_s5_im"][sl]).reshape(NS, 4096)
        m["cwin"] = f32(inp["cache_win"][sl]).reshape(NS, 512, 512)
        m["ptab"] = np.ascontiguousarray(np.asarray(inp["page_table"][sl], dtype=np.int32)).reshape(NS * 16)
        m.update(make_core_consts(c % 2))
        in_maps.append(m)
    res = run_bass_kernel_spmd(nc, in_maps, core_ids=list(range(8))).results
    B = 4
    cat = lambda nm: np.concatenate([np.asarray(res[c][nm], dtype=np.float32) for c in range(8)], axis=0)
    y_prompt = np.zeros((B, NTILE, 128, D), np.float32)
    for c in range(8):
        yp = np.asarray(res[c]["y_p"], dtype=np.float32).reshape(NTILE // 2, 128, D)
        y_prompt[c // 2, (c % 2)::2] = yp
    y_prompt = y_prompt.reshape(B, T, D)
    y_sample = cat("y_s").reshape(128, 1, D)
    s5_re_p = np.stack([res[2 * b]["s5_re_p"] for b in range(B)]).astype(np.float32)
    s5_im_p = np.stack([res[2 * b]["s5_im_p"] for b in range(B)]).astype(np.float32)
    kv_p = np.stack([res[2 * b]["kv_p"] for b in range(B)]).astype(np.float32).reshape(B, T, 4, 4, 64)
    win_p = np.stack([res[2 * b]["win_p"] for b in range(B)]).astype(np.float32).reshape(B, 512, 2, 4, 64)
    s5_re_s = cat("s5_re_s").reshape(128, 64, 64)
    s5_im_s = cat("s5_im_s").reshape(128, 64, 64)
    kv_s = cat("kv_s").reshape(128, 1, 4, 4, 64)
    win_s = cat("win_s").reshape(128, 1, 2, 4, 64)
    return (y_prompt, y_sample, s5_re_p, s5_im_p, kv_p, win_p, s5_re_s, s5_im_s, kv_s, win_s)
```

```python
import math
import os
import numpy as np
STOP = float(os.environ.get('S5STOP', '99'))
NOSELF = os.environ.get('NOSELF', '0') == '1'
from contextlib import ExitStack
import concourse.bass as bass
import concourse.mybir as mybir
from concourse.bass_types import AP
from concourse.bass_utils import run_bass_kernel_spmd

F32 = mybir.dt.float32
BF16 = mybir.dt.bfloat16
I32 = mybir.dt.int32
AF = mybir.ActivationFunctionType
ALU = mybir.AluOpType
AX = mybir.AxisListType

D = 1024
T = 4096
NS = 16
EPS = 1e-6
TWO_PI = 2.0 * math.pi


class Sched:
    def __init__(self, nc, es, ndma=10):
        self.nc = nc
        self.eng = {"pe": nc.tensor, "dve": nc.vector, "act": nc.scalar, "pool": nc.gpsimd, "sp": nc.sync}
        self.sem = {k: es.enter_context(nc.semaphore("s_" + k)) for k in self.eng}
        self.cnt = {k: 0 for k in self.eng}
        self.dsem = {k: [es.enter_context(nc.semaphore("d_%s%d" % (k, i))) for i in range(ndma)] for k in ("sp", "act", "pool")}
        self.dcnt = {k: [0] * ndma for k in self.dsem}
        self.drr = {k: 0 for k in self.dsem}
        self.waited = {k: {} for k in self.eng}
        self.semobj = {}
        self.wr = {}
        self.rd = {}

    def _sid(self, s):
        i = id(s)
        self.semobj[i] = s
        return i

    def _wait(self, e, deps):
        w = self.waited[e]
        for sid, val in deps.items():
            if w.get(sid, 0) >= val:
                continue
            self.eng[e].wait_ge(self.semobj[sid], val)
            w[sid] = val

    def _deps(self, reads, writes, own_sid=None, skip_own=False):
        deps = {}

        def add(d):
            for sid, val in d.items():
                if skip_own and sid == own_sid:
                    continue
                if deps.get(sid, 0) < val:
                    deps[sid] = val
        for k in reads:
            add(self.wr.get(k, {}))
        for k in writes:
            add(self.wr.get(k, {}))
            add(self.rd.get(k, {}))
        return deps

    def op(self, e, fn, reads=(), writes=()):
        s = self.sem[e]
        sid = self._sid(s)
        deps = self._deps(reads, writes, own_sid=sid, skip_own=(e == "pe" or NOSELF))
        self._wait(e, deps)
        inst = fn(self.eng[e])
        self.cnt[e] += 1
        inst.then_inc(s, 1)
        val = self.cnt[e]
        for k in reads:
            self.rd.setdefault(k, {})[sid] = val
        for k in writes:
            self.wr[k] = {sid: val}
            self.rd[k] = {}
        return inst

    def dma(self, q, out, in_, reads=(), writes=(), add_writer=False, **kw):
        pool = self.dsem[q]
        i = self.drr[q]
        self.drr[q] = (i + 1) % len(pool)
        s = pool[i]
        sid = self._sid(s)
        deps = self._deps(reads, writes)
        if self.dcnt[q][i] > 0:
            deps[sid] = max(deps.get(sid, 0), self.dcnt[q][i])
        self._wait(q, deps)
        inst = self.eng[q].dma_start(out=out, in_=in_, **kw)
        self.dcnt[q][i] += 16
        inst.then_inc(s, 16)
        val = self.dcnt[q][i]
        for k in reads:
            self.rd.setdefault(k, {})[sid] = val
        for k in writes:
            if add_writer and k in self.wr:
                self.wr[k][sid] = val
            else:
                self.wr[k] = {sid: val}
                self.rd[k] = {}
        return inst

    def idma(self, out, in_, idx_ap, reads=(), writes=()):
        q = "pool"
        pool = self.dsem[q]
        i = self.drr[q]
        self.drr[q] = (i + 1) % len(pool)
        sm = pool[i]
        sid = self._sid(sm)
        deps = self._deps(reads, writes)
        if self.dcnt[q][i] > 0:
            deps[sid] = max(deps.get(sid, 0), self.dcnt[q][i])
        self._wait(q, deps)
        inst = self.eng[q].indirect_dma_start(out=out, out_offset=None, in_=in_, in_offset=bass.IndirectOffsetOnAxis(ap=idx_ap, axis=0))
        self.dcnt[q][i] += 16
        inst.then_inc(sm, 16)
        val = self.dcnt[q][i]
        for k in reads:
            self.rd.setdefault(k, {})[sid] = val
        for k in writes:
            self.wr[k] = {sid: val}
            self.rd[k] = {}
        return inst

    def barrier(self):
        deps = {}
        for e in self.eng:
            if self.cnt[e] > 0:
                deps[self._sid(self.sem[e])] = self.cnt[e]
        for q in self.dsem:
            for i, sm in enumerate(self.dsem[q]):
                if self.dcnt[q][i] > 0:
                    deps[self._sid(sm)] = self.dcnt[q][i]
        for e in self.eng:
            self._wait(e, dict(deps))

    def finish(self):
        deps = {}
        for k in list(self.wr.keys()):
            for sid, val in self.wr[k].items():
                deps[sid] = max(deps.get(sid, 0), val)
        for q in self.dsem:
            for i, s in enumerate(self.dsem[q]):
                if self.dcnt[q][i] > 0:
                    sid = self._sid(s)
                    deps[sid] = max(deps.get(sid, 0), self.dcnt[q][i])
        self._wait("sp", deps)


class KB:
    def __init__(self, nc, es):
        self.nc = nc
        self.es = es
        self.S = Sched(nc, es)
        self.dr = {}
        self._q = 0

    def sb(self, es, name, shape, dt):
        return es.enter_context(self.nc.sbuf_tensor(name, shape, dt))

    def ps(self, es, name, shape, dt):
        return es.enter_context(self.nc.psum_tensor(name, shape, dt))

    def dram(self, name, shape, dt, kind):
        t = self.nc.dram_tensor(name, shape, dt, kind=kind).ap()
        self.dr[name] = t
        return t

    def V(self, fn, r=(), w=()):
        return self.S.op("dve", fn, r, w)

    def A(self, fn, r=(), w=()):
        return self.S.op("act", fn, r, w)

    def G(self, fn, r=(), w=()):
        return self.S.op("pool", fn, r, w)

    def P(self, fn, r=(), w=()):
        return self.S.op("pe", fn, r, w)

    def dma(self, out, in_, r=(), w=(), q=None, **kw):
        if q is None:
            q = ("sp", "act")[self._q % 2]
            self._q += 1
        return self.S.dma(q, out, in_, r, w, **kw)


def bc(ap, shape):
    return ap.broadcast_to(shape)


def build_consts(K, es):
    c = {}
    idf = K.sb(es, "ident_f", [128, 128], F32)
    idb = K.sb(es, "ident_b", [128, 128], BF16)
    K.G(lambda e: e.memset(idf[:], 1.0), w=["idf"])
    K.G(lambda e: e.affine_select(out=idf[:], in_=idf[:], pattern=[[-1, 128]], compare_op=ALU.is_equal,
                                  fill=0.0, base=0, channel_multiplier=1), r=["idf"], w=["idf"])
    K.V(lambda e: e.tensor_copy(out=idb[:], in_=idf[:]), r=["idf"], w=["idb"])
    c["idf"], c["idb"] = idf, idb
    return c


def s5_stage(K, es_outer, C, I, O):
    nc = K.nc
    idf, idb = C["idf"], C["idb"]
    V, A, G, P, dma = K.V, K.A, K.G, K.P, K.dma
    with ExitStack() as es:
        _s5_body(K, es, C, I, O)
    K.S.barrier()


def _s5_body(K, es, C, I, O):
    idf, idb = C["idf"], C["idb"]
    V, A, G, P, dma = K.V, K.A, K.G, K.P, K.dma

    Win = K.sb(es, "s5Win", [128, 32, 2, 2, 128], BF16)
    Wout = K.sb(es, "s5Wout", [128, 32, 2, 128], BF16)
    Mint = K.sb(es, "s5Mint", [128, 64, 128], BF16)
    CA = K.sb(es, "s5CA", [128, 2, 32], F32)
    CBn = K.sb(es, "s5CBn", [128, 32], F32)
    CBp = K.sb(es, "s5CBp", [128, 32], F32)
    A1 = K.sb(es, "s5A1", [128, 2, 32], F32)
    gain0 = K.sb(es, "gain0", [128, D], F32)
    dskip = K.sb(es, "dskip", [128, D], F32)
    dma(gain0[:], AP(tensor=I["norm_mix"].tensor, offset=0, ap=[[0, 128], [1, D]]), w=["gain0"])
    dma(dskip[:], AP(tensor=I["s5_d"].tensor, offset=0, ap=[[0, 128], [1, D]]), w=["dskip"])

    with ExitStack() as pes:
        raw = K.sb(pes, "s5raw", [64, 3, 2, 64], F32)
        PRM = K.sb(pes, "s5prm", [128, 3, 32], F32)
        Wk = K.sb(pes, "s5wk", [128, 24, 32], F32)
        Wi = K.sb(pes, "s5wi", [128, 32], I32)
        PW = K.sb(pes, "s5pw", [128, 16, 2, 32], F32)
        Bt = K.sb(pes, "s5bt", [128, 2, 32, 16], F32)
        Ct = K.sb(pes, "s5ct", [128, 2, 32, 16], F32)
        BB = K.sb(pes, "s5bb", [128, 2, 32, 16], F32)
        craw = K.sb(pes, "s5craw", [128, 2, 2, 64], F32)
        T3 = K.sb(pes, "s5t3", [128, 4, 32, 16], F32)
        Ubf = K.sb(pes, "s5U", [128, 32, 2, 128], BF16)
        Vpad = K.sb(pes, "s5V", [128, 32, 2, 2, 128], BF16)
        maskLT = K.sb(pes, "s5mask", [128, 128], F32)
        pp = K.ps(pes, "s5pp", [128, 8, 64], F32)
        pc = [K.ps(pes, "s5pc%d" % i, [128, 512], F32) for i in range(2)]
        pm = [K.ps(pes, "s5pm%d" % i, [128, 512], F32) for i in range(2)]
        pw = [K.ps(pes, "s5pw%d" % i, [128, 1024], BF16) for i in range(2)]

        for i, nm in enumerate(["s5_lam_re", "s5_lam_im"]):
            dma(raw[:, i, :, :], AP(tensor=I[nm].tensor, offset=0, ap=[[64, 64], [0, 2], [1, 64]]), w=["raw"], add_writer=True)
        ldt = K.sb(pes, "s5ldt", [64, 1], F32)
        dma(ldt[:], AP(tensor=I["s5_log_dt"].tensor, offset=0, ap=[[1, 64], [1, 1]]), w=["ldt"])
        V(lambda e: e.tensor_copy(out=raw[:, 2, :, :].rearrange("p a b -> p (a b)"), in_=bc(ldt[:, 0:1], [64, 128])), r=["ldt"], w=["raw2"])
        for i in range(3):
            P(lambda e: e.transpose(out=pp[:, i, :], in_=raw[:, i, :, :], identity=idf[0:64, 0:64]), r=["raw", "raw2", "idf"], w=["pp"])
        for gl in range(2):
            sl = slice(64 * gl, 64 * gl + 64)
            V(lambda e: e.tensor_copy(out=PRM[sl, :, :], in_=pp[sl, 0:3, :].rearrange("p i (pr gl) -> p i pr gl", gl=2)[:, :, :, gl]),
              r=["pp"], w=["PRM"])
        if STOP <= 1:
            return
        LR, LI, LD = PRM[:, 0, :], PRM[:, 1, :], PRM[:, 2, :]
        _n = [0]

        def wk():
            _n[0] += 1
            return Wk[:, _n[0] - 1, :]
        rk, wkk = ["PRM", "Wk"], ["Wk"]

        def vtt(out, a, b, op):
            V(lambda e: e.tensor_tensor(out=out, in0=a, in1=b, op=op), r=rk + ["PW", "Bt", "Ct", "BB"], w=wkk)

        def vts(out, a, s1, s2, op0, op1=None):
            if op1 is None:
                V(lambda e: e.tensor_scalar(out=out, in0=a, scalar1=s1, scalar2=None, op0=op0), r=rk, w=wkk)
            else:
                V(lambda e: e.tensor_scalar(out=out, in0=a, scalar1=s1, scalar2=s2, op0=op0, op1=op1), r=rk, w=wkk)

        def sin_turns(out, turns):
            tf, fr, m = wk(), wk(), wk()
            V(lambda e: e.tensor_copy(out=Wi[:], in_=turns), r=rk, w=["Wi"])
            V(lambda e: e.tensor_copy(out=tf, in_=Wi[:]), r=["Wi"], w=wkk)
            vtt(fr, turns, tf, ALU.subtract)
            vts(m, fr, 0.5, None, ALU.is_gt)
            vtt(fr, fr, m, ALU.subtract)
            vts(m, fr, -0.5, None, ALU.is_lt)
            vtt(fr, fr, m, ALU.add)
            A(lambda e: e.activation(out=out, in_=fr, func=AF.Sin, scale=TWO_PI), r=rk, w=wkk)

        dt_, th, turns, turnc, sn, cs, ld, mag = [wk() for _ in range(8)]
        A(lambda e: e.activation(out=dt_, in_=LD, func=AF.Exp), r=rk, w=wkk)
        vtt(th, LI, dt_, ALU.mult)
        vts(turns, th, 1.0 / TWO_PI, None, ALU.mult)
        vts(turnc, turns, 0.25, None, ALU.add)
        sin_turns(sn, turns)
        sin_turns(cs, turnc)
        vtt(ld, LR, dt_, ALU.mult)
        A(lambda e: e.activation(out=mag, in_=ld, func=AF.Exp), r=rk, w=wkk)
        ar, ai = PW[:, 8, 0, :], PW[:, 8, 1, :]
        V(lambda e: e.tensor_tensor(out=ar, in0=mag, in1=cs, op=ALU.mult), r=rk, w=["PW"])
        V(lambda e: e.tensor_tensor(out=ai, in0=mag, in1=sn, op=ALU.mult), r=rk, w=["PW"])
        _n[0] = 8
        den, t1, t2, nr, fre, fim, q = [wk() for _ in range(7)]
        vtt(den, LR, LR, ALU.mult)
        vtt(t1, LI, LI, ALU.mult)
        vtt(den, den, t1, ALU.add)
        V(lambda e: e.reciprocal(out=den, in_=den), r=rk, w=wkk)
        V(lambda e: e.tensor_scalar(out=nr, in0=ar, scalar1=-1.0, scalar2=None, op0=ALU.add), r=rk + ["PW"], w=wkk)
        vtt(t1, nr, LR, ALU.mult)
        vtt(t2, ai, LI, ALU.mult)
        vtt(t1, t1, t2, ALU.add)
        vtt(fre, t1, den, ALU.mult)
        vtt(t1, ai, LR, ALU.mult)
        vtt(t2, nr, LI, ALU.mult)
        vtt(t1, t1, t2, ALU.subtract)
        vtt(fim, t1, den, ALU.mult)
        A(lambda e: e.activation(out=q, in_=ld, func=AF.Exp, scale=-2.0), r=rk, w=wkk)
        V(lambda e: e.memset(PW[:, 7, 0, :], 1.0), w=["PW"])
        V(lambda e: e.memset(PW[:, 7, 1, :], 0.0), w=["PW"])
        V(lambda e: e.tensor_tensor(out=PW[:, 6, 0, :], in0=ar, in1=q, op=ALU.mult), r=rk + ["PW"], w=["PW"])
        V(lambda e: e.scalar_tensor_tensor(out=PW[:, 6, 1, :], in0=ai, scalar=-1.0, in1=q, op0=ALU.mult, op1=ALU.mult),
          r=rk + ["PW"], w=["PW"])

        def cmul(o, x, y):
            a_, b_ = wk(), wk()
            _n[0] -= 2
            V(lambda e: e.tensor_tensor(out=a_, in0=PW[:, x, 0, :], in1=PW[:, y, 0, :], op=ALU.mult), r=["PW", "Wk"], w=wkk)
            V(lambda e: e.tensor_tensor(out=b_, in0=PW[:, x, 1, :], in1=PW[:, y, 1, :], op=ALU.mult), r=["PW", "Wk"], w=wkk)
            V(lambda e: e.tensor_tensor(out=PW[:, o, 0, :], in0=a_, in1=b_, op=ALU.subtract), r=["PW", "Wk"], w=["PW"])
            V(lambda e: e.tensor_tensor(out=a_, in0=PW[:, x, 0, :], in1=PW[:, y, 1, :], op=ALU.mult), r=["PW", "Wk"], w=wkk)
            V(lambda e: e.tensor_tensor(out=b_, in0=PW[:, x, 1, :], in1=PW[:, y, 0, :], op=ALU.mult), r=["PW", "Wk"], w=wkk)
            V(lambda e: e.tensor_tensor(out=PW[:, o, 1, :], in0=a_, in1=b_, op=ALU.add), r=["PW", "Wk"], w=["PW"])
        for k in range(2, 9):
            cmul(7 + k, 7 + k - 1, 8)
        for k in range(2, 8):
            cmul(7 - k, 7 - k + 1, 6)
        V(lambda e: e.tensor_copy(out=CA[:, 0, :], in_=PW[:, 15, 0, :]), r=["PW"], w=["CA"])
        V(lambda e: e.tensor_copy(out=CA[:, 1, :], in_=PW[:, 15, 0, :]), r=["PW"], w=["CA"])
        V(lambda e: e.tensor_copy(out=CBp[:], in_=PW[:, 15, 1, :]), r=["PW"], w=["CBp"])
        V(lambda e: e.tensor_scalar(out=CBn[:], in0=PW[:, 15, 1, :], scalar1=-1.0, scalar2=None, op0=ALU.mult), r=["PW"], w=["CBn"])
        V(lambda e: e.tensor_copy(out=A1[:], in_=PW[:, 8, :, :]), r=["PW"], w=["A1"])

        if STOP <= 2:
            return
        for i, nm in enumerate(["s5_b_re", "s5_b_im"]):
            for q4 in range(8):
                dma(Bt[:, i, 4 * q4:4 * q4 + 4, :], I[nm].rearrange("(pr gl) p c -> (gl p) pr c", gl=2)[:, 4 * q4:4 * q4 + 4, :],
                    w=["Bt"], add_writer=True)
        fre_b = bc(fre.unsqueeze(2), [128, 32, 16])
        fim_b = bc(fim.unsqueeze(2), [128, 32, 16])
        vtt(T3[:, 0], Bt[:, 0], fre_b, ALU.mult)
        vtt(T3[:, 1], Bt[:, 1], fim_b, ALU.mult)
        V(lambda e: e.tensor_tensor(out=BB[:, 0], in0=T3[:, 0], in1=T3[:, 1], op=ALU.subtract), r=["Wk"], w=["BB"])
        vtt(T3[:, 0], Bt[:, 1], fre_b, ALU.mult)
        vtt(T3[:, 1], Bt[:, 0], fim_b, ALU.mult)
        V(lambda e: e.tensor_tensor(out=BB[:, 1], in0=T3[:, 0], in1=T3[:, 1], op=ALU.add), r=["Wk"], w=["BB"])

        if STOP <= 2.3:
            return
        for i, nm in enumerate(["s5_c_re", "s5_c_im"]):
            cflat = I[nm]
            for k in range(8):
                for hh in range(2):
                    dma(craw[:, k % 2, hh, :], AP(tensor=cflat.tensor, offset=k * 128 * 64, ap=[[64, 128], [1, 64]]),
                        w=["craw%d" % (k % 2)], add_writer=(hh == 1))
                P(lambda e: e.transpose(out=pc[k % 2][:, 0:128], in_=craw[:, k % 2, :, :], identity=idf[:]),
                  r=["craw%d" % (k % 2), "idf"], w=["pc%d" % (k % 2)])
                for gl in range(2):
                    sl = slice(64 * gl, 64 * gl + 64)
                    V(lambda e: e.tensor_copy(
                        out=Ct[sl, i, 4 * k:4 * k + 4, :],
                        in_=pc[k % 2][sl, 0:128].rearrange("p (pr gl c) -> p pr gl c", gl=2, c=16)[:, :, gl, :]),
                      r=["pc%d" % (k % 2)], w=["Ct"])

        if STOP <= 2.5:
            return
        Uv = Ubf[:].rearrange("p pr r (s c) -> p pr r s c", c=16)
        for s in range(8):
            pi = 14 - s
            Pr = bc(PW[:, pi, 0, :].unsqueeze(2), [128, 32, 16])
            Pi = bc(PW[:, pi, 1, :].unsqueeze(2), [128, 32, 16])
            vtt(T3[:, 0], BB[:, 0], Pr, ALU.mult)
            vtt(T3[:, 1], BB[:, 1], Pi, ALU.mult)
            V(lambda e: e.tensor_tensor(out=Uv[:, :, 0, s, :], in0=T3[:, 0], in1=T3[:, 1], op=ALU.subtract), r=["Wk"], w=["Ubf"])
            vtt(T3[:, 2], BB[:, 1], Pr, ALU.mult)
            vtt(T3[:, 3], BB[:, 0], Pi, ALU.mult)
            V(lambda e: e.tensor_tensor(out=Uv[:, :, 1, s, :], in0=T3[:, 2], in1=T3[:, 3], op=ALU.add), r=["Wk"], w=["Ubf"])

        if STOP <= 2.7:
            return
        G(lambda e: e.memset(Vpad[:], 0.0), w=["Vpad"])
        G(lambda e: e.memset(Win[:], 0.0), w=["Win"])
        Vv = Vpad[:].rearrange("p pr g r (s c) -> p pr g r s c", c=16)
        Wv = Wout[:].rearrange("p pr r (s c) -> p pr r s c", c=16)
        for s in range(8):
            for which in range(2):
                pi = s if which == 0 else s + 8
                Pr = bc(PW[:, pi, 0, :].unsqueeze(2), [128, 32, 16])
                Pi = bc(PW[:, pi, 1, :].unsqueeze(2), [128, 32, 16])
                vtt(T3[:, 0], Ct[:, 0], Pr, ALU.mult)
                vtt(T3[:, 1], Ct[:, 1], Pi, ALU.mult)
                vtt(T3[:, 0], T3[:, 0], T3[:, 1], ALU.subtract)
                vtt(T3[:, 2], Ct[:, 0], Pi, ALU.mult)
                vtt(T3[:, 3], Ct[:, 1], Pr, ALU.mult)
                vtt(T3[:, 2], T3[:, 2], T3[:, 3], ALU.add)
                if which == 0:
                    for gl in range(2):
                        sl = slice(64 * gl, 64 * gl + 64)
                        V(lambda e: e.tensor_copy(out=Vv[sl, :, gl, 0, s, :], in_=T3[sl, 0]), r=["Wk"], w=["Vpad"])
                        V(lambda e: e.tensor_scalar(out=Vv[sl, :, gl, 1, s, :], in0=T3[sl, 2], scalar1=-1.0, scalar2=None,
                                                    op0=ALU.mult), r=["Wk"], w=["Vpad"])
                else:
                    V(lambda e: e.tensor_copy(out=Wv[:, :, 0, s, :], in_=T3[:, 0]), r=["Wk"], w=["Wout"])
                    V(lambda e: e.tensor_scalar(out=Wv[:, :, 1, s, :], in0=T3[:, 2], scalar1=-1.0, scalar2=None, op0=ALU.mult),
                      r=["Wk"], w=["Wout"])

        if STOP <= 2.9:
            return
        G(lambda e: e.memset(maskLT[:], 1.0), w=["maskLT"])
        G(lambda e: e.affine_select(out=maskLT[:].rearrange("p (s c) -> p s c", c=16), in_=maskLT[:].rearrange("p (s c) -> p s c", c=16),
                                    pattern=[[16, 8], [0, 16]], compare_op=ALU.is_ge, fill=0.0, base=15, channel_multiplier=-1),
          r=["maskLT"], w=["maskLT"])

        if STOP <= 3:
            return
        for pr in range(32):
            for gl in range(2):
                g = 2 * pr + gl
                pmx = pm[g % 2]
                key = "pm%d" % (g % 2)
                P(lambda e: e.matmul(pmx[:, 0:128], lhsT=Ubf[:, pr, 0, :], rhs=Vpad[:, pr, gl, 0, :], start=True, stop=False),
                  r=["Ubf", "Vpad"], w=[key])
                P(lambda e: e.matmul(pmx[:, 0:128], lhsT=Ubf[:, pr, 1, :], rhs=Vpad[:, pr, gl, 1, :], start=False, stop=True),
                  r=["Ubf", "Vpad"], w=[key])
                V(lambda e: e.tensor_tensor(out=Mint[:, g, :], in0=pmx[:, 0:128], in1=maskLT[:], op=ALU.mult), r=[key, "maskLT"], w=["Mint"])
            for ri in range(2):
                pwx = pw[ri]
                key = "pw%d" % ri
                P(lambda e: e.transpose(out=pwx[:, 0:128], in_=Ubf[:, pr, ri, :], identity=idb[:]), r=["Ubf", "idb"], w=[key])
                A(lambda e: e.copy(out=Win[:, pr, 0, ri, 0:64], in_=pwx[:, 0:64]), r=[key], w=["Win"])
                A(lambda e: e.copy(out=Win[:, pr, 1, ri, 64:128], in_=pwx[:, 64:128]), r=[key], w=["Win"])

    if STOP <= 4:
        return
    with ExitStack() as ses:
        NCH = 64
        xh = K.sb(ses, "s5xh", [NCH, 4, D], F32)
        junk = K.sb(ses, "s5junk", [NCH, D], F32)
        useg = K.sb(ses, "s5useg", [NCH, 64, 8, 16], BF16)
        uT = K.sb(ses, "s5uT", [128, 64, NCH], BF16)
        X = K.sb(ses, "s5X", [128, 2, 32, NCH], F32)
        Hbf = K.sb(ses, "s5H", [128, 2, 32, NCH + 1], BF16)
        carry = K.sb(ses, "s5carry", [128, 2, 32], F32)
        t1 = K.sb(ses, "s5t1", [128, 2, 32], F32)
        t2 = K.sb(ses, "s5t2", [128, 2, 32], F32)
        gtok = K.sb(ses, "s5gtok", [NCH, 8, D], BF16)
        ysb = K.sb(ses, "s5ysb", [128, 8, NCH], F32)
        yc = K.sb(ses, "s5yc", [NCH, 8, 128], F32)
        yc2 = K.sb(ses, "s5yc2", [NCH, 8, 128], F32)
        ss = K.sb(ses, "s5ss", [NCH, 8], F32)
        ptr = [K.ps(ses, "s5ptr%d" % i, [128, 16, NCH], BF16) for i in range(2)]
        px = [K.ps(ses, "s5px%d" % i, [128, 4, 2, NCH], F32) for i in range(2)]
        py = K.ps(ses, "s5py", [128, 8, NCH], F32)
        pyt = [K.ps(ses, "s5pyt%d" % i, [NCH, 4, 128], F32) for i in range(2)]

        V(lambda e: e.memset(carry[:], 0.0), w=["carry"])
        xp = I["xp"]
        for seg in range(T // (8 * NCH)):
            rows = xp[seg * 8 * NCH:(seg + 1) * 8 * NCH, :].rearrange("(n s) d -> n s d", s=8)
            for h in range(2):
                dma(xh[:], rows[:, 4 * h:4 * h + 4, :], w=["xh"])
                for s in range(4):
                    A(lambda e: e.activation(out=junk[:], in_=xh[:, s, :], func=AF.Square, accum_out=ss[:, 4 * h + s:4 * h + s + 1]),
                      r=["xh"], w=["junk", "ss"])
                V(lambda e: e.tensor_scalar(out=ss[:, 4 * h:4 * h + 4], in0=ss[:, 4 * h:4 * h + 4], scalar1=1.0 / D, scalar2=EPS,
                                            op0=ALU.mult, op1=ALU.add), r=["ss"], w=["ss"])
                A(lambda e: e.activation(out=ss[:, 4 * h:4 * h + 4], in_=ss[:, 4 * h:4 * h + 4], func=AF.Sqrt), r=["ss"], w=["ss"])
                V(lambda e: e.reciprocal(out=ss[:, 4 * h:4 * h + 4], in_=ss[:, 4 * h:4 * h + 4]), r=["ss"], w=["ss"])
                for s in range(4):
                    V(lambda e: e.scalar_tensor_tensor(out=useg[:, :, 4 * h + s, :], in0=xh[:, s, :].rearrange("n (g c) -> n g c", c=16),
                                                       scalar=ss[:, 4 * h + s:4 * h + s + 1],
                                                       in1=gain0[0:NCH, :].rearrange("n (g c) -> n g c", c=16), op0=ALU.mult, op1=ALU.mult),
                      r=["xh", "ss", "gain0"], w=["useg"])
            if STOP <= 5:
                return
            for g8 in range(8):
                pt_ = ptr[g8 % 2]
                key = "ptr%d" % (g8 % 2)
                for j in range(8):
                    g = 8 * g8 + j
                    P(lambda e: e.transpose(out=pt_[:, j, :], in_=useg[:, g, :, :], identity=idb[0:NCH, 0:NCH]),
                      r=["useg", "idb"], w=[key])
                A(lambda e: e.copy(out=uT[:, 8 * g8:8 * g8 + 8, :], in_=pt_[:, 0:8, :]), r=[key], w=["uT"])
            if STOP <= 6:
                return
            for q in range(8):
                px_ = px[q % 2]
                key = "px%d" % (q % 2)
                for a in range(4):
                    pr = 4 * q + a
                    for ri in range(2):
                        P(lambda e: e.matmul(px_[:, a, ri, :], lhsT=Win[:, pr, 0, ri, :], rhs=uT[:, 2 * pr, :], start=True, stop=False),
                          r=["Win", "uT"], w=[key])
                        P(lambda e: e.matmul(px_[:, a, ri, :], lhsT=Win[:, pr, 1, ri, :], rhs=uT[:, 2 * pr + 1, :], start=False, stop=True),
                          r=["Win", "uT"], w=[key])
                A(lambda e: e.copy(out=X[:, :, 4 * q:4 * q + 4, :], in_=px_[:].rearrange("p a r n -> p r a n")), r=[key], w=["X"])
            if STOP <= 7:
                return
            V(lambda e: e.tensor_copy(out=Hbf[:, :, :, 0], in_=carry[:]), r=["carry"], w=["Hbf"])
            for n in range(NCH):
                Sp = carry[:] if n == 0 else X[:, :, :, n - 1]
                V(lambda e: e.tensor_tensor(out=t1[:], in0=Sp, in1=CA[:], op=ALU.mult), r=["X", "carry", "CA"], w=["t1"])
                V(lambda e: e.tensor_tensor(out=t2[:, 0, :], in0=Sp[:, 1, :], in1=CBn[:], op=ALU.mult), r=["X", "carry", "CBn"], w=["t2"])
                V(lambda e: e.tensor_tensor(out=t2[:, 1, :], in0=Sp[:, 0, :], in1=CBp[:], op=ALU.mult), r=["X", "carry", "CBp"], w=["t2"])
                V(lambda e: e.tensor_tensor(out=t1[:], in0=t1[:], in1=t2[:], op=ALU.add), r=["t1", "t2"], w=["t1"])
                V(lambda e: e.tensor_tensor(out=X[:, :, :, n], in0=X[:, :, :, n], in1=t1[:], op=ALU.add), r=["t1", "X"], w=["X"])
            V(lambda e: e.tensor_copy(out=carry[:], in_=X[:, :, :, NCH - 1]), r=["X"], w=["carry"])
            A(lambda e: e.copy(out=Hbf[:, :, :, 1:NCH + 1], in_=X[:]), r=["X"], w=["Hbf"])
            if STOP <= 8:
                return
            for g8 in range(8):
                for j in range(8):
                    g = 8 * g8 + j
                    pr, gl = g // 2, g % 2
                    sl = slice(64 * gl, 64 * gl + 64)
                    P(lambda e: e.matmul(py[:, j, :], lhsT=Mint[:, g, :], rhs=uT[:, g, :], start=True, stop=False),
                      r=["Mint", "uT"], w=["py"])
                    P(lambda e: e.matmul(py[:, j, :], lhsT=Wout[sl, pr, 0, :], rhs=Hbf[sl, 0, pr, 0:NCH], start=False, stop=False),
                      r=["Wout", "Hbf"], w=["py"])
                    P(lambda e: e.matmul(py[:, j, :], lhsT=Wout[sl, pr, 1, :], rhs=Hbf[sl, 1, pr, 0:NCH], start=False, stop=True),
                      r=["Wout", "Hbf"], w=["py"])
                A(lambda e: e.copy(out=ysb[:], in_=py[:]), r=["py"], w=["ysb"])
                for j in range(8):
                    pyt_ = pyt[j // 4]
                    P(lambda e: e.transpose(out=pyt_[:, j % 4, :], in_=ysb[:, j, :], identity=idf[:]), r=["ysb", "idf"], w=["pyt%d" % (j // 4)])
                cs_ = slice(128 * g8, 128 * g8 + 128)
                for hh in range(2):
                    V(lambda e: e.tensor_copy(
                        out=yc[:, :, 64 * hh:64 * hh + 64].rearrange("n s (j c) -> n s j c", c=16),
                        in_=pyt[hh][:].rearrange("n j (s c) -> n s j c", c=16)), r=["pyt%d" % hh], w=["yc"])
                V(lambda e: e.tensor_tensor(out=yc2[:].rearrange("n s (j c) -> n s j c", c=16),
                                            in0=useg[:, 8 * g8:8 * g8 + 8, :, :].rearrange("n j s c -> n s j c"),
                                            in1=bc(dskip[0:NCH, cs_].unsqueeze(1), [NCH, 8, 128]).rearrange("n s (j c) -> n s j c", c=16), op=ALU.mult),
                  r=["useg", "dskip"], w=["yc2"])
                V(lambda e: e.tensor_tensor(out=yc[:], in0=yc[:], in1=yc2[:], op=ALU.add), r=["yc", "yc2"], w=["yc"])
                V(lambda e: e.tensor_tensor(out=yc2[:], in0=yc[:], in1=yc[:], op=ALU.mult), r=["yc"], w=["yc2"])
                V(lambda e: e.tensor_scalar(out=yc2[:], in0=yc2[:], scalar1=0.044715, scalar2=1.0, op0=ALU.mult, op1=ALU.add), r=["yc2"], w=["yc2"])
                V(lambda e: e.tensor_tensor(out=yc2[:], in0=yc2[:], in1=yc[:], op=ALU.mult), r=["yc", "yc2"], w=["yc2"])
                A(lambda e: e.activation(out=yc2[:], in_=yc2[:], func=AF.Sigmoid, scale=1.5957691216057308), r=["yc2"], w=["yc2"])
                V(lambda e: e.tensor_tensor(out=gtok[:, :, cs_], in0=yc[:], in1=yc2[:], op=ALU.mult), r=["yc", "yc2"], w=["gtok"])
            dma(O["g_scr"][seg * 8 * NCH:(seg + 1) * 8 * NCH, :].rearrange("(n s) d -> n s d", s=8), gtok[:], r=["gtok"], w=["g_scr"],
                add_writer=True)
        stT = K.sb(ses, "s5stT", [32, 2, 128], F32)
        for ri, nm in enumerate(["s5_re_p", "s5_im_p"]):
            P(lambda e: e.transpose(out=py[0:32, ri, :].rearrange("p n -> p n") if False else pyt[0][0:32, ri, :], in_=carry[:, ri, :], identity=idf[:]),
              r=["carry", "idf"], w=["pyt0"])
            V(lambda e: e.tensor_copy(out=stT[:, ri, :], in_=pyt[0][0:32, ri, :]), r=["pyt0"], w=["stT"])
            dma(O[nm].rearrange("(pr gl) p -> pr (gl p)", gl=2), stT[:, ri, :], r=["stT"], w=[nm])


    K.S.barrier()
    _s5_sample(K, C, I, O, Win, Wout, Mint, A1, gain0, dskip)


def gelu_tanh(K, y, ykey, tmp, tkey, out, okey):
    V, A = K.V, K.A
    V(lambda e: e.tensor_tensor(out=tmp, in0=y, in1=y, op=ALU.mult), r=[ykey], w=[tkey])
    V(lambda e: e.tensor_scalar(out=tmp, in0=tmp, scalar1=0.044715, scalar2=1.0, op0=ALU.mult, op1=ALU.add), r=[tkey], w=[tkey])
    V(lambda e: e.tensor_tensor(out=tmp, in0=tmp, in1=y, op=ALU.mult), r=[ykey, tkey], w=[tkey])
    A(lambda e: e.activation(out=tmp, in_=tmp, func=AF.Sigmoid, scale=1.5957691216057308), r=[tkey], w=[tkey])
    V(lambda e: e.tensor_tensor(out=out, in0=y, in1=tmp, op=ALU.mult), r=[ykey, tkey], w=[okey])


def _s5_sample(K, C, I, O, Win, Wout, Mint, A1, gain0, dskip):
    idf, idb = C["idf"], C["idb"]
    V, A, G, P, dma = K.V, K.A, K.G, K.P, K.dma
    N = NS
    with ExitStack() as es:
        xs = K.sb(es, "ss_x", [N, D], F32)
        ss = K.sb(es, "ss_ss", [N, 1], F32)
        us = K.sb(es, "ss_us", [N, D], F32)
        u0 = K.sb(es, "ss_u0", [N, 64, 8, 16], BF16)
        uT0 = K.sb(es, "ss_uT0", [128, 64, N], BF16)
        uT7 = K.sb(es, "ss_uT7", [128, 64, N], BF16)
        st = K.sb(es, "ss_st", [N, 2, 4096], F32)
        stO = st
        H0 = K.sb(es, "ss_H0", [128, 2, 32, N], F32)
        H0b = K.sb(es, "ss_H0b", [128, 2, 32, N], BF16)
        X7 = K.sb(es, "ss_X7", [128, 2, 32, N], F32)
        H1 = K.sb(es, "ss_H1", [128, 2, 32, N], F32)
        t1 = K.sb(es, "ss_t1", [128, 2, 32, N], F32)
        t2 = K.sb(es, "ss_t2", [128, 2, 32, N], F32)
        ys = K.sb(es, "ss_ys", [N, D], F32)
        y2 = K.sb(es, "ss_y2", [N, D], F32)
        junk = y2
        gsb = K.sb(es, "ss_g", [N, D], BF16)
        ptrA = K.ps(es, "ss_ptrA", [128, 64, N], BF16)
        ptrB = K.ps(es, "ss_ptrB", [128, 64, N], BF16)
        pxs = [K.ps(es, "ss_px%d" % i, [128, 16, 2, N], F32) for i in range(2)]
        ph = [K.ps(es, "ss_ph%d" % i, [128, 32, N], F32) for i in range(2)]
        pys = [K.ps(es, "ss_py%d" % i, [N, 32, 16], F32) for i in range(2)]

        dma(xs[:], I["xs"], w=["sxs"])
        dma(st[:, 0, :], I["st_re"], w=["sst"])
        dma(st[:, 1, :], I["st_im"], w=["sst"], add_writer=True)
        A(lambda e: e.activation(out=junk[:], in_=xs[:], func=AF.Square, accum_out=ss[:, 0:1]), r=["sxs"], w=["sy2", "sss"])
        V(lambda e: e.tensor_scalar(out=ss[:], in0=ss[:], scalar1=1.0 / D, scalar2=EPS, op0=ALU.mult, op1=ALU.add), r=["sss"], w=["sss"])
        A(lambda e: e.activation(out=ss[:], in_=ss[:], func=AF.Sqrt), r=["sss"], w=["sss"])
        V(lambda e: e.reciprocal(out=ss[:], in_=ss[:]), r=["sss"], w=["sss"])
        V(lambda e: e.scalar_tensor_tensor(out=us[:], in0=xs[:], scalar=ss[:, 0:1], in1=gain0[0:N, :], op0=ALU.mult, op1=ALU.mult),
          r=["sxs", "sss", "gain0"], w=["sus"])
        usv = us[:].rearrange("n (g c) -> n g c", c=16)
        G(lambda e: e.memset(u0[:], 0.0), w=["su0"])
        V(lambda e: e.tensor_copy(out=u0[:, :, 0, :], in_=usv), r=["sus", "su0"], w=["su0"])
        for g in range(64):
            P(lambda e: e.transpose(out=ptrA[:, g, :], in_=u0[:, g, :, :], identity=idb[0:N, 0:N]), r=["su0", "idb"], w=["sptrA"])
        G(lambda e: e.memset(u0[:], 0.0), w=["su0"])
        V(lambda e: e.tensor_copy(out=u0[:, :, 7, :], in_=usv), r=["sus", "su0"], w=["su0"])
        for g in range(64):
            P(lambda e: e.transpose(out=ptrB[:, g, :], in_=u0[:, g, :, :], identity=idb[0:N, 0:N]), r=["su0", "idb"], w=["sptrB"])
        A(lambda e: e.copy(out=uT0[:], in_=ptrA[:]), r=["sptrA"], w=["suT0"])
        A(lambda e: e.copy(out=uT7[:], in_=ptrB[:]), r=["sptrB"], w=["suT7"])
        for pr in range(32):
            h_, a_ = pr // 16, pr % 16
            for ri in range(2):
                P(lambda e: e.matmul(pxs[h_][:, a_, ri, :], lhsT=Win[:, pr, 0, ri, :], rhs=uT7[:, 2 * pr, :], start=True, stop=False),
                  r=["Win", "suT7"], w=["spx%d" % h_])
                P(lambda e: e.matmul(pxs[h_][:, a_, ri, :], lhsT=Win[:, pr, 1, ri, :], rhs=uT7[:, 2 * pr + 1, :], start=False, stop=True),
                  r=["Win", "suT7"], w=["spx%d" % h_])
        for h_ in range(2):
            A(lambda e: e.copy(out=X7[:, :, 16 * h_:16 * h_ + 16, :], in_=pxs[h_][:].rearrange("p a r n -> p r a n")), r=["spx%d" % h_], w=["sX7"])
        for ri in range(2):
            for pr in range(32):
                P(lambda e: e.transpose(out=ph[ri][:, pr, :], in_=st[:, ri, pr * 128:(pr + 1) * 128], identity=idf[0:N, 0:N]),
                  r=["sst", "idf"], w=["sph%d" % ri])
            V(lambda e: e.tensor_copy(out=H0[:, ri, :, :], in_=ph[ri][:]), r=["sph%d" % ri], w=["sH0"])
        A(lambda e: e.copy(out=H0b[:], in_=H0[:]), r=["sH0"], w=["sH0b"])
        a1r = bc(A1[:, 0, :].unsqueeze(2), [128, 32, N])
        a1i = bc(A1[:, 1, :].unsqueeze(2), [128, 32, N])
        for ri in range(2):
            V(lambda e: e.tensor_tensor(out=t1[:, ri], in0=H0[:, ri], in1=a1r, op=ALU.mult), r=["sH0", "A1"], w=["st1"])
            V(lambda e: e.tensor_tensor(out=t2[:, ri], in0=H0[:, 1 - ri], in1=a1i, op=ALU.mult), r=["sH0", "A1"], w=["st2"])
        V(lambda e: e.tensor_tensor(out=H1[:, 0], in0=t1[:, 0], in1=t2[:, 0], op=ALU.subtract), r=["st1", "st2"], w=["sH1"])
        V(lambda e: e.tensor_tensor(out=H1[:, 1], in0=t1[:, 1], in1=t2[:, 1], op=ALU.add), r=["st1", "st2"], w=["sH1"])
        V(lambda e: e.tensor_tensor(out=H1[:], in0=H1[:], in1=X7[:], op=ALU.add), r=["sH1", "sX7"], w=["sH1"])
        idx = 0
        for ri in range(2):
            for q in range(8):
                b_ = idx % 2
                idx += 1
                pv = pxs[b_][0:N].rearrange("p a r n -> p (a r n)")
                for j in range(4):
                    pr = 4 * q + j
                    P(lambda e: e.transpose(out=pv[:, j * 128:(j + 1) * 128], in_=H1[:, ri, pr, :], identity=idf[:]),
                      r=["sH1", "idf"], w=["spx%d" % b_])
                V(lambda e: e.tensor_copy(out=stO[:, ri, 512 * q:512 * q + 512], in_=pv[:, 0:512]), r=["spx%d" % b_], w=["sst"])
        dma(O["s5_re_s"], stO[:, 0, :], r=["sst"], w=["s5_re_s"])
        dma(O["s5_im_s"], stO[:, 1, :], r=["sst"], w=["s5_im_s"])
        for g in range(64):
            pr, gl = g // 2, g % 2
            sl = slice(64 * gl, 64 * gl + 64)
            h_ = g // 32
            P(lambda e: e.matmul(pys[h_][:, g % 32, :], lhsT=uT0[:, g, :], rhs=Mint[:, g, 0:16], start=True, stop=False),
              r=["suT0", "Mint"], w=["spy%d" % h_])
            P(lambda e: e.matmul(pys[h_][:, g % 32, :], lhsT=H0b[sl, 0, pr, :], rhs=Wout[sl, pr, 0, 0:16], start=False, stop=False),
              r=["sH0b", "Wout"], w=["spy%d" % h_])
            P(lambda e: e.matmul(pys[h_][:, g % 32, :], lhsT=H0b[sl, 1, pr, :], rhs=Wout[sl, pr, 1, 0:16], start=False, stop=True),
              r=["sH0b", "Wout"], w=["spy%d" % h_])
        V(lambda e: e.tensor_tensor(out=y2[:], in0=us[:], in1=dskip[0:N, :], op=ALU.mult), r=["sus", "dskip"], w=["sy2"])
        for h_ in range(2):
            V(lambda e: e.tensor_tensor(out=ys[:, 512 * h_:512 * h_ + 512], in0=pys[h_][:].rearrange("n g c -> n (g c)"),
                                        in1=y2[:, 512 * h_:512 * h_ + 512], op=ALU.add), r=["spy%d" % h_, "sy2"], w=["sys"])
        gelu_tanh(K, ys[:], "sys", y2[:], "sy2", gsb[:], "sgsb")
        dma(O["gs_scr"], gsb[:], r=["sgsb"], w=["gs_scr"])
        V(lambda e: e.tensor_tensor(out=ys[:], in0=ys[:], in1=y2[:], op=ALU.mult), r=["sys", "sy2", "sgsb"], w=["sys"])
        dma(O["gs32_scr"], ys[:], r=["sys"], w=["gs32_scr"])

def load_w_bf16(K, wt, src, ncols, step=1024):
    v = src.rearrange("(k p) n -> p k n", p=128)
    for c0 in range(0, ncols, step):
        c1 = min(ncols, c0 + step)
        K.S.dma("pool", wt[:, :, c0:c1], v[:, :, c0:c1], (), ["w_" + wt.name], add_writer=(c0 > 0))


def rms_to_bf16(K, x_t, xkey, gain, h_t, hkey, junk, ss, sfx, Pn=128):
    V, A = K.V, K.A
    A(lambda e: e.activation(out=junk[0:Pn, :], in_=x_t[0:Pn, :], func=AF.Square, accum_out=ss[0:Pn, 0:1]), r=[xkey], w=["junk" + sfx, "ss" + sfx])
    V(lambda e: e.tensor_scalar(out=ss[0:Pn, 0:1], in0=ss[0:Pn, 0:1], scalar1=1.0 / D, scalar2=EPS, op0=ALU.mult, op1=ALU.add), r=["ss" + sfx], w=["ss" + sfx])
    A(lambda e: e.activation(out=ss[0:Pn, 0:1], in_=ss[0:Pn, 0:1], func=AF.Sqrt), r=["ss" + sfx], w=["ss" + sfx])
    V(lambda e: e.reciprocal(out=ss[0:Pn, 0:1], in_=ss[0:Pn, 0:1]), r=["ss" + sfx], w=["ss" + sfx])
    V(lambda e: e.scalar_tensor_tensor(out=h_t[0:Pn, :], in0=x_t[0:Pn, :], scalar=ss[0:Pn, 0:1], in1=gain[0:Pn, :], op0=ALU.mult, op1=ALU.mult),
      r=[xkey, "ss" + sfx, "gain"], w=[hkey])


def transpose_tiles(K, src, skey, nk, dstT, dkey, ptb, idb, Pn=128):
    for k0 in range(0, nk, 8):
        k1 = min(nk, k0 + 8)
        for k in range(k0, k1):
            K.P(lambda e: e.transpose(out=ptb[:, k - k0, 0:Pn], in_=src[0:Pn, k * 128:(k + 1) * 128], identity=idb[0:Pn, 0:Pn]),
                r=[skey, "idb"], w=["ptb"])
        K.A(lambda e: e.copy(out=dstT[:, k0:k1, 0:Pn], in_=ptb[:, 0:k1 - k0, 0:Pn]), r=["ptb"], w=[dkey])


NTILE = T // 128


def tile_rows(t):
    return (128, slice(t * 128, (t + 1) * 128)) if t < NTILE else (NS, None)


def pick(big, small, t):
    Pn, rs = tile_rows(t)
    return big[rs, :] if rs is not None else small


def glu_stage(K, C, I, O):
    idb = C["idb"]
    V, A, G, P, dma = K.V, K.A, K.G, K.P, K.dma
    with ExitStack() as es:
        wg = K.sb(es, "wglu", [128, 8, 2048], BF16)
        load_w_bf16(K, wg, I["s5_w_glu"], 2048)
        gt = [K.sb(es, "glu_g%d" % i, [128, D], BF16) for i in range(2)]
        xt = [K.sb(es, "glu_x%d" % i, [128, D], F32) for i in range(2)]
        gT = [K.sb(es, "glu_gT%d" % i, [128, 8, 128], BF16) for i in range(2)]
        sg = [K.sb(es, "glu_sg%d" % i, [128, D], F32) for i in range(2)]
        ptb = K.ps(es, "glu_ptb", [128, 8, 128], BF16)
        pa = [K.ps(es, "glu_pa%d" % i, [128, 512], F32) for i in range(4)]
        for t in range(NTILE):
            b = t % 2
            sx = str(b)
            Pn, _ = tile_rows(t)
            dma(gt[b][0:Pn, :], pick(O["g_scr"], O["gs_scr"], t), r=["g_scr", "gs_scr"], w=["gt" + sx])
            dma(xt[b][0:Pn, :], pick(I["xp"], I["xs"], t), w=["xt" + sx])
            transpose_tiles(K, gt[b], "gt" + sx, 8, gT[b], "gT" + sx, ptb, idb, Pn)
            for c in range(4):
                for k in range(8):
                    P(lambda e: e.matmul(pa[c][0:Pn, :], lhsT=gT[b][:, k, 0:Pn], rhs=wg[:, k, c * 512:(c + 1) * 512], start=(k == 0), stop=(k == 7)),
                      r=["gT" + sx, "w_wglu"], w=["pa%d" % c])
            for hh in range(2):
                cs = slice(512 * hh, 512 * hh + 512)
                A(lambda e: e.activation(out=sg[b][0:Pn, cs], in_=pa[2 + hh][0:Pn, :], func=AF.Sigmoid), r=["pa%d" % (2 + hh)], w=["sg" + sx])
                V(lambda e: e.tensor_tensor(out=sg[b][0:Pn, cs], in0=pa[hh][0:Pn, :], in1=sg[b][0:Pn, cs], op=ALU.mult), r=["pa%d" % hh, "sg" + sx], w=["sg" + sx])
            V(lambda e: e.tensor_tensor(out=xt[b][0:Pn, :], in0=xt[b][0:Pn, :], in1=sg[b][0:Pn, :], op=ALU.add), r=["xt" + sx, "sg" + sx], w=["xt" + sx])
            dma(pick(O["y1_scr"], O["y1s_scr"], t), xt[b][0:Pn, :], r=["xt" + sx], w=["y1_scr"], add_writer=True)
    K.S.barrier()


def ffn_stage(K, C, I, O):
    idb = C["idb"]
    V, A, G, P, dma = K.V, K.A, K.G, K.P, K.dma
    FF = 2816
    CW = 352
    with ExitStack() as es:
        w1 = K.sb(es, "ffw1", [128, 8, 2 * FF], BF16)
        w2 = K.sb(es, "ffw2", [128, 22, D], BF16)
        load_w_bf16(K, w1, I["ffn_w_in"], 2 * FF)
        load_w_bf16(K, w2, I["ffn_w_out"], D)
        gain = K.sb(es, "ff_gain", [128, D], F32)
        dma(gain[:], AP(tensor=I["norm_ffn"].tensor, offset=0, ap=[[0, 128], [1, D]]), w=["gain"])
        yt = [K.sb(es, "ff_y%d" % i, [128, D], F32) for i in range(2)]
        hb = [K.sb(es, "ff_h%d" % i, [128, D], BF16) for i in range(2)]
        hT = [K.sb(es, "ff_hT%d" % i, [128, 8, 128], BF16) for i in range(2)]
        act = [K.sb(es, "ff_act%d" % i, [128, FF], BF16) for i in range(2)]
        actT = [K.sb(es, "ff_actT%d" % i, [128, 22, 128], BF16) for i in range(2)]
        sl_ = [K.sb(es, "ff_s%d" % i, [128, CW], F32) for i in range(2)]
        junk = K.sb(es, "ff_junk", [128, D], F32)
        ss = [K.sb(es, "ff_ss%d" % i, [128, 1], F32) for i in range(2)]
        ptb = K.ps(es, "ff_ptb", [128, 8, 128], BF16)
        pa = [K.ps(es, "ff_pa%d" % i, [128, 512], F32) for i in range(2)]
        pb = [K.ps(es, "ff_pb%d" % i, [128, 512], F32) for i in range(2)]
        po = [K.ps(es, "ff_po%d" % i, [128, 512], F32) for i in range(2)]
        for t in range(NTILE):
            b = t % 2
            sx = str(b)
            Pn, _ = tile_rows(t)
            dma(yt[b][0:Pn, :], pick(O["y1_scr"], O["y1s_scr"], t), r=["y1_scr"], w=["yt" + sx])
            rms_to_bf16(K, yt[b], "yt" + sx, gain, hb[b], "hb" + sx, junk, ss[b], sx, Pn)
            transpose_tiles(K, hb[b], "hb" + sx, 8, hT[b], "hT" + sx, ptb, idb, Pn)
            for j in range(8):
                jb = j % 2
                for k in range(8):
                    P(lambda e: e.matmul(pa[jb][0:Pn, 0:CW], lhsT=hT[b][:, k, 0:Pn], rhs=w1[:, k, CW * j:CW * j + CW], start=(k == 0), stop=(k == 7)),
                      r=["hT" + sx, "w_ffw1"], w=["fpa%d" % jb])
                for k in range(8):
                    P(lambda e: e.matmul(pb[jb][0:Pn, 0:CW], lhsT=hT[b][:, k, 0:Pn], rhs=w1[:, k, FF + CW * j:FF + CW * j + CW], start=(k == 0), stop=(k == 7)),
                      r=["hT" + sx, "w_ffw1"], w=["fpb%d" % jb])
                A(lambda e: e.activation(out=sl_[jb][0:Pn, :], in_=pa[jb][0:Pn, 0:CW], func=AF.Silu), r=["fpa%d" % jb], w=["fsl%d" % jb])
                V(lambda e: e.tensor_tensor(out=act[b][0:Pn, CW * j:CW * j + CW], in0=pb[jb][0:Pn, 0:CW], in1=sl_[jb][0:Pn, :], op=ALU.mult),
                  r=["fpb%d" % jb, "fsl%d" % jb], w=["act" + sx])
            transpose_tiles(K, act[b], "act" + sx, 22, actT[b], "actT" + sx, ptb, idb, Pn)
            for c in range(2):
                for k in range(22):
                    P(lambda e: e.matmul(po[c][0:Pn, :], lhsT=actT[b][:, k, 0:Pn], rhs=w2[:, k, c * 512:(c + 1) * 512], start=(k == 0), stop=(k == 21)),
                      r=["actT" + sx, "w_ffw2"], w=["fpo%d" % c])
                V(lambda e: e.tensor_tensor(out=yt[b][0:Pn, c * 512:(c + 1) * 512], in0=po[c][0:Pn, :], in1=yt[b][0:Pn, c * 512:(c + 1) * 512], op=ALU.add),
                  r=["fpo%d" % c, "yt" + sx], w=["yt" + sx])
            dma(pick(O["y2_scr"], O["y2s_scr"], t), yt[b][0:Pn, :], r=["yt" + sx], w=["y2_scr"], add_writer=True)
    K.S.barrier()


def nsa_proj_stage(K, C, I, O):
    idb = C["idb"]
    V, A, G, P, dma = K.V, K.A, K.G, K.P, K.dma
    NCOL = 2608
    with ExitStack() as es:
        wn = K.sb(es, "nsw", [128, 8, NCOL], BF16)
        load_w_bf16(K, wn, I["nsa_w_in"], NCOL)
        gain = K.sb(es, "ns_gain", [128, D], F32)
        dma(gain[:], AP(tensor=I["norm_mix"].tensor, offset=D, ap=[[0, 128], [1, D]]), w=["gain"])
        yt = [K.sb(es, "ns_y%d" % i, [128, D], F32) for i in range(2)]
        hb = [K.sb(es, "ns_h%d" % i, [128, D], BF16) for i in range(2)]
        hT = [K.sb(es, "ns_hT%d" % i, [128, 8, 128], BF16) for i in range(2)]
        kv = [K.sb(es, "ns_kv%d" % i, [128, 1536], F32) for i in range(2)]
        kvb = [K.sb(es, "ns_kvb%d" % i, [128, 1536], BF16) for i in range(2)]
        qb = [K.sb(es, "ns_qb%d" % i, [128, 1024], BF16) for i in range(2)]
        gt_ = [K.sb(es, "ns_gt%d" % i, [128, 48], F32) for i in range(2)]
        junk = K.sb(es, "ns_junk", [128, D], F32)
        ss = [K.sb(es, "ns_ss%d" % i, [128, 1], F32) for i in range(2)]
        ptb = K.ps(es, "ns_ptb", [128, 8, 128], BF16)
        pk = [K.ps(es, "ns_pk%d" % i, [128, 512], F32) for i in range(6)]
        for t in range(NTILE):
            b = t % 2
            sx = str(b)
            Pn, rs = tile_rows(t)
            dma(yt[b][0:Pn, :], pick(O["y2_scr"], O["y2s_scr"], t), r=["y2_scr"], w=["yt" + sx])
            rms_to_bf16(K, yt[b], "yt" + sx, gain, hb[b], "hb" + sx, junk, ss[b], sx, Pn)
            transpose_tiles(K, hb[b], "hb" + sx, 8, hT[b], "hT" + sx, ptb, idb, Pn)
            for c in range(6):
                c0 = 512 * c
                cw = min(512, NCOL - c0)
                for k in range(8):
                    P(lambda e: e.matmul(pk[c][0:Pn, 0:cw], lhsT=hT[b][:, k, 0:Pn], rhs=wn[:, k, c0:c0 + cw], start=(k == 0), stop=(k == 7)),
                      r=["hT" + sx, "w_nsw"], w=["npk%d" % c])
            for c in range(2):
                A(lambda e: e.activation(out=qb[b][0:Pn, 512 * c:512 * c + 512], in_=pk[c][0:Pn, :], func=AF.Identity, scale=0.125),
                  r=["npk%d" % c], w=["qb" + sx])
            for c in range(3):
                A(lambda e: e.copy(out=kv[b][0:Pn, 512 * c:512 * c + 512], in_=pk[2 + c][0:Pn, :]), r=["npk%d" % (2 + c)], w=["kv" + sx])
                V(lambda e: e.tensor_copy(out=kvb[b][0:Pn, 512 * c:512 * c + 512], in_=kv[b][0:Pn, 512 * c:512 * c + 512]), r=["kv" + sx], w=["kvb" + sx])
            A(lambda e: e.activation(out=gt_[b][0:Pn, :], in_=pk[5][0:Pn, 0:48], func=AF.Sigmoid), r=["npk5"], w=["gt_" + sx])
            dma(pick(O["kv_p"], O["kv_s"], t), kv[b][0:Pn, 0:1024], r=["kv" + sx], w=["kv_p"], add_writer=True)
            NSDBG = int(os.environ.get("NSDBG", "0"))
            if not NSDBG & 1:
                dma(pick(O["q_scr"], O["qs_scr"], t), qb[b][0:Pn, :], r=["qb" + sx], w=["q_scr"], add_writer=True)
            if not NSDBG & 2:
                dma(pick(O["kvb_scr"], O["kvbs_scr"], t), kvb[b][0:Pn, :], r=["kvb" + sx], w=["kvb_scr"], add_writer=True)
            if not NSDBG & 4:
                dma(pick(O["gate_scr"], O["gates_scr"], t), gt_[b][0:Pn, :], r=["gt_" + sx], w=["gate_scr"], add_writer=True)
            if rs is None:
                dma(O["win_s"], kv[b][0:Pn, 1024:1536], r=["kv" + sx], w=["win_p"], add_writer=True)
            elif t >= NTILE - 4:
                tt = t - (NTILE - 4)
                dma(O["win_p"][tt * 128:(tt + 1) * 128, :], kv[b][0:Pn, 1024:1536], r=["kv" + sx], w=["win_p"], add_writer=True)
    K.S.barrier()


class Lin32:
    def __init__(self, K, es, C, kcmax, tag):
        self.K, self.C, self.tag = K, C, tag
        self.wch = [K.sb(es, "l32w%s%d" % (tag, i), [128, kcmax, 512], F32) for i in range(2)]
        self.ps = [K.ps(es, "l32p%s%d" % (tag, i), [128, 512], F32) for i in range(2)]
        self.pt = K.ps(es, "l32t%s" % tag, [128, 32, NS], F32)
        self.i = 0

    def transpose(self, src, skey, kc, dstT, dkey):
        K, idf = self.K, self.C["idf"]
        for k in range(kc):
            K.P(lambda e: e.transpose(out=self.pt[:, k, :], in_=src[:, k * 128:(k + 1) * 128], identity=idf[0:NS, 0:NS]),
                r=[skey, "idf"], w=["l32t" + self.tag])
        K.V(lambda e: e.tensor_copy(out=dstT[:, 0:kc, :], in_=self.pt[:, 0:kc, :]), r=["l32t" + self.tag], w=[dkey])

    def run(self, xT, xkey, kc, Wd, ncols, evac):
        K = self.K
        Wv = Wd.rearrange("(k p) n -> p k n", p=128)
        for c0 in range(0, ncols, 512):
            cw = min(512, ncols - c0)
            b = self.i % 2
            self.i += 1
            wk = "l32w%s%d" % (self.tag, b)
            pk = "l32p%s%d" % (self.tag, b)
            K.dma(self.wch[b][:, 0:kc, 0:cw], Wv[:, :, c0:c0 + cw], w=[wk])
            for k in range(kc):
                K.P(lambda e: e.matmul(self.ps[b][0:NS, 0:cw], lhsT=xT[:, k, :], rhs=self.wch[b][:, k, 0:cw], start=(k == 0), stop=(k == kc - 1)),
                    r=[xkey, wk], w=[pk])
            evac(c0, cw, self.ps[b][0:NS, 0:cw], pk)


def rms32(K, x, xkey, gain, out, okey, junk, ss, tag):
    V, A = K.V, K.A
    N = NS
    A(lambda e: e.activation(out=junk[0:N, :], in_=x, func=AF.Square, accum_out=ss[0:N, 0:1]), r=[xkey], w=["junk" + tag, "ss" + tag])
    V(lambda e: e.tensor_scalar(out=ss[0:N, :], in0=ss[0:N, :], scalar1=1.0 / D, scalar2=EPS, op0=ALU.mult, op1=ALU.add), r=["ss" + tag], w=["ss" + tag])
    A(lambda e: e.activation(out=ss[0:N, :], in_=ss[0:N, :], func=AF.Sqrt), r=["ss" + tag], w=["ss" + tag])
    V(lambda e: e.reciprocal(out=ss[0:N, :], in_=ss[0:N, :]), r=["ss" + tag], w=["ss" + tag])
    V(lambda e: e.scalar_tensor_tensor(out=out, in0=x, scalar=ss[0:N, 0:1], in1=gain[0:N, :], op0=ALU.mult, op1=ALU.mult),
      r=[xkey, "ss" + tag, "gain" + tag], w=[okey])


def sample_l0_f32(K, C, I, O):
    V, A, G, P, dma = K.V, K.A, K.G, K.P, K.dma
    N = NS
    FF = 2816
    with ExitStack() as es:
        L = Lin32(K, es, C, 22, "a")
        x = K.sb(es, "f_x", [N, D], F32)
        g32 = K.sb(es, "f_g", [N, D], F32)
        xT = K.sb(es, "f_xT", [128, 22, N], F32)
        big = K.sb(es, "f_big", [N, 2 * FF], F32)
        act = K.sb(es, "f_act", [N, FF], F32)
        h = K.sb(es, "f_h", [N, D], F32)
        junk = K.sb(es, "f_junk", [N, D], F32)
        ss = K.sb(es, "f_ss", [N, 1], F32)
        gain_f = K.sb(es, "f_gainf", [N, D], F32)
        gain_m = K.sb(es, "f_gainm", [N, D], F32)
        qb = K.sb(es, "f_qb", [N, D], BF16)
        kvb = K.sb(es, "f_kvb", [N, 1536], BF16)
        dma(x[:], I["xs"], w=["f_x"])
        dma(g32[:], O["gs32_scr"], r=["gs32_scr"], w=["f_g"])
        dma(gain_f[:], AP(tensor=I["norm_ffn"].tensor, offset=0, ap=[[0, N], [1, D]]), w=["gainf"])
        dma(gain_m[:], AP(tensor=I["norm_mix"].tensor, offset=D, ap=[[0, N], [1, D]]), w=["gainm"])

        def to_big(c0, cw, ps, pk):
            A(lambda e: e.copy(out=big[:, c0:c0 + cw], in_=ps), r=[pk], w=["f_big"])
        L.transpose(g32, "f_g", 8, xT, "f_xT")
        L.run(xT, "f_xT", 8, I["s5_w_glu"], 2048, to_big)
        A(lambda e: e.activation(out=big[:, 1024:2048], in_=big[:, 1024:2048], func=AF.Sigmoid), r=["f_big"], w=["f_big"])
        V(lambda e: e.tensor_tensor(out=big[:, 0:1024], in0=big[:, 0:1024], in1=big[:, 1024:2048], op=ALU.mult), r=["f_big"], w=["f_big"])
        V(lambda e: e.tensor_tensor(out=x[:], in0=x[:], in1=big[:, 0:1024], op=ALU.add), r=["f_big", "f_x"], w=["f_x"])
        rms32(K, x[:], "f_x", gain_f, h[:], "f_h", junk, ss, "f")
        L.transpose(h, "f_h", 8, xT, "f_xT")
        L.run(xT, "f_xT", 8, I["ffn_w_in"], 2 * FF, to_big)
        A(lambda e: e.activation(out=act[:], in_=big[:, 0:FF], func=AF.Silu), r=["f_big"], w=["f_act"])
        V(lambda e: e.tensor_tensor(out=act[:], in0=act[:], in1=big[:, FF:2 * FF], op=ALU.mult), r=["f_big", "f_act"], w=["f_act"])
        L.transpose(act, "f_act", 22, xT, "f_xT")

        def add_x(c0, cw, ps, pk):
            V(lambda e: e.tensor_tensor(out=x[:, c0:c0 + cw], in0=ps, in1=x[:, c0:c0 + cw], op=ALU.add), r=[pk, "f_x"], w=["f_x"])
        L.run(xT, "f_xT", 22, I["ffn_w_out"], D, add_x)
        dma(O["y2s_scr"], x[:], r=["f_x"], w=["y2_scr"], add_writer=True)
        rms32(K, x[:], "f_x", gain_m, h[:], "f_h", junk, ss, "m")
        L.transpose(h, "f_h", 8, xT, "f_xT")
        L.run(xT, "f_xT", 8, I["nsa_w_in"], 2608, to_big)
        A(lambda e: e.activation(out=qb[:], in_=big[:, 0:1024], func=AF.Identity, scale=0.125), r=["f_big"], w=["f_qb"])
        V(lambda e: e.tensor_copy(out=kvb[:], in_=big[:, 1024:2560]), r=["f_big"], w=["f_kvb"])
        A(lambda e: e.activation(out=act[:, 0:48], in_=big[:, 2560:2608], func=AF.Sigmoid), r=["f_big", "f_act"], w=["f_act"])
        dma(O["kv_s"], big[:, 1024:2048], r=["f_big"], w=["kv_p"], add_writer=True)
        dma(O["win_s"], big[:, 2048:2560], r=["f_big"], w=["win_p"], add_writer=True)
        dma(O["qs_scr"], qb[:], r=["f_qb"], w=["q_scr"], add_writer=True)
        dma(O["kvbs_scr"], kvb[:], r=["f_kvb"], w=["kvb_scr"], add_writer=True)
        dma(O["gates_scr"], act[:, 0:48], r=["f_act"], w=["gate_scr"], add_writer=True)
    K.S.barrier()


NEG = -30000.0
NCB = 255


def nsa_attn_prompt(K, C, I, O):
    idf, idb = C["idf"], C["idb"]
    V, A, G, P, dma = K.V, K.A, K.G, K.P, K.dma
    nc = K.nc
    with ExitStack() as es:
        KselE = K.sb(es, "na_KselE", [128, 4, T], BF16)
        KwinE = K.sb(es, "na_KwinE", [128, 4, T], BF16)
        Vsel = K.sb(es, "na_Vsel", [128, NTILE, 4, 65], BF16)
        Vwin = K.sb(es, "na_Vwin", [128, NTILE, 4, 65], BF16)
        kcT = K.sb(es, "na_kcT", [64, 4, 256], BF16)
        vca = K.sb(es, "na_vca", [128, 2, 4, 129], BF16)
        chb = K.sb(es, "na_chb", [128, 16], F32)
        W4 = K.sb(es, "na_W4", [128, 128], F32)
        for g in range(4):
            K.S.dma("pool", KselE[64:128, g, :], I["c_E"], (), ["KselE"], add_writer=True)
            K.S.dma("pool", KwinE[64:128, g, :], I["c_E"], (), ["KwinE"], add_writer=True)
        G(lambda e: e.memset(Vsel[:, :, :, 64:65], 1.0), w=["Vsel"])
        G(lambda e: e.memset(Vwin[:, :, :, 64:65], 1.0), w=["Vwin"])
        G(lambda e: e.memset(vca[:, :, :, 64:65], 1.0), w=["vca"])
        for nt in range(2):
            for g in range(4):
                K.S.dma("pool", vca[:, nt, g, 65:129], I["c_cover"][nt * 128:(nt + 1) * 128, :], (), ["vca"], add_writer=True)
        dma(chb[:], AP(tensor=I["rel_bias"].tensor, offset=31 * 16, ap=[[0, 128], [1, 16]]), w=["chb"])
        G(lambda e: e.memset(W4[:], 0.0), w=["W4"])
        G(lambda e: e.affine_select(out=W4[:], in_=W4[:], pattern=[[-1, 128]], compare_op=ALU.is_ge, fill=NEG, base=0, channel_multiplier=1),
          r=["W4"], w=["W4"])

        with ExitStack() as bes:
            tabs = K.sb(bes, "na_tab", [32, 16], F32)
            ohb = K.sb(bes, "na_ohb", [32, 256], F32)
            Fraw = K.sb(bes, "na_Fraw", [16, 256], F32)
            Fpad = K.sb(bes, "na_Fpad", [16, 384], F32)
            FC = K.sb(bes, "na_FC", [16, 8192], F32)
            pF = K.ps(bes, "na_pF", [128, 512], F32)
            dma(tabs[:], I["rel_bias"], w=["tabs"])
            dma(ohb[:], I["c_ohb"], w=["ohb"])
            P(lambda e: e.matmul(pF[0:16, 0:256], lhsT=tabs[:], rhs=ohb[:], start=True, stop=True), r=["tabs", "ohb"], w=["pF"])
            V(lambda e: e.tensor_copy(out=Fraw[:], in_=pF[0:16, 0:256]), r=["pF"], w=["Fraw"])
            G(lambda e: e.memset(Fpad[:], NEG), w=["Fpad"])
            V(lambda e: e.tensor_scalar(out=Fpad[:, 127:383], in0=Fraw[:], scalar1=Fraw[:, 255:256], scalar2=None, op0=ALU.subtract),
              r=["Fraw", "Fpad"], w=["Fpad"])
            dma(O["fpad_scr"], Fpad[:], r=["Fpad"], w=["fpad_scr"])
            G(lambda e: e.memset(FC[:, 0:4111], NEG), w=["FC"])
            V(lambda e: e.tensor_copy(out=FC[:, 4111:4367], in_=Fraw[:]), r=["Fraw", "FC"], w=["FC"])
            V(lambda e: e.tensor_copy(out=FC[:, 4367:8192], in_=bc(Fraw[:, 255:256], [16, 8192 - 4367])), r=["Fraw", "FC"], w=["FC"])
            dma(O["fc_scr"], FC[:], r=["FC"], w=["fc_scr"])

        K.S.barrier()
        with ExitStack() as ces:
            KcT = K.sb(ces, "na_KcT", [64, 2, 4, T], BF16)
            kvt = [K.sb(ces, "na_kvt%d" % i, [128, 1536], BF16) for i in range(2)]
            W1 = K.sb(ces, "na_W1", [64, 2, 32, 128], BF16)
            W2 = K.sb(ces, "na_W2", [128, 2, 64], BF16)
            pe_f = K.sb(ces, "na_pef", [32, 2, 64], F32)
            pe_b = K.sb(ces, "na_peb", [32, 2, 64], BF16)
            peT = K.sb(ces, "na_peT", [64, 2, 32], BF16)
            cv = K.sb(ces, "na_cv", [128, 2], F32)
            pre = K.sb(ces, "na_pre", [128, 256], F32)
            tmpg = K.sb(ces, "na_tmpg", [128, 256], F32)
            hidT = K.sb(ces, "na_hidT", [128, 256], BF16)
            ptk = [K.ps(ces, "na_ptk%d" % i, [64, 8, 128], BF16) for i in range(2)]
            ph = K.ps(ces, "na_ph", [128, 512], F32)
            pk2 = K.ps(ces, "na_pk2", [128, 512], F32)
            for kvi in range(2):
                K.S.dma("pool", W1[:, kvi, :, :], I["nsa_phi_w1"][kvi].rearrange("l d e -> d l e"), (), ["naW1"], add_writer=True)
                K.S.dma("pool", W2[:, kvi, :], I["nsa_phi_w2"][kvi], (), ["naW2"], add_writer=True)
            dma(pe_f[:], I["nsa_phi_pe"].rearrange("k l d -> l k d"), w=["pe_f"])
            V(lambda e: e.tensor_copy(out=pe_b[:], in_=pe_f[:]), r=["pe_f"], w=["pe_b"])
            for kvi in range(2):
                P(lambda e: e.transpose(out=ptk[0][:, kvi, 0:32], in_=pe_b[:, kvi, :], identity=idb[0:32, 0:32]), r=["pe_b", "idb"], w=["ptk0"])
            V(lambda e: e.tensor_copy(out=peT[:], in_=ptk[0][:, 0:2, 0:32]), r=["ptk0"], w=["peT"])
            for kvi in range(2):
                for l in range(32):
                    P(lambda e: e.matmul(ph[:, kvi:kvi + 1], lhsT=W1[:, kvi, l, :], rhs=peT[:, kvi, l:l + 1], start=(l == 0), stop=(l == 31)),
                      r=["naW1", "peT"], w=["ph"])
            V(lambda e: e.tensor_copy(out=cv[:], in_=ph[:, 0:2]), r=["ph"], w=["cv"])
            G(lambda e: e.memset(hidT[:], 0.0), w=["hidT"])
            for t in range(NTILE):
                b = t % 2
                cs = slice(t * 128, (t + 1) * 128)
                dma(kvt[b][:], O["kvb_scr"][cs, :], r=["kvb_scr"], w=["kvt%d" % b])
                for j in range(8):
                    P(lambda e: e.transpose(out=ptk[0][:, j, :], in_=kvt[b][:, j * 64:(j + 1) * 64], identity=idb[:]), r=["kvt%d" % b, "idb"], w=["ptk0"])
                A(lambda e: e.copy(out=KcT[:, :, :, cs], in_=ptk[0][:].rearrange("p (k g) n -> p k g n", g=4)), r=["ptk0"], w=["KcT"])
                for j in range(4):
                    P(lambda e: e.transpose(out=ptk[1][:, j, :], in_=kvt[b][:, 512 + j * 64:512 + (j + 1) * 64], identity=idb[:]), r=["kvt%d" % b, "idb"], w=["ptk1"])
                    P(lambda e: e.transpose(out=ptk[1][:, 4 + j, :], in_=kvt[b][:, 1024 + j * 64:1024 + (j + 1) * 64], identity=idb[:]), r=["kvt%d" % b, "idb"], w=["ptk1"])
                V(lambda e: e.tensor_copy(out=KselE[0:64, :, cs], in_=ptk[1][:, 0:4, :]), r=["ptk1"], w=["KselE"])
                V(lambda e: e.tensor_copy(out=KwinE[0:64, :, cs], in_=ptk[1][:, 4:8, :]), r=["ptk1", "KselE"], w=["KwinE"])
                G(lambda e: e.tensor_copy(out=Vsel[:, t, :, 0:64], in_=kvt[b][:, 768:1024].rearrange("p (g d) -> p g d", d=64)), r=["kvt%d" % b], w=["Vsel"])
                G(lambda e: e.tensor_copy(out=Vwin[:, t, :, 0:64], in_=kvt[b][:, 1280:1536].rearrange("p (g d) -> p g d", d=64)), r=["kvt%d" % b], w=["Vwin"])
            for kvi in range(2):
                for g in range(4):
                    for l in range(32):
                        P(lambda e: e.matmul(ph[:, 0:NCB], lhsT=W1[:, kvi, l, :], rhs=KcT[:, kvi, g, l:l + 16 * (NCB - 1) + 1:16],
                                             start=(l == 0), stop=(l == 31)), r=["naW1", "KcT"], w=["ph"])
                    V(lambda e: e.tensor_scalar(out=pre[:, 0:NCB], in0=ph[:, 0:NCB], scalar1=cv[:, kvi:kvi + 1], scalar2=None, op0=ALU.add),
                      r=["ph", "cv"], w=["pre"])
                    gelu_tanh(K, pre[:, 0:NCB], "pre", tmpg[:, 0:NCB], "tmpg", hidT[:, 255:0:-1], "hidT")
                    if kvi == 0:
                        P(lambda e: e.matmul(pk2[0:64, 0:256], lhsT=W2[:, 0, :], rhs=hidT[:], start=True, stop=True), r=["naW2", "hidT"], w=["pk2"])
                        V(lambda e: e.tensor_copy(out=kcT[:, g, :], in_=pk2[0:64, 0:256]), r=["pk2"], w=["kcT"])
                    else:
                        for nt in range(2):
                            P(lambda e: e.matmul(pk2[:, 64 * nt:64 * nt + 64], lhsT=hidT[:, nt * 128:(nt + 1) * 128], rhs=W2[:, 1, :], start=True, stop=True),
                              r=["naW2", "hidT"], w=["pk2"])
                        V(lambda e: e.tensor_copy(out=vca[:, :, g, 0:64], in_=pk2[:, 0:128].rearrange("p (n f) -> p n f", f=64)), r=["pk2"], w=["vca"])
        K.S.barrier()

        with ExitStack() as qes:
            wo = K.sb(qes, "na_wo", [128, 8, D], BF16)
            parT = K.sb(qes, "na_par", [128, 2], F32)
            biasT = K.sb(qes, "na_biasT", [128, 3, 16, 128], BF16)
            TW = K.sb(qes, "na_TW", [128, 2, 128], F32)
            dma(parT[:], I["par"], w=["parT"])
            p0, p1 = parT[:, 0:1], parT[:, 1:2]
            load_w_bf16(K, wo, I["nsa_w_o"], D)
            with ExitStack() as hes:
                hk = K.sb(hes, "na_hk", [128, 16, 256], F32)
                biasD = K.sb(hes, "na_biasD", [128, 16, 256], F32)
                negt = K.sb(hes, "na_negt", [128, 4, 128], F32)
                Jm = K.sb(hes, "na_J", [128, 128], F32)
                pJ = K.ps(hes, "na_pJ", [128, 512], F32)
                dma(hk[:], AP(tensor=O["fpad_scr"].tensor, offset=0, ap=[[1, 128], [384, 16], [1, 256]]), r=["fpad_scr"], w=["hk"])
                G(lambda e: e.memset(Jm[:], 1.0), w=["Jm"])
                G(lambda e: e.affine_select(out=Jm[:], in_=Jm[:], pattern=[[1, 128]], compare_op=ALU.is_equal, fill=0.0, base=-127, channel_multiplier=1),
                  r=["Jm"], w=["Jm"])
                hkf = hk[:].rearrange("p h x -> p (h x)")
                bDf = biasD[:].rearrange("p h x -> p (h x)")
                for c in range(8):
                    P(lambda e: e.matmul(pJ[:], lhsT=Jm[:], rhs=hkf[:, c * 512:(c + 1) * 512], start=True, stop=True), r=["Jm", "hk"], w=["pJ"])
                    V(lambda e: e.tensor_copy(out=bDf[:, c * 512:(c + 1) * 512], in_=pJ[:]), r=["pJ"], w=["biasD"])
                bD0 = biasD[:, :, 0:128]
                bD1 = biasD[:, :, 128:256]
                V(lambda e: e.tensor_scalar(out=biasT[:, 0], in0=bD0, scalar1=p1, scalar2=None, op0=ALU.mult), r=["biasD", "parT"], w=["biasT"])
                G(lambda e: e.memset(negt[:], NEG), w=["negt"])
                for hq in range(4):
                    V(lambda e: e.scalar_tensor_tensor(out=biasT[:, 0, 4 * hq:4 * hq + 4, :], in0=negt[:], scalar=p0, in1=biasT[:, 0, 4 * hq:4 * hq + 4, :],
                                                       op0=ALU.mult, op1=ALU.add), r=["negt", "parT", "biasT"], w=["biasT"])
                V(lambda e: e.tensor_scalar(out=biasT[:, 1], in0=bD1, scalar1=p1, scalar2=None, op0=ALU.mult), r=["biasD", "parT", "biasT"], w=["biasT"])
                V(lambda e: e.scalar_tensor_tensor(out=biasT[:, 1], in0=bD0, scalar=p0, in1=biasT[:, 1], op0=ALU.mult, op1=ALU.add), r=["biasD", "parT", "biasT"], w=["biasT"])
                V(lambda e: e.tensor_scalar(out=biasT[:, 2], in0=bD1, scalar1=p0, scalar2=None, op0=ALU.mult), r=["biasD", "parT", "biasT"], w=["biasT"])
                V(lambda e: e.tensor_scalar(out=TW[:, 0, :], in0=W4[:], scalar1=p0, scalar2=None, op0=ALU.mult), r=["W4", "parT"], w=["TW"])
                V(lambda e: e.scalar_tensor_tensor(out=TW[:, 0, :], in0=negt[:, 0, :], scalar=p1, in1=TW[:, 0, :], op0=ALU.mult, op1=ALU.add), r=["negt", "parT", "TW"], w=["TW"])
                V(lambda e: e.tensor_scalar(out=TW[:, 1, :], in0=W4[:], scalar1=p1, scalar2=None, op0=ALU.mult), r=["W4", "parT", "TW"], w=["TW"])


            K.S.barrier()
            NJ = NTILE // 2
            qt_sb = [K.sb(qes, "na_q%d" % i, [128, D], BF16) for i in range(2)]
            gate = [K.sb(qes, "na_gate%d" % i, [128, 48], F32) for i in range(2)]
            yres = [K.sb(qes, "na_y%d" % i, [128, D], F32) for i in range(2)]
            bW1 = K.sb(qes, "na_bW", [128, 16, 256], F32)
            bW = [bW1, bW1]
            bC1 = [K.sb(qes, "na_bC_%d" % n, [128, 16, 128], BF16) for n in range(2)]
            bC = [bC1, bC1]
            ka = [K.sb(qes, "na_ka%d" % i, [128, 2, 64], F32) for i in range(2)]
            qidx = K.sb(qes, "na_qidx", [128, NJ], I32)
            QSel = K.sb(qes, "na_QSel", [128, 16, 128], BF16)
            QWin = K.sb(qes, "na_QWin", [128, 16, 128], BF16)
            NBUF = 3
            sc = [K.sb(qes, "na_sc%d" % i, [128, 4, 128], F32) for i in range(NBUF)]
            PT = [K.sb(qes, "na_PT%d" % i, [128, 4, 128], BF16) for i in range(NBUF)]
            rc = [K.sb(qes, "na_rc%d" % i, [128, 3, 4], F32) for i in range(2)]
            ocs = [K.sb(qes, "na_ocs%d" % i, [128, 4, 64], F32) for i in range(2)]
            imp = K.sb(qes, "na_imp", [128, 64], F32)
            wk1 = K.sb(qes, "na_wk1", [128, 64], F32)
            m8 = K.sb(qes, "na_m8", [128, 16], F32)
            selm = K.sb(qes, "na_selm", [128, 64], F32)
            tmpm = K.sb(qes, "na_tmpm", [128, 64], F32)
            mbw = [K.sb(qes, "na_mbw%d" % i, [128, 4, 128], BF16) for i in range(2)]
            otile = [K.sb(qes, "na_o%d" % i, [128, D], BF16) for i in range(2)]
            oacc = K.sb(qes, "na_oacc", [128, 64], F32)
            oT = K.sb(qes, "na_oT", [128, 8, 128], BF16)
            psb = [K.ps(qes, "na_ps%d" % i, [128, 4, 128], F32) for i in range(NBUF)]
            posel = K.ps(qes, "na_posel", [128, 512], F32)
            powin = K.ps(qes, "na_powin", [128, 512], F32)
            poc = K.ps(qes, "na_poc", [128, 512], F32)
            pimp = K.ps(qes, "na_pimp", [128, 4, 128], F32)
            pmisc = K.ps(qes, "na_pmisc", [128, 8, 128], BF16)
            posel_v = posel[:, 0:260].rearrange("p (h c) -> p h c", c=65)
            powin_v = powin[:, 0:260].rearrange("p (h c) -> p h c", c=65)
            poc_v = poc[:, 0:260].rearrange("p (h c) -> p h c", c=65)

            dma(qidx[:], I["c_qidx"], w=["qidx"])
            for i_ in range(2):
                G(lambda e: e.memset(mbw[i_][:], 0.0), w=["mbw%d" % i_])
            V(lambda e: e.tensor_copy(out=QWin[64:128, :, :], in_=bc(chb[64:128, :].unsqueeze(2), [64, 16, 128])), r=["chb"],
              w=["QWin0", "QWin1", "QWin2", "QWin3"])
            sidx = [0]

            def emit_qk(blk):
                lhsT_full, g, kt, qrhs, qkey = blk["lhsT"], blk["g"], blk["kt"], blk["qrhs"], blk["qkey"]
                i = sidx[0] % NBUF
                sidx[0] += 1
                blk["i"] = i
                ks = slice(kt * 128, (kt + 1) * 128)
                P(lambda e: e.matmul(psb[i][:], lhsT=lhsT_full[:, g, ks], rhs=qrhs[:, 4 * g:4 * g + 4, :], start=True, stop=True),
                  r=["KselE", "KwinE", qkey], w=["psb%d" % i])

            def emit_exp_pv(blk):
                i, g, kt = blk["i"], blk["g"], blk["kt"]
                bias_ap = blk["bias"]
                if bias_ap is not None:
                    V(lambda e: e.tensor_tensor(out=sc[i][:], in0=psb[i][:], in1=bias_ap, op=ALU.add), r=["psb%d" % i, "biasT", "TW"], w=["sc%d" % i])
                    A(lambda e: e.activation(out=PT[i][:], in_=sc[i][:], func=AF.Exp), r=["sc%d" % i], w=["PT%d" % i])
                else:
                    A(lambda e: e.activation(out=PT[i][:], in_=psb[i][:], func=AF.Exp), r=[], w=["PT%d" % i, "psb%d" % i])
                for h in range(4):
                    P(lambda e: e.matmul(blk["po_v"][:, h, :], lhsT=PT[i][:, h, :], rhs=blk["vaug"][:, kt, g, :], start=(blk["first"] and h == 0),
                                         stop=blk["last"], skip_group_check=True), r=["PT%d" % i, "Vsel", "Vwin"], w=[blk["pokey"]])

            def nts_of(j):
                return [1] if j <= 7 else [0, 1]

            def tile_loads(j):
                b = j % 2
                sx = str(b)
                K.S.idma(qt_sb[b][:], O["q_scr"], qidx[:, j:j + 1], ["qidx", "q_scr"], ["qsb" + sx])
                K.S.idma(gate[b][:], O["gate_scr"], qidx[:, j:j + 1], ["qidx", "gate_scr"], ["gate" + sx])
                K.S.idma(yres[b][:], O["y2_scr"], qidx[:, j:j + 1], ["qidx", "y2_scr"], ["yres" + sx])
                dma(ka[b][:], I["c_ka"][j], w=["ka" + sx])
                for nt in nts_of(j):
                    dma(bW[b][:], AP(tensor=O["fc_scr"].tensor, offset=256 * j + 2048 * nt, ap=[[16, 128], [8192, 16], [1, 256]]),
                        r=["fc_scr"], w=["bW"])
                    G(lambda e: e.tensor_scalar(out=bC[b][nt][:], in0=bW[b][:, :, 0:128], scalar1=p0, scalar2=None, op0=ALU.mult),
                      r=["bW", "parT"], w=["bC_%d" % nt])
                    V(lambda e: e.scalar_tensor_tensor(out=bC[b][nt][:], in0=bW[b][:, :, 128:256], scalar=p1, in1=bC[b][nt][:], op0=ALU.mult, op1=ALU.add),
                      r=["bW", "parT", "bC_%d" % nt], w=["bC_%d" % nt])

            def phase_a(j, g):
                b = j % 2
                sx = str(b)
                gp = g % 2
                nts = nts_of(j)
                qk, wk_ = "QSel%d" % g, "QWin%d" % g
                for h in range(4):
                    hh = 4 * g + h
                    P(lambda e: e.transpose(out=pmisc[0:64, h, :], in_=qt_sb[b][:, hh * 64:(hh + 1) * 64], identity=idb[:]), r=["qsb" + sx, "idb"], w=["pmisc"])
                A(lambda e: e.copy(out=QSel[0:64, 4 * g:4 * g + 4, :], in_=pmisc[0:64, 0:4, :]), r=["pmisc"], w=[qk])
                V(lambda e: e.tensor_copy(out=QWin[0:64, 4 * g:4 * g + 4, :], in_=QSel[0:64, 4 * g:4 * g + 4, :]), r=[qk], w=[wk_])
                for ni, nt in enumerate(nts):
                    i = sidx[0] % NBUF
                    sidx[0] += 1
                    P(lambda e: e.matmul(psb[i][:], lhsT=kcT[:, g, nt * 128:(nt + 1) * 128], rhs=QSel[0:64, 4 * g:4 * g + 4, :], start=True, stop=True),
                      r=["kcT", qk], w=["psb%d" % i])
                    V(lambda e: e.tensor_tensor(out=sc[i][:], in0=psb[i][:], in1=bC[b][nt][:, 4 * g:4 * g + 4, :], op=ALU.add),
                      r=["psb%d" % i, "bC_%d" % nt], w=["sc%d" % i])
                    A(lambda e: e.activation(out=PT[i][:], in_=sc[i][:], func=AF.Exp), r=["sc%d" % i], w=["PT%d" % i])
                    for h in range(4):
                        P(lambda e: e.matmul(poc_v[:, h, :], lhsT=PT[i][:, h, :], rhs=vca[:, nt, g, 0:65], start=(ni == 0 and h == 0),
                                             stop=(ni == len(nts) - 1), skip_group_check=True), r=["PT%d" % i, "vca"], w=["poc"])
                        P(lambda e: e.matmul(pimp[:, h, 0:64], lhsT=PT[i][:, h, :], rhs=vca[:, nt, g, 65:129], start=(ni == 0 and h == 0),
                                             stop=(ni == len(nts) - 1), skip_group_check=True), r=["PT%d" % i, "vca"], w=["pimp"])
                rk = "rc%d" % gp
                V(lambda e: e.tensor_scalar(out=rc[gp][:, 0, :], in0=poc_v[:, :, 64], scalar1=1e-30, scalar2=None, op0=ALU.add), r=["poc"], w=[rk])
                V(lambda e: e.reciprocal(out=rc[gp][:, 0, :], in_=rc[gp][:, 0, :]), r=[rk], w=[rk])
                V(lambda e: e.tensor_scalar(out=imp[:], in0=pimp[:, 0, 0:64], scalar1=rc[gp][:, 0, 0:1], scalar2=None, op0=ALU.mult), r=["pimp", rk], w=["imp"])
                for h in range(1, 4):
                    V(lambda e: e.scalar_tensor_tensor(out=imp[:], in0=pimp[:, h, 0:64], scalar=rc[gp][:, 0, h:h + 1], in1=imp[:], op0=ALU.mult, op1=ALU.add),
                      r=["pimp", rk, "imp"], w=["imp"])
                V(lambda e: e.tensor_tensor(out=ocs[gp][:], in0=poc_v[:, :, 0:64], in1=bc(rc[gp][:, 0, :].unsqueeze(2), [128, 4, 64]), op=ALU.mult),
                  r=["poc", rk], w=["ocs%d" % gp])
                V(lambda e: e.tensor_tensor(out=imp[:], in0=imp[:], in1=ka[b][:, 0, :], op=ALU.mult), r=["imp", "ka" + sx], w=["imp"])
                V(lambda e: e.tensor_tensor(out=imp[:], in0=imp[:], in1=ka[b][:, 1, :], op=ALU.add), r=["imp", "ka" + sx], w=["imp"])
                V(lambda e: e.max(out=m8[:, 0:8], in_=imp[:]), r=["imp"], w=["m8"])
                V(lambda e: e.match_replace(out=wk1[:], in_to_replace=m8[:, 0:8], in_values=imp[:], imm_value=-3e38), r=["imp", "m8"], w=["wk1"])
                V(lambda e: e.max(out=m8[:, 8:16], in_=wk1[:]), r=["wk1"], w=["m8"])
                V(lambda e: e.tensor_scalar(out=selm[:], in0=imp[:], scalar1=m8[:, 15:16], scalar2=None, op0=ALU.is_ge), r=["imp", "m8"], w=["selm"])
                V(lambda e: e.tensor_scalar(out=tmpm[:], in0=selm[:], scalar1=-NEG, scalar2=NEG, op0=ALU.mult, op1=ALU.add), r=["selm"], w=["tmpm"])
                for h in range(4):
                    V(lambda e: e.scalar_tensor_tensor(out=mbw[g % 2][:, h, 64:128], in0=selm[:], scalar=chb[:, 4 * g + h:4 * g + h + 1], in1=tmpm[:],
                                                       op0=ALU.mult, op1=ALU.add), r=["selm", "tmpm", "chb"], w=["mbw%d" % (g % 2)])

            def phase_a2(j, g):
                qk = "QSel%d" % g
                for h in range(4):
                    P(lambda e: e.transpose(out=pmisc[:, h, :], in_=mbw[g % 2][:, h, :], identity=idb[:]), r=["mbw%d" % (g % 2), "idb"], w=["pmisc"])
                A(lambda e: e.copy(out=QSel[64:128, 4 * g:4 * g + 4, :], in_=pmisc[64:128, 0:4, :]), r=["pmisc"], w=[qk])

            def phase_b(j, g):
                b = j % 2
                sx = str(b)
                gp = g % 2
                rk = "rc%d" % gp
                ktop = 2 * j + 1
                blocks = []
                for kt in range(ktop + 1):
                    kr = ktop - kt
                    bias_ap = biasT[:, kr, 4 * g:4 * g + 4, :] if kr <= 2 else None
                    blocks.append(dict(lhsT=KselE, g=g, kt=kt, qrhs=QSel, qkey="QSel%d" % g, bias=bias_ap, vaug=Vsel, po_v=posel_v, pokey="posel",
                                       first=(kt == 0), last=(kt == ktop)))
                k0 = max(0, ktop - 5)
                for kt in range(k0, ktop + 1):
                    kr = ktop - kt
                    if kr <= 2:
                        bias_ap = biasT[:, kr, 4 * g:4 * g + 4, :]
                    elif kr == 5:
                        bias_ap = bc(TW[:, 0, :].unsqueeze(1), [128, 4, 128])
                    elif kr == 4:
                        bias_ap = bc(TW[:, 1, :].unsqueeze(1), [128, 4, 128])
                    else:
                        bias_ap = None
                    blocks.append(dict(lhsT=KwinE, g=g, kt=kt, qrhs=QWin, qkey="QWin%d" % g, bias=bias_ap, vaug=Vwin, po_v=powin_v, pokey="powin",
                                       first=(kt == k0), last=(kt == ktop)))
                n = len(blocks)
                LA = NBUF - 1
                for i in range(n + LA):
                    if i < n:
                        emit_qk(blocks[i])
                    if i >= LA:
                        emit_exp_pv(blocks[i - LA])
                V(lambda e: e.tensor_scalar(out=rc[gp][:, 1, :], in0=posel_v[:, :, 64], scalar1=1e-30, scalar2=None, op0=ALU.add), r=["posel"], w=[rk])
                V(lambda e: e.tensor_scalar(out=rc[gp][:, 2, :], in0=powin_v[:, :, 64], scalar1=1e-30, scalar2=None, op0=ALU.add), r=["powin"], w=[rk])
                V(lambda e: e.reciprocal(out=rc[gp][:, 1:3, :], in_=rc[gp][:, 1:3, :]), r=[rk], w=[rk])
                V(lambda e: e.memset(rc[gp][:, 0, :], 1.0), r=[rk, "ocs%d" % gp], w=[rk])
                V(lambda e: e.tensor_tensor(out=rc[gp][:], in0=rc[gp][:], in1=gate[b][:].rearrange("p (r h) -> p r h", h=16)[:, :, 4 * g:4 * g + 4], op=ALU.mult),
                  r=[rk, "gate" + sx], w=[rk])
                for h in range(4):
                    hh = 4 * g + h
                    V(lambda e: e.tensor_scalar(out=oacc[:], in0=ocs[gp][:, h, :], scalar1=rc[gp][:, 0, h:h + 1], scalar2=None, op0=ALU.mult),
                      r=["ocs%d" % gp, rk], w=["oacc"])
                    V(lambda e: e.scalar_tensor_tensor(out=oacc[:], in0=posel_v[:, h, 0:64], scalar=rc[gp][:, 1, h:h + 1], in1=oacc[:], op0=ALU.mult, op1=ALU.add),
                      r=["posel", rk, "oacc"], w=["oacc"])
                    V(lambda e: e.scalar_tensor_tensor(out=otile[b][:, hh * 64:(hh + 1) * 64], in0=powin_v[:, h, 0:64], scalar=rc[gp][:, 2, h:h + 1], in1=oacc[:],
                                                       op0=ALU.mult, op1=ALU.add), r=["powin", rk, "oacc"], w=["otile" + sx])

            def tile_end(j):
                b = j % 2
                sx = str(b)
                rs = slice(j * 128, (j + 1) * 128)
                for k in range(8):
                    P(lambda e: e.transpose(out=pmisc[:, k, :], in_=otile[b][:, k * 128:(k + 1) * 128], identity=idb[:]), r=["otile" + sx, "idb"], w=["pmisc"])
                A(lambda e: e.copy(out=oT[:], in_=pmisc[:]), r=["pmisc"], w=["oT"])
                for c in range(2):
                    pso = psb[c][:].rearrange("p h n -> p (h n)")
                    for k in range(8):
                        P(lambda e: e.matmul(pso, lhsT=oT[:, k, :], rhs=wo[:, k, c * 512:(c + 1) * 512], start=(k == 0), stop=(k == 7)),
                          r=["oT", "w_na_wo"], w=["psb%d" % c])
                    V(lambda e: e.tensor_tensor(out=yres[b][:, c * 512:(c + 1) * 512], in0=pso, in1=yres[b][:, c * 512:(c + 1) * 512], op=ALU.add),
                      r=["psb%d" % c, "yres" + sx], w=["yres" + sx])
                dma(O["y3_scr"][rs, :], yres[b][:], r=["yres" + sx], w=["y3_scr"], add_writer=True)

            items = [(j, g) for j in range(NJ) for g in range(4)]
            tile_loads(0)
            phase_a(*items[0])
            phase_a2(*items[0])
            for ii, (j, g) in enumerate(items):
                if ii + 1 < len(items):
                    nj, ng = items[ii + 1]
                    if ng == 0:
                        tile_loads(nj)
                    phase_a(nj, ng)
                phase_b(j, g)
                if ii + 1 < len(items):
                    phase_a2(*items[ii + 1])
                if g == 3:
                    tile_end(j)
    K.S.barrier()


NKT_S = 17
NWT_S = 5
OFFC = 4111


def nsa_attn_sample(K, C, I, O):
    idf, idb = C["idf"], C["idb"]
    V, A, G, P, dma = K.V, K.A, K.G, K.P, K.dma
    N = NS
    with ExitStack() as es:
        W1 = K.sb(es, "sa_W1", [64, 2, 32, 128], BF16)
        W2 = K.sb(es, "sa_W2", [128, 2, 64], BF16)
        cv = K.sb(es, "sa_cv", [128, 2], F32)
        biasC = K.sb(es, "sa_bC", [128, 16], F32)
        biasS = K.sb(es, "sa_bS", [128, NKT_S, 16], F32)
        biasW = K.sb(es, "sa_bW", [128, NWT_S, 16], F32)
        QT = K.sb(es, "sa_QT", [128, 16, N], BF16)
        idx = K.sb(es, "sa_idx", [128, N * 16], I32)
        covs = K.sb(es, "sa_cov", [128, 64], BF16)
        Erow = K.sb(es, "sa_E", [128, NKT_S * 128], BF16)
        ones4 = K.sb(es, "sa_ones4", [1, 4], F32)
        for kvi in range(2):
            K.S.dma("pool", W1[:, kvi, :, :], I["nsa_phi_w1"][kvi].rearrange("l d e -> d l e"), (), ["saW1"], add_writer=True)
            K.S.dma("pool", W2[:, kvi, :], I["nsa_phi_w2"][kvi], (), ["saW2"], add_writer=True)
        K.S.dma("pool", covs[:], I["c_cover_s"], (), ["covs"])
        K.S.dma("pool", Erow[64:128, :], I["c_E"][:, 0:NKT_S * 128], (), ["Erow"])
        V(lambda e: e.memset(ones4[:], 1.0), w=["ones4"])
        with ExitStack() as bes:
            pe_f = K.sb(bes, "sa_pef", [32, 2, 64], F32)
            pe_b = K.sb(bes, "sa_peb", [32, 2, 64], BF16)
            peT = K.sb(bes, "sa_peT", [64, 2, 32], BF16)
            hk = K.sb(bes, "sa_hk", [128, NKT_S + NWT_S, 16], F32)
            Jm = K.sb(bes, "sa_J", [128, 128], F32)
            pti = K.sb(bes, "sa_pti", [128, N * 16], I32)
            ptf = K.sb(bes, "sa_ptf", [128, N * 16], F32)
            iot = K.sb(bes, "sa_iot", [128, 1], F32)
            qs = K.sb(bes, "sa_qs", [N, D], BF16)
            pA = K.ps(bes, "sa_pA", [128, 1024], BF16)
            pB = K.ps(bes, "sa_pB", [128, 512], F32)
            dma(pe_f[:], I["nsa_phi_pe"].rearrange("k l d -> l k d"), w=["pe_f"])
            V(lambda e: e.tensor_copy(out=pe_b[:], in_=pe_f[:]), r=["pe_f"], w=["pe_b"])
            for kvi in range(2):
                P(lambda e: e.transpose(out=pA[0:64, kvi * 32:kvi * 32 + 32], in_=pe_b[:, kvi, :], identity=idb[0:32, 0:32]), r=["pe_b", "idb"], w=["pA"])
            V(lambda e: e.tensor_copy(out=peT[:].rearrange("p k l -> p (k l)"), in_=pA[0:64, 0:64]), r=["pA"], w=["peT"])
            for kvi in range(2):
                for l in range(32):
                    P(lambda e: e.matmul(pB[:, kvi:kvi + 1], lhsT=W1[:, kvi, l, :], rhs=peT[:, kvi, l:l + 1], start=(l == 0), stop=(l == 31)),
                      r=["saW1", "peT"], w=["pB"])
            V(lambda e: e.tensor_copy(out=cv[:], in_=pB[:, 0:2]), r=["pB"], w=["cv"])
            bC2 = K.sb(bes, "sa_bC2", [128, 16, 2], F32)
            dma(bC2[:], AP(tensor=O["fc_scr"].tensor, offset=OFFC + 2017 - 16 * 127, ap=[[16, 128], [8192, 16], [1, 2]]), r=["fc_scr"], w=["bC2"])
            V(lambda e: e.tensor_copy(out=biasC[:], in_=bC2[:, :, 0]), r=["bC2"], w=["biasC"])
            hk2 = K.sb(bes, "sa_hk2", [128, NKT_S + NWT_S, 16, 2], F32)
            for kt in range(NKT_S):
                dma(hk2[:, kt, :, :], AP(tensor=O["fc_scr"].tensor, offset=OFFC + 2048 - 128 * kt - 127, ap=[[1, 128], [8192, 16], [1, 2]]),
                    r=["fc_scr"], w=["hk2"], add_writer=True)
            for wt in range(NWT_S):
                dma(hk2[:, NKT_S + wt, :, :], AP(tensor=O["fc_scr"].tensor, offset=OFFC + 512 - 128 * wt - 127, ap=[[1, 128], [8192, 16], [1, 2]]),
                    r=["fc_scr"], w=["hk2"], add_writer=True)
            V(lambda e: e.tensor_copy(out=hk[:], in_=hk2[:, :, :, 0]), r=["hk2"], w=["hk"])
            G(lambda e: e.memset(Jm[:], 1.0), w=["Jm"])
            G(lambda e: e.affine_select(out=Jm[:], in_=Jm[:], pattern=[[1, 128]], compare_op=ALU.is_equal, fill=0.0, base=-127, channel_multiplier=1),
              r=["Jm"], w=["Jm"])
            ncol = (NKT_S + NWT_S) * 16
            P(lambda e: e.matmul(pB[:, 0:ncol], lhsT=Jm[:], rhs=hk[:].rearrange("p t h -> p (t h)"), start=True, stop=True), r=["Jm", "hk", "cv"], w=["pB"])
            V(lambda e: e.tensor_copy(out=biasS[:].rearrange("p t h -> p (t h)"), in_=pB[:, 0:NKT_S * 16]), r=["pB"], w=["biasS"])
            V(lambda e: e.tensor_copy(out=biasW[:].rearrange("p t h -> p (t h)"), in_=pB[:, NKT_S * 16:ncol]), r=["pB"], w=["biasW"])
            dma(pti[:], AP(tensor=I["ptab"].tensor, offset=0, ap=[[0, 128], [1, N * 16]]), w=["pti"])
            G(lambda e: e.iota(out=iot[:], pattern=[[0, 1]], base=0, channel_multiplier=1, allow_small_or_imprecise_dtypes=True), w=["iot"])
            V(lambda e: e.tensor_copy(out=ptf[:], in_=pti[:]), r=["pti"], w=["ptf"])
            V(lambda e: e.tensor_scalar(out=ptf[:], in0=ptf[:], scalar1=128.0, scalar2=iot[:, 0:1], op0=ALU.mult, op1=ALU.add), r=["ptf", "iot"], w=["ptf"])
            V(lambda e: e.tensor_copy(out=idx[:], in_=ptf[:]), r=["ptf"], w=["idx"])
            dma(qs[:], O["qs_scr"], r=["q_scr"], w=["sqs"])
            for hh in range(16):
                P(lambda e: e.transpose(out=pA[0:64, 64 + hh * N:64 + (hh + 1) * N], in_=qs[:, hh * 64:(hh + 1) * 64], identity=idb[0:N, 0:N]),
                  r=["sqs", "idb", "peT"], w=["pA"])
            V(lambda e: e.tensor_copy(out=QT[0:64, :, :].rearrange("p h n -> p (h n)"), in_=pA[0:64, 64:64 + 16 * N]), r=["pA"], w=["QT"])
        K.S.barrier()

        with ExitStack() as tes:
            pg = [K.sb(tes, "sa_pg%d" % i, [128, D], F32) for i in range(3)]
            wn = K.sb(tes, "sa_wn", [128, 4, 512], F32)
            newf = K.sb(tes, "sa_newf", [N, 1536], BF16)
            KcT = K.sb(tes, "sa_KcT", [64, 2, 4, 2048], BF16)
            KsE = K.sb(tes, "sa_KsE", [128, 4, NKT_S * 128], BF16)
            KwE = K.sb(tes, "sa_KwE", [64, 4, NWT_S * 128], BF16)
            Vs = K.sb(tes, "sa_Vs", [128, NKT_S, 4, 65], BF16)
            Vw = K.sb(tes, "sa_Vw", [128, NWT_S, 4, 65], BF16)
            kcT = K.sb(tes, "sa_kcT", [64, 4, 128], BF16)
            vca = K.sb(tes, "sa_vca", [128, 4, 129], BF16)
            pre = [K.sb(tes, "sa_pre%d" % i, [128, 128], F32) for i in range(2)]
            tmpg = [K.sb(tes, "sa_tmpg%d" % i, [128, 128], F32) for i in range(2)]
            hidT = [K.sb(tes, "sa_hidT%d" % i, [128, 128], BF16) for i in range(2)]
            sc = K.sb(tes, "sa_sc", [128, NKT_S, 4], F32)
            PTs = K.sb(tes, "sa_PT", [128, NKT_S, 4], BF16)
            oc = K.sb(tes, "sa_oc", [4, 129], F32)
            osw = K.sb(tes, "sa_osw", [4, 2, 65], F32)
            rcc = K.sb(tes, "sa_rcc", [4, 1], F32)
            impr = K.sb(tes, "sa_impr", [1, 64], F32)
            wk1 = K.sb(tes, "sa_wk1", [1, 64], F32)
            m8 = K.sb(tes, "sa_m8", [1, 16], F32)
            mbr = K.sb(tes, "sa_mbr", [1, 128], F32)
            ptA = K.ps(tes, "sa_ptA", [64, 4, 128], F32)
            ptB = K.ps(tes, "sa_ptB", [64, 4, 128], F32)
            ph = [K.ps(tes, "sa_ph%d" % i, [128, 512], F32) for i in range(2)]
            pk2 = K.ps(tes, "sa_pk2", [128, 512], F32)
            psS = K.ps(tes, "sa_psS", [128, 512], F32)
            po = K.ps(tes, "sa_po", [128, 512], F32)
            ptN = K.ps(tes, "sa_ptN", [128, 1024], BF16)

            dma(newf[:], O["kvbs_scr"], r=["kvb_scr"], w=["newf"])
            G(lambda e: e.memset(Vs[:, :, :, 64:65], 1.0), w=["sVs"])
            G(lambda e: e.memset(Vw[:, :, :, 64:65], 1.0), w=["sVw"])
            G(lambda e: e.memset(vca[:, :, 64:65], 1.0), w=["svca"])
            for g in range(4):
                V(lambda e: e.tensor_copy(out=vca[:, g, 65:129], in_=covs[:]), r=["covs", "svca"], w=["svca"])
                V(lambda e: e.tensor_copy(out=KsE[64:128, g, :], in_=Erow[64:128, :]), r=["Erow"], w=["sKsE"])
            G(lambda e: e.memset(KsE[0:64, :, 16 * 128:17 * 128], 0.0), r=["sKsE"], w=["sKsE"])
            G(lambda e: e.memset(KwE[:, :, 4 * 128:5 * 128], 0.0), w=["sKwE"])
            G(lambda e: e.memset(Vs[:, 16, :, 0:64], 0.0), r=["sVs"], w=["sVs"])
            G(lambda e: e.memset(Vw[:, 4, :, 0:64], 0.0), r=["sVw"], w=["sVw"])
            for i_ in range(2):
                G(lambda e: e.memset(hidT[i_][:], 0.0), w=["shidT%d" % i_])
            G(lambda e: e.memset(mbr[:], 0.0), w=["mbr"])
            for j in range(4):
                P(lambda e: e.transpose(out=ptN[0:64, j * N:(j + 1) * N], in_=newf[:, 512 + j * 64:512 + (j + 1) * 64], identity=idb[0:N, 0:N]),
                  r=["newf", "idb"], w=["ptN"])
                P(lambda e: e.transpose(out=ptN[0:64, (4 + j) * N:(5 + j) * N], in_=newf[:, 1024 + j * 64:1024 + (j + 1) * 64], identity=idb[0:N, 0:N]),
                  r=["newf", "idb"], w=["ptN"])
            newT = K.sb(tes, "sa_newT", [64, 8, N], BF16)
            V(lambda e: e.tensor_copy(out=newT[:].rearrange("p j n -> p (j n)"), in_=ptN[0:64, 0:8 * N]), r=["ptN"], w=["newT"])
            newrow = K.sb(tes, "sa_newrow", [1, 1536], BF16)

            for tk in range(N):
                dma(wn[:], I["cwin"][tk].rearrange("(w r) c -> r w c", r=128), w=["wn"])
                for wt in range(4):
                    for g in range(4):
                        P(lambda e: e.transpose(out=ptA[:, g, :], in_=wn[:, wt, g * 64:(g + 1) * 64], identity=idf[:]), r=["wn", "idf"], w=["sptA"])
                    A(lambda e: e.copy(out=KwE[:, :, wt * 128:(wt + 1) * 128], in_=ptA[:]), r=["sptA"], w=["sKwE"])
                    V(lambda e: e.tensor_copy(out=Vw[:, wt, :, 0:64], in_=wn[:, wt, 256:512].rearrange("p (g d) -> p g d", d=64)), r=["wn", "sVw"], w=["sVw"])
                V(lambda e: e.tensor_copy(out=KsE[0:64, :, 16 * 128:16 * 128 + 1], in_=newT[:, 0:4, tk:tk + 1]), r=["newT", "sKsE"], w=["sKsE"])
                V(lambda e: e.tensor_copy(out=KwE[:, :, 4 * 128:4 * 128 + 1], in_=newT[:, 4:8, tk:tk + 1]), r=["newT", "sKwE"], w=["sKwE"])
                dma(newrow[:], O["kvbs_scr"][tk:tk + 1, :], r=["kvb_scr"], w=["newrow"])
                V(lambda e: e.tensor_copy(out=Vs[0:1, 16, :, 0:64], in_=newrow[:, 768:1024].rearrange("p (g d) -> p g d", d=64)), r=["newrow", "sVs"], w=["sVs"])
                V(lambda e: e.tensor_copy(out=Vw[0:1, 4, :, 0:64], in_=newrow[:, 1280:1536].rearrange("p (g d) -> p g d", d=64)), r=["newrow", "sVw"], w=["sVw"])
                for pgi in range(16):
                    b = (tk * 16 + pgi) % 3
                    K.S.idma(pg[b][:], I["ckv"], idx[:, tk * 16 + pgi:tk * 16 + pgi + 1], ["idx"], ["spg%d" % b])
                    cs = slice(pgi * 128, (pgi + 1) * 128)
                    for g in range(4):
                        P(lambda e: e.transpose(out=ptA[:, g, :], in_=pg[b][:, g * 64:(g + 1) * 64], identity=idf[:]), r=["spg%d" % b, "idf"], w=["sptA"])
                    A(lambda e: e.copy(out=KcT[:, 0, :, cs], in_=ptA[:]), r=["sptA"], w=["sKcT"])
                    for g in range(4):
                        P(lambda e: e.transpose(out=ptB[:, g, :], in_=pg[b][:, 256 + g * 64:256 + (g + 1) * 64], identity=idf[:]), r=["spg%d" % b, "idf"], w=["sptB"])
                    V(lambda e: e.tensor_copy(out=KcT[:, 1, :, cs], in_=ptB[:]), r=["sptB", "sKcT"], w=["sKcT"])
                    for g in range(4):
                        P(lambda e: e.transpose(out=ptA[:, g, :], in_=pg[b][:, 512 + g * 64:512 + (g + 1) * 64], identity=idf[:]), r=["spg%d" % b, "idf"], w=["sptA"])
                    A(lambda e: e.copy(out=KsE[0:64, :, cs], in_=ptA[:]), r=["sptA"], w=["sKsE"])
                    V(lambda e: e.tensor_copy(out=Vs[:, pgi, :, 0:64], in_=pg[b][:, 768:1024].rearrange("p (g d) -> p g d", d=64)), r=["spg%d" % b, "sVs"], w=["sVs"])
                for kvi in range(2):
                    for g in range(4):
                        cb = (kvi * 4 + g) % 2
                        cx = str(cb)
                        for l in range(32):
                            P(lambda e: e.matmul(ph[cb][:, 0:127], lhsT=W1[:, kvi, l, :], rhs=KcT[:, kvi, g, l:l + 16 * 126 + 1:16], start=(l == 0), stop=(l == 31)),
                              r=["saW1", "sKcT"], w=["sph" + cx])
                        V(lambda e: e.tensor_scalar(out=pre[cb][:, 0:127], in0=ph[cb][:, 0:127], scalar1=cv[:, kvi:kvi + 1], scalar2=None, op0=ALU.add),
                          r=["sph" + cx, "cv"], w=["spre" + cx])
                        gelu_tanh(K, pre[cb][:, 0:127], "spre" + cx, tmpg[cb][:, 0:127], "stmpg" + cx, hidT[cb][:, 127:0:-1], "shidT" + cx)
                        if kvi == 0:
                            P(lambda e: e.matmul(pk2[0:64, 0:128], lhsT=W2[:, 0, :], rhs=hidT[cb][:], start=True, stop=True), r=["saW2", "shidT" + cx], w=["spk2"])
                            V(lambda e: e.tensor_copy(out=kcT[:, g, :], in_=pk2[0:64, 0:128]), r=["spk2"], w=["skcT"])
                        else:
                            P(lambda e: e.matmul(pk2[:, 0:64], lhsT=hidT[cb][:], rhs=W2[:, 1, :], start=True, stop=True), r=["saW2", "shidT" + cx], w=["spk2"])
                            V(lambda e: e.tensor_copy(out=vca[:, g, 0:64], in_=pk2[:, 0:64]), r=["spk2", "svca"], w=["svca"])
                for g in range(4):
                    qcol = QT[:, 4 * g:4 * g + 4, tk]
                    P(lambda e: e.matmul(psS[:, 0:4], lhsT=kcT[:, g, :], rhs=QT[0:64, 4 * g:4 * g + 4, tk], start=True, stop=True), r=["skcT", "QT"], w=["spsS"])
                    V(lambda e: e.tensor_tensor(out=sc[:, 0, :], in0=psS[:, 0:4], in1=biasC[:, 4 * g:4 * g + 4], op=ALU.add), r=["spsS", "biasC"], w=["ssc"])
                    A(lambda e: e.activation(out=PTs[:, 0, :], in_=sc[:, 0, :], func=AF.Exp), r=["ssc"], w=["sPT"])
                    P(lambda e: e.matmul(po[0:4, 0:129], lhsT=PTs[:, 0, :], rhs=vca[:, g, :], start=True, stop=True), r=["sPT", "svca"], w=["spo"])
                    V(lambda e: e.tensor_copy(out=oc[:], in_=po[0:4, 0:129]), r=["spo"], w=["soc"])
                    V(lambda e: e.tensor_scalar(out=rcc[:], in0=oc[:, 64:65], scalar1=1e-30, scalar2=None, op0=ALU.add), r=["soc"], w=["srcc"])
                    V(lambda e: e.reciprocal(out=rcc[:], in_=rcc[:]), r=["srcc"], w=["srcc"])
                    dma(O["os_scr"][tk, 0, 4 * g:4 * g + 4, :], oc[:, 0:65], r=["soc"], w=["os_scr"], add_writer=True)
                    P(lambda e: e.matmul(po[0:1, 256:320], lhsT=rcc[:], rhs=oc[:, 65:129], start=True, stop=True), r=["srcc", "soc"], w=["spo"])
                    V(lambda e: e.tensor_copy(out=impr[:], in_=po[0:1, 256:320]), r=["spo"], w=["simpr"])
                    V(lambda e: e.memset(impr[:, 0:1], 1e4), r=["simpr"], w=["simpr"])
                    V(lambda e: e.memset(impr[:, 31:33], 1e4), r=["simpr"], w=["simpr"])
                    V(lambda e: e.memset(impr[:, 33:64], -1e30), r=["simpr"], w=["simpr"])
                    V(lambda e: e.max(out=m8[:, 0:8], in_=impr[:]), r=["simpr"], w=["sm8"])
                    V(lambda e: e.match_replace(out=wk1[:], in_to_replace=m8[:, 0:8], in_values=impr[:], imm_value=-3e38), r=["simpr", "sm8"], w=["swk1"])
                    V(lambda e: e.max(out=m8[:, 8:16], in_=wk1[:]), r=["swk1"], w=["sm8"])
                    V(lambda e: e.tensor_scalar(out=wk1[:], in0=impr[:], scalar1=m8[:, 15:16], scalar2=None, op0=ALU.is_ge), r=["simpr", "sm8", "swk1"], w=["swk1"])
                    V(lambda e: e.tensor_scalar(out=mbr[:, 64:128], in0=wk1[:], scalar1=-NEG, scalar2=NEG, op0=ALU.mult, op1=ALU.add), r=["swk1", "mbr"], w=["mbr"])
                    P(lambda e: e.matmul(po[:, 320:324], lhsT=mbr[:], rhs=ones4[:], start=True, stop=True), r=["mbr", "ones4", "simpr"], w=["spo"])
                    V(lambda e: e.tensor_copy(out=QT[64:128, 4 * g:4 * g + 4, tk], in_=po[64:128, 320:324]), r=["spo"], w=["QT"])
                    for kt in range(NKT_S):
                        P(lambda e: e.matmul(psS[:, 4 + 4 * kt:8 + 4 * kt], lhsT=KsE[:, g, kt * 128:(kt + 1) * 128], rhs=qcol, start=True, stop=True),
                          r=["sKsE", "QT"], w=["spsS"])
                    V(lambda e: e.tensor_tensor(out=sc[:], in0=psS[:, 4:4 + 4 * NKT_S].rearrange("p (t h) -> p t h", h=4), in1=biasS[:, :, 4 * g:4 * g + 4], op=ALU.add),
                      r=["spsS", "biasS"], w=["ssc"])
                    A(lambda e: e.activation(out=PTs[:], in_=sc[:], func=AF.Exp), r=["ssc"], w=["sPT"])
                    for kt in range(NKT_S):
                        P(lambda e: e.matmul(po[0:4, 0:65], lhsT=PTs[:, kt, :], rhs=Vs[:, kt, g, :], start=(kt == 0), stop=(kt == NKT_S - 1)),
                          r=["sPT", "sVs"], w=["spo"])
                    V(lambda e: e.tensor_copy(out=osw[:, 0, :], in_=po[0:4, 0:65]), r=["spo"], w=["sosw"])
                    for wt in range(NWT_S):
                        P(lambda e: e.matmul(psS[:, 4 * wt:4 * wt + 4], lhsT=KwE[:, g, wt * 128:(wt + 1) * 128], rhs=QT[0:64, 4 * g:4 * g + 4, tk], start=True, stop=True),
                          r=["sKwE", "QT"], w=["spsS"])
                    V(lambda e: e.tensor_tensor(out=sc[:, 0:NWT_S, :], in0=psS[:, 0:4 * NWT_S].rearrange("p (t h) -> p t h", h=4), in1=biasW[:, :, 4 * g:4 * g + 4], op=ALU.add),
                      r=["spsS", "biasW"], w=["ssc"])
                    A(lambda e: e.activation(out=PTs[:, 0:NWT_S, :], in_=sc[:, 0:NWT_S, :], func=AF.Exp), r=["ssc"], w=["sPT"])
                    for wt in range(NWT_S):
                        P(lambda e: e.matmul(po[0:4, 0:65], lhsT=PTs[:, wt, :], rhs=Vw[:, wt, g, :], start=(wt == 0), stop=(wt == NWT_S - 1)),
                          r=["sPT", "sVw"], w=["spo"])
                    V(lambda e: e.tensor_copy(out=osw[:, 1, :], in_=po[0:4, 0:65]), r=["spo", "sosw"], w=["sosw"])
                    dma(O["os_scr"][tk, 1, 4 * g:4 * g + 4, :], osw[:, 0, :], r=["sosw"], w=["os_scr"], add_writer=True)
                    dma(O["os_scr"][tk, 2, 4 * g:4 * g + 4, :], osw[:, 1, :], r=["sosw"], w=["os_scr"], add_writer=True)
        K.S.barrier()

        with ExitStack() as mes:
            osb = K.sb(mes, "sa_osb", [N, 3, 16, 65], F32)
            gt = K.sb(mes, "sa_gt", [N, 3, 16], F32)
            wgt = K.sb(mes, "sa_wgt", [N, 3, 16], F32)
            o1 = K.sb(mes, "sa_o1", [N, 16, 64], F32)
            o2 = K.sb(mes, "sa_o2", [N, 16, 64], F32)
            yr = K.sb(mes, "sa_yr", [N, D], F32)
            dma(osb[:], O["os_scr"], r=["os_scr"], w=["osb"])
            dma(gt[:], O["gates_scr"].rearrange("n (r h) -> n r h", h=16), r=["gate_scr"], w=["sgt"])
            dma(yr[:], O["y2s_scr"], r=["y2_scr"], w=["syr"])
            V(lambda e: e.tensor_scalar(out=wgt[:], in0=osb[:, :, :, 64], scalar1=1e-30, scalar2=None, op0=ALU.add), r=["osb"], w=["swgt"])
            V(lambda e: e.reciprocal(out=wgt[:], in_=wgt[:]), r=["swgt"], w=["swgt"])
            V(lambda e: e.tensor_tensor(out=wgt[:], in0=wgt[:], in1=gt[:], op=ALU.mult), r=["swgt", "sgt"], w=["swgt"])
            V(lambda e: e.tensor_tensor(out=o1[:], in0=osb[:, 0, :, 0:64], in1=bc(wgt[:, 0, :].unsqueeze(2), [N, 16, 64]), op=ALU.mult), r=["osb", "swgt"], w=["so1"])
            for br in (1, 2):
                V(lambda e: e.tensor_tensor(out=o2[:], in0=osb[:, br, :, 0:64], in1=bc(wgt[:, br, :].unsqueeze(2), [N, 16, 64]), op=ALU.mult), r=["osb", "swgt"], w=["so2"])
                V(lambda e: e.tensor_tensor(out=o1[:], in0=o1[:], in1=o2[:], op=ALU.add), r=["so1", "so2"], w=["so1"])
            L = Lin32(K, mes, C, 8, "o")
            oT32 = K.sb(mes, "sa_oT32", [128, 8, N], F32)
            L.transpose(o1[:].rearrange("n h d -> n (h d)"), "so1", 8, oT32, "soT32")

            def add_y(c0, cw, ps, pk):
                V(lambda e: e.tensor_tensor(out=yr[:, c0:c0 + cw], in0=ps, in1=yr[:, c0:c0 + cw], op=ALU.add), r=[pk, "syr"], w=["syr"])
            L.run(oT32, "soT32", 8, I["nsa_w_o"], D, add_y)
            dma(O["y3s_scr"], yr[:], r=["syr"], w=["y3s_scr"])
    K.S.barrier()


NMT = NTILE // 2 + 1


def moe_stage(K, C, I, O):
    idf, idb = C["idf"], C["idb"]
    V, A, G, P, dma = K.V, K.A, K.G, K.P, K.dma
    EF = 1408
    CW = 352
    with ExitStack() as es:
        acc = K.sb(es, "mo_acc", [128, NMT, D], F32)
        hT = K.sb(es, "mo_hT", [128, NMT, 8, 128], BF16)
        gts = K.sb(es, "mo_gts", [128, NMT, 8], F32)
        hT32s = K.sb(es, "mo_hT32s", [128, 8, NS], F32)
        par = K.sb(es, "mo_par", [128, 2], F32)
        gain = K.sb(es, "mo_gain", [128, D], F32)
        gfin = K.sb(es, "mo_gfin", [128, D], F32)
        rt = K.sb(es, "mo_rt", [128, 8, 8], F32)
        dma(par[:], I["par"], w=["par"])
        dma(gain[:], AP(tensor=I["norm_ffn"].tensor, offset=D, ap=[[0, 128], [1, D]]), w=["gain"])
        dma(gfin[:], AP(tensor=I["norm_final"].tensor, offset=0, ap=[[0, 128], [1, D]]), w=["gfin"])
        dma(rt[:], I["moe_router"].rearrange("(k p) e -> p k e", p=128), w=["rt"])
        with ExitStack() as es2:
            ya = K.sb(es2, "mo_ya", [128, D], F32)
            yb = K.sb(es2, "mo_yb", [128, D], F32)
            h32 = K.sb(es2, "mo_h32", [128, D], F32)
            hb = K.sb(es2, "mo_hb", [128, D], BF16)
            hT32 = K.sb(es2, "mo_hT32", [128, 8, 128], F32)
            junk = K.sb(es2, "mo_junk", [128, D], F32)
            ss = K.sb(es2, "mo_ss", [128, 1], F32)
            lg = K.sb(es2, "mo_lg", [128, 8], F32)
            ex = K.sb(es2, "mo_ex", [128, 8], F32)
            mk = K.sb(es2, "mo_mk", [128, 8], F32)
            m8 = K.sb(es2, "mo_m8", [128, 8], F32)
            sm = K.sb(es2, "mo_sm", [128, 1], F32)
            ptb = K.ps(es2, "mo_ptb", [128, 8, 128], BF16)
            pt32 = [K.ps(es2, "mo_pt32%d" % i, [128, 4, 128], F32) for i in range(2)]
            plg = K.ps(es2, "mo_plg", [128, 512], F32)
            for j in range(NMT):
                Pn = 128 if j < NMT - 1 else NS
                if j < NMT - 1:
                    dma(acc[:, j, :], O["y3_scr"][j * 128:(j + 1) * 128, :], r=["y3_scr"], w=["acc"])
                else:
                    dma(acc[0:Pn, j, :], O["y3s_scr"], r=["y3s_scr"], w=["acc"])
                xin = acc[0:Pn, j, :]
                A(lambda e: e.activation(out=junk[0:Pn, :], in_=xin, func=AF.Square, accum_out=ss[0:Pn, 0:1]), r=["acc"], w=["junk", "ss"])
                V(lambda e: e.tensor_scalar(out=ss[0:Pn, :], in0=ss[0:Pn, :], scalar1=1.0 / D, scalar2=EPS, op0=ALU.mult, op1=ALU.add), r=["ss"], w=["ss"])
                A(lambda e: e.activation(out=ss[0:Pn, :], in_=ss[0:Pn, :], func=AF.Sqrt), r=["ss"], w=["ss"])
                V(lambda e: e.reciprocal(out=ss[0:Pn, :], in_=ss[0:Pn, :]), r=["ss"], w=["ss"])
                V(lambda e: e.scalar_tensor_tensor(out=h32[0:Pn, :], in0=xin, scalar=ss[0:Pn, 0:1], in1=gain[0:Pn, :], op0=ALU.mult, op1=ALU.mult),
                  r=["acc", "ss", "gain"], w=["h32"])
                V(lambda e: e.tensor_copy(out=hb[0:Pn, :], in_=h32[0:Pn, :]), r=["h32"], w=["hb"])
                for k in range(8):
                    P(lambda e: e.transpose(out=ptb[:, k, 0:Pn], in_=hb[0:Pn, k * 128:(k + 1) * 128], identity=idb[0:Pn, 0:Pn]), r=["hb", "idb"], w=["ptb"])
                A(lambda e: e.copy(out=hT[:, j, :, 0:Pn], in_=ptb[:, :, 0:Pn]), r=["ptb"], w=["hT"])
                for k in range(8):
                    P(lambda e: e.transpose(out=pt32[k // 4][:, k % 4, 0:Pn], in_=h32[0:Pn, k * 128:(k + 1) * 128], identity=idf[0:Pn, 0:Pn]),
                      r=["h32", "idf"], w=["pt32%d" % (k // 4)])
                for hh in range(2):
                    V(lambda e: e.tensor_copy(out=hT32[:, 4 * hh:4 * hh + 4, 0:Pn], in_=pt32[hh][:, :, 0:Pn]), r=["pt32%d" % hh], w=["hT32"])
                if j == NMT - 1:
                    V(lambda e: e.tensor_copy(out=hT32s[:], in_=hT32[:, :, 0:NS]), r=["hT32"], w=["hT32s"])
                for k in range(8):
                    P(lambda e: e.matmul(plg[0:Pn, 0:8], lhsT=hT32[:, k, 0:Pn], rhs=rt[:, k, :], start=(k == 0), stop=(k == 7)), r=["hT32", "rt"], w=["plg"])
                V(lambda e: e.tensor_copy(out=lg[0:Pn, :], in_=plg[0:Pn, 0:8]), r=["plg"], w=["lg"])
                V(lambda e: e.max(out=m8[0:Pn, :], in_=lg[0:Pn, :]), r=["lg"], w=["m8"])
                V(lambda e: e.tensor_scalar(out=ex[0:Pn, :], in0=lg[0:Pn, :], scalar1=m8[0:Pn, 0:1], scalar2=None, op0=ALU.subtract), r=["lg", "m8"], w=["ex"])
                A(lambda e: e.activation(out=ex[0:Pn, :], in_=ex[0:Pn, :], func=AF.Exp), r=["ex"], w=["ex"])
                V(lambda e: e.tensor_scalar(out=mk[0:Pn, :], in0=lg[0:Pn, :], scalar1=m8[0:Pn, 1:2], scalar2=None, op0=ALU.is_ge), r=["lg", "m8"], w=["mk"])
                V(lambda e: e.tensor_tensor(out=ex[0:Pn, :], in0=ex[0:Pn, :], in1=mk[0:Pn, :], op=ALU.mult), r=["ex", "mk"], w=["ex"])
                V(lambda e: e.reduce_sum(out=sm[0:Pn, :], in_=ex[0:Pn, :], axis=AX.X), r=["ex"], w=["sm"])
                V(lambda e: e.reciprocal(out=sm[0:Pn, :], in_=sm[0:Pn, :]), r=["sm"], w=["sm"])
                V(lambda e: e.tensor_scalar(out=gts[0:Pn, j, :], in0=ex[0:Pn, :], scalar1=sm[0:Pn, 0:1], scalar2=None, op0=ALU.mult), r=["ex", "sm"], w=["gts"])
        K.S.barrier()
        with ExitStack() as es3:
            w1 = K.sb(es3, "mo_w1", [128, 8, 2 * EF], BF16)
            w2 = K.sb(es3, "mo_w2", [128, 11, D], BF16)
            act = [K.sb(es3, "mo_act%d" % i, [128, EF], BF16) for i in range(2)]
            actT = [K.sb(es3, "mo_actT%d" % i, [128, 11, 128], BF16) for i in range(2)]
            sl_ = [K.sb(es3, "mo_s%d" % i, [128, CW], F32) for i in range(2)]
            ptb = K.ps(es3, "mo_ptb2", [128, 8, 128], BF16)
            pa = [K.ps(es3, "mo_pa%d" % i, [128, 512], F32) for i in range(2)]
            pb = [K.ps(es3, "mo_pb%d" % i, [128, 512], F32) for i in range(2)]
            po = [K.ps(es3, "mo_po%d" % i, [128, 512], F32) for i in range(2)]
            for ex_ in range(8):
                v1 = I["moe_w_in"][ex_].rearrange("(k p) n -> p k n", p=128)
                for c0 in range(0, 2 * EF, 704):
                    K.S.dma("pool", w1[:, :, c0:c0 + 704], v1[:, :, c0:c0 + 704], (), ["mo_w1"], add_writer=(c0 > 0))
                v2 = I["moe_w_out"][ex_].rearrange("(k p) n -> p k n", p=128)
                for c0 in range(0, D, 512):
                    K.S.dma("pool", w2[:, :, c0:c0 + 512], v2[:, :, c0:c0 + 512], (), ["mo_w2"], add_writer=(c0 > 0))
                for j in range(NMT - 1):
                    Pn = 128
                    b = j % 2
                    sx = str(b)
                    for jj in range(4):
                        jb = jj % 2
                        for k in range(8):
                            P(lambda e: e.matmul(pa[jb][0:Pn, 0:CW], lhsT=hT[:, j, k, 0:Pn], rhs=w1[:, k, CW * jj:CW * jj + CW], start=(k == 0), stop=(k == 7)),
                              r=["hT", "mo_w1"], w=["mpa%d" % jb])
                        for k in range(8):
                            P(lambda e: e.matmul(pb[jb][0:Pn, 0:CW], lhsT=hT[:, j, k, 0:Pn], rhs=w1[:, k, EF + CW * jj:EF + CW * jj + CW], start=(k == 0), stop=(k == 7)),
                              r=["hT", "mo_w1"], w=["mpb%d" % jb])
                        A(lambda e: e.activation(out=sl_[jb][0:Pn, :], in_=pa[jb][0:Pn, 0:CW], func=AF.Silu), r=["mpa%d" % jb], w=["msl%d" % jb])
                        V(lambda e: e.tensor_tensor(out=act[b][0:Pn, CW * jj:CW * jj + CW], in0=pb[jb][0:Pn, 0:CW], in1=sl_[jb][0:Pn, :], op=ALU.mult),
                          r=["mpb%d" % jb, "msl%d" % jb], w=["mact" + sx])
                    for k0 in range(0, 11, 8):
                        k1 = min(11, k0 + 8)
                        for k in range(k0, k1):
                            P(lambda e: e.transpose(out=ptb[:, k - k0, 0:Pn], in_=act[b][0:Pn, k * 128:(k + 1) * 128], identity=idb[0:Pn, 0:Pn]),
                              r=["mact" + sx, "idb"], w=["mptb"])
                        A(lambda e: e.copy(out=actT[b][:, k0:k1, 0:Pn], in_=ptb[:, 0:k1 - k0, 0:Pn]), r=["mptb"], w=["mactT" + sx])
                    for c in range(2):
                        for k in range(11):
                            P(lambda e: e.matmul(po[c][0:Pn, :], lhsT=actT[b][:, k, 0:Pn], rhs=w2[:, k, c * 512:(c + 1) * 512], start=(k == 0), stop=(k == 10)),
                              r=["mactT" + sx, "mo_w2"], w=["mpo%d" % c])
                        V(lambda e: e.scalar_tensor_tensor(out=acc[0:Pn, j, c * 512:(c + 1) * 512], in0=po[c][0:Pn, :], scalar=gts[0:Pn, j, ex_:ex_ + 1],
                                                           in1=acc[0:Pn, j, c * 512:(c + 1) * 512], op0=ALU.mult, op1=ALU.add),
                          r=["mpo%d" % c, "gts", "acc"], w=["acc"])
        K.S.barrier()
        with ExitStack() as es4:
            L = Lin32(K, es4, C, 11, "m")
            bigm = K.sb(es4, "mo_big", [NS, 2 * EF], F32)
            actm = K.sb(es4, "mo_actm", [NS, EF], F32)
            actTm = K.sb(es4, "mo_actTm", [128, 11, NS], F32)
            js = NMT - 1

            def to_bigm(c0, cw, ps, pk):
                A(lambda e: e.copy(out=bigm[:, c0:c0 + cw], in_=ps), r=[pk], w=["mo_big"])
            for ex_ in range(8):
                L.run(hT32s, "hT32s", 8, I["moe_w_in"][ex_], 2 * EF, to_bigm)
                A(lambda e: e.activation(out=actm[:], in_=bigm[:, 0:EF], func=AF.Silu), r=["mo_big"], w=["mo_actm"])
                V(lambda e: e.tensor_tensor(out=actm[:], in0=actm[:], in1=bigm[:, EF:2 * EF], op=ALU.mult), r=["mo_big", "mo_actm"], w=["mo_actm"])
                L.transpose(actm, "mo_actm", 11, actTm, "mo_actTm")

                def acc_add(c0, cw, ps, pk):
                    V(lambda e: e.scalar_tensor_tensor(out=acc[0:NS, js, c0:c0 + cw], in0=ps, scalar=gts[0:NS, js, ex_:ex_ + 1],
                                                       in1=acc[0:NS, js, c0:c0 + cw], op0=ALU.mult, op1=ALU.add), r=[pk, "gts", "acc"], w=["acc"])
                L.run(actTm, "mo_actTm", 11, I["moe_w_out"][ex_], D, acc_add)
        K.S.barrier()
        with ExitStack() as es3:
            junk2 = K.sb(es3, "mo_junk2", [128, D], F32)
            ss2 = K.sb(es3, "mo_ss2", [128, 1], F32)
            for j in range(NMT):
                Pn = 128 if j < NMT - 1 else NS
                xin = acc[0:Pn, j, :]
                A(lambda e: e.activation(out=junk2[0:Pn, :], in_=xin, func=AF.Square, accum_out=ss2[0:Pn, 0:1]), r=["acc"], w=["junk2", "ss2"])
                V(lambda e: e.tensor_scalar(out=ss2[0:Pn, :], in0=ss2[0:Pn, :], scalar1=1.0 / D, scalar2=EPS, op0=ALU.mult, op1=ALU.add), r=["ss2"], w=["ss2"])
                A(lambda e: e.activation(out=ss2[0:Pn, :], in_=ss2[0:Pn, :], func=AF.Sqrt), r=["ss2"], w=["ss2"])
                V(lambda e: e.reciprocal(out=ss2[0:Pn, :], in_=ss2[0:Pn, :]), r=["ss2"], w=["ss2"])
                V(lambda e: e.scalar_tensor_tensor(out=junk2[0:Pn, :], in0=xin, scalar=ss2[0:Pn, 0:1], in1=gfin[0:Pn, :], op0=ALU.mult, op1=ALU.mult),
                  r=["acc", "ss2", "gfin", "junk2"], w=["junk2"])
                if j < NMT - 1:
                    dma(O["y_p"][j * 128:(j + 1) * 128, :], junk2[:], r=["junk2"], w=["y_p"], add_writer=True)
                else:
                    dma(O["y_s"], junk2[0:Pn, :], r=["junk2"], w=["y_s"])
    K.S.barrier()


IN_SPECS = [("norm_mix", [2, D]), ("norm_ffn", [2, D]), ("s5_lam_re", [64, 64]), ("s5_lam_im", [64, 64]), ("s5_log_dt", [64]),
            ("s5_b_re", [64, 64, 16]), ("s5_b_im", [64, 64, 16]), ("s5_c_re", [64, 16, 64]), ("s5_c_im", [64, 16, 64]),
            ("s5_d", [D]), ("s5_w_glu", [D, 2 * D]), ("ffn_w_in", [D, 5632]), ("ffn_w_out", [2816, D]), ("nsa_w_in", [D, 2608]),
            ("rel_bias", [32, 16]), ("nsa_phi_pe", [2, 32, 64]), ("nsa_phi_w1", [2, 32, 64, 128]), ("nsa_phi_w2", [2, 128, 64]),
            ("nsa_w_o", [D, D]), ("norm_final", [D]), ("moe_router", [D, 8]), ("moe_w_in", [8, D, 2816]), ("moe_w_out", [8, 1408, D])]
CONST_SPECS = [("c_ohb", [32, 256]), ("c_cover", [256, 64]), ("c_E", [64, T]), ("c_cover_s", [128, 64])]


def t5_bucket_np(n):
    n = np.maximum(n, 0)
    logpart = 16 + (np.log(np.maximum(n, 1).astype(np.float32) / 16) / math.log(8) * 16).astype(np.int32)
    return np.where(n < 16, n, np.minimum(logpart, 31))


def make_core_consts(par):
    nj = NTILE // 2
    qidx = np.zeros((128, nj), np.int32)
    ka = np.zeros((nj, 128, 2, 64), np.float32)
    jj = np.arange(64)
    for j in range(nj):
        qt = 2 * j + par
        qpos = qt * 128 + np.arange(128)
        qidx[:, j] = qpos
        jt = qpos // 64
        forced = (jj[None, :] == 0) | (jj[None, :] == jt[:, None]) | (jj[None, :] == jt[:, None] - 1)
        vis = (jj[None, :] * 64) <= qpos[:, None]
        keep = (vis & ~forced).astype(np.float32)
        add = np.where(vis, np.where(forced, 1e4, 0.0), -1e30).astype(np.float32)
        ka[j, :, 0, :] = keep
        ka[j, :, 1, :] = add
    return {"c_qidx": qidx, "c_ka": ka, "par": np.tile(np.array([[1.0 - par, float(par)]], np.float32), (128, 1))}


def make_consts():
    c = {}
    bk = t5_bucket_np(np.arange(256))
    ohb = np.zeros((32, 256), np.float32)
    ohb[bk, np.arange(256)] = 1.0
    c["c_ohb"] = ohb
    cover = np.zeros((256, 64), np.float32)
    off = (np.arange(4)[:, None] - np.arange(2)[None, :]).reshape(-1)
    for j in range(64):
        for o in off:
            n = 4 * j + o
            if 0 <= n < NCB:
                cover[n, j] += 1.0
    c["c_cover"] = np.ascontiguousarray(cover[::-1])
    E = np.zeros((64, T), np.float32)
    E[np.arange(T) // 64, np.arange(T)] = 1.0
    c["c_E"] = E
    cov_s = np.zeros((128, 64), np.float32)
    for j in range(33):
        for o in off:
            n = 4 * j + o
            if 0 <= n < 127:
                cov_s[127 - n, j] += 1.0
    c["c_cover_s"] = cov_s
    return c


def s5_stage_w(K, C, I, O):
    s5_stage(K, None, C, I, O)


def build(upto=99, only=None):
    nc = bass.Bass("TRN2", target_bir_lowering=False)
    with ExitStack() as es:
        K = KB(nc, es)
        I, O = {}, {}
        I["xp"] = K.dram("xp", [T, D], F32, "ExternalInput")
        for nm, shp in IN_SPECS + CONST_SPECS:
            I[nm] = K.dram(nm, shp, F32, "ExternalInput")
        O["s5_re_p"] = K.dram("s5_re_p", [64, 64], F32, "ExternalOutput")
        O["s5_im_p"] = K.dram("s5_im_p", [64, 64], F32, "ExternalOutput")
        O["kv_p"] = K.dram("kv_p", [T, 1024], F32, "ExternalOutput")
        O["win_p"] = K.dram("win_p", [512, 512], F32, "ExternalOutput")
        I["xs"] = K.dram("xs", [NS, D], F32, "ExternalInput")
        I["st_re"] = K.dram("st_re", [NS, 4096], F32, "ExternalInput")
        I["st_im"] = K.dram("st_im", [NS, 4096], F32, "ExternalInput")
        O["s5_re_s"] = K.dram("s5_re_s", [NS, 4096], F32, "ExternalOutput")
        O["s5_im_s"] = K.dram("s5_im_s", [NS, 4096], F32, "ExternalOutput")
        O["kv_s"] = K.dram("kv_s", [NS, 1024], F32, "ExternalOutput")
        O["win_s"] = K.dram("win_s", [NS, 512], F32, "ExternalOutput")
        dbg = "ExternalOutput" if (upto < 99 or only is not None) else "Internal"
        O["g_scr"] = K.dram("g_scr", [T, D], BF16, dbg)
        O["y1_scr"] = K.dram("y1_scr", [T, D], F32, dbg)
        O["y2_scr"] = K.dram("y2_scr", [T, D], F32, dbg)
        O["gs_scr"] = K.dram("gs_scr", [NS, D], BF16, dbg)
        O["gs32_scr"] = K.dram("gs32_scr", [NS, D], F32, dbg)
        O["y1s_scr"] = K.dram("y1s_scr", [NS, D], F32, dbg)
        O["y2s_scr"] = K.dram("y2s_scr", [NS, D], F32, dbg)
        O["q_scr"] = K.dram("q_scr", [T, 1024], BF16, dbg)
        O["qs_scr"] = K.dram("qs_scr", [NS, 1024], BF16, dbg)
        O["kvb_scr"] = K.dram("kvb_scr", [T, 1536], BF16, dbg)
        O["kvbs_scr"] = K.dram("kvbs_scr", [NS, 1536], BF16, dbg)
        O["gate_scr"] = K.dram("gate_scr", [T, 48], F32, dbg)
        O["gates_scr"] = K.dram("gates_scr", [NS, 48], F32, dbg)
        O["fpad_scr"] = K.dram("fpad_scr", [16, 384], F32, "Internal")
        O["fc_scr"] = K.dram("fc_scr", [16, 8192], F32, "Internal")
        O["y3_scr"] = K.dram("y3_scr", [T // 2, D], F32, dbg)
        O["y3s_scr"] = K.dram("y3s_scr", [NS, D], F32, dbg)
        O["os_scr"] = K.dram("os_scr", [NS, 3, 16, 65], F32, dbg)
        if only is None or 6 in only:
            I["ckv"] = K.dram("ckv", [2560 * 128, 1024], F32, "ExternalInput")
        I["cwin"] = K.dram("cwin", [NS, 512, 512], F32, "ExternalInput")
        I["ptab"] = K.dram("ptab", [NS * 16], I32, "ExternalInput")
        I["par"] = K.dram("par", [128, 2], F32, "ExternalInput")
        I["c_qidx"] = K.dram("c_qidx", [128, NTILE // 2], I32, "ExternalInput")
        I["c_ka"] = K.dram("c_ka", [NTILE // 2, 128, 2, 64], F32, "ExternalInput")
        O["y_p"] = K.dram("y_p", [T // 2, D], F32, "ExternalOutput")
        O["y_s"] = K.dram("y_s", [NS, D], F32, "ExternalOutput")
        C = build_consts(K, es)
        stages = [(1, s5_stage_w), (1.5, sample_l0_f32), (2, glu_stage), (3, ffn_stage), (4, nsa_proj_stage), (5, nsa_attn_prompt), (6, nsa_attn_sample), (7, moe_stage)]
        for idx, fn in stages:
            if (only is None and idx <= upto) or (only is not None and idx in only):
                fn(K, C, I, O)
        K.S.finish()
    return nc


def kernel(**inp):
    f32 = lambda a: np.ascontiguousarray(np.asarray(a, dtype=np.float32))
    nc = build()
    shared = {nm: f32(inp[nm]) for nm, _ in IN_SPECS}
    shared.update(make_consts())
    shared["ckv"] = f32(inp["cache_kv"]).reshape(2560 * 128, 1024)
    in_maps = []
    for c in range(8):
        m = dict(shared)
        m["xp"] = f32(inp["x_prompt"][c // 2])
        sl = slice(NS * c, NS * (c + 1))
        m["xs"] = f32(inp["x_sample"][sl, 0, :])
        m["st_re"] = f32(inp["state_s5_re"][sl]).reshape(NS, 4096)
        m["st_im"] = f32(inp["state_s5_im"][sl]).reshape(NS, 4096)
        m["cwin"] = f32(inp["cache_win"][sl]).reshape(NS, 512, 512)
        m["ptab"] = np.ascontiguousarray(np.asarray(inp["page_table"][sl], dtype=np.int32)).reshape(NS * 16)
        m.update(make_core_consts(c % 2))
        in_maps.append(m)
    res = run_bass_kernel_spmd(nc, in_maps, core_ids=list(range(8))).results
    B = 4
    cat = lambda nm: np.concatenate([np.asarray(res[c][nm], dtype=np.float32) for c in range(8)], axis=0)
    y_prompt = np.zeros((B, NTILE, 128, D), np.float32)
    for c in range(8):
        yp = np.asarray(res[c]["y_p"], dtype=np.float32).reshape(NTILE // 2, 128, D)
        y_prompt[c // 2, (c % 2)::2] = yp
    y_prompt = y_prompt.reshape(B, T, D)
    y_sample = cat("y_s").reshape(128, 1, D)
    s5_re_p = np.stack([res[2 * b]["s5_re_p"] for b in range(B)]).astype(np.float32)
    s5_im_p = np.stack([res[2 * b]["s5_im_p"] for b in range(B)]).astype(np.float32)
    kv_p = np.stack([res[2 * b]["kv_p"] for b in range(B)]).astype(np.float32).reshape(B, T, 4, 4, 64)
    win_p = np.stack([res[2 * b]["win_p"] for b in range(B)]).astype(np.float32).reshape(B, 512, 2, 4, 64)
    s5_re_s = cat("s5_re_s").reshape(128, 64, 64)
    s5_im_s = cat("s5_im_s").reshape(128, 64, 64)
    kv_s = cat("kv_s").reshape(128, 1, 4, 4, 64)
    win_s = cat("win_s").reshape(128, 1, 2, 4, 64)
    return (y_prompt, y_sample, s5_re_p, s5_im_p, kv_p, win_p, s5_re_s, s5_im_s, kv_s, win_s)
```
